# Optimizing a Trainium2 kernel written in Bass

```python
import math
import jax, jax.numpy as jnp
from jax import lax
import numpy as np

D_MODEL = 1024
BATCH = 8
SEQ = 2048
DEPTH = 4

HEAD_DIM = 64
N_HEADS_TOTAL = D_MODEL // HEAD_DIM
N_HEADS_MOBA = N_HEADS_TOTAL // 2
N_HEADS_SWA = N_HEADS_TOTAL - N_HEADS_MOBA
N_KV_SWA = 2
GQA_GROUP = N_HEADS_SWA // N_KV_SWA
MIX_WIDTH = N_HEADS_TOTAL * HEAD_DIM
MOBA_BLOCK = 256
MOBA_TOPK = 3
MOBA_QCHUNK = 16
SWA_WINDOW = 128
N_BUCKETS = 32
MAX_DISTANCE = 128
D_FF = 4 * D_MODEL
EPS = 1e-6
W_A = N_HEADS_MOBA * HEAD_DIM
W_QB = N_HEADS_SWA * HEAD_DIM
W_KVB = N_KV_SWA * HEAD_DIM
D_IN = 3 * W_A + W_QB + 2 * W_KVB

kernel_name = "hybrid_moba_swa_sandwich_adaln"


def rmsnorm(x, g):
    xf = x.astype(jnp.float32)
    y = xf * lax.rsqrt(jnp.mean(xf * xf, axis=-1, keepdims=True) + EPS)
    return (y * g.astype(jnp.float32)).astype(x.dtype)


def t5_bucket(dist):
    n = jnp.maximum(dist, 0)
    max_exact = N_BUCKETS // 2
    nf = jnp.maximum(n, 1).astype(jnp.float32)
    large = max_exact + (jnp.log(nf / max_exact) / math.log(MAX_DISTANCE / max_exact)
                         * (N_BUCKETS - max_exact)).astype(jnp.int32)
    large = jnp.minimum(large, N_BUCKETS - 1)
    return jnp.where(n < max_exact, n, large)


def moba_attention(q, k, v, bias_hb):
    B, H, S, dh = q.shape
    nb = -(-S // MOBA_BLOCK)
    sp = nb * MOBA_BLOCK
    padw = ((0, 0), (0, 0), (0, sp - S), (0, 0))
    q, k, v = jnp.pad(q, padw), jnp.pad(k, padw), jnp.pad(v, padw)
    kb = k.reshape(B, H, nb, MOBA_BLOCK, dh)
    vb = v.reshape(B, H, nb, MOBA_BLOCK, dh)
    k_mean = jnp.mean(kb.astype(jnp.float32), axis=3)
    gate = jnp.einsum('bhsd,bhnd->bhsn', q.astype(jnp.float32), k_mean)
    q_block = jnp.arange(sp) // MOBA_BLOCK
    fully_past = jnp.arange(nb)[None, :] < q_block[:, None]
    gate = jnp.where(fully_past, gate, -jnp.inf)
    n_sel = min(MOBA_TOPK, nb)
    sel_score, sel_idx = lax.top_k(gate, n_sel)
    sel_valid = jnp.isfinite(sel_score)
    scale = dh ** -0.5
    b_idx = jnp.arange(B)[:, None, None, None]
    h_idx = jnp.arange(H)[None, :, None, None]
    offs = jnp.arange(MOBA_BLOCK)

    def chunk(ci):
        q0 = ci * MOBA_QCHUNK
        qc = lax.dynamic_slice_in_dim(q, q0, MOBA_QCHUNK, axis=2)
        idx = lax.dynamic_slice_in_dim(sel_idx, q0, MOBA_QCHUNK, axis=2)
        valid = lax.dynamic_slice_in_dim(sel_valid, q0, MOBA_QCHUNK, axis=2)
        qpos = q0 + jnp.arange(MOBA_QCHUNK)
        k_sel = kb[b_idx, h_idx, idx]
        v_sel = vb[b_idx, h_idx, idx]
        s_sel = jnp.einsum('bhqd,bhqnjd->bhqnj', qc, k_sel).astype(jnp.float32) * scale
        kpos_sel = idx[..., None] * MOBA_BLOCK + offs
        s_sel = s_sel + bias_hb[h_idx[..., None], t5_bucket(qpos[:, None, None] - kpos_sel)]
        s_sel = jnp.where(valid[..., None], s_sel, -jnp.inf)
        ob = q0 // MOBA_BLOCK
        k_own = lax.dynamic_slice_in_dim(k, ob * MOBA_BLOCK, MOBA_BLOCK, axis=2)
        v_own = lax.dynamic_slice_in_dim(v, ob * MOBA_BLOCK, MOBA_BLOCK, axis=2)
        s_own = jnp.einsum('bhqd,bhjd->bhqj', qc, k_own).astype(jnp.float32) * scale
        dist = qpos[:, None] - (ob * MOBA_BLOCK + offs)[None, :]
        s_own = s_own + bias_hb[:, t5_bucket(dist)][None]
        s_own = jnp.where(dist >= 0, s_own, -jnp.inf)
        logits = jnp.concatenate(
            [s_sel.reshape(B, H, MOBA_QCHUNK, n_sel * MOBA_BLOCK), s_own], axis=-1)
        p = jax.nn.softmax(logits, axis=-1)
        p_sel = p[..., :n_sel * MOBA_BLOCK].reshape(B, H, MOBA_QCHUNK, n_sel, MOBA_BLOCK).astype(v.dtype)
        p_own = p[..., n_sel * MOBA_BLOCK:].astype(v.dtype)
        return (jnp.einsum('bhqnj,bhqnjd->bhqd', p_sel, v_sel)
                + jnp.einsum('bhqj,bhjd->bhqd', p_own, v_own))

    outs = lax.map(chunk, jnp.arange(sp // MOBA_QCHUNK))
    out = jnp.moveaxis(outs, 0, 2).reshape(B, H, sp, dh)
    return out[:, :, :S]


def swa_attention(q, k, v, bias_hb, sinks):
    B, HKV, G, S, dh = q.shape
    W = SWA_WINDOW
    nbq = S // W
    qb = q.reshape(B, HKV, G, nbq, W, dh)
    kb = k.reshape(B, HKV, nbq, W, dh)
    vb = v.reshape(B, HKV, nbq, W, dh)
    pad_prev = ((0, 0), (0, 0), (1, 0), (0, 0), (0, 0))
    k_band = jnp.concatenate([jnp.pad(kb[:, :, :-1], pad_prev), kb], axis=3)
    v_band = jnp.concatenate([jnp.pad(vb[:, :, :-1], pad_prev), vb], axis=3)
    s = jnp.einsum('bkgnqd,bknjd->bkgnqj', qb, k_band).astype(jnp.float32) * (dh ** -0.5)
    blk = jnp.arange(nbq)
    qpos = blk[:, None] * W + jnp.arange(W)[None, :]
    kpos = (blk[:, None] - 1) * W + jnp.arange(2 * W)[None, :]
    dist = qpos[:, :, None] - kpos[:, None, :]
    allowed = (dist >= 0) & (dist < W) & (kpos[:, None, :] >= 0)
    bias = bias_hb[:, t5_bucket(dist)].reshape(HKV, G, nbq, W, 2 * W)
    s = jnp.where(allowed, s + bias, -jnp.inf)
    sink = jnp.broadcast_to(sinks.astype(jnp.float32).reshape(1, HKV, G, 1, 1, 1), s.shape[:-1] + (1,))
    p = jax.nn.softmax(jnp.concatenate([s, sink], axis=-1), axis=-1)[..., :-1]
    out = jnp.einsum('bkgnqj,bknjd->bkgnqd', p.astype(v.dtype), v_band)
    return out.reshape(B, HKV, G, S, dh)


def token_mixer(h, w_in, w_out, sinks, bias_hb):
    B, S, _ = h.shape
    proj = h @ w_in
    qa, ka, va, qb, kb, vb = jnp.split(
        proj, [W_A, 2 * W_A, 3 * W_A, 3 * W_A + W_QB, 3 * W_A + W_QB + W_KVB], axis=-1)
    heads = lambda t, n: t.reshape(B, S, n, HEAD_DIM).transpose(0, 2, 1, 3)
    out_a = moba_attention(heads(qa, N_HEADS_MOBA), heads(ka, N_HEADS_MOBA),
                           heads(va, N_HEADS_MOBA), bias_hb[:N_HEADS_MOBA])
    out_a = out_a.transpose(0, 2, 1, 3).reshape(B, S, W_A)
    q_swa = qb.reshape(B, S, N_KV_SWA, GQA_GROUP, HEAD_DIM).transpose(0, 2, 3, 1, 4)
    out_b = swa_attention(q_swa, heads(kb, N_KV_SWA), heads(vb, N_KV_SWA),
                          bias_hb[N_HEADS_MOBA:], sinks)
    out_b = out_b.transpose(0, 3, 1, 2, 4).reshape(B, S, W_QB)
    return jnp.concatenate([out_a, out_b], axis=-1) @ w_out


def setup_inputs(seed: int = 0) -> dict:
    key = jax.random.key(seed)
    ks = jax.random.split(key, 13)
    f32 = jnp.float32
    nrm = lambda k, shape, s: jax.random.normal(k, shape, f32) * s
    return {
        "x": nrm(ks[0], (BATCH, SEQ, D_MODEL), 1.0),
        "c": nrm(ks[1], (BATCH, D_MODEL), 1.0),
        "w_in": nrm(ks[2], (DEPTH, D_MODEL, D_IN), D_MODEL ** -0.5),
        "w_out": nrm(ks[3], (DEPTH, MIX_WIDTH, D_MODEL), MIX_WIDTH ** -0.5),
        "sinks": nrm(ks[4], (DEPTH, N_HEADS_SWA), 0.5),
        "rel_bias": nrm(ks[5], (N_BUCKETS, N_HEADS_TOTAL), 0.5),
        "w_ada": nrm(ks[6], (DEPTH, D_MODEL, 6 * D_MODEL), D_MODEL ** -0.5),
        "b_ada": nrm(ks[7], (DEPTH, 6 * D_MODEL), 0.02),
        "norm_gains": 1.0 + nrm(ks[8], (DEPTH, 4, D_MODEL), 0.05),
        "w1": nrm(ks[9], (DEPTH, D_MODEL, D_FF), D_MODEL ** -0.5),
        "b1": nrm(ks[10], (DEPTH, D_FF), 0.02),
        "w2": nrm(ks[11], (DEPTH, D_FF, D_MODEL), D_FF ** -0.5),
        "b2": nrm(ks[12], (DEPTH, D_MODEL), 0.02),
    }


def reference(x, c, w_in, w_out, sinks, rel_bias, w_ada, b_ada, norm_gains, w1, b1, w2, b2):
    bias_hb = rel_bias.T
    c_act = jax.nn.silu(c)
    for l in range(DEPTH):
        mod = c_act @ w_ada[l] + b_ada[l]
        sh1, sc1, g1, sh2, sc2, g2 = [m[:, None, :] for m in jnp.split(mod, 6, axis=-1)]
        h = rmsnorm(x, norm_gains[l, 0]) * (1.0 + sc1) + sh1
        y = token_mixer(h, w_in[l], w_out[l], sinks[l], bias_hb)
        x = x + g1 * rmsnorm(y, norm_gains[l, 1])
        h = rmsnorm(x, norm_gains[l, 2]) * (1.0 + sc2) + sh2
        y = jnp.square(jax.nn.relu(h @ w1[l] + b1[l])) @ w2[l] + b2[l]
        x = x + g2 * rmsnorm(y, norm_gains[l, 3])
    return x
```

```python
import math
import numpy as np
import concourse.bass as bass
import concourse.mybir as mybir
from concourse.bass_utils import run_bass_kernel_spmd

F32 = mybir.dt.float32
BF16 = mybir.dt.bfloat16
AF = mybir.ActivationFunctionType
ALU = mybir.AluOpType
AX = mybir.AxisListType

D = 1024
S = 2048
DEPTH = 4
DFF = 4096
DIN = 2304
EPS = 1e-6
NEG = -30000.0
BIG = 1024.0
MOBA_ROUND = "trunc"
SWA_ROUND = "trunc"


class Sched:
    def __init__(self):
        self.ops = []
        self.last_w = {}
        self.readers = {}

    def marker(self, reads=(), writes=()):
        k = getattr(self, "_mk", 0)
        self._mk = k + 1
        col = k % 8
        dm = self.dummy
        self.add("dve", (lambda e: e.memset(dm[:, col:col + 1], 0.0)), reads=reads, writes=list(writes) + [f"dummy{col}"])

    def barrier(self):
        names = set(self.last_w) | set(self.readers)
        names.add("__bar__")
        self.marker(writes=sorted(names))

    def add(self, eng, fn, reads=(), writes=(), dma=None, ndma=1, total=False):
        idx = len(self.ops)
        reads = tuple(reads) + ("__bar__",)
        writes = tuple(writes)
        deps = set()
        for r in reads:
            w = self.last_w.get(r)
            if w is not None:
                deps.add(w)
        for w_ in writes:
            w = self.last_w.get(w_)
            if w is not None:
                deps.add(w)
            for rd in self.readers.get(w_, ()):
                deps.add(rd)
        for r in reads:
            self.readers.setdefault(r, []).append(idx)
        for w_ in writes:
            self.last_w[w_] = idx
            self.readers[w_] = []
        self.ops.append(dict(eng=eng, fn=fn, deps=deps, dma=dma, ndma=ndma, total=total,
                             reads=set(reads), writes=set(writes)))
        return idx

    def finalize(self, nc, semctx):
        ops = self.ops
        need = [False] * len(ops)
        for i, o in enumerate(ops):
            keep = set()
            for d in o["deps"]:
                p = ops[d]
                if p["dma"] is not None or o["dma"] is not None:
                    keep.add(d)
                elif p["eng"] != o["eng"]:
                    keep.add(d)
                else:
                    if o["eng"] != "pe" and (p["writes"] & (o["reads"] | o["writes"])):
                        keep.add(d)
            o["deps"] = keep
            for d in keep:
                need[d] = True
        sems = {}

        def getsem(name):
            if name not in sems:
                sems[name] = semctx(name)
            return sems[name]

        cnt = {}
        totals = {}
        for i, o in enumerate(ops):
            if o["dma"] is not None:
                key = "d_" + o["dma"]
                cnt[key] = cnt.get(key, 0) + 16 * o["ndma"]
                o["sig"] = (key, cnt[key])
                if o["total"]:
                    totals[key] = True
            elif need[i]:
                key = "e_" + o["eng"]
                cnt[key] = cnt.get(key, 0) + 1
                o["sig"] = (key, cnt[key])
            else:
                o["sig"] = None
        for o in ops:
            if o["dma"] is not None and o["total"]:
                o["sig"] = (o["sig"][0], cnt[o["sig"][0]])
        for o in ops:
            w = {}
            for d in o["deps"]:
                k, v = ops[d]["sig"]
                if w.get(k, 0) < v:
                    w[k] = v
            o["waits"] = w
        for k in cnt:
            getsem(k)
        self.sems = sems
        self.cnt = cnt

    def emit(self, eng, e):
        waited = {}
        n = 0
        for o in self.ops:
            if o["eng"] != eng:
                continue
            for k in sorted(o["waits"]):
                v = o["waits"][k]
                if waited.get(k, 0) < v:
                    e.wait_ge(self.sems[k], v)
                    waited[k] = v
            ins = o["fn"](e)
            n += 1
            if o["dma"] is not None:
                if not isinstance(ins, (list, tuple)):
                    ins = [ins]
                assert len(ins) == o["ndma"], (len(ins), o["ndma"])
                for i_ in ins:
                    i_.then_inc(self.sems[o["sig"][0]], 16)
            elif o["sig"] is not None:
                if isinstance(ins, (list, tuple)):
                    ins = ins[-1]
                ins.then_inc(self.sems[o["sig"][0]], 1)
        return n


def _t5_bucket_np(dist, mode):
    n = np.maximum(dist, 0).astype(np.int32)
    nf = np.maximum(n, 1).astype(np.float32)
    val = (np.log(nf / np.float32(16)) / np.float32(math.log(128 / 16)) * np.float32(16)).astype(np.float32)
    if mode == "trunc":
        li = val.astype(np.int32)
    else:
        li = np.rint(val).astype(np.int32)
    large = np.minimum(16 + li, 31)
    return np.where(n < 16, n, large)


def _dtile_index():
    k = np.arange(128)[:, None]
    q = np.arange(128)[None, :]
    out = np.zeros((2, 2, 128, 128), np.int64)
    for hg, mode in ((0, MOBA_ROUND), (1, SWA_ROUND)):
        d0 = q - k
        b0 = _t5_bucket_np(d0, mode)
        out[hg, 1] = np.where(d0 >= 0, b0, 32)
        d1 = 128 + q - k
        b1 = _t5_bucket_np(d1, mode)
        if hg == 0:
            out[hg, 0] = b1
        else:
            out[hg, 0] = np.where(d1 < 128, b1, 32)
    return out


class Prog:
    def __init__(self, phases, debug=False, ngroups=8, stop=99):
        self.phases = phases
        self.stop = stop
        self.ngroups = ngroups
        self.debug = debug
        self.dbg_names = []
        self.nc = bass.Bass("TRN2", target_bir_lowering=False)
        self.sc = Sched()
        self.build()

    def dram_in(self, name, shape, dt=F32):
        return self.nc.dram_tensor(name, list(shape), dt, kind="ExternalInput").ap()

    def build(self):
        nc = self.nc
        sc = self.sc
        self.d_xT = self.dram_in("xT", [D, S])
        self.d_cT = self.dram_in("cT", [128, 8])
        self.d_wada = self.dram_in("wada", [DEPTH, 24, 128, 8, 256])
        self.d_badac = self.dram_in("badac", [128, DEPTH * 48])
        self.d_gainc = self.dram_in("gainc", [128, 128])
        self.d_b1c = self.dram_in("b1c", [128, 128])
        self.d_b2c = self.dram_in("b2c", [128, 32])
        self.d_w1r = self.dram_in("w1r", [DEPTH, 16, 128, 8, 256])
        self.d_w2r = self.dram_in("w2r", [DEPTH, 8, 2, 128, 16, 128])
        self.d_winr = self.dram_in("winr", [DEPTH, 128, 8, DIN])
        self.d_woutr = self.dram_in("woutr", [DEPTH, 128, 8, D])
        self.d_sinks = self.dram_in("sinks", [DEPTH, 8])
        self.d_rbT = self.dram_in("rbT", [1, 16 * 32])
        self.d_dtile = self.dram_in("dtile", [128, 16 * 2 * 128])
        self.d_ident = self.dram_in("ident", [128, 128])
        self.d_hsel = self.dram_in("hsel", [128, 2 * 128])
        self.d_hind = self.dram_in("hind", [128, 2])
        self.d_indall = self.dram_in("indall", [72, 8 * 128])
        self.d_bmask = self.dram_in("bmask", [128, 3 * 64])
        self.d_out = nc.dram_tensor("outT", [D, S], F32, kind="ExternalOutput").ap()

        total_words = 53200
        self.pool = nc.alloc_sbuf_tensor("pool", [128, total_words], F32)
        self.off = 0

        def alloc(words):
            o = self.off
            self.off += (words + 7) // 8 * 8
            assert self.off <= total_words, (self.off, total_words)
            return o

        def view(o, words, dt=F32):
            v = self.pool[:, o:o + words]
            if dt != F32:
                v = v.bitcast(dt)
            return v

        self.view = view
        o_x = alloc(8 * S)
        self.XT = view(o_x, 8 * S).rearrange("p (c t) -> p c t", c=8)
        self.COLS = view(alloc(320), 320)
        self.GAINC = view(alloc(128), 128)
        self.B1C = view(alloc(128), 128)
        self.B2C = view(alloc(32), 32)
        self.BADAC = view(alloc(192), 192)
        self.MODC = view(alloc(48), 48)
        self.CT = view(alloc(8), 8)
        self.CACT = view(alloc(8), 8, BF16)[:, 0:8]
        self.IDENT = view(alloc(64), 64, BF16)
        self.ONES = view(alloc(64), 64, BF16)
        self.SQ = [view(alloc(512), 512, BF16).rearrange("p (a t) -> p a t", a=2) for _ in range(2)]
        self.rstd_off = self.off
        self.RSTD = [view(alloc(512), 512) for _ in range(2)]
        self.XN = [view(alloc(512), 512) for _ in range(2)]
        self.wada_off = self.off
        self.WADA = [view(alloc(1024), 1024, BF16).rearrange("p (k n) -> p k n", k=8) for _ in range(2)]
        self.DUMMY = view(alloc(8), 8)
        sc.dummy = self.DUMMY
        self.EPSC = view(alloc(8), 8)
        self.phase_base = self.off

        self.PS = [nc.alloc_psum_tensor(f"psb{i}", [128, 512], F32) for i in range(8)]

        self.preamble()
        done_mod = set()
        for (kind, l) in self.phases:
            if l not in done_mod:
                self.mod_layer(l)
                done_mod.add(l)
            sc.barrier()
            if kind == "attn":
                self.attn_phase(l)
            else:
                self.ffn_phase(l)
        sc.barrier()
        self.epilogue()

        class _SemCtx:
            pass
        semlist = []

        def semctx(name):
            cm = nc.semaphore(name)
            h = cm.__enter__()
            semlist.append(cm)
            return h

        sc.finalize(nc, semctx)
        with nc.Block() as block:
            @block.tensor
            def _(e):
                sc.emit("pe", e)

            @block.scalar
            def _(e):
                sc.emit("act", e)

            @block.vector
            def _(e):
                sc.emit("dve", e)

            @block.gpsimd
            def _(e):
                sc.emit("pool", e)

            @block.sync
            def _(e):
                sc.emit("sp", e)

    def dump(self, name, ap, reads):
        if not getattr(self, "debug", False):
            return
        shp = list(ap.shape)
        d = self.nc.dram_tensor("dbg_" + name, shp, ap.dtype, kind="ExternalOutput").ap()
        self.sc.add("sp", (lambda e: e.dma_start(out=d, in_=ap)), reads=list(reads), writes=["dbg_" + name], dma="dbg_" + name)
        self.dbg_names.append("dbg_" + name)

    def psb(self, i, dt=F32):
        v = self.PS[i][:, :]
        if dt != F32:
            v = v.bitcast(dt)
        return v

    def preamble(self):
        sc = self.sc
        XT = self.XT
        xs = self.d_xT.rearrange("(c p) t -> p c t", p=128)
        for c in range(8):
            sc.add("sp", (lambda e, c=c: e.dma_start(out=XT[:, c, :], in_=xs[:, c, :])),
                   writes=[f"xTc{c}"], dma=f"xin{c}")
        small = [(self.GAINC, self.d_gainc, "gainc"), (self.B1C, self.d_b1c, "b1c"), (self.B2C, self.d_b2c, "b2c"),
                 (self.BADAC, self.d_badac, "badac"), (self.CT, self.d_cT, "ct")]
        for (dst, src, nm) in small:
            sc.add("sp", (lambda e, dst=dst, src=src: e.dma_start(out=dst, in_=src[:, :])),
                   writes=[nm], dma="c_" + nm)
        sc.add("pool", (lambda e: e.dma_start(out=self.IDENT, in_=self.d_ident[:, :])), writes=["ident"], dma="c_ident")
        sc.add("dve", (lambda e: e.memset(self.ONES, 1.0)), writes=["ones"])
        sc.add("dve", (lambda e: e.memset(self.EPSC, float(D * EPS))), writes=["epsc"])
        sc.add("act", (lambda e: e.activation(out=self.CACT, in_=self.CT, func=AF.Silu)), reads=["ct"], writes=["cact"])
        sc.marker(reads=[f"xTc{c}" for c in range(8)], writes=[f"xT{tb}" for tb in range(8)] + ["rgnA_ok", "rgnB_ok"])

    def epilogue(self):
        sc = self.sc
        XT = self.XT
        od = self.d_out.rearrange("(c p) t -> p c t", p=128)
        for c in range(8):
            sc.add("sp", (lambda e, c=c: e.dma_start(out=od[:, c, :], in_=XT[:, c, :])),
                   reads=[f"xT{tb}" for tb in range(8)], writes=[f"out{c}"], dma=f"xout{c}")
        sc.add("sp", (lambda e: e.nop()), reads=[f"out{c}" for c in range(8)])

    def mod_layer(self, l):
        sc = self.sc
        ps = self.psb(7)
        for pc in range(24):
            buf = self.WADA[pc % 2]
            bn = f"wada{pc % 2}"
            src = self.d_wada[l, pc]
            sc.add("pool", (lambda e, buf=buf, src=src: e.dma_start(out=buf, in_=src)), reads=["wada_ok"], writes=[bn], dma=bn)

            def mm(e, buf=buf, pc=pc):
                last = None
                for j in range(2):
                    col = pc * 2 + j
                    for kc in range(8):
                        last = e.matmul(ps[:, col:col + 1], buf[:, kc, j * 128:(j + 1) * 128],
                                        self.CACT[:, kc:kc + 1], start=(kc == 0), stop=(kc == 7))
                return last
            sc.add("pe", mm, reads=[bn, "cact"], writes=["ps7"])
        MODC = self.MODC
        sc.add("dve", (lambda e: e.tensor_tensor(out=MODC, in0=ps[:, 0:48], in1=self.BADAC[:, l * 48:(l + 1) * 48], op=ALU.add)),
               reads=["ps7", "badac"], writes=["modc"])
        C = self.COLS
        b = l * 64
        G = self.GAINC
        g0 = (l * 4) * 8

        def cols(e):
            e.scalar_tensor_tensor(out=C[:, b + 0:b + 8], in0=MODC[:, 8:16], scalar=1.0, in1=G[:, g0 + 0:g0 + 8], op0=ALU.add, op1=ALU.mult)
            e.tensor_copy(out=C[:, b + 8:b + 16], in_=MODC[:, 0:8])
            e.tensor_tensor(out=C[:, b + 16:b + 24], in0=MODC[:, 16:24], in1=G[:, g0 + 8:g0 + 16], op=ALU.mult)
            e.scalar_tensor_tensor(out=C[:, b + 24:b + 32], in0=MODC[:, 32:40], scalar=1.0, in1=G[:, g0 + 16:g0 + 24], op0=ALU.add, op1=ALU.mult)
            e.tensor_copy(out=C[:, b + 32:b + 40], in_=MODC[:, 24:32])
            return e.tensor_tensor(out=C[:, b + 40:b + 48], in0=MODC[:, 40:48], in1=G[:, g0 + 24:g0 + 32], op=ALU.mult)
        sc.add("dve", cols, reads=["modc", "gainc"], writes=[f"colsraw{l}"])

        def cols2(e):
            e.tensor_scalar(out=C[:, b + 0:b + 8], in0=C[:, b + 0:b + 8], scalar1=32.0, scalar2=None, op0=ALU.mult)
            e.tensor_scalar(out=C[:, b + 16:b + 32], in0=C[:, b + 16:b + 32], scalar1=32.0, scalar2=None, op0=ALU.mult)
            return e.tensor_scalar(out=C[:, b + 40:b + 48], in0=C[:, b + 40:b + 48], scalar1=32.0, scalar2=None, op0=ALU.mult)
        sc.add("dve", cols2, reads=[f"colsraw{l}"], writes=[f"cols{l}"])
        self.dump(f"cols{l}", C[:, b:b + 48], [f"cols{l}"])
        self.dump(f"modc{l}", MODC, [f"cols{l}"])

    def rmsnorm_in(self, l, sub, t0, n, HT, ht_res, psbank, extra_reads=(), sq_names=None):
        sc = self.sc
        XT = self.XT
        tbs = [f"xT{tb}" for tb in range(t0 // 256, (t0 + n) // 256)]
        ps = self.psb(psbank)
        cb = l * 64 + (0 if sub == 0 else 24)
        C = self.COLS
        for cp in range(4):
            sq = self.SQ[cp % 2]
            sqn = f"sq{cp % 2}"
            sqw = [sqn] if sq_names is None else sq_names[cp % 2]
            sc.add("act", (lambda e, cp=cp, sq=sq: e.activation(out=sq[:, :, 0:n], in_=XT[:, 2 * cp:2 * cp + 2, t0:t0 + n], func=AF.Square)),
                   reads=tbs, writes=sqw)

            def mm(e, cp=cp, sq=sq):
                last = None
                for j in range(2):
                    c = 2 * cp + j
                    last = e.matmul(ps[:, 0:n], self.ONES, sq[:, j, 0:n], start=(c == 0), stop=(c == 7))
                return last
            sc.add("pe", mm, reads=[sqn, "ones"], writes=[f"ps{psbank}"])
        rs = self.RSTD[0]
        sc.add("act", (lambda e: e.activation(out=rs[:, 0:n], in_=ps[:, 0:n], func=AF.Ln, bias=self.EPSC[:, 0:1], scale=1.0)),
               reads=[f"ps{psbank}", "epsc"], writes=["rstd0p", "rstd0"])
        sc.add("act", (lambda e: e.activation(out=rs[:, 0:n], in_=rs[:, 0:n], func=AF.Exp, scale=-0.5)),
               reads=["rstd0p"], writes=["rstd0", "rstd0p"])
        for c in range(8):
            xn = self.XN[c % 2]
            xnn = f"xn{c % 2}"
            sc.add("dve", (lambda e, c=c, xn=xn: e.tensor_tensor(out=xn[:, 0:n], in0=XT[:, c, t0:t0 + n], in1=rs[:, 0:n], op=ALU.mult)),
                   reads=tbs + ["rstd0"], writes=[xnn])
            sc.add("act", (lambda e, c=c, xn=xn: e.activation(out=HT[:, c, 0:n], in_=xn[:, 0:n], func=AF.Identity,
                                                               scale=C[:, cb + c:cb + c + 1], bias=C[:, cb + 8 + c:cb + 9 + c])),
                   reads=[xnn, f"cols{l}"] + list(extra_reads), writes=[ht_res])

    def resid_update(self, l, sub, t0, n, Y, y_res, ssbank):
        sc = self.sc
        XT = self.XT
        tbs = [f"xT{tb}" for tb in range(t0 // 256, (t0 + n) // 256)]
        ps = self.psb(ssbank)
        C = self.COLS
        cb = l * 64 + (16 if sub == 0 else 40)
        rs = self.RSTD[1]
        sc.add("act", (lambda e: e.activation(out=rs[:, 0:n], in_=ps[:, 0:n], func=AF.Ln, bias=self.EPSC[:, 0:1], scale=1.0)),
               reads=[f"ps{ssbank}", "epsc"], writes=["rstd1p", "rstd1"])
        sc.add("act", (lambda e: e.activation(out=rs[:, 0:n], in_=rs[:, 0:n], func=AF.Exp, scale=-0.5)),
               reads=["rstd1p"], writes=["rstd1", "rstd1p"])
        if t0 == 0 and sub == 1:
            self.dump("rstd1", rs, ["rstd1"])
            self.dump("ysb", Y, [y_res(c) for c in range(8)])
        for c in range(8):
            sc.add("dve", (lambda e, c=c: e.scalar_tensor_tensor(out=Y[:, c, 0:n], in0=Y[:, c, 0:n], scalar=C[:, cb + c:cb + c + 1], in1=rs[:, 0:n],
                                                                 op0=ALU.mult, op1=ALU.mult)),
                   reads=[y_res(c), "rstd1", f"cols{l}"], writes=[y_res(c)])
            sc.add("dve", (lambda e, c=c: e.tensor_tensor(out=XT[:, c, t0:t0 + n], in0=XT[:, c, t0:t0 + n], in1=Y[:, c, 0:n], op=ALU.add)),
                   reads=[y_res(c)] + tbs, writes=tbs)

    def ffn_phase(self, l):
        sc = self.sc
        view = self.view
        base = self.phase_base
        o = base
        HID = view(o, 32 * 1024 // 2, BF16).rearrange("p (m t) -> p m t", m=32); o += 16384
        rgn = o; o += 8192
        HT = view(rgn, 4096, BF16).rearrange("p (c t) -> p c t", c=8)
        W1B = [view(rgn + 4096 + i * 1024, 1024, BF16).rearrange("p (k n) -> p k n", k=8) for i in range(3)]
        RL = [view(rgn + 4096 + 3072 + i * 512, 512) for i in range(2)]
        YSB = view(rgn, 8192).rearrange("p (c t) -> p c t", c=8)
        W2B = [view(o + i * 1024, 1024, BF16).rearrange("p (k n) -> p k n", k=16) for i in range(3)]; o += 3072
        assert o <= 53200, o
        B1C, B2C = self.B1C, self.B2C
        w1cnt = 0
        w2cnt = 0
        for H in range(2):
            T0 = H * 1024
            region_users = ["rgnA_ok"]
            for tg in range(2):
                self.rmsnorm_in(l, 1, T0 + tg * 512, 512, HT[:, :, tg * 512:(tg + 1) * 512], f"ht{tg}", 6,
                                extra_reads=region_users)
            if H == 0:
                self.dump("ht", HT, ["ht0", "ht1"])
            psi = 0
            for g in range(16):
                wb = W1B[w1cnt % 3]; wn = f"w1b{w1cnt % 3}"; w1cnt += 1
                src = self.d_w1r[l, g]
                sc.add("pool", (lambda e, wb=wb, src=src: e.dma_start(out=wb, in_=src)), reads=region_users, writes=[wn], dma=wn)
                for mm_ in range(2):
                    m = 2 * g + mm_
                    for tg in range(2):
                        bank = psi % 4; psi += 1
                        ps = self.psb(bank)

                        def mm(e, wb=wb, mm_=mm_, tg=tg, ps=ps):
                            last = None
                            for c in range(8):
                                last = e.matmul(ps, wb[:, c, mm_ * 128:(mm_ + 1) * 128], HT[:, c, tg * 512:(tg + 1) * 512],
                                                start=(c == 0), stop=(c == 7))
                            return last
                        sc.add("pe", mm, reads=[wn, f"ht{tg}"], writes=[f"ps{bank}"])
                        rl = RL[psi % 2]; rln = f"rl{psi % 2}"
                        sc.add("act", (lambda e, rl=rl, ps=ps, m=m: e.activation(out=rl, in_=ps, func=AF.Relu,
                                                                                 bias=B1C[:, l * 32 + m:l * 32 + m + 1])),
                               reads=[f"ps{bank}", "b1c"] + region_users, writes=[rln])
                        sc.add("dve", (lambda e, rl=rl, m=m, tg=tg: e.tensor_tensor(out=HID[:, m, tg * 512:(tg + 1) * 512], in0=rl, in1=rl, op=ALU.mult)),
                               reads=[rln], writes=[f"hid{m}_{tg}"])
            if H == 0:
                self.dump("hid", HID, [f"hid{m}_{tg}" for m in range(32) for tg in range(2)])
            sc.marker(writes=["ht0", "ht1", "w1b0", "w1b1", "w1b2", "rl0", "rl1", "rgnB_ok"])
            ht_users = ["rgnB_ok"]
            for o_ in range(8):
                wbs = []
                for kh in range(2):
                    wb = W2B[w2cnt % 3]; wn = f"w2b{w2cnt % 3}"; w2cnt += 1
                    src = self.d_w2r[l, o_, kh]
                    sc.add("pool", (lambda e, wb=wb, src=src: e.dma_start(out=wb, in_=src)), writes=[wn], dma=wn)
                    wbs.append((wb, wn))
                banks = [(o_ % 2) * 2, (o_ % 2) * 2 + 1]
                for kh in range(2):
                    wb, wn = wbs[kh]
                    for tg in range(2):
                        ps = self.psb(banks[tg])

                        def mm(e, wb=wb, kh=kh, tg=tg, ps=ps):
                            last = None
                            for kk in range(16):
                                m = kh * 16 + kk
                                last = e.matmul(ps, wb[:, kk, :], HID[:, m, tg * 512:(tg + 1) * 512],
                                                start=(m == 0), stop=(m == 31))
                            return last
                        sc.add("pe", mm, reads=[wn] + [f"hid{kh * 16 + kk}_{tg}" for kk in range(16)], writes=[f"ps{banks[tg]}"])
                for tg in range(2):
                    ps = self.psb(banks[tg])
                    ysl = YSB[:, o_, tg * 512:(tg + 1) * 512]
                    sc.add("act", (lambda e, ps=ps, ysl=ysl, o_=o_: e.activation(out=ysl, in_=ps, func=AF.Identity,
                                                                                  bias=B2C[:, l * 8 + o_:l * 8 + o_ + 1])),
                           reads=[f"ps{banks[tg]}", "b2c"] + ht_users, writes=[f"ysb{o_}t{tg}"])
                    sq = self.SQ[tg][:, 0, :]
                    sc.add("dve", (lambda e, ysl=ysl, sq=sq: e.tensor_tensor(out=sq, in0=ysl, in1=ysl, op=ALU.mult)),
                           reads=[f"ysb{o_}t{tg}"], writes=[f"sq{tg}"])
                    ssb = 4 + tg
                    sc.add("pe", (lambda e, sq=sq, ssb=ssb, o_=o_: e.matmul(self.psb(ssb), self.ONES, sq, start=(o_ == 0), stop=(o_ == 7))),
                           reads=[f"sq{tg}", "ones"], writes=[f"ps{ssb}"])
            for tg in range(2):
                self.resid_update(l, 1, T0 + tg * 512, 512, YSB[:, :, tg * 512:(tg + 1) * 512],
                                  (lambda c, tg=tg: f"ysb{c}t{tg}"), 4 + tg)
            sc.marker(writes=[f"ysb{c}t{tg}" for c in range(8) for tg in range(2)] + ["rgnA_ok"])

    def attn_phase(self, l):
        sc = self.sc
        view = self.view
        XT = self.XT
        o = self.phase_base
        KT = view(o, 5120, BF16).rearrange("p (c t) -> p c t", c=5); o += 5120
        VA = view(o, 5200, BF16).rearrange("p (t h d) -> p t h d", t=16, h=10); o += 5200
        WIN = view(o, 9216, BF16).rearrange("p (k n) -> p k n", k=8); o += 9216
        WOUT = view(o, 4096, BF16).rearrange("p (k n) -> p k n", k=8); o += 4096
        DT = view(o, 2048, BF16).rearrange("p (h t q) -> p h t q", h=16, t=2); o += 2048
        rA = o; o += 1024
        rC = o; o += 1024
        rB = o; o += 1024
        HTG = view(rA, 1024, BF16).rearrange("p (c t) -> p c t", c=8)
        OG = view(rA, 1024, BF16).rearrange("p (q f) -> p q f", q=2)
        QTG = view(rC, 1024, BF16).rearrange("p (c t) -> p c t", c=8)
        QSQ = view(rB, 1024, BF16).rearrange("p (c t) -> p c t", c=8)
        OTG = view(rB, 1024, BF16).rearrange("p (c t) -> p c t", c=8)
        YSBA = view(rA, 2048).rearrange("p (c t) -> p c t", c=8)
        AUGT1 = [view(o + i * 128, 128, BF16) for i in range(3)]; o += 384
        IND = view(o, 512, BF16).rearrange("p (j k) -> p j k", j=8); o += 512
        HIND = view(o, 8, BF16)[:, 0:2]; o += 8
        KMEANT = view(o, 32, BF16).rearrange("p (c r j) -> p c r j", c=4, r=2); o += 32
        KSUM = view(o, 8, F32); o += 8
        KMAX2 = view(o, 8, F32); o += 8
        KMXG = view(o, 8, F32); o += 8
        KM16 = view(o, 16, F32); o += 16
        QMXG = view(o, 8, F32); o += 8
        QM16 = view(o, 16, F32); o += 16
        SINKS = view(o, 8, F32); o += 8
        BM8 = view(o, 16, F32); o += 16
        RB = self.XN[0].rearrange("p (h b) -> p h b", h=16)
        B31 = view(o, 16, F32); o += 16
        KSQ = view(rB, 640, BF16).rearrange("p (c t) -> p c t", c=5)
        RDEN = view(o, 8, F32); o += 8
        assert o <= 53200, o
        wsc = self.wada_off
        CMP = view(wsc, 1024).rearrange("p (a j k) -> p a j k", a=16, j=8)
        AUGB = view(wsc + 1024, 128, BF16).rearrange("p (q s j) -> p q s j", q=2, s=16)
        BMASK = view(wsc + 1600, 192).rearrange("p (k b j) -> p k b j", k=3, b=8)
        GM = view(wsc + 1792, 128).rearrange("p (a j) -> p a j", a=16)
        SEL = view(wsc + 1920, 128).rearrange("p (a j) -> p a j", a=16)
        rs_off = self.rstd_off
        SELF = view(rs_off + 256, 256).rearrange("p (q s j) -> p q s j", q=2, s=16)
        SH8 = view(rs_off + 512 + 256, 32).rearrange("p (q s) -> p q s", q=2)
        SINKT = view(rs_off + 512 + 288, 16).rearrange("p (q s) -> p q s", q=2)
        SQRT_T = view(rs_off + 512 + 304, 32).rearrange("p (q s) -> p q s", q=2)
        TMPS = [self.XN[0], self.XN[1]]
        PTS = [self.SQ[0].rearrange("p a t -> p (a t)")[:, 0:512], self.SQ[0].rearrange("p a t -> p (a t)")[:, 512:1024],
               self.SQ[1].rearrange("p a t -> p (a t)")[:, 0:512], self.SQ[1].rearrange("p a t -> p (a t)")[:, 512:1024]]
        PTN = ["sq0", "sq0b", "sq1", "sq1b"]
        IDENT = self.IDENT.rearrange("p (a b) -> p a b", a=1)[:, 0, :]

        sc.marker(writes=["wada0", "wada1", "rstd0", "rstd1", "rstd0p", "rstd1p", "wadafree"])
        sc.add("pool", (lambda e: e.dma_start(out=WIN, in_=self.d_winr[l])), writes=["win"], dma="win")
        sc.add("pool", (lambda e: e.dma_start(out=DT.rearrange("p h t q -> p (h t q)"), in_=self.d_dtile[:, :])), writes=["dt"], dma="dt")
        sc.add("pool", (lambda e: e.dma_start(out=IND.rearrange("p j k -> p (j k)")[0:72, :], in_=self.d_indall[:, :])), writes=["ind"], dma="ind")
        sc.add("pool", (lambda e: e.dma_start(out=HIND, in_=self.d_hind[:, :])), writes=["hind"], dma="hind")
        sc.add("sp", (lambda e: e.dma_start(out=BMASK.rearrange("p k b j -> p (k b j)"), in_=self.d_bmask[:, :])), reads=["wadafree"], writes=["bmask"], dma="bmask")
        sc.add("sp", (lambda e: e.dma_start(out=SINKS, in_=self.d_sinks[l:l + 1, :].partition_broadcast(128))), writes=["sinks"], dma="sinks")
        sc.add("sp", (lambda e: e.dma_start(out=RB.rearrange("p h b -> p (h b)"), in_=self.d_rbT[0:1, :].partition_broadcast(128))), writes=["xn0"], dma="rb")
        sc.add("pool", (lambda e: e.dma_start(out=WOUT, in_=self.d_woutr[l])), writes=["wout"], dma="wout")

        def init1(e):
            e.memset(KMEANT, 0.0)
            e.memset(KMAX2, 0.0)
            e.memset(VA[:, :, :, 64:65], 1.0)
            e.memset(AUGB, 0.0)
            e.memset(SELF, 1.0)
            e.tensor_reduce(out=BM8, in_=RB, axis=AX.X, op=ALU.max)
            e.tensor_copy(out=B31, in_=RB[:, :, 31])
            return e.memset(KM16, 0.0)
        sc.add("dve", init1, reads=["xn0", "wadafree"], writes=["kmeant", "kmax2", "va_ones", "augb", "selfm", "bm8raw", "b31", "km16"])

        sc.add("dve", (lambda e: e.tensor_tensor(out=BM8[:, 8:16], in0=BM8[:, 8:16], in1=SINKS, op=ALU.max)),
               reads=["bm8raw", "sinks"], writes=["bm8raw"])
        sc.add("dve", (lambda e: e.tensor_scalar(out=BM8, in0=BM8, scalar1=8.0, scalar2=None, op0=ALU.mult)),
               reads=["bm8raw"], writes=["bm8", "bm8raw"])

        QCOL, KCOL, VCOL, QBCOL, KBCOL, VBCOL = 0, 512, 1024, 1536, 2048, 2176
        sbank = [0]
        ptc = [0]

        def group(g):
            t0 = g * 256
            b = g
            xres = [f"xT{g}"]
            self.rmsnorm_in(l, 0, t0, 256, HTG, "rA", 7, sq_names=(["sq0", "sq0b"], ["sq1", "sq1b"]))
            pbank = [0]

            def nextbank():
                bk = 4 + pbank[0] % 4
                pbank[0] += 1
                return bk
            for ci in range(8):
                col = QCOL + ci * 128 if ci < 4 else QBCOL + (ci - 4) * 128
                bk = nextbank()
                ps = self.psb(bk)

                def mm(e, col=col, ps=ps):
                    last = None
                    for kc in range(8):
                        last = e.matmul(ps[:, 0:256], WIN[:, kc, col:col + 128], HTG[:, kc, :], start=(kc == 0), stop=(kc == 7))
                    return last
                sc.add("pe", mm, reads=["win", "rA"], writes=[f"ps{bk}"])
                sc.add("dve", (lambda e, ci=ci, ps=ps: e.tensor_copy(out=QTG[:, ci, :], in_=ps[:, 0:256])),
                       reads=[f"ps{bk}"], writes=["rC"])
            sc.add("dve", (lambda e: e.memset(KSUM, 0.0)), writes=[f"ksum{c}" for c in range(4)])
            for ci in range(5):
                col = KCOL + ci * 128 if ci < 4 else KBCOL
                bk = nextbank()
                ps = self.psb(bk)

                def mm(e, col=col, ps=ps):
                    last = None
                    for kc in range(8):
                        last = e.matmul(ps[:, 0:256], WIN[:, kc, col:col + 128], HTG[:, kc, :], start=(kc == 0), stop=(kc == 7))
                    return last
                sc.add("pe", mm, reads=["win", "rA"], writes=[f"ps{bk}"])
                if ci < 4:
                    sc.add("act", (lambda e, ci=ci, ps=ps: e.activation(out=KT[:, ci, t0:t0 + 256], in_=ps[:, 0:256], func=AF.Copy,
                                                                           accum_out=KSUM[:, ci:ci + 1])),
                           reads=[f"ps{bk}"], writes=[f"kt{ci}_{g}", f"ksum{ci}"])
                else:
                    sc.add("act", (lambda e, ci=ci, ps=ps: e.activation(out=KT[:, ci, t0:t0 + 256], in_=ps[:, 0:256], func=AF.Copy)),
                           reads=[f"ps{bk}"], writes=[f"kt{ci}_{g}"])
            for qt in range(2):
                tile_i = g * 2 + qt
                bk = nextbank()
                ps = self.psb(bk)

                def mmv(e, qt=qt, ps=ps):
                    last = None
                    for kc in range(8):
                        last = e.matmul(ps[:, 0:512], HTG[:, kc, qt * 128:(qt + 1) * 128], WIN[:, kc, VCOL:VCOL + 512], start=(kc == 0), stop=(kc == 7))
                    return last
                sc.add("pe", mmv, reads=["win", "rA"], writes=[f"ps{bk}"])
                sc.add("act", (lambda e, tile_i=tile_i, ps=ps: e.activation(out=VA[:, tile_i, 0:8, 0:64],
                                                                               in_=ps[:, 0:512].rearrange("p (h d) -> p h d", h=8), func=AF.Copy)),
                       reads=[f"ps{bk}", "va_ones"], writes=[f"va{g}_{qt}a"])
                bk2 = nextbank()
                ps2 = self.psb(bk2)

                def mmv2(e, qt=qt, ps2=ps2):
                    last = None
                    for kc in range(8):
                        last = e.matmul(ps2[:, 0:128], HTG[:, kc, qt * 128:(qt + 1) * 128], WIN[:, kc, VBCOL:VBCOL + 128], start=(kc == 0), stop=(kc == 7))
                    return last
                sc.add("pe", mmv2, reads=["win", "rA"], writes=[f"ps{bk2}"])
                sc.add("dve", (lambda e, tile_i=tile_i, ps2=ps2: e.tensor_copy(out=VA[:, tile_i, 8:10, 0:64],
                                                                                 in_=ps2[:, 0:128].rearrange("p (h d) -> p h d", h=2))),
                       reads=[f"ps{bk2}", "va_ones"], writes=[f"va{g}_{qt}b"])
            if getattr(self, 'stop', 99) <= 2:
                return
            def kmw(e, b=b):
                e.tensor_scalar(out=KMEANT[0:64, :, 0, b], in0=KSUM[0:64, 0:4], scalar1=1.0 / 256.0, scalar2=None, op0=ALU.mult)
                return e.tensor_scalar(out=KMEANT[64:128, :, 1, b], in0=KSUM[64:128, 0:4], scalar1=1.0 / 256.0, scalar2=None, op0=ALU.mult)
            sc.add("dve", kmw, reads=[f"ksum{c}" for c in range(4)], writes=["kmeant"])
            sc.add("dve", (lambda e: e.tensor_tensor(out=KSQ, in0=KT[:, 0:5, t0:t0 + 256], in1=KT[:, 0:5, t0:t0 + 256], op=ALU.mult)),
                   reads=[f"kt{c}_{g}" for c in range(5)], writes=["rB", "rBb"])
            if getattr(self, 'stop', 99) <= 2.2:
                return
            for bi, cs in enumerate([(0, 1), (2, 3), (4,)]):
                bk = 4 + bi
                ps = self.psb(bk).rearrange("p (a t) -> p a t", a=2)

                def mmk(e, cs=cs, ps=ps):
                    last = None
                    for a, c in enumerate(cs):
                        last = e.matmul(ps[:, a, :], self.ONES, KSQ[:, c, :], start=True, stop=True)
                    return last
                sc.add("pe", mmk, reads=["rB", "ones"], writes=[f"ps{bk}"])
                sc.add("dve", (lambda e, cs=cs, ps=ps: e.tensor_reduce(out=KMXG[:, cs[0]:cs[0] + len(cs)], in_=ps[:, 0:len(cs), :], axis=AX.X, op=ALU.max)),
                       reads=[f"ps{bk}"], writes=["kmxg"])

            if getattr(self, 'stop', 99) <= 2.4:
                return
            sc.add("dve", (lambda e: e.tensor_tensor(out=KMAX2[:, 0:5], in0=KMAX2[:, 0:5], in1=KMXG[:, 0:5], op=ALU.max)),
                   reads=["kmxg", "kmax2"], writes=["kmax2"])

            def kmax(e):
                e.tensor_copy(out=KM16[:, 0:8].rearrange("p (c r) -> p c r", r=2), in_=KMAX2[:, 0:4].unsqueeze(2).to_broadcast([128, 4, 2]))
                return e.tensor_copy(out=KM16[:, 8:16], in_=KMAX2[:, 4:5].to_broadcast([128, 8]))
            sc.add("dve", kmax, reads=["kmax2"], writes=["km16"])
            if getattr(self, 'stop', 99) <= 2.6:
                return
            sc.add("dve", (lambda e: e.tensor_tensor(out=QSQ, in0=QTG, in1=QTG, op=ALU.mult)), reads=["rC"], writes=["rB", "rBb"])
            ps7 = self.psb(7)
            GATE = ps7[:, 0:128].rearrange("p (a j) -> p a j", a=16)

            for bi in range(4):
                psq = self.psb(bi).rearrange("p (a t) -> p a t", a=2)

                def mmq(e, bi=bi, psq=psq):
                    last = None
                    for a in range(2):
                        last = e.matmul(psq[:, a, :], self.ONES, QSQ[:, 2 * bi + a, :], start=True, stop=True)
                    return last
                sc.add("pe", mmq, reads=["rB", "ones"], writes=[f"ps{bi}"])
                sc.add("dve", (lambda e, bi=bi, psq=psq: e.tensor_reduce(out=QMXG[:, 2 * bi:2 * bi + 2], in_=psq, axis=AX.X, op=ALU.max)),
                       reads=[f"ps{bi}"], writes=["qmxg"])
            sc.add("dve", (lambda e: e.tensor_copy(out=QM16.rearrange("p (c r) -> p c r", r=2), in_=QMXG.unsqueeze(2).to_broadcast([128, 8, 2]))),
                   reads=["qmxg"], writes=["qm16"])

            def mmg(e):
                last = None
                for qt in range(2):
                    for c in range(4):
                        last = e.matmul(ps7[:, (qt * 8 + 2 * c) * 8:(qt * 8 + 2 * c + 2) * 8], QTG[:, c, qt * 128:(qt + 1) * 128],
                                        KMEANT[:, c, :, :].rearrange("p r j -> p (r j)"), start=True, stop=True)
                return last
            sc.add("pe", mmg, reads=["rC", "kmeant"], writes=["ps7"])
            if getattr(self, 'stop', 99) <= 3:
                return
            NEGM = BMASK[:, 0, b, :]
            ELIG = BMASK[:, 1, b, :]
            OWN = BMASK[:, 2, b, :]

            sc.add("dve", (lambda e: e.tensor_tensor(out=GM, in0=GATE, in1=NEGM.unsqueeze(1).to_broadcast([128, 16, 8]), op=ALU.add)),
                   reads=["ps7", "bmask"], writes=["gm"])
            sc.add("dve", (lambda e: e.tensor_tensor(out=CMP, in0=GM.unsqueeze(2).to_broadcast([128, 16, 8, 8]),
                                                     in1=GM.unsqueeze(3).to_broadcast([128, 16, 8, 8]), op=ALU.is_gt)),
                   reads=["gm"], writes=["cmp"])
            sc.add("dve", (lambda e: e.tensor_reduce(out=SEL, in_=CMP, axis=AX.X, op=ALU.add)), reads=["cmp"], writes=["selr"])
            sc.add("dve", (lambda e: e.scalar_tensor_tensor(out=SEL, in0=SEL, scalar=3.0, in1=ELIG.unsqueeze(1).to_broadcast([128, 16, 8]),
                                                            op0=ALU.is_lt, op1=ALU.mult)),
                   reads=["selr", "bmask"], writes=["selr"])
            sc.add("dve", (lambda e: e.tensor_tensor(out=SELF[:, :, 0:8, :], in0=SEL.rearrange("p (q h) j -> p q h j", q=2),
                                                     in1=OWN.unsqueeze(1).unsqueeze(1).to_broadcast([128, 2, 8, 8]), op=ALU.add)),
                   reads=["selr", "bmask", "selfm"], writes=["selfm"])
            sc.add("dve", (lambda e: e.tensor_tensor(out=SQRT_T, in0=QM16.unsqueeze(1).to_broadcast([128, 2, 16]),
                                                     in1=KM16.unsqueeze(1).to_broadcast([128, 2, 16]), op=ALU.mult)),
                   reads=["qm16", "km16"], writes=["sqrt_t"])
            sc.add("act", (lambda e: e.activation(out=SQRT_T, in_=SQRT_T, func=AF.Ln, bias=self.EPSC[:, 0:1], scale=1.0)),
                   reads=["sqrt_t", "epsc"], writes=["sqrt_t"])
            sc.add("act", (lambda e: e.activation(out=SQRT_T, in_=SQRT_T, func=AF.Exp, scale=0.5)),
                   reads=["sqrt_t"], writes=["sqrt_t"])
            AUGBv = AUGB

            def augops(e):
                e.tensor_tensor(out=SH8, in0=SQRT_T, in1=BM8.unsqueeze(1).to_broadcast([128, 2, 16]), op=ALU.add)
                return e.tensor_scalar(out=SELF, in0=SELF, scalar1=BIG, scalar2=-BIG, op0=ALU.mult, op1=ALU.add)
            sc.add("dve", augops, reads=["sqrt_t", "bm8", "selfm"], writes=["sh8", "selfm"])
            sc.add("dve", (lambda e: e.tensor_tensor(out=AUGBv[:, :, 0:16, 0:8], in0=SELF, in1=SH8.unsqueeze(3).to_broadcast([128, 2, 16, 8]), op=ALU.subtract)),
                   reads=["sh8", "selfm"], writes=["augb"])

            def sinkops(e):
                e.memset(SELF[:, :, 8:16, :], 1.0)
                return e.scalar_tensor_tensor(out=SINKT, in0=AUGBv[:, :, 8:16, 0], scalar=0.125, in1=SINKS.unsqueeze(1).to_broadcast([128, 2, 8]),
                                              op0=ALU.mult, op1=ALU.add)
            sc.add("dve", sinkops, reads=["augb", "sinks"], writes=["sinkt", "selfm"])
            sc.add("act", (lambda e: e.activation(out=SINKT, in_=SINKT, func=AF.Exp)), reads=["sinkt"], writes=["sinkt"])
            if getattr(self, 'stop', 99) <= 4:
                return
            for grp in range(2):
                def prep(s16):
                    ps7b = self.psb(7, BF16)
                    AT = AUGT1[s16 % 3]

                    def tr(e):
                        last = None
                        for qt in range(2):
                            last = e.transpose(ps7b[0:8, qt * 128:(qt + 1) * 128], AUGB[:, qt, s16, :], IDENT)
                        return last
                    sc.add("pe", tr, reads=["augb", "ident"], writes=["ps7"])
                    sc.add("act", (lambda e: e.activation(out=AT[0:8, 0:256], in_=ps7b[0:8, 0:256], func=AF.Copy)),
                           reads=["ps7"], writes=[f"augt{s16 % 3}"])
                if grp == 0:
                    prep(0)

                def head(hs8):
                    s16 = grp * 8 + hs8
                    if s16 + 1 < 16:
                        prep(s16 + 1)
                    AT = AUGT1[s16 % 3]
                    pb = 0
                    if grp == 0:
                        ck, r0, cq, vh = hs8 // 2, (hs8 % 2) * 64, hs8 // 2, hs8
                    else:
                        i_, r_ = hs8 // 2, hs8 % 2
                        ck, r0, cq, vh = 4, r_ * 64, 4 + i_, 8 + r_
                    quad, hsl = hs8 // 4, hs8 % 4
                    augres = f"augt{s16 % 3}"
                    far = []
                    if grp == 0 and b >= 1:
                        for kc in range(0, 2 * b - 1):
                            far.append((kc, 0, 256))
                        far.append((2 * b - 1, 128, 128))
                    near = [(2 * b - 1, 0, 0), (2 * b, 0, 1), (2 * b, 1, 0), (2 * b + 1, 1, 1)]
                    if b == 0:
                        near = near[1:]
                    n_qt = [0, 0]
                    for (kc, q0, qn) in far:
                        for sub in range(qn // 128):
                            n_qt[(q0 + sub * 128) // 128] += 1
                    for (kc, qt, _) in near:
                        n_qt[qt] += 1
                    done_qt = [0, 0]
                    banks = []
                    cur, tot = [], 0
                    for p_ in far:
                        if tot + p_[2] > 512:
                            banks.append(cur)
                            cur, tot = [], 0
                        cur.append(p_)
                        tot += p_[2]
                    if cur:
                        banks.append(cur)
                    for pieces in banks:
                        bk = 4 + sbank[0] % 3
                        sbank[0] += 1
                        ps = self.psb(bk)
                        pti = ptc[0] % 4
                        ptc[0] += 1
                        PT = PTS[pti]
                        offs = []
                        off = 0
                        for p_ in pieces:
                            offs.append(off)
                            off += p_[2]
                        tot = off

                        def mms(e, pieces=pieces, offs=offs, ps=ps):
                            last = None
                            for (kc, q0, qn), of in zip(pieces, offs):
                                e.matmul(ps[:, of:of + qn], KT[r0:r0 + 64, ck, kc * 128:(kc + 1) * 128], QTG[r0:r0 + 64, cq, q0:q0 + qn],
                                         start=True, stop=False)
                                last = e.matmul(ps[:, of:of + qn], IND[pb:pb + 8, kc // 2, :], AT[0:8, q0:q0 + qn],
                                                start=False, stop=True)
                            return last
                        sc.add("pe", mms, reads=[f"kt{ck}_{p_[0] // 2}" for p_ in pieces] + ["rC", "ind", augres], writes=[f"ps{bk}"])
                        sc.add("act", (lambda e, PT=PT, ps=ps, tot=tot: e.activation(out=PT[:, 0:tot], in_=ps[:, 0:tot], func=AF.Exp,
                                                                                       scale=0.125, bias=B31[:, s16:s16 + 1])),
                               reads=[f"ps{bk}", "b31"], writes=[PTN[pti]])
                        flags = []
                        for (kc, q0, qn), of in zip(pieces, offs):
                            for sub in range(qn // 128):
                                qt = (q0 + sub * 128) // 128
                                st = done_qt[qt] == 0
                                done_qt[qt] += 1
                                sp_ = done_qt[qt] == n_qt[qt]
                                flags.append((kc, qt, of + sub * 128, st, sp_))

                        def pv(e, flags=flags, PT=PT):
                            last = None
                            for (kc, qt, of, st, sp_) in flags:
                                last = e.matmul(self.psb(qt * 2 + quad).rearrange("p (h d) -> p h d", d=65)[:, hsl, 0:65] if False else
                                                self.oacc(qt * 2 + quad)[:, hsl, :], PT[:, of:of + 128], VA[:, kc, vh, :], start=st, stop=sp_)
                            return last
                        sc.add("pe", pv, reads=[PTN[pti]] + [f"va{p_[0] // 2}_{p_[0] % 2}{'a' if grp == 0 else 'b'}" for p_ in pieces],
                               writes=[f"ps{quad}", f"ps{2 + quad}"])
                    bk = 4 + sbank[0] % 3
                    sbank[0] += 1
                    ps = self.psb(bk).rearrange("p (a t) -> p a t", a=4)
                    pti = ptc[0] % 4
                    ptc[0] += 1
                    PT = PTS[pti].rearrange("p (a t) -> p a t", a=4)
                    TMP = TMPS[pti % 2].rearrange("p (a t) -> p a t", a=4)
                    tmpn = f"xn{pti % 2}"
                    ti0 = 4 - len(near)

                    def mmn(e, near=near, ps=ps, ti0=ti0):
                        last = None
                        for k_, (kc, qt, di) in enumerate(near):
                            ti = ti0 + k_
                            e.matmul(ps[:, ti, :], KT[r0:r0 + 64, ck, kc * 128:(kc + 1) * 128], QTG[r0:r0 + 64, cq, qt * 128:(qt + 1) * 128],
                                     start=True, stop=False)
                            last = e.matmul(ps[:, ti, :], IND[pb:pb + 8, kc // 2, :], AT[0:8, qt * 128:(qt + 1) * 128],
                                            start=False, stop=True)
                        return last
                    sc.add("pe", mmn, reads=[f"kt{ck}_{kc // 2}" for (kc, _, _) in near] + ["rC", "ind", augres], writes=[f"ps{bk}"])

                    def biasadd(e, ps=ps, TMP=TMP, ti0=ti0):
                        if ti0 == 0:
                            e.scalar_tensor_tensor(out=TMP[:, 0:2, :], in0=ps[:, 0:2, :], scalar=0.125, in1=DT[:, s16, 0:2, :], op0=ALU.mult, op1=ALU.add)
                        else:
                            e.scalar_tensor_tensor(out=TMP[:, 1:2, :], in0=ps[:, 1:2, :], scalar=0.125, in1=DT[:, s16, 1:2, :], op0=ALU.mult, op1=ALU.add)
                        return e.scalar_tensor_tensor(out=TMP[:, 2:4, :], in0=ps[:, 2:4, :], scalar=0.125, in1=DT[:, s16, 0:2, :], op0=ALU.mult, op1=ALU.add)
                    sc.add("dve", biasadd, reads=[f"ps{bk}", "dt"], writes=[tmpn])
                    sc.add("act", (lambda e, PT=PT, TMP=TMP, ti0=ti0: e.activation(out=PT[:, ti0:4, :], in_=TMP[:, ti0:4, :], func=AF.Exp)),
                           reads=[tmpn], writes=[PTN[pti]])
                    flags = []
                    for k_, (kc, qt, di) in enumerate(near):
                        st = done_qt[qt] == 0
                        done_qt[qt] += 1
                        sp_ = done_qt[qt] == n_qt[qt]
                        flags.append((kc, qt, ti0 + k_, st, sp_))

                    def pvn(e, flags=flags, PT=PT):
                        last = None
                        for (kc, qt, ti, st, sp_) in flags:
                            last = e.matmul(self.oacc(qt * 2 + quad)[:, hsl, :], PT[:, ti, :], VA[:, kc, vh, :], start=st, stop=sp_)
                        return last
                    sc.add("pe", pvn, reads=[PTN[pti]] + [f"va{kc // 2}_{kc % 2}{'a' if grp == 0 else 'b'}" for (kc, _, _) in near],
                           writes=[f"ps{quad}", f"ps{2 + quad}"])
                for hs8 in range(8):
                    head(hs8)
                for qt in range(2):
                    for quad in range(2):
                        bk = qt * 2 + quad
                        oa = self.oacc(bk)
                        if grp == 0:
                            outv = OG[:, qt, quad * 256:(quad + 1) * 256].rearrange("p (h d) -> p h d", h=4)
                            inv = oa[:, :, 0:64]
                        else:
                            outv = OG[:, qt, 512:1024].rearrange("p (r i d) -> p i r d", r=2, i=4)[:, 2 * quad:2 * quad + 2, :, :]
                            inv = oa[:, :, 0:64].rearrange("p (i r) d -> p i r d", r=2)

                        def nrm(e, oa=oa, outv=outv, inv=inv, qt=qt, quad=quad, grp=grp):
                            if grp == 0:
                                e.reciprocal(out=RDEN[:, 0:4], in_=oa[:, :, 64])
                            else:
                                e.tensor_tensor(out=RDEN[:, 0:4], in0=oa[:, :, 64], in1=SINKT[:, qt, 4 * quad:4 * quad + 4], op=ALU.add)
                                e.reciprocal(out=RDEN[:, 0:4], in_=RDEN[:, 0:4])
                            if grp == 0:
                                rb_ = RDEN[:, 0:4].unsqueeze(2).to_broadcast([128, 4, 64])
                            else:
                                rb_ = RDEN[:, 0:4].rearrange("p (i r) -> p i r", r=2).unsqueeze(3).to_broadcast([128, 2, 2, 64])
                            return e.tensor_tensor(out=outv, in0=inv, in1=rb_, op=ALU.mult)
                        def nrm1(e, oa=oa, qt=qt, quad=quad, grp=grp):
                            if grp == 0:
                                return e.reciprocal(out=RDEN[:, 4 * (bk % 2):4 * (bk % 2) + 4], in_=oa[:, :, 64])
                            return e.tensor_tensor(out=RDEN[:, 4 * (bk % 2):4 * (bk % 2) + 4], in0=oa[:, :, 64], in1=SINKT[:, qt, 4 * quad:4 * quad + 4], op=ALU.add)
                        rdn = f"rden{bk % 2}"
                        RD = RDEN[:, 4 * (bk % 2):4 * (bk % 2) + 4]
                        if grp == 0:
                            sc.add("dve", (lambda e, oa=oa, RD=RD: e.reciprocal(out=RD, in_=oa[:, :, 64])), reads=[f"ps{bk}"], writes=[rdn])
                        else:
                            sc.add("dve", (lambda e, oa=oa, RD=RD, qt=qt, quad=quad: e.tensor_tensor(out=RD, in0=oa[:, :, 64], in1=SINKT[:, qt, 4 * quad:4 * quad + 4], op=ALU.add)),
                                   reads=[f"ps{bk}", "sinkt"], writes=[rdn])
                            sc.add("dve", (lambda e, RD=RD: e.reciprocal(out=RD, in_=RD)), reads=[rdn], writes=[rdn])
                        if grp == 0:
                            rb_ = RD.unsqueeze(2).to_broadcast([128, 4, 64])
                        else:
                            rb_ = RD.rearrange("p (i r) -> p i r", r=2).unsqueeze(3).to_broadcast([128, 2, 2, 64])
                        sc.add("dve", (lambda e, outv=outv, inv=inv, rb_=rb_: e.tensor_tensor(out=outv, in0=inv, in1=rb_, op=ALU.mult)),
                               reads=[f"ps{bk}", rdn], writes=["rA"])
            if getattr(self, 'stop', 99) <= 7:
                return
            for half in range(2):
                bk = 4 + half
                psT = self.psb(bk, BF16).rearrange("p (c t) -> p c t", c=4)

                def trO(e, half=half, psT=psT):
                    last = None
                    for cc in range(4):
                        c = half * 4 + cc
                        for qt in range(2):
                            last = e.transpose(psT[:, cc, qt * 128:(qt + 1) * 128], OG[:, qt, c * 128:(c + 1) * 128], IDENT)
                    return last
                sc.add("pe", trO, reads=["rA", "ident"], writes=[f"ps{bk}"])
                if half == 0:
                    sc.add("act", (lambda e, psT=psT: e.activation(out=OTG[:, 0:4, :], in_=psT, func=AF.Copy)), reads=[f"ps{bk}"], writes=["rB"])
                else:
                    sc.add("dve", (lambda e, psT=psT: e.tensor_copy(out=OTG[:, 4:8, :], in_=psT)), reads=[f"ps{bk}"], writes=["rBb"])
            if getattr(self, 'stop', 99) <= 8:
                return
            for o_ in range(8):
                bk = 4 + o_ % 3
                ps = self.psb(bk)

                def mmo(e, o_=o_, ps=ps):
                    last = None
                    for c in range(8):
                        last = e.matmul(ps[:, 0:256], WOUT[:, c, o_ * 128:(o_ + 1) * 128], OTG[:, c, :], start=(c == 0), stop=(c == 7))
                    return last
                sc.add("pe", mmo, reads=["wout", "rB", "rBb"], writes=[f"ps{bk}"])
                ysl = YSBA[:, o_, :]
                sc.add("act", (lambda e, ps=ps, ysl=ysl: e.activation(out=ysl, in_=ps[:, 0:256], func=AF.Copy)),
                       reads=[f"ps{bk}", "rA", "rC"], writes=[f"ysa{o_}"])
                sqb = PTS[o_ % 4]
                sc.add("dve", (lambda e, ysl=ysl, sqb=sqb: e.tensor_tensor(out=sqb[:, 0:256], in0=ysl, in1=ysl, op=ALU.mult)),
                       reads=[f"ysa{o_}"], writes=[PTN[o_ % 4]])
                sc.add("pe", (lambda e, sqb=sqb, o_=o_: e.matmul(self.psb(7)[:, 0:256], self.ONES, sqb[:, 0:256], start=(o_ == 0), stop=(o_ == 7))),
                       reads=[PTN[o_ % 4], "ones"], writes=["ps7"])
            if getattr(self, 'stop', 99) <= 9:
                return
            self.resid_update(l, 0, t0, 256, YSBA, (lambda c: f"ysa{c}"), 7)
            sc.marker(reads=[f"ysa{c}" for c in range(8)], writes=["rA", "rC"])

        for g in range(getattr(self, 'ngroups', 8)):
            group(g)
        sc.marker(writes=["bmask", "gm", "cmp", "selr", "augb", "selfm", "sh8", "sinkt", "sqrt_t", "wada_ok"])

    def oacc(self, bk):
        return self.PS[bk][:, 0:260].rearrange("p (h d) -> p h d", d=65)


def _host_prep(inputs):
    f = np.float32
    w_ada = np.asarray(inputs["w_ada"], f)
    w1 = np.asarray(inputs["w1"], f)
    w2 = np.asarray(inputs["w2"], f)
    w_in = np.asarray(inputs["w_in"], f)
    w_out = np.asarray(inputs["w_out"], f)
    sh = {}
    sh["wada"] = np.ascontiguousarray(w_ada.reshape(DEPTH, 8, 128, 24, 256).transpose(0, 3, 2, 1, 4))
    sh["badac"] = np.ascontiguousarray(np.asarray(inputs["b_ada"], f).reshape(DEPTH, 48, 128).transpose(2, 0, 1).reshape(128, DEPTH * 48))
    sh["gainc"] = np.ascontiguousarray(np.asarray(inputs["norm_gains"], f).reshape(DEPTH, 4, 8, 128).transpose(3, 0, 1, 2).reshape(128, 128))
    sh["b1c"] = np.ascontiguousarray(np.asarray(inputs["b1"], f).reshape(DEPTH, 32, 128).transpose(2, 0, 1).reshape(128, 128))
    sh["b2c"] = np.ascontiguousarray(np.asarray(inputs["b2"], f).reshape(DEPTH, 8, 128).transpose(2, 0, 1).reshape(128, 32))
    sh["w1r"] = np.ascontiguousarray(w1.reshape(DEPTH, 8, 128, 16, 256).transpose(0, 3, 2, 1, 4))
    sh["w2r"] = np.ascontiguousarray(w2.reshape(DEPTH, 2, 16, 128, 8, 128).transpose(0, 4, 1, 3, 2, 5))
    perm = [(k // 2) + 4 * (k % 2) for k in range(8)]
    colidx = np.arange(DIN)
    qb = colidx[1536:2048].reshape(8, 64)[perm].reshape(-1)
    colidx = np.concatenate([colidx[:1536], qb, colidx[2048:]])
    w_in_p = w_in[:, :, colidx]
    sh["winr"] = np.ascontiguousarray(w_in_p.reshape(DEPTH, 8, 128, DIN).transpose(0, 2, 1, 3))
    sh["woutr"] = np.ascontiguousarray(w_out.reshape(DEPTH, 8, 128, D).transpose(0, 2, 1, 3))
    sh["sinks"] = np.ascontiguousarray(np.asarray(inputs["sinks"], f)[:, perm])
    rb = np.asarray(inputs["rel_bias"], f)
    hperm = list(range(8)) + [8 + p for p in perm]
    rb = rb[:, hperm]
    sh["rbT"] = np.ascontiguousarray(rb.T).reshape(1, 16 * 32)
    tab = np.concatenate([rb, np.full((1, 16), NEG, f)], axis=0)
    idx = _dtile_index()
    dt = np.zeros((128, 16, 2, 128), f)
    for h in range(16):
        hg = 0 if h < 8 else 1
        for t in range(2):
            dt[:, h, t, :] = tab[idx[hg, t], h]
    sh["dtile"] = dt.reshape(128, 16 * 2 * 128)
    sh["ident"] = np.eye(128, dtype=f)
    hsel = np.zeros((128, 2, 128), f)
    hsel[0:64, 0, :] = 1.0
    hsel[64:128, 1, :] = 1.0
    sh["hsel"] = hsel.reshape(128, 256)
    hind = np.zeros((128, 2), f)
    hind[0:64, 0] = 1.0
    hind[64:128, 1] = 1.0
    sh["hind"] = hind
    ind = np.zeros((72, 8, 128), f)
    for j in range(8):
        for pb in (0, 32, 64):
            ind[pb + j, j, :] = 1.0
    sh["indall"] = ind.reshape(72, 1024)
    bm = np.zeros((128, 3, 8, 8), f)
    for b in range(8):
        for j in range(8):
            bm[:, 0, b, j] = 0.0 if j < b else -1e30
            bm[:, 1, b, j] = 1.0 if j < b else 0.0
            bm[:, 2, b, j] = 1.0 if j == b else 0.0
    sh["bmask"] = bm.reshape(128, 192)
    x = np.asarray(inputs["x"], f)
    c = np.asarray(inputs["c"], f)
    per = []
    for b in range(x.shape[0]):
        m = dict(sh)
        m["xT"] = np.ascontiguousarray(x[b].T)
        m["cT"] = np.ascontiguousarray(c[b].reshape(8, 128).T)
        per.append(m)
    return per


_PROG_CACHE = {}


def _get_prog(phases, debug=False, ngroups=8, stop=99):
    key = (tuple(phases), debug, ngroups, stop)
    if key not in _PROG_CACHE:
        _PROG_CACHE[key] = Prog(list(phases), debug=debug, ngroups=ngroups, stop=stop)
    return _PROG_CACHE[key]


def run_phases(inputs, phases, n_cores=8, trace=False, debug=False, ngroups=8, stop=99):
    per = _host_prep(inputs)[:n_cores]
    prog = _get_prog(phases, debug, ngroups, stop)
    res = run_bass_kernel_spmd(prog.nc, per, core_ids=list(range(n_cores)), trace=trace)
    outs = [np.ascontiguousarray(r["outT"].T) for r in res.results]
    return np.stack(outs, axis=0), res


def kernel(**inputs):
    phases = []
    for l in range(DEPTH):
        phases += [("attn", l), ("ffn", l)]
    out, _ = run_phases(inputs, phases)
    return out.astype(np.float32)
```

```python
import math
import numpy as np
import concourse.bass as bass
import concourse.mybir as mybir
from concourse.bass_utils import run_bass_kernel_spmd

F32 = mybir.dt.float32
BF16 = mybir.dt.bfloat16
AF = mybir.ActivationFunctionType
ALU = mybir.AluOpType
AX = mybir.AxisListType

D = 1024
S = 2048
DEPTH = 4
DFF = 4096
DIN = 2304
EPS = 1e-6
NEG = -30000.0
BIG = 1024.0
MOBA_ROUND = "trunc"
SWA_ROUND = "trunc"


class Sched:
    def __init__(self):
        self.ops = []
        self.last_w = {}
        self.readers = {}

    def marker(self, reads=(), writes=()):
        k = getattr(self, "_mk", 0)
        self._mk = k + 1
        col = k % 8
        dm = self.dummy
        self.add("dve", (lambda e: e.memset(dm[:, col:col + 1], 0.0)), reads=reads, writes=list(writes) + [f"dummy{col}"])

    def barrier(self):
        names = set(self.last_w) | set(self.readers)
        names.add("__bar__")
        self.marker(writes=sorted(names))

    def add(self, eng, fn, reads=(), writes=(), dma=None, ndma=1, total=False):
        idx = len(self.ops)
        reads = tuple(reads) + ("__bar__",)
        writes = tuple(writes)
        deps = set()
        for r in reads:
            w = self.last_w.get(r)
            if w is not None:
                deps.add(w)
        for w_ in writes:
            w = self.last_w.get(w_)
            if w is not None:
                deps.add(w)
            for rd in self.readers.get(w_, ()):
                deps.add(rd)
        for r in reads:
            self.readers.setdefault(r, []).append(idx)
        for w_ in writes:
            self.last_w[w_] = idx
            self.readers[w_] = []
        self.ops.append(dict(eng=eng, fn=fn, deps=deps, dma=dma, ndma=ndma, total=total,
                             reads=set(reads), writes=set(writes)))
        return idx

    def finalize(self, nc, semctx):
        ops = self.ops
        need = [False] * len(ops)
        for i, o in enumerate(ops):
            keep = set()
            for d in o["deps"]:
                p = ops[d]
                if p["dma"] is not None or o["dma"] is not None:
                    keep.add(d)
                elif p["eng"] != o["eng"]:
                    keep.add(d)
                else:
                    if o["eng"] != "pe" and (p["writes"] & (o["reads"] | o["writes"])):
                        keep.add(d)
            o["deps"] = keep
            for d in keep:
                need[d] = True
        sems = {}

        def getsem(name):
            if name not in sems:
                sems[name] = semctx(name)
            return sems[name]

        cnt = {}
        totals = {}
        for i, o in enumerate(ops):
            if o["dma"] is not None:
                key = "d_" + o["dma"]
                cnt[key] = cnt.get(key, 0) + 16 * o["ndma"]
                o["sig"] = (key, cnt[key])
                if o["total"]:
                    totals[key] = True
            elif need[i]:
                key = "e_" + o["eng"]
                cnt[key] = cnt.get(key, 0) + 1
                o["sig"] = (key, cnt[key])
            else:
                o["sig"] = None
        for o in ops:
            if o["dma"] is not None and o["total"]:
                o["sig"] = (o["sig"][0], cnt[o["sig"][0]])
        for o in ops:
            w = {}
            for d in o["deps"]:
                k, v = ops[d]["sig"]
                if w.get(k, 0) < v:
                    w[k] = v
            o["waits"] = w
        for k in cnt:
            getsem(k)
        self.sems = sems
        self.cnt = cnt

    def emit(self, eng, e):
        waited = {}
        n = 0
        for o in self.ops:
            if o["eng"] != eng:
                continue
            for k in sorted(o["waits"]):
                v = o["waits"][k]
                if waited.get(k, 0) < v:
                    e.wait_ge(self.sems[k], v)
                    waited[k] = v
            ins = o["fn"](e)
            n += 1
            if o["dma"] is not None:
                if not isinstance(ins, (list, tuple)):
                    ins = [ins]
                assert len(ins) == o["ndma"], (len(ins), o["ndma"])
                for i_ in ins:
                    i_.then_inc(self.sems[o["sig"][0]], 16)
            elif o["sig"] is not None:
                if isinstance(ins, (list, tuple)):
                    ins = ins[-1]
                ins.then_inc(self.sems[o["sig"][0]], 1)
        return n


def _t5_bucket_np(dist, mode):
    n = np.maximum(dist, 0).astype(np.int32)
    nf = np.maximum(n, 1).astype(np.float32)
    val = (np.log(nf / np.float32(16)) / np.float32(math.log(128 / 16)) * np.float32(16)).astype(np.float32)
    if mode == "trunc":
        li = val.astype(np.int32)
    else:
        li = np.rint(val).astype(np.int32)
    large = np.minimum(16 + li, 31)
    return np.where(n < 16, n, large)


def _dtile_index():
    k = np.arange(128)[:, None]
    q = np.arange(128)[None, :]
    out = np.zeros((2, 2, 128, 128), np.int64)
    for hg, mode in ((0, MOBA_ROUND), (1, SWA_ROUND)):
        d0 = q - k
        b0 = _t5_bucket_np(d0, mode)
        out[hg, 1] = np.where(d0 >= 0, b0, 32)
        d1 = 128 + q - k
        b1 = _t5_bucket_np(d1, mode)
        if hg == 0:
            out[hg, 0] = b1
        else:
            out[hg, 0] = np.where(d1 < 128, b1, 32)
    return out


class Prog:
    def __init__(self, phases, debug=False, ngroups=8, stop=99):
        self.phases = phases
        self.stop = stop
        self.ngroups = ngroups
        self.debug = debug
        self.dbg_names = []
        self.nc = bass.Bass("TRN2", target_bir_lowering=False)
        self.sc = Sched()
        self.build()

    def dram_in(self, name, shape, dt=F32):
        return self.nc.dram_tensor(name, list(shape), dt, kind="ExternalInput").ap()

    def build(self):
        nc = self.nc
        sc = self.sc
        self.d_xT = self.dram_in("xT", [D, S])
        self.d_cT = self.dram_in("cT", [128, 8])
        self.d_wada = self.dram_in("wada", [DEPTH, 24, 128, 8, 256])
        self.d_badac = self.dram_in("badac", [128, DEPTH * 48])
        self.d_gainc = self.dram_in("gainc", [128, 128])
        self.d_b1c = self.dram_in("b1c", [128, 128])
        self.d_b2c = self.dram_in("b2c", [128, 32])
        self.d_w1r = self.dram_in("w1r", [DEPTH, 16, 128, 8, 256])
        self.d_w2r = self.dram_in("w2r", [DEPTH, 8, 2, 128, 16, 128])
        self.d_winr = self.dram_in("winr", [DEPTH, 128, 8, DIN])
        self.d_woutr = self.dram_in("woutr", [DEPTH, 128, 8, D])
        self.d_sinks = self.dram_in("sinks", [DEPTH, 8])
        self.d_rbT = self.dram_in("rbT", [1, 16 * 32])
        self.d_dtile = self.dram_in("dtile", [128, 16 * 2 * 128])
        self.d_ident = self.dram_in("ident", [128, 128])
        self.d_hsel = self.dram_in("hsel", [128, 2 * 128])
        self.d_hind = self.dram_in("hind", [128, 2])
        self.d_indall = self.dram_in("indall", [72, 8 * 128])
        self.d_bmask = self.dram_in("bmask", [128, 3 * 64])
        self.d_out = nc.dram_tensor("outT", [D, S], F32, kind="ExternalOutput").ap()

        total_words = 53200
        self.pool = nc.alloc_sbuf_tensor("pool", [128, total_words], F32)
        self.off = 0

        def alloc(words):
            o = self.off
            self.off += (words + 7) // 8 * 8
            assert self.off <= total_words, (self.off, total_words)
            return o

        def view(o, words, dt=F32):
            v = self.pool[:, o:o + words]
            if dt != F32:
                v = v.bitcast(dt)
            return v

        self.view = view
        o_x = alloc(8 * S)
        self.XT = view(o_x, 8 * S).rearrange("p (c t) -> p c t", c=8)
        self.COLS = view(alloc(320), 320)
        self.GAINC = view(alloc(128), 128)
        self.B1C = view(alloc(128), 128)
        self.B2C = view(alloc(32), 32)
        self.BADAC = view(alloc(192), 192)
        self.MODC = view(alloc(48), 48)
        self.CT = view(alloc(8), 8)
        self.CACT = view(alloc(8), 8, BF16)[:, 0:8]
        self.IDENT = view(alloc(64), 64, BF16)
        self.ONES = view(alloc(64), 64, BF16)
        self.SQ = [view(alloc(512), 512, BF16).rearrange("p (a t) -> p a t", a=2) for _ in range(2)]
        self.rstd_off = self.off
        self.RSTD = [view(alloc(512), 512) for _ in range(2)]
        self.XN = [view(alloc(512), 512) for _ in range(2)]
        self.wada_off = self.off
        self.WADA = [view(alloc(1024), 1024, BF16).rearrange("p (k n) -> p k n", k=8) for _ in range(2)]
        self.DUMMY = view(alloc(8), 8)
        sc.dummy = self.DUMMY
        self.EPSC = view(alloc(8), 8)
        self.phase_base = self.off

        self.PS = [nc.alloc_psum_tensor(f"psb{i}", [128, 512], F32) for i in range(8)]

        self.preamble()
        done_mod = set()
        self.side = []
        for pi, (kind, l) in enumerate(self.phases):
            if l not in done_mod:
                self.mod_layer(l)
                done_mod.add(l)
            sc.barrier()
            if kind == "attn":
                self.attn_phase(l)
            else:
                nxt = [ll for (_, ll) in self.phases[pi + 1:] if ll not in done_mod]
                if nxt:
                    self.side = self.mod_jobs(nxt[0])
                    done_mod.add(nxt[0])
                self.ffn_phase(l)
                self.run_side(100)
        sc.barrier()
        self.epilogue()

        class _SemCtx:
            pass
        semlist = []

        def semctx(name):
            cm = nc.semaphore(name)
            h = cm.__enter__()
            semlist.append(cm)
            return h

        sc.finalize(nc, semctx)
        with nc.Block() as block:
            @block.tensor
            def _(e):
                sc.emit("pe", e)

            @block.scalar
            def _(e):
                sc.emit("act", e)

            @block.vector
            def _(e):
                sc.emit("dve", e)

            @block.gpsimd
            def _(e):
                sc.emit("pool", e)

            @block.sync
            def _(e):
                sc.emit("sp", e)

    def dump(self, name, ap, reads):
        if not getattr(self, "debug", False):
            return
        shp = list(ap.shape)
        d = self.nc.dram_tensor("dbg_" + name, shp, ap.dtype, kind="ExternalOutput").ap()
        self.sc.add("sp", (lambda e: e.dma_start(out=d, in_=ap)), reads=list(reads), writes=["dbg_" + name], dma="dbg_" + name)
        self.dbg_names.append("dbg_" + name)

    def psb(self, i, dt=F32):
        v = self.PS[i][:, :]
        if dt != F32:
            v = v.bitcast(dt)
        return v

    def preamble(self):
        sc = self.sc
        XT = self.XT
        xs = self.d_xT.rearrange("(c p) t -> p c t", p=128)
        for c in range(8):
            sc.add("sp", (lambda e, c=c: e.dma_start(out=XT[:, c, :], in_=xs[:, c, :])),
                   writes=[f"xTc{c}"], dma=f"xin{c}")
        small = [(self.GAINC, self.d_gainc, "gainc"), (self.B1C, self.d_b1c, "b1c"), (self.B2C, self.d_b2c, "b2c"),
                 (self.BADAC, self.d_badac, "badac"), (self.CT, self.d_cT, "ct")]
        for (dst, src, nm) in small:
            sc.add("sp", (lambda e, dst=dst, src=src: e.dma_start(out=dst, in_=src[:, :])),
                   writes=[nm], dma="c_" + nm)
        sc.add("pool", (lambda e: e.dma_start(out=self.IDENT, in_=self.d_ident[:, :])), writes=["ident"], dma="c_ident")
        sc.add("dve", (lambda e: e.memset(self.ONES, 1.0)), writes=["ones"])
        sc.add("dve", (lambda e: e.memset(self.EPSC, float(D * EPS))), writes=["epsc"])
        sc.add("act", (lambda e: e.activation(out=self.CACT, in_=self.CT, func=AF.Silu)), reads=["ct"], writes=["cact"])
        sc.marker(reads=[f"xTc{c}" for c in range(8)], writes=[f"xT{tb}" for tb in range(8)] + ["rgnA_ok", "rgnB_ok"])

    def epilogue(self):
        sc = self.sc
        XT = self.XT
        od = self.d_out.rearrange("(c p) t -> p c t", p=128)
        for c in range(8):
            sc.add("sp", (lambda e, c=c: e.dma_start(out=od[:, c, :], in_=XT[:, c, :])),
                   reads=[f"xT{tb}" for tb in range(8)], writes=[f"out{c}"], dma=f"xout{c}")
        sc.add("sp", (lambda e: e.nop()), reads=[f"out{c}" for c in range(8)])

    def mod_layer(self, l):
        for j in self.mod_jobs(l):
            j()

    def run_side(self, n=1):
        for _ in range(n):
            if self.side:
                self.side.pop(0)()

    def mod_jobs(self, l):
        jobs = []
        for pc in range(24):
            jobs.append(lambda pc=pc: self.mod_piece(l, pc))
        jobs.append(lambda: self.mod_finish(l))
        return jobs

    def mod_piece(self, l, pc):
        sc = self.sc
        ps = self.psb(7)
        if True:
            buf = self.WADA[pc % 2]
            bn = f"wada{pc % 2}"
            src = self.d_wada[l, pc]
            sc.add("pool", (lambda e, buf=buf, src=src: e.dma_start(out=buf, in_=src)), reads=["wada_ok"], writes=[bn], dma=bn)

            def mm(e, buf=buf, pc=pc):
                last = None
                for j in range(2):
                    col = pc * 2 + j
                    for kc in range(8):
                        last = e.matmul(ps[:, col:col + 1], buf[:, kc, j * 128:(j + 1) * 128],
                                        self.CACT[:, kc:kc + 1], start=(kc == 0), stop=(kc == 7))
                return last
            sc.add("pe", mm, reads=[bn, "cact"], writes=["ps7"])
    def mod_finish(self, l):
        sc = self.sc
        ps = self.psb(7)
        MODC = self.MODC
        sc.add("dve", (lambda e: e.tensor_tensor(out=MODC, in0=ps[:, 0:48], in1=self.BADAC[:, l * 48:(l + 1) * 48], op=ALU.add)),
               reads=["ps7", "badac"], writes=["modc"])
        C = self.COLS
        b = l * 64
        G = self.GAINC
        g0 = (l * 4) * 8

        def cols(e):
            e.scalar_tensor_tensor(out=C[:, b + 0:b + 8], in0=MODC[:, 8:16], scalar=1.0, in1=G[:, g0 + 0:g0 + 8], op0=ALU.add, op1=ALU.mult)
            e.tensor_copy(out=C[:, b + 8:b + 16], in_=MODC[:, 0:8])
            e.tensor_tensor(out=C[:, b + 16:b + 24], in0=MODC[:, 16:24], in1=G[:, g0 + 8:g0 + 16], op=ALU.mult)
            e.scalar_tensor_tensor(out=C[:, b + 24:b + 32], in0=MODC[:, 32:40], scalar=1.0, in1=G[:, g0 + 16:g0 + 24], op0=ALU.add, op1=ALU.mult)
            e.tensor_copy(out=C[:, b + 32:b + 40], in_=MODC[:, 24:32])
            return e.tensor_tensor(out=C[:, b + 40:b + 48], in0=MODC[:, 40:48], in1=G[:, g0 + 24:g0 + 32], op=ALU.mult)
        sc.add("dve", cols, reads=["modc", "gainc"], writes=[f"colsraw{l}"])

        def cols2(e):
            e.tensor_scalar(out=C[:, b + 0:b + 8], in0=C[:, b + 0:b + 8], scalar1=32.0, scalar2=None, op0=ALU.mult)
            e.tensor_scalar(out=C[:, b + 16:b + 32], in0=C[:, b + 16:b + 32], scalar1=32.0, scalar2=None, op0=ALU.mult)
            return e.tensor_scalar(out=C[:, b + 40:b + 48], in0=C[:, b + 40:b + 48], scalar1=32.0, scalar2=None, op0=ALU.mult)
        sc.add("dve", cols2, reads=[f"colsraw{l}"], writes=[f"cols{l}"])
        self.dump(f"cols{l}", C[:, b:b + 48], [f"cols{l}"])
        self.dump(f"modc{l}", MODC, [f"cols{l}"])

    def rmsnorm_in(self, l, sub, t0, n, HT, ht_res, psbank, extra_reads=(), sq_names=None):
        sc = self.sc
        XT = self.XT
        tbs = [f"xT{tb}" for tb in range(t0 // 256, (t0 + n) // 256)]
        ps = self.psb(psbank)
        cb = l * 64 + (0 if sub == 0 else 24)
        C = self.COLS
        for cp in range(4):
            sq = self.SQ[cp % 2]
            sqn = f"sq{cp % 2}"
            sqw = [sqn] if sq_names is None else sq_names[cp % 2]
            sc.add("act", (lambda e, cp=cp, sq=sq: e.activation(out=sq[:, :, 0:n], in_=XT[:, 2 * cp:2 * cp + 2, t0:t0 + n], func=AF.Square)),
                   reads=tbs, writes=sqw)

            def mm(e, cp=cp, sq=sq):
                last = None
                for j in range(2):
                    c = 2 * cp + j
                    last = e.matmul(ps[:, 0:n], self.ONES, sq[:, j, 0:n], start=(c == 0), stop=(c == 7))
                return last
            sc.add("pe", mm, reads=[sqn, "ones"], writes=[f"ps{psbank}"])
        rs = self.RSTD[0]
        sc.add("act", (lambda e: e.activation(out=rs[:, 0:n], in_=ps[:, 0:n], func=AF.Ln, bias=self.EPSC[:, 0:1], scale=1.0)),
               reads=[f"ps{psbank}", "epsc"], writes=["rstd0p", "rstd0"])
        sc.add("act", (lambda e: e.activation(out=rs[:, 0:n], in_=rs[:, 0:n], func=AF.Exp, scale=-0.5)),
               reads=["rstd0p"], writes=["rstd0", "rstd0p"])
        for c in range(8):
            xn = self.XN[c % 2]
            xnn = f"xn{c % 2}"
            sc.add("dve", (lambda e, c=c, xn=xn: e.tensor_tensor(out=xn[:, 0:n], in0=XT[:, c, t0:t0 + n], in1=rs[:, 0:n], op=ALU.mult)),
                   reads=tbs + ["rstd0"], writes=[xnn])
            sc.add("act", (lambda e, c=c, xn=xn: e.activation(out=HT[:, c, 0:n], in_=xn[:, 0:n], func=AF.Identity,
                                                               scale=C[:, cb + c:cb + c + 1], bias=C[:, cb + 8 + c:cb + 9 + c])),
                   reads=[xnn, f"cols{l}"] + list(extra_reads), writes=[ht_res])

    def resid_update(self, l, sub, t0, n, Y, y_res, ssbank):
        sc = self.sc
        XT = self.XT
        tbs = [f"xT{tb}" for tb in range(t0 // 256, (t0 + n) // 256)]
        ps = self.psb(ssbank)
        C = self.COLS
        cb = l * 64 + (16 if sub == 0 else 40)
        rs = self.RSTD[1]
        sc.add("act", (lambda e: e.activation(out=rs[:, 0:n], in_=ps[:, 0:n], func=AF.Ln, bias=self.EPSC[:, 0:1], scale=1.0)),
               reads=[f"ps{ssbank}", "epsc"], writes=["rstd1p", "rstd1"])
        sc.add("act", (lambda e: e.activation(out=rs[:, 0:n], in_=rs[:, 0:n], func=AF.Exp, scale=-0.5)),
               reads=["rstd1p"], writes=["rstd1", "rstd1p"])
        if t0 == 0 and sub == 1:
            self.dump("rstd1", rs, ["rstd1"])
            self.dump("ysb", Y, [y_res(c) for c in range(8)])
        for c in range(8):
            sc.add("dve", (lambda e, c=c: e.scalar_tensor_tensor(out=Y[:, c, 0:n], in0=Y[:, c, 0:n], scalar=C[:, cb + c:cb + c + 1], in1=rs[:, 0:n],
                                                                 op0=ALU.mult, op1=ALU.mult)),
                   reads=[y_res(c), "rstd1", f"cols{l}"], writes=[y_res(c)])
            sc.add("dve", (lambda e, c=c: e.tensor_tensor(out=XT[:, c, t0:t0 + n], in0=XT[:, c, t0:t0 + n], in1=Y[:, c, 0:n], op=ALU.add)),
                   reads=[y_res(c)] + tbs, writes=tbs)

    def ffn_phase(self, l):
        sc = self.sc
        view = self.view
        base = self.phase_base
        o = base
        HID = view(o, 32 * 1024 // 2, BF16).rearrange("p (m t) -> p m t", m=32); o += 16384
        rgn = o; o += 8192
        HT = view(rgn, 4096, BF16).rearrange("p (c t) -> p c t", c=8)
        W1B = [view(rgn + 4096 + i * 1024, 1024, BF16).rearrange("p (k n) -> p k n", k=8) for i in range(3)]
        RL = [view(rgn + 4096 + 3072 + i * 512, 512) for i in range(2)]
        YSB = view(rgn, 8192).rearrange("p (c t) -> p c t", c=8)
        W2B = [view(o + i * 1024, 1024, BF16).rearrange("p (k n) -> p k n", k=16) for i in range(3)]; o += 3072
        assert o <= 53200, o
        B1C, B2C = self.B1C, self.B2C
        w1cnt = 0
        w2cnt = 0
        for H in range(2):
            T0 = H * 1024
            region_users = ["rgnA_ok"]
            for tg in range(2):
                self.rmsnorm_in(l, 1, T0 + tg * 512, 512, HT[:, :, tg * 512:(tg + 1) * 512], f"ht{tg}", 6,
                                extra_reads=region_users)
            if H == 0:
                self.dump("ht", HT, ["ht0", "ht1"])
            psi = 0
            for g in range(16):
                self.run_side(1)
                wb = W1B[w1cnt % 3]; wn = f"w1b{w1cnt % 3}"; w1cnt += 1
                src = self.d_w1r[l, g]
                sc.add("pool", (lambda e, wb=wb, src=src: e.dma_start(out=wb, in_=src)), reads=region_users, writes=[wn], dma=wn)
                for mm_ in range(2):
                    m = 2 * g + mm_
                    for tg in range(2):
                        bank = psi % 4; psi += 1
                        ps = self.psb(bank)

                        def mm(e, wb=wb, mm_=mm_, tg=tg, ps=ps):
                            last = None
                            for c in range(8):
                                last = e.matmul(ps, wb[:, c, mm_ * 128:(mm_ + 1) * 128], HT[:, c, tg * 512:(tg + 1) * 512],
                                                start=(c == 0), stop=(c == 7))
                            return last
                        sc.add("pe", mm, reads=[wn, f"ht{tg}"], writes=[f"ps{bank}"])
                        rl = RL[psi % 2]; rln = f"rl{psi % 2}"
                        sc.add("act", (lambda e, rl=rl, ps=ps, m=m: e.activation(out=rl, in_=ps, func=AF.Relu,
                                                                                 bias=B1C[:, l * 32 + m:l * 32 + m + 1])),
                               reads=[f"ps{bank}", "b1c"] + region_users, writes=[rln])
                        sc.add("dve", (lambda e, rl=rl, m=m, tg=tg: e.tensor_tensor(out=HID[:, m, tg * 512:(tg + 1) * 512], in0=rl, in1=rl, op=ALU.mult)),
                               reads=[rln], writes=[f"hid{m}_{tg}"])
            if H == 0:
                self.dump("hid", HID, [f"hid{m}_{tg}" for m in range(32) for tg in range(2)])
            sc.marker(writes=["ht0", "ht1", "w1b0", "w1b1", "w1b2", "rl0", "rl1", "rgnB_ok"])
            ht_users = ["rgnB_ok"]
            for o_ in range(8):
                wbs = []
                for kh in range(2):
                    wb = W2B[w2cnt % 3]; wn = f"w2b{w2cnt % 3}"; w2cnt += 1
                    src = self.d_w2r[l, o_, kh]
                    sc.add("pool", (lambda e, wb=wb, src=src: e.dma_start(out=wb, in_=src)), writes=[wn], dma=wn)
                    wbs.append((wb, wn))
                banks = [(o_ % 2) * 2, (o_ % 2) * 2 + 1]
                for kh in range(2):
                    wb, wn = wbs[kh]
                    for tg in range(2):
                        ps = self.psb(banks[tg])

                        def mm(e, wb=wb, kh=kh, tg=tg, ps=ps):
                            last = None
                            for kk in range(16):
                                m = kh * 16 + kk
                                last = e.matmul(ps, wb[:, kk, :], HID[:, m, tg * 512:(tg + 1) * 512],
                                                start=(m == 0), stop=(m == 31))
                            return last
                        sc.add("pe", mm, reads=[wn] + [f"hid{kh * 16 + kk}_{tg}" for kk in range(16)], writes=[f"ps{banks[tg]}"])
                for tg in range(2):
                    ps = self.psb(banks[tg])
                    ysl = YSB[:, o_, tg * 512:(tg + 1) * 512]
                    sc.add("act", (lambda e, ps=ps, ysl=ysl, o_=o_: e.activation(out=ysl, in_=ps, func=AF.Identity,
                                                                                  bias=B2C[:, l * 8 + o_:l * 8 + o_ + 1])),
                           reads=[f"ps{banks[tg]}", "b2c"] + ht_users, writes=[f"ysb{o_}t{tg}"])
                    sq = self.SQ[tg][:, 0, :]
                    sc.add("dve", (lambda e, ysl=ysl, sq=sq: e.tensor_tensor(out=sq, in0=ysl, in1=ysl, op=ALU.mult)),
                           reads=[f"ysb{o_}t{tg}"], writes=[f"sq{tg}"])
                    ssb = 4 + tg
                    sc.add("pe", (lambda e, sq=sq, ssb=ssb, o_=o_: e.matmul(self.psb(ssb), self.ONES, sq, start=(o_ == 0), stop=(o_ == 7))),
                           reads=[f"sq{tg}", "ones"], writes=[f"ps{ssb}"])
            for tg in range(2):
                self.resid_update(l, 1, T0 + tg * 512, 512, YSB[:, :, tg * 512:(tg + 1) * 512],
                                  (lambda c, tg=tg: f"ysb{c}t{tg}"), 4 + tg)
            sc.marker(writes=[f"ysb{c}t{tg}" for c in range(8) for tg in range(2)] + ["rgnA_ok"])

    def attn_phase(self, l):
        sc = self.sc
        view = self.view
        XT = self.XT
        o = self.phase_base
        KT = view(o, 5120, BF16).rearrange("p (c t) -> p c t", c=5); o += 5120
        VA = view(o, 5200, BF16).rearrange("p (t h d) -> p t h d", t=16, h=10); o += 5200
        WIN = view(o, 9216, BF16).rearrange("p (k n) -> p k n", k=8); o += 9216
        WOUT = view(o, 4096, BF16).rearrange("p (k n) -> p k n", k=8); o += 4096
        DT = view(o, 2048, BF16).rearrange("p (h t q) -> p h t q", h=16, t=2); o += 2048
        rA = o; o += 1024
        rC = o; o += 1024
        rB = o; o += 1024
        HTG = view(rA, 1024, BF16).rearrange("p (c t) -> p c t", c=8)
        OG = view(rA, 1024, BF16).rearrange("p (q f) -> p q f", q=2)
        QTG = view(rC, 1024, BF16).rearrange("p (c t) -> p c t", c=8)
        QSQ = view(rB, 1024, BF16).rearrange("p (c t) -> p c t", c=8)
        OTG = view(rB, 1024, BF16).rearrange("p (c t) -> p c t", c=8)
        YSBA = view(rA, 2048).rearrange("p (c t) -> p c t", c=8)
        AUGT1 = [view(o + i * 128, 128, BF16) for i in range(3)]; o += 384
        IND = view(o, 512, BF16).rearrange("p (j k) -> p j k", j=8); o += 512
        HIND = view(o, 8, BF16)[:, 0:2]; o += 8
        KMEANT = view(o, 32, BF16).rearrange("p (c r j) -> p c r j", c=4, r=2); o += 32
        KSUM = view(o, 8, F32); o += 8
        KMAX2 = view(o, 8, F32); o += 8
        KMXG = view(o, 8, F32); o += 8
        KM16 = view(o, 16, F32); o += 16
        QMXG = view(o, 8, F32); o += 8
        QM16 = view(o, 16, F32); o += 16
        SINKS = view(o, 8, F32); o += 8
        BM8 = view(o, 16, F32); o += 16
        RB = self.XN[0].rearrange("p (h b) -> p h b", h=16)
        B31 = view(o, 16, F32); o += 16
        KSQ = view(rB, 640, BF16).rearrange("p (c t) -> p c t", c=5)
        RDEN = view(o, 8, F32); o += 8
        assert o <= 53200, o
        wsc = self.wada_off
        CMP = view(wsc, 1024).rearrange("p (a j k) -> p a j k", a=16, j=8)
        AUGB = view(wsc + 1024, 128, BF16).rearrange("p (q s j) -> p q s j", q=2, s=16)
        BMASK = view(wsc + 1600, 192).rearrange("p (k b j) -> p k b j", k=3, b=8)
        GM = view(wsc + 1792, 128).rearrange("p (a j) -> p a j", a=16)
        SEL = view(wsc + 1920, 128).rearrange("p (a j) -> p a j", a=16)
        rs_off = self.rstd_off
        SELF = view(rs_off + 256, 256).rearrange("p (q s j) -> p q s j", q=2, s=16)
        SH8 = view(rs_off + 512 + 256, 32).rearrange("p (q s) -> p q s", q=2)
        SINKT = view(rs_off + 512 + 288, 16).rearrange("p (q s) -> p q s", q=2)
        SQRT_T = view(rs_off + 512 + 304, 32).rearrange("p (q s) -> p q s", q=2)
        TMPS = [self.XN[0], self.XN[1]]
        PTS = [self.SQ[0].rearrange("p a t -> p (a t)")[:, 0:512], self.SQ[0].rearrange("p a t -> p (a t)")[:, 512:1024],
               self.SQ[1].rearrange("p a t -> p (a t)")[:, 0:512], self.SQ[1].rearrange("p a t -> p (a t)")[:, 512:1024]]
        PTN = ["sq0", "sq0b", "sq1", "sq1b"]
        IDENT = self.IDENT.rearrange("p (a b) -> p a b", a=1)[:, 0, :]

        sc.marker(writes=["wada0", "wada1", "rstd0", "rstd1", "rstd0p", "rstd1p", "wadafree"])
        sc.add("pool", (lambda e: e.dma_start(out=WIN, in_=self.d_winr[l])), writes=["win"], dma="win")
        sc.add("pool", (lambda e: e.dma_start(out=DT.rearrange("p h t q -> p (h t q)"), in_=self.d_dtile[:, :])), writes=["dt"], dma="dt")
        sc.add("pool", (lambda e: e.dma_start(out=IND.rearrange("p j k -> p (j k)")[0:72, :], in_=self.d_indall[:, :])), writes=["ind"], dma="ind")
        sc.add("pool", (lambda e: e.dma_start(out=HIND, in_=self.d_hind[:, :])), writes=["hind"], dma="hind")
        sc.add("sp", (lambda e: e.dma_start(out=BMASK.rearrange("p k b j -> p (k b j)"), in_=self.d_bmask[:, :])), reads=["wadafree"], writes=["bmask"], dma="bmask")
        sc.add("sp", (lambda e: e.dma_start(out=SINKS, in_=self.d_sinks[l:l + 1, :].partition_broadcast(128))), writes=["sinks"], dma="sinks")
        sc.add("sp", (lambda e: e.dma_start(out=RB.rearrange("p h b -> p (h b)"), in_=self.d_rbT[0:1, :].partition_broadcast(128))), writes=["xn0"], dma="rb")
        sc.add("pool", (lambda e: e.dma_start(out=WOUT, in_=self.d_woutr[l])), writes=["wout"], dma="wout")

        def init1(e):
            e.memset(KMEANT, 0.0)
            e.memset(KMAX2, 0.0)
            e.memset(VA[:, :, :, 64:65], 1.0)
            e.memset(AUGB, 0.0)
            e.memset(SELF, 1.0)
            e.tensor_reduce(out=BM8, in_=RB, axis=AX.X, op=ALU.max)
            e.tensor_copy(out=B31, in_=RB[:, :, 31])
            return e.memset(KM16, 0.0)
        sc.add("dve", init1, reads=["xn0", "wadafree"], writes=["kmeant", "kmax2", "va_ones", "augb", "selfm", "bm8raw", "b31", "km16"])

        sc.add("dve", (lambda e: e.tensor_tensor(out=BM8[:, 8:16], in0=BM8[:, 8:16], in1=SINKS, op=ALU.max)),
               reads=["bm8raw", "sinks"], writes=["bm8raw"])
        sc.add("dve", (lambda e: e.tensor_scalar(out=BM8, in0=BM8, scalar1=8.0, scalar2=None, op0=ALU.mult)),
               reads=["bm8raw"], writes=["bm8", "bm8raw"])

        QCOL, KCOL, VCOL, QBCOL, KBCOL, VBCOL = 0, 512, 1024, 1536, 2048, 2176
        sbank = [0]
        ptc = [0]

        def group(g):
            t0 = g * 256
            b = g
            xres = [f"xT{g}"]
            self.rmsnorm_in(l, 0, t0, 256, HTG, "rA", 7, sq_names=(["sq0", "sq0b"], ["sq1", "sq1b"]))
            pbank = [0]

            def nextbank():
                bk = 4 + pbank[0] % 4
                pbank[0] += 1
                return bk
            for ci in range(8):
                col = QCOL + ci * 128 if ci < 4 else QBCOL + (ci - 4) * 128
                bk = nextbank()
                ps = self.psb(bk)

                def mm(e, col=col, ps=ps):
                    last = None
                    for kc in range(8):
                        last = e.matmul(ps[:, 0:256], WIN[:, kc, col:col + 128], HTG[:, kc, :], start=(kc == 0), stop=(kc == 7))
                    return last
                sc.add("pe", mm, reads=["win", "rA"], writes=[f"ps{bk}"])
                sc.add("dve", (lambda e, ci=ci, ps=ps: e.tensor_copy(out=QTG[:, ci, :], in_=ps[:, 0:256])),
                       reads=[f"ps{bk}"], writes=["rC"])
            sc.add("dve", (lambda e: e.memset(KSUM, 0.0)), writes=[f"ksum{c}" for c in range(4)])
            for ci in range(5):
                col = KCOL + ci * 128 if ci < 4 else KBCOL
                bk = nextbank()
                ps = self.psb(bk)

                def mm(e, col=col, ps=ps):
                    last = None
                    for kc in range(8):
                        last = e.matmul(ps[:, 0:256], WIN[:, kc, col:col + 128], HTG[:, kc, :], start=(kc == 0), stop=(kc == 7))
                    return last
                sc.add("pe", mm, reads=["win", "rA"], writes=[f"ps{bk}"])
                if ci < 4:
                    sc.add("act", (lambda e, ci=ci, ps=ps: e.activation(out=KT[:, ci, t0:t0 + 256], in_=ps[:, 0:256], func=AF.Copy,
                                                                           accum_out=KSUM[:, ci:ci + 1])),
                           reads=[f"ps{bk}"], writes=[f"kt{ci}_{g}", f"ksum{ci}"])
                else:
                    sc.add("act", (lambda e, ci=ci, ps=ps: e.activation(out=KT[:, ci, t0:t0 + 256], in_=ps[:, 0:256], func=AF.Copy)),
                           reads=[f"ps{bk}"], writes=[f"kt{ci}_{g}"])
            for qt in range(2):
                tile_i = g * 2 + qt
                bk = nextbank()
                ps = self.psb(bk)

                def mmv(e, qt=qt, ps=ps):
                    last = None
                    for kc in range(8):
                        last = e.matmul(ps[:, 0:512], HTG[:, kc, qt * 128:(qt + 1) * 128], WIN[:, kc, VCOL:VCOL + 512], start=(kc == 0), stop=(kc == 7))
                    return last
                sc.add("pe", mmv, reads=["win", "rA"], writes=[f"ps{bk}"])
                sc.add("act", (lambda e, tile_i=tile_i, ps=ps: e.activation(out=VA[:, tile_i, 0:8, 0:64],
                                                                               in_=ps[:, 0:512].rearrange("p (h d) -> p h d", h=8), func=AF.Copy)),
                       reads=[f"ps{bk}", "va_ones"], writes=[f"va{g}_{qt}a"])
                bk2 = nextbank()
                ps2 = self.psb(bk2)

                def mmv2(e, qt=qt, ps2=ps2):
                    last = None
                    for kc in range(8):
                        last = e.matmul(ps2[:, 0:128], HTG[:, kc, qt * 128:(qt + 1) * 128], WIN[:, kc, VBCOL:VBCOL + 128], start=(kc == 0), stop=(kc == 7))
                    return last
                sc.add("pe", mmv2, reads=["win", "rA"], writes=[f"ps{bk2}"])
                sc.add("dve", (lambda e, tile_i=tile_i, ps2=ps2: e.tensor_copy(out=VA[:, tile_i, 8:10, 0:64],
                                                                                 in_=ps2[:, 0:128].rearrange("p (h d) -> p h d", h=2))),
                       reads=[f"ps{bk2}", "va_ones"], writes=[f"va{g}_{qt}b"])
            if getattr(self, 'stop', 99) <= 2:
                return
            def kmw(e, b=b):
                e.tensor_scalar(out=KMEANT[0:64, :, 0, b], in0=KSUM[0:64, 0:4], scalar1=1.0 / 256.0, scalar2=None, op0=ALU.mult)
                return e.tensor_scalar(out=KMEANT[64:128, :, 1, b], in0=KSUM[64:128, 0:4], scalar1=1.0 / 256.0, scalar2=None, op0=ALU.mult)
            sc.add("dve", kmw, reads=[f"ksum{c}" for c in range(4)], writes=["kmeant"])
            sc.add("dve", (lambda e: e.tensor_tensor(out=KSQ, in0=KT[:, 0:5, t0:t0 + 256], in1=KT[:, 0:5, t0:t0 + 256], op=ALU.mult)),
                   reads=[f"kt{c}_{g}" for c in range(5)], writes=["rB", "rBb"])
            if getattr(self, 'stop', 99) <= 2.2:
                return
            for bi, cs in enumerate([(0, 1), (2, 3), (4,)]):
                bk = 4 + bi
                ps = self.psb(bk).rearrange("p (a t) -> p a t", a=2)

                def mmk(e, cs=cs, ps=ps):
                    last = None
                    for a, c in enumerate(cs):
                        last = e.matmul(ps[:, a, :], self.ONES, KSQ[:, c, :], start=True, stop=True)
                    return last
                sc.add("pe", mmk, reads=["rB", "ones"], writes=[f"ps{bk}"])
                sc.add("dve", (lambda e, cs=cs, ps=ps: e.tensor_reduce(out=KMXG[:, cs[0]:cs[0] + len(cs)], in_=ps[:, 0:len(cs), :], axis=AX.X, op=ALU.max)),
                       reads=[f"ps{bk}"], writes=["kmxg"])

            if getattr(self, 'stop', 99) <= 2.4:
                return
            sc.add("dve", (lambda e: e.tensor_tensor(out=KMAX2[:, 0:5], in0=KMAX2[:, 0:5], in1=KMXG[:, 0:5], op=ALU.max)),
                   reads=["kmxg", "kmax2"], writes=["kmax2"])

            def kmax(e):
                e.tensor_copy(out=KM16[:, 0:8].rearrange("p (c r) -> p c r", r=2), in_=KMAX2[:, 0:4].unsqueeze(2).to_broadcast([128, 4, 2]))
                return e.tensor_copy(out=KM16[:, 8:16], in_=KMAX2[:, 4:5].to_broadcast([128, 8]))
            sc.add("dve", kmax, reads=["kmax2"], writes=["km16"])
            if getattr(self, 'stop', 99) <= 2.6:
                return
            sc.add("dve", (lambda e: e.tensor_tensor(out=QSQ, in0=QTG, in1=QTG, op=ALU.mult)), reads=["rC"], writes=["rB", "rBb"])
            ps7 = self.psb(7)
            GATE = ps7[:, 0:128].rearrange("p (a j) -> p a j", a=16)

            for bi in range(4):
                psq = self.psb(bi).rearrange("p (a t) -> p a t", a=2)

                def mmq(e, bi=bi, psq=psq):
                    last = None
                    for a in range(2):
                        last = e.matmul(psq[:, a, :], self.ONES, QSQ[:, 2 * bi + a, :], start=True, stop=True)
                    return last
                sc.add("pe", mmq, reads=["rB", "ones"], writes=[f"ps{bi}"])
                sc.add("dve", (lambda e, bi=bi, psq=psq: e.tensor_reduce(out=QMXG[:, 2 * bi:2 * bi + 2], in_=psq, axis=AX.X, op=ALU.max)),
                       reads=[f"ps{bi}"], writes=["qmxg"])
            sc.add("dve", (lambda e: e.tensor_copy(out=QM16.rearrange("p (c r) -> p c r", r=2), in_=QMXG.unsqueeze(2).to_broadcast([128, 8, 2]))),
                   reads=["qmxg"], writes=["qm16"])

            def mmg(e):
                last = None
                for qt in range(2):
                    for c in range(4):
                        last = e.matmul(ps7[:, (qt * 8 + 2 * c) * 8:(qt * 8 + 2 * c + 2) * 8], QTG[:, c, qt * 128:(qt + 1) * 128],
                                        KMEANT[:, c, :, :].rearrange("p r j -> p (r j)"), start=True, stop=True)
                return last
            sc.add("pe", mmg, reads=["rC", "kmeant"], writes=["ps7"])
            if getattr(self, 'stop', 99) <= 3:
                return
            NEGM = BMASK[:, 0, b, :]
            ELIG = BMASK[:, 1, b, :]
            OWN = BMASK[:, 2, b, :]

            sc.add("dve", (lambda e: e.tensor_tensor(out=GM, in0=GATE, in1=NEGM.unsqueeze(1).to_broadcast([128, 16, 8]), op=ALU.add)),
                   reads=["ps7", "bmask"], writes=["gm"])
            sc.add("dve", (lambda e: e.tensor_tensor(out=CMP, in0=GM.unsqueeze(2).to_broadcast([128, 16, 8, 8]),
                                                     in1=GM.unsqueeze(3).to_broadcast([128, 16, 8, 8]), op=ALU.is_gt)),
                   reads=["gm"], writes=["cmp"])
            sc.add("dve", (lambda e: e.tensor_reduce(out=SEL, in_=CMP, axis=AX.X, op=ALU.add)), reads=["cmp"], writes=["selr"])
            sc.add("dve", (lambda e: e.scalar_tensor_tensor(out=SEL, in0=SEL, scalar=3.0, in1=ELIG.unsqueeze(1).to_broadcast([128, 16, 8]),
                                                            op0=ALU.is_lt, op1=ALU.mult)),
                   reads=["selr", "bmask"], writes=["selr"])
            sc.add("dve", (lambda e: e.tensor_tensor(out=SELF[:, :, 0:8, :], in0=SEL.rearrange("p (q h) j -> p q h j", q=2),
                                                     in1=OWN.unsqueeze(1).unsqueeze(1).to_broadcast([128, 2, 8, 8]), op=ALU.add)),
                   reads=["selr", "bmask", "selfm"], writes=["selfm"])
            sc.add("dve", (lambda e: e.tensor_tensor(out=SQRT_T, in0=QM16.unsqueeze(1).to_broadcast([128, 2, 16]),
                                                     in1=KM16.unsqueeze(1).to_broadcast([128, 2, 16]), op=ALU.mult)),
                   reads=["qm16", "km16"], writes=["sqrt_t"])
            sc.add("act", (lambda e: e.activation(out=SQRT_T, in_=SQRT_T, func=AF.Ln, bias=self.EPSC[:, 0:1], scale=1.0)),
                   reads=["sqrt_t", "epsc"], writes=["sqrt_t"])
            sc.add("act", (lambda e: e.activation(out=SQRT_T, in_=SQRT_T, func=AF.Exp, scale=0.5)),
                   reads=["sqrt_t"], writes=["sqrt_t"])
            AUGBv = AUGB

            def augops(e):
                e.tensor_tensor(out=SH8, in0=SQRT_T, in1=BM8.unsqueeze(1).to_broadcast([128, 2, 16]), op=ALU.add)
                return e.tensor_scalar(out=SELF, in0=SELF, scalar1=BIG, scalar2=-BIG, op0=ALU.mult, op1=ALU.add)
            sc.add("dve", augops, reads=["sqrt_t", "bm8", "selfm"], writes=["sh8", "selfm"])
            sc.add("dve", (lambda e: e.tensor_tensor(out=AUGBv[:, :, 0:16, 0:8], in0=SELF, in1=SH8.unsqueeze(3).to_broadcast([128, 2, 16, 8]), op=ALU.subtract)),
                   reads=["sh8", "selfm"], writes=["augb"])

            def sinkops(e):
                e.memset(SELF[:, :, 8:16, :], 1.0)
                return e.scalar_tensor_tensor(out=SINKT, in0=AUGBv[:, :, 8:16, 0], scalar=0.125, in1=SINKS.unsqueeze(1).to_broadcast([128, 2, 8]),
                                              op0=ALU.mult, op1=ALU.add)
            sc.add("dve", sinkops, reads=["augb", "sinks"], writes=["sinkt", "selfm"])
            sc.add("act", (lambda e: e.activation(out=SINKT, in_=SINKT, func=AF.Exp)), reads=["sinkt"], writes=["sinkt"])
            if getattr(self, 'stop', 99) <= 4:
                return
            for grp in range(2):
                pvq = []

                def flush(keep):
                    while len(pvq) > keep:
                        a_, k_ = pvq.pop(0)
                        sc.add(*a_, **k_)

                def prep(s16):
                    ps7b = self.psb(7, BF16)
                    AT = AUGT1[s16 % 3]

                    def tr(e):
                        last = None
                        for qt in range(2):
                            last = e.transpose(ps7b[0:8, qt * 128:(qt + 1) * 128], AUGB[:, qt, s16, :], IDENT)
                        return last
                    sc.add("pe", tr, reads=["augb", "ident"], writes=["ps7"])
                    sc.add("act", (lambda e: e.activation(out=AT[0:8, 0:256], in_=ps7b[0:8, 0:256], func=AF.Copy)),
                           reads=["ps7"], writes=[f"augt{s16 % 3}"])
                if grp == 0:
                    prep(0)
                    prep(1)

                def head(hs8):
                    s16 = grp * 8 + hs8
                    if s16 + 2 < 16:
                        prep(s16 + 2)
                    AT = AUGT1[s16 % 3]
                    pb = 0
                    if grp == 0:
                        ck, r0, cq, vh = hs8 // 2, (hs8 % 2) * 64, hs8 // 2, hs8
                    else:
                        i_, r_ = hs8 // 2, hs8 % 2
                        ck, r0, cq, vh = 4, r_ * 64, 4 + i_, 8 + r_
                    quad, hsl = hs8 // 4, hs8 % 4
                    augres = f"augt{s16 % 3}"
                    far = []
                    if grp == 0 and b >= 1:
                        for kc in range(0, 2 * b - 1):
                            far.append((kc, 0, 256))
                        far.append((2 * b - 1, 128, 128))
                    near = [(2 * b - 1, 0, 0), (2 * b, 0, 1), (2 * b, 1, 0), (2 * b + 1, 1, 1)]
                    if b == 0:
                        near = near[1:]
                    n_qt = [0, 0]
                    for (kc, q0, qn) in far:
                        for sub in range(qn // 128):
                            n_qt[(q0 + sub * 128) // 128] += 1
                    for (kc, qt, _) in near:
                        n_qt[qt] += 1
                    done_qt = [0, 0]
                    banks = []
                    cur, tot = [], 0
                    for p_ in far:
                        if tot + p_[2] > 512:
                            banks.append(cur)
                            cur, tot = [], 0
                        cur.append(p_)
                        tot += p_[2]
                    if cur:
                        banks.append(cur)
                    for pieces in banks:
                        bk = 4 + sbank[0] % 3
                        sbank[0] += 1
                        ps = self.psb(bk)
                        pti = ptc[0] % 4
                        ptc[0] += 1
                        PT = PTS[pti]
                        offs = []
                        off = 0
                        for p_ in pieces:
                            offs.append(off)
                            off += p_[2]
                        tot = off

                        def mms(e, pieces=pieces, offs=offs, ps=ps):
                            last = None
                            for (kc, q0, qn), of in zip(pieces, offs):
                                e.matmul(ps[:, of:of + qn], KT[r0:r0 + 64, ck, kc * 128:(kc + 1) * 128], QTG[r0:r0 + 64, cq, q0:q0 + qn],
                                         start=True, stop=False)
                                last = e.matmul(ps[:, of:of + qn], IND[pb:pb + 8, kc // 2, :], AT[0:8, q0:q0 + qn],
                                                start=False, stop=True)
                            return last
                        flush(1)
                        sc.add("pe", mms, reads=[f"kt{ck}_{p_[0] // 2}" for p_ in pieces] + ["rC", "ind", augres], writes=[f"ps{bk}"])
                        sc.add("act", (lambda e, PT=PT, ps=ps, tot=tot: e.activation(out=PT[:, 0:tot], in_=ps[:, 0:tot], func=AF.Exp,
                                                                                       scale=0.125, bias=B31[:, s16:s16 + 1])),
                               reads=[f"ps{bk}", "b31"], writes=[PTN[pti]])
                        flags = []
                        for (kc, q0, qn), of in zip(pieces, offs):
                            for sub in range(qn // 128):
                                qt = (q0 + sub * 128) // 128
                                st = done_qt[qt] == 0
                                done_qt[qt] += 1
                                sp_ = done_qt[qt] == n_qt[qt]
                                flags.append((kc, qt, of + sub * 128, st, sp_))

                        def pv(e, flags=flags, PT=PT):
                            last = None
                            for (kc, qt, of, st, sp_) in flags:
                                last = e.matmul(self.psb(qt * 2 + quad).rearrange("p (h d) -> p h d", d=65)[:, hsl, 0:65] if False else
                                                self.oacc(qt * 2 + quad)[:, hsl, :], PT[:, of:of + 128], VA[:, kc, vh, :], start=st, stop=sp_)
                            return last
                        pvq.append((("pe", pv), dict(reads=[PTN[pti]] + [f"va{p_[0] // 2}_{p_[0] % 2}{'a' if grp == 0 else 'b'}" for p_ in pieces],
                                                     writes=[f"ps{quad}", f"ps{2 + quad}"])))
                    bk = 4 + sbank[0] % 3
                    sbank[0] += 1
                    ps = self.psb(bk).rearrange("p (a t) -> p a t", a=4)
                    pti = ptc[0] % 4
                    ptc[0] += 1
                    PT = PTS[pti].rearrange("p (a t) -> p a t", a=4)
                    TMP = TMPS[pti % 2].rearrange("p (a t) -> p a t", a=4)
                    tmpn = f"xn{pti % 2}"
                    ti0 = 4 - len(near)

                    def mmn(e, near=near, ps=ps, ti0=ti0):
                        last = None
                        for k_, (kc, qt, di) in enumerate(near):
                            ti = ti0 + k_
                            e.matmul(ps[:, ti, :], KT[r0:r0 + 64, ck, kc * 128:(kc + 1) * 128], QTG[r0:r0 + 64, cq, qt * 128:(qt + 1) * 128],
                                     start=True, stop=False)
                            last = e.matmul(ps[:, ti, :], IND[pb:pb + 8, kc // 2, :], AT[0:8, qt * 128:(qt + 1) * 128],
                                            start=False, stop=True)
                        return last
                    flush(1)
                    sc.add("pe", mmn, reads=[f"kt{ck}_{kc // 2}" for (kc, _, _) in near] + ["rC", "ind", augres], writes=[f"ps{bk}"])

                    def biasadd(e, ps=ps, TMP=TMP, ti0=ti0):
                        if ti0 == 0:
                            e.scalar_tensor_tensor(out=TMP[:, 0:2, :], in0=ps[:, 0:2, :], scalar=0.125, in1=DT[:, s16, 0:2, :], op0=ALU.mult, op1=ALU.add)
                        else:
                            e.scalar_tensor_tensor(out=TMP[:, 1:2, :], in0=ps[:, 1:2, :], scalar=0.125, in1=DT[:, s16, 1:2, :], op0=ALU.mult, op1=ALU.add)
                        return e.scalar_tensor_tensor(out=TMP[:, 2:4, :], in0=ps[:, 2:4, :], scalar=0.125, in1=DT[:, s16, 0:2, :], op0=ALU.mult, op1=ALU.add)
                    sc.add("dve", biasadd, reads=[f"ps{bk}", "dt"], writes=[tmpn])
                    sc.add("act", (lambda e, PT=PT, TMP=TMP, ti0=ti0: e.activation(out=PT[:, ti0:4, :], in_=TMP[:, ti0:4, :], func=AF.Exp)),
                           reads=[tmpn], writes=[PTN[pti]])
                    flags = []
                    for k_, (kc, qt, di) in enumerate(near):
                        st = done_qt[qt] == 0
                        done_qt[qt] += 1
                        sp_ = done_qt[qt] == n_qt[qt]
                        flags.append((kc, qt, ti0 + k_, st, sp_))

                    def pvn(e, flags=flags, PT=PT):
                        last = None
                        for (kc, qt, ti, st, sp_) in flags:
                            last = e.matmul(self.oacc(qt * 2 + quad)[:, hsl, :], PT[:, ti, :], VA[:, kc, vh, :], start=st, stop=sp_)
                        return last
                    pvq.append((("pe", pvn), dict(reads=[PTN[pti]] + [f"va{kc // 2}_{kc % 2}{'a' if grp == 0 else 'b'}" for (kc, _, _) in near],
                                                  writes=[f"ps{quad}", f"ps{2 + quad}"])))
                for hs8 in range(8):
                    head(hs8)
                flush(0)
                for qt in range(2):
                    for quad in range(2):
                        bk = qt * 2 + quad
                        oa = self.oacc(bk)
                        if grp == 0:
                            outv = OG[:, qt, quad * 256:(quad + 1) * 256].rearrange("p (h d) -> p h d", h=4)
                            inv = oa[:, :, 0:64]
                        else:
                            outv = OG[:, qt, 512:1024].rearrange("p (r i d) -> p i r d", r=2, i=4)[:, 2 * quad:2 * quad + 2, :, :]
                            inv = oa[:, :, 0:64].rearrange("p (i r) d -> p i r d", r=2)

                        def nrm(e, oa=oa, outv=outv, inv=inv, qt=qt, quad=quad, grp=grp):
                            if grp == 0:
                                e.reciprocal(out=RDEN[:, 0:4], in_=oa[:, :, 64])
                            else:
                                e.tensor_tensor(out=RDEN[:, 0:4], in0=oa[:, :, 64], in1=SINKT[:, qt, 4 * quad:4 * quad + 4], op=ALU.add)
                                e.reciprocal(out=RDEN[:, 0:4], in_=RDEN[:, 0:4])
                            if grp == 0:
                                rb_ = RDEN[:, 0:4].unsqueeze(2).to_broadcast([128, 4, 64])
                            else:
                                rb_ = RDEN[:, 0:4].rearrange("p (i r) -> p i r", r=2).unsqueeze(3).to_broadcast([128, 2, 2, 64])
                            return e.tensor_tensor(out=outv, in0=inv, in1=rb_, op=ALU.mult)
                        def nrm1(e, oa=oa, qt=qt, quad=quad, grp=grp):
                            if grp == 0:
                                return e.reciprocal(out=RDEN[:, 4 * (bk % 2):4 * (bk % 2) + 4], in_=oa[:, :, 64])
                            return e.tensor_tensor(out=RDEN[:, 4 * (bk % 2):4 * (bk % 2) + 4], in0=oa[:, :, 64], in1=SINKT[:, qt, 4 * quad:4 * quad + 4], op=ALU.add)
                        rdn = f"rden{bk % 2}"
                        RD = RDEN[:, 4 * (bk % 2):4 * (bk % 2) + 4]
                        if grp == 0:
                            sc.add("dve", (lambda e, oa=oa, RD=RD: e.reciprocal(out=RD, in_=oa[:, :, 64])), reads=[f"ps{bk}"], writes=[rdn])
                        else:
                            sc.add("dve", (lambda e, oa=oa, RD=RD, qt=qt, quad=quad: e.tensor_tensor(out=RD, in0=oa[:, :, 64], in1=SINKT[:, qt, 4 * quad:4 * quad + 4], op=ALU.add)),
                                   reads=[f"ps{bk}", "sinkt"], writes=[rdn])
                            sc.add("dve", (lambda e, RD=RD: e.reciprocal(out=RD, in_=RD)), reads=[rdn], writes=[rdn])
                        if grp == 0:
                            rb_ = RD.unsqueeze(2).to_broadcast([128, 4, 64])
                        else:
                            rb_ = RD.rearrange("p (i r) -> p i r", r=2).unsqueeze(3).to_broadcast([128, 2, 2, 64])
                        sc.add("dve", (lambda e, outv=outv, inv=inv, rb_=rb_: e.tensor_tensor(out=outv, in0=inv, in1=rb_, op=ALU.mult)),
                               reads=[f"ps{bk}", rdn], writes=["rA"])
            if getattr(self, 'stop', 99) <= 7:
                return
            for half in range(2):
                bk = 4 + half
                psT = self.psb(bk, BF16).rearrange("p (c t) -> p c t", c=4)

                def trO(e, half=half, psT=psT):
                    last = None
                    for cc in range(4):
                        c = half * 4 + cc
                        for qt in range(2):
                            last = e.transpose(psT[:, cc, qt * 128:(qt + 1) * 128], OG[:, qt, c * 128:(c + 1) * 128], IDENT)
                    return last
                sc.add("pe", trO, reads=["rA", "ident"], writes=[f"ps{bk}"])
                if half == 0:
                    sc.add("act", (lambda e, psT=psT: e.activation(out=OTG[:, 0:4, :], in_=psT, func=AF.Copy)), reads=[f"ps{bk}"], writes=["rB"])
                else:
                    sc.add("dve", (lambda e, psT=psT: e.tensor_copy(out=OTG[:, 4:8, :], in_=psT)), reads=[f"ps{bk}"], writes=["rBb"])
            if getattr(self, 'stop', 99) <= 8:
                return
            for o_ in range(8):
                bk = 4 + o_ % 3
                ps = self.psb(bk)

                def mmo(e, o_=o_, ps=ps):
                    last = None
                    for c in range(8):
                        last = e.matmul(ps[:, 0:256], WOUT[:, c, o_ * 128:(o_ + 1) * 128], OTG[:, c, :], start=(c == 0), stop=(c == 7))
                    return last
                sc.add("pe", mmo, reads=["wout", "rB", "rBb"], writes=[f"ps{bk}"])
                ysl = YSBA[:, o_, :]
                sc.add("act", (lambda e, ps=ps, ysl=ysl: e.activation(out=ysl, in_=ps[:, 0:256], func=AF.Copy)),
                       reads=[f"ps{bk}", "rA", "rC"], writes=[f"ysa{o_}"])
                sqb = PTS[o_ % 4]
                sc.add("dve", (lambda e, ysl=ysl, sqb=sqb: e.tensor_tensor(out=sqb[:, 0:256], in0=ysl, in1=ysl, op=ALU.mult)),
                       reads=[f"ysa{o_}"], writes=[PTN[o_ % 4]])
                sc.add("pe", (lambda e, sqb=sqb, o_=o_: e.matmul(self.psb(7)[:, 0:256], self.ONES, sqb[:, 0:256], start=(o_ == 0), stop=(o_ == 7))),
                       reads=[PTN[o_ % 4], "ones"], writes=["ps7"])
            if getattr(self, 'stop', 99) <= 9:
                return
            self.resid_update(l, 0, t0, 256, YSBA, (lambda c: f"ysa{c}"), 7)
            sc.marker(reads=[f"ysa{c}" for c in range(8)], writes=["rA", "rC"])

        for g in range(getattr(self, 'ngroups', 8)):
            group(g)
        sc.marker(writes=["bmask", "gm", "cmp", "selr", "augb", "selfm", "sh8", "sinkt", "sqrt_t", "wada_ok"])

    def oacc(self, bk):
        return self.PS[bk][:, 0:260].rearrange("p (h d) -> p h d", d=65)


def _host_prep(inputs):
    f = np.float32
    w_ada = np.asarray(inputs["w_ada"], f)
    w1 = np.asarray(inputs["w1"], f)
    w2 = np.asarray(inputs["w2"], f)
    w_in = np.asarray(inputs["w_in"], f)
    w_out = np.asarray(inputs["w_out"], f)
    sh = {}
    sh["wada"] = np.ascontiguousarray(w_ada.reshape(DEPTH, 8, 128, 24, 256).transpose(0, 3, 2, 1, 4))
    sh["badac"] = np.ascontiguousarray(np.asarray(inputs["b_ada"], f).reshape(DEPTH, 48, 128).transpose(2, 0, 1).reshape(128, DEPTH * 48))
    sh["gainc"] = np.ascontiguousarray(np.asarray(inputs["norm_gains"], f).reshape(DEPTH, 4, 8, 128).transpose(3, 0, 1, 2).reshape(128, 128))
    sh["b1c"] = np.ascontiguousarray(np.asarray(inputs["b1"], f).reshape(DEPTH, 32, 128).transpose(2, 0, 1).reshape(128, 128))
    sh["b2c"] = np.ascontiguousarray(np.asarray(inputs["b2"], f).reshape(DEPTH, 8, 128).transpose(2, 0, 1).reshape(128, 32))
    sh["w1r"] = np.ascontiguousarray(w1.reshape(DEPTH, 8, 128, 16, 256).transpose(0, 3, 2, 1, 4))
    sh["w2r"] = np.ascontiguousarray(w2.reshape(DEPTH, 2, 16, 128, 8, 128).transpose(0, 4, 1, 3, 2, 5))
    perm = [(k // 2) + 4 * (k % 2) for k in range(8)]
    colidx = np.arange(DIN)
    qb = colidx[1536:2048].reshape(8, 64)[perm].reshape(-1)
    colidx = np.concatenate([colidx[:1536], qb, colidx[2048:]])
    w_in_p = w_in[:, :, colidx]
    sh["winr"] = np.ascontiguousarray(w_in_p.reshape(DEPTH, 8, 128, DIN).transpose(0, 2, 1, 3))
    sh["woutr"] = np.ascontiguousarray(w_out.reshape(DEPTH, 8, 128, D).transpose(0, 2, 1, 3))
    sh["sinks"] = np.ascontiguousarray(np.asarray(inputs["sinks"], f)[:, perm])
    rb = np.asarray(inputs["rel_bias"], f)
    hperm = list(range(8)) + [8 + p for p in perm]
    rb = rb[:, hperm]
    sh["rbT"] = np.ascontiguousarray(rb.T).reshape(1, 16 * 32)
    tab = np.concatenate([rb, np.full((1, 16), NEG, f)], axis=0)
    idx = _dtile_index()
    dt = np.zeros((128, 16, 2, 128), f)
    for h in range(16):
        hg = 0 if h < 8 else 1
        for t in range(2):
            dt[:, h, t, :] = tab[idx[hg, t], h]
    sh["dtile"] = dt.reshape(128, 16 * 2 * 128)
    sh["ident"] = np.eye(128, dtype=f)
    hsel = np.zeros((128, 2, 128), f)
    hsel[0:64, 0, :] = 1.0
    hsel[64:128, 1, :] = 1.0
    sh["hsel"] = hsel.reshape(128, 256)
    hind = np.zeros((128, 2), f)
    hind[0:64, 0] = 1.0
    hind[64:128, 1] = 1.0
    sh["hind"] = hind
    ind = np.zeros((72, 8, 128), f)
    for j in range(8):
        for pb in (0, 32, 64):
            ind[pb + j, j, :] = 1.0
    sh["indall"] = ind.reshape(72, 1024)
    bm = np.zeros((128, 3, 8, 8), f)
    for b in range(8):
        for j in range(8):
            bm[:, 0, b, j] = 0.0 if j < b else -1e30
            bm[:, 1, b, j] = 1.0 if j < b else 0.0
            bm[:, 2, b, j] = 1.0 if j == b else 0.0
    sh["bmask"] = bm.reshape(128, 192)
    x = np.asarray(inputs["x"], f)
    c = np.asarray(inputs["c"], f)
    per = []
    for b in range(x.shape[0]):
        m = dict(sh)
        m["xT"] = np.ascontiguousarray(x[b].T)
        m["cT"] = np.ascontiguousarray(c[b].reshape(8, 128).T)
        per.append(m)
    return per


_PROG_CACHE = {}


def _get_prog(phases, debug=False, ngroups=8, stop=99):
    key = (tuple(phases), debug, ngroups, stop)
    if key not in _PROG_CACHE:
        _PROG_CACHE[key] = Prog(list(phases), debug=debug, ngroups=ngroups, stop=stop)
    return _PROG_CACHE[key]


def run_phases(inputs, phases, n_cores=8, trace=False, debug=False, ngroups=8, stop=99):
    per = _host_prep(inputs)[:n_cores]
    prog = _get_prog(phases, debug, ngroups, stop)
    res = run_bass_kernel_spmd(prog.nc, per, core_ids=list(range(n_cores)), trace=trace)
    outs = [np.ascontiguousarray(r["outT"].T) for r in res.results]
    return np.stack(outs, axis=0), res


def kernel(**inputs):
    phases = []
    for l in range(DEPTH):
        phases += [("attn", l), ("ffn", l)]
    out, _ = run_phases(inputs, phases)
    return out.astype(np.float32)
```

```python
import math
import numpy as np
import concourse.bass as bass
import concourse.mybir as mybir
from concourse.bass_utils import run_bass_kernel_spmd

F32 = mybir.dt.float32
BF16 = mybir.dt.bfloat16
AF = mybir.ActivationFunctionType
ALU = mybir.AluOpType
AX = mybir.AxisListType

D = 1024
S = 2048
DEPTH = 4
DFF = 4096
DIN = 2304
EPS = 1e-6
NEG = -30000.0
BIG = 1024.0
MOBA_ROUND = "trunc"
SWA_ROUND = "trunc"


class Sched:
    def __init__(self):
        self.ops = []
        self.last_w = {}
        self.readers = {}

    def marker(self, reads=(), writes=()):
        k = getattr(self, "_mk", 0)
        self._mk = k + 1
        col = k % 8
        dm = self.dummy
        self.add("dve", (lambda e: e.memset(dm[:, col:col + 1], 0.0)), reads=reads, writes=list(writes) + [f"dummy{col}"])

    def barrier(self):
        names = set(self.last_w) | set(self.readers)
        names.add("__bar__")
        self.marker(writes=sorted(names))

    def add(self, eng, fn, reads=(), writes=(), dma=None, ndma=1, total=False):
        idx = len(self.ops)
        reads = tuple(reads) + ("__bar__",)
        writes = tuple(writes)
        deps = set()
        for r in reads:
            w = self.last_w.get(r)
            if w is not None:
                deps.add(w)
        for w_ in writes:
            w = self.last_w.get(w_)
            if w is not None:
                deps.add(w)
            for rd in self.readers.get(w_, ()):
                deps.add(rd)
        for r in reads:
            self.readers.setdefault(r, []).append(idx)
        for w_ in writes:
            self.last_w[w_] = idx
            self.readers[w_] = []
        self.ops.append(dict(eng=eng, fn=fn, deps=deps, dma=dma, ndma=ndma, total=total,
                             reads=set(reads), writes=set(writes)))
        return idx

    def finalize(self, nc, semctx):
        ops = self.ops
        need = [False] * len(ops)
        for i, o in enumerate(ops):
            keep = set()
            for d in o["deps"]:
                p = ops[d]
                if p["dma"] is not None or o["dma"] is not None:
                    keep.add(d)
                elif p["eng"] != o["eng"]:
                    keep.add(d)
                else:
                    if o["eng"] != "pe" and (p["writes"] & (o["reads"] | o["writes"])):
                        keep.add(d)
            o["deps"] = keep
            for d in keep:
                need[d] = True
        sems = {}

        def getsem(name):
            if name not in sems:
                sems[name] = semctx(name)
            return sems[name]

        cnt = {}
        totals = {}
        for i, o in enumerate(ops):
            if o["dma"] is not None:
                key = "d_" + o["dma"]
                cnt[key] = cnt.get(key, 0) + 16 * o["ndma"]
                o["sig"] = (key, cnt[key])
                if o["total"]:
                    totals[key] = True
            elif need[i]:
                key = "e_" + o["eng"]
                cnt[key] = cnt.get(key, 0) + 1
                o["sig"] = (key, cnt[key])
            else:
                o["sig"] = None
        for o in ops:
            if o["dma"] is not None and o["total"]:
                o["sig"] = (o["sig"][0], cnt[o["sig"][0]])
        for o in ops:
            w = {}
            for d in o["deps"]:
                k, v = ops[d]["sig"]
                if w.get(k, 0) < v:
                    w[k] = v
            o["waits"] = w
        for k in cnt:
            getsem(k)
        self.sems = sems
        self.cnt = cnt

    def emit(self, eng, e):
        waited = {}
        n = 0
        for o in self.ops:
            if o["eng"] != eng:
                continue
            for k in sorted(o["waits"]):
                v = o["waits"][k]
                if waited.get(k, 0) < v:
                    e.wait_ge(self.sems[k], v)
                    waited[k] = v
            ins = o["fn"](e)
            n += 1
            if o["dma"] is not None:
                if not isinstance(ins, (list, tuple)):
                    ins = [ins]
                assert len(ins) == o["ndma"], (len(ins), o["ndma"])
                for i_ in ins:
                    i_.then_inc(self.sems[o["sig"][0]], 16)
            elif o["sig"] is not None:
                if isinstance(ins, (list, tuple)):
                    ins = ins[-1]
                ins.then_inc(self.sems[o["sig"][0]], 1)
        return n


def _t5_bucket_np(dist, mode):
    n = np.maximum(dist, 0).astype(np.int32)
    nf = np.maximum(n, 1).astype(np.float32)
    val = (np.log(nf / np.float32(16)) / np.float32(math.log(128 / 16)) * np.float32(16)).astype(np.float32)
    if mode == "trunc":
        li = val.astype(np.int32)
    else:
        li = np.rint(val).astype(np.int32)
    large = np.minimum(16 + li, 31)
    return np.where(n < 16, n, large)


def _dtile_index():
    k = np.arange(128)[:, None]
    q = np.arange(128)[None, :]
    out = np.zeros((2, 2, 128, 128), np.int64)
    for hg, mode in ((0, MOBA_ROUND), (1, SWA_ROUND)):
        d0 = q - k
        b0 = _t5_bucket_np(d0, mode)
        out[hg, 1] = np.where(d0 >= 0, b0, 32)
        d1 = 128 + q - k
        b1 = _t5_bucket_np(d1, mode)
        if hg == 0:
            out[hg, 0] = b1
        else:
            out[hg, 0] = np.where(d1 < 128, b1, 32)
    return out


class Prog:
    def __init__(self, phases, debug=False, ngroups=8, stop=99):
        self.phases = phases
        self.stop = stop
        self.ngroups = ngroups
        self.debug = debug
        self.dbg_names = []
        self.nc = bass.Bass("TRN2", target_bir_lowering=False)
        self.sc = Sched()
        self.build()

    def dram_in(self, name, shape, dt=F32):
        return self.nc.dram_tensor(name, list(shape), dt, kind="ExternalInput").ap()

    def build(self):
        nc = self.nc
        sc = self.sc
        self.d_xT = self.dram_in("xT", [D, S])
        self.d_cT = self.dram_in("cT", [128, 8])
        self.d_wada = self.dram_in("wada", [DEPTH, 24, 128, 8, 256])
        self.d_badac = self.dram_in("badac", [128, DEPTH * 48])
        self.d_gainc = self.dram_in("gainc", [128, 128])
        self.d_b1c = self.dram_in("b1c", [128, 128])
        self.d_b2c = self.dram_in("b2c", [128, 32])
        self.d_w1r = self.dram_in("w1r", [DEPTH, 16, 128, 8, 256])
        self.d_w2r = self.dram_in("w2r", [DEPTH, 8, 2, 128, 16, 128])
        self.d_winr = self.dram_in("winr", [DEPTH, 128, 8, DIN])
        self.d_woutr = self.dram_in("woutr", [DEPTH, 128, 8, D])
        self.d_sinks = self.dram_in("sinks", [DEPTH, 8])
        self.d_rbT = self.dram_in("rbT", [1, 16 * 32])
        self.d_dtile = self.dram_in("dtile", [128, 16 * 2 * 128])
        self.d_ident = self.dram_in("ident", [128, 128])
        self.d_hsel = self.dram_in("hsel", [128, 2 * 128])
        self.d_hind = self.dram_in("hind", [128, 2])
        self.d_indall = self.dram_in("indall", [72, 8 * 128])
        self.d_bmask = self.dram_in("bmask", [128, 3 * 64])
        self.d_out = nc.dram_tensor("outT", [D, S], F32, kind="ExternalOutput").ap()

        total_words = 53200
        self.pool = nc.alloc_sbuf_tensor("pool", [128, total_words], F32)
        self.off = 0

        def alloc(words):
            o = self.off
            self.off += (words + 7) // 8 * 8
            assert self.off <= total_words, (self.off, total_words)
            return o

        def view(o, words, dt=F32):
            v = self.pool[:, o:o + words]
            if dt != F32:
                v = v.bitcast(dt)
            return v

        self.view = view
        o_x = alloc(8 * S)
        self.XT = view(o_x, 8 * S).rearrange("p (c t) -> p c t", c=8)
        self.COLS = view(alloc(320), 320)
        self.GAINC = view(alloc(128), 128)
        self.B1C = view(alloc(128), 128)
        self.B2C = view(alloc(32), 32)
        self.BADAC = view(alloc(192), 192)
        self.MODC = view(alloc(48), 48)
        self.CT = view(alloc(8), 8)
        self.CACT = view(alloc(8), 8, BF16)[:, 0:8]
        self.IDENT = view(alloc(64), 64, BF16)
        self.ONES = view(alloc(64), 64, BF16)
        self.SQ = [view(alloc(512), 512, BF16).rearrange("p (a t) -> p a t", a=2) for _ in range(2)]
        self.rstd_off = self.off
        self.RSTD = [view(alloc(512), 512) for _ in range(2)]
        self.XN = [view(alloc(512), 512) for _ in range(2)]
        self.wada_off = self.off
        self.WADA = [view(alloc(1024), 1024, BF16).rearrange("p (k n) -> p k n", k=8) for _ in range(2)]
        self.DUMMY = view(alloc(8), 8)
        sc.dummy = self.DUMMY
        self.EPSC = view(alloc(8), 8)
        self.phase_base = self.off

        self.PS = [nc.alloc_psum_tensor(f"psb{i}", [128, 512], F32) for i in range(8)]

        self.preamble()
        done_mod = set()
        self.side = []
        for pi, (kind, l) in enumerate(self.phases):
            if l not in done_mod:
                self.mod_layer(l)
                done_mod.add(l)
            sc.barrier()
            if kind == "attn":
                self.attn_phase(l)
            else:
                nxt = [ll for (_, ll) in self.phases[pi + 1:] if ll not in done_mod]
                if nxt:
                    self.side = self.mod_jobs(nxt[0])
                    done_mod.add(nxt[0])
                self.ffn_phase(l)
                self.run_side(100)
        sc.barrier()
        self.epilogue()

        class _SemCtx:
            pass
        semlist = []

        def semctx(name):
            cm = nc.semaphore(name)
            h = cm.__enter__()
            semlist.append(cm)
            return h

        sc.finalize(nc, semctx)
        with nc.Block() as block:
            @block.tensor
            def _(e):
                sc.emit("pe", e)

            @block.scalar
            def _(e):
                sc.emit("act", e)

            @block.vector
            def _(e):
                sc.emit("dve", e)

            @block.gpsimd
            def _(e):
                sc.emit("pool", e)

            @block.sync
            def _(e):
                sc.emit("sp", e)

    def dump(self, name, ap, reads):
        if not getattr(self, "debug", False):
            return
        shp = list(ap.shape)
        d = self.nc.dram_tensor("dbg_" + name, shp, ap.dtype, kind="ExternalOutput").ap()
        self.sc.add("sp", (lambda e: e.dma_start(out=d, in_=ap)), reads=list(reads), writes=["dbg_" + name], dma="dbg_" + name)
        self.dbg_names.append("dbg_" + name)

    def psb(self, i, dt=F32):
        v = self.PS[i][:, :]
        if dt != F32:
            v = v.bitcast(dt)
        return v

    def preamble(self):
        sc = self.sc
        XT = self.XT
        xs = self.d_xT.rearrange("(c p) t -> p c t", p=128)
        for c in range(8):
            sc.add("sp", (lambda e, c=c: e.dma_start(out=XT[:, c, :], in_=xs[:, c, :])),
                   writes=[f"xTc{c}"], dma=f"xin{c}")
        small = [(self.GAINC, self.d_gainc, "gainc"), (self.B1C, self.d_b1c, "b1c"), (self.B2C, self.d_b2c, "b2c"),
                 (self.BADAC, self.d_badac, "badac"), (self.CT, self.d_cT, "ct")]
        for (dst, src, nm) in small:
            sc.add("sp", (lambda e, dst=dst, src=src: e.dma_start(out=dst, in_=src[:, :])),
                   writes=[nm], dma="c_" + nm)
        sc.add("pool", (lambda e: e.dma_start(out=self.IDENT, in_=self.d_ident[:, :])), writes=["ident"], dma="c_ident")
        sc.add("dve", (lambda e: e.memset(self.ONES, 1.0)), writes=["ones"])
        sc.add("dve", (lambda e: e.memset(self.EPSC, float(D * EPS))), writes=["epsc"])
        sc.add("act", (lambda e: e.activation(out=self.CACT, in_=self.CT, func=AF.Silu)), reads=["ct"], writes=["cact"])
        sc.marker(reads=[f"xTc{c}" for c in range(8)], writes=[f"xT{tb}" for tb in range(8)] + ["rgnA_ok", "rgnB_ok"])

    def epilogue(self):
        sc = self.sc
        XT = self.XT
        od = self.d_out.rearrange("(c p) t -> p c t", p=128)
        for c in range(8):
            sc.add("sp", (lambda e, c=c: e.dma_start(out=od[:, c, :], in_=XT[:, c, :])),
                   reads=[f"xT{tb}" for tb in range(8)], writes=[f"out{c}"], dma=f"xout{c}")
        sc.add("sp", (lambda e: e.nop()), reads=[f"out{c}" for c in range(8)])

    def mod_layer(self, l):
        for j in self.mod_jobs(l):
            j()

    def run_side(self, n=1):
        for _ in range(n):
            if self.side:
                self.side.pop(0)()

    def mod_jobs(self, l):
        jobs = []
        for pc in range(24):
            jobs.append(lambda pc=pc: self.mod_piece(l, pc))
        jobs.append(lambda: self.mod_finish(l))
        return jobs

    def mod_piece(self, l, pc):
        sc = self.sc
        ps = self.psb(7)
        if True:
            buf = self.WADA[pc % 2]
            bn = f"wada{pc % 2}"
            src = self.d_wada[l, pc]
            sc.add("pool", (lambda e, buf=buf, src=src: e.dma_start(out=buf, in_=src)), reads=["wada_ok"], writes=[bn], dma=bn)

            def mm(e, buf=buf, pc=pc):
                last = None
                for j in range(2):
                    col = pc * 2 + j
                    for kc in range(8):
                        last = e.matmul(ps[:, col:col + 1], buf[:, kc, j * 128:(j + 1) * 128],
                                        self.CACT[:, kc:kc + 1], start=(kc == 0), stop=(kc == 7))
                return last
            sc.add("pe", mm, reads=[bn, "cact"], writes=["ps7"])
    def mod_finish(self, l):
        sc = self.sc
        ps = self.psb(7)
        MODC = self.MODC
        sc.add("dve", (lambda e: e.tensor_tensor(out=MODC, in0=ps[:, 0:48], in1=self.BADAC[:, l * 48:(l + 1) * 48], op=ALU.add)),
               reads=["ps7", "badac"], writes=["modc"])
        C = self.COLS
        b = l * 64
        G = self.GAINC
        g0 = (l * 4) * 8

        def cols(e):
            e.scalar_tensor_tensor(out=C[:, b + 0:b + 8], in0=MODC[:, 8:16], scalar=1.0, in1=G[:, g0 + 0:g0 + 8], op0=ALU.add, op1=ALU.mult)
            e.tensor_copy(out=C[:, b + 8:b + 16], in_=MODC[:, 0:8])
            e.tensor_tensor(out=C[:, b + 16:b + 24], in0=MODC[:, 16:24], in1=G[:, g0 + 8:g0 + 16], op=ALU.mult)
            e.scalar_tensor_tensor(out=C[:, b + 24:b + 32], in0=MODC[:, 32:40], scalar=1.0, in1=G[:, g0 + 16:g0 + 24], op0=ALU.add, op1=ALU.mult)
            e.tensor_copy(out=C[:, b + 32:b + 40], in_=MODC[:, 24:32])
            return e.tensor_tensor(out=C[:, b + 40:b + 48], in0=MODC[:, 40:48], in1=G[:, g0 + 24:g0 + 32], op=ALU.mult)
        sc.add("dve", cols, reads=["modc", "gainc"], writes=[f"colsraw{l}"])

        def cols2(e):
            e.tensor_scalar(out=C[:, b + 0:b + 8], in0=C[:, b + 0:b + 8], scalar1=32.0, scalar2=None, op0=ALU.mult)
            e.tensor_scalar(out=C[:, b + 16:b + 32], in0=C[:, b + 16:b + 32], scalar1=32.0, scalar2=None, op0=ALU.mult)
            return e.tensor_scalar(out=C[:, b + 40:b + 48], in0=C[:, b + 40:b + 48], scalar1=32.0, scalar2=None, op0=ALU.mult)
        sc.add("dve", cols2, reads=[f"colsraw{l}"], writes=[f"cols{l}"])
        self.dump(f"cols{l}", C[:, b:b + 48], [f"cols{l}"])
        self.dump(f"modc{l}", MODC, [f"cols{l}"])

    def rmsnorm_in(self, l, sub, t0, n, HT, ht_res, psbank, extra_reads=(), sq_names=None):
        sc = self.sc
        XT = self.XT
        tbs = [f"xT{tb}" for tb in range(t0 // 256, (t0 + n) // 256)]
        ps = self.psb(psbank)
        cb = l * 64 + (0 if sub == 0 else 24)
        C = self.COLS
        for cp in range(4):
            sq = self.SQ[cp % 2]
            sqn = f"sq{cp % 2}"
            sqw = [sqn] if sq_names is None else sq_names[cp % 2]
            sc.add("act", (lambda e, cp=cp, sq=sq: e.activation(out=sq[:, :, 0:n], in_=XT[:, 2 * cp:2 * cp + 2, t0:t0 + n], func=AF.Square)),
                   reads=tbs, writes=sqw)

            def mm(e, cp=cp, sq=sq):
                last = None
                for j in range(2):
                    c = 2 * cp + j
                    last = e.matmul(ps[:, 0:n], self.ONES, sq[:, j, 0:n], start=(c == 0), stop=(c == 7))
                return last
            sc.add("pe", mm, reads=[sqn, "ones"], writes=[f"ps{psbank}"])
        rs = self.RSTD[0]
        sc.add("act", (lambda e: e.activation(out=rs[:, 0:n], in_=ps[:, 0:n], func=AF.Ln, bias=self.EPSC[:, 0:1], scale=1.0)),
               reads=[f"ps{psbank}", "epsc"], writes=["rstd0p", "rstd0"])
        sc.add("act", (lambda e: e.activation(out=rs[:, 0:n], in_=rs[:, 0:n], func=AF.Exp, scale=-0.5)),
               reads=["rstd0p"], writes=["rstd0", "rstd0p"])
        for c in range(8):
            xn = self.XN[c % 2]
            xnn = f"xn{c % 2}"
            sc.add("dve", (lambda e, c=c, xn=xn: e.tensor_tensor(out=xn[:, 0:n], in0=XT[:, c, t0:t0 + n], in1=rs[:, 0:n], op=ALU.mult)),
                   reads=tbs + ["rstd0"], writes=[xnn])
            sc.add("act", (lambda e, c=c, xn=xn: e.activation(out=HT[:, c, 0:n], in_=xn[:, 0:n], func=AF.Identity,
                                                               scale=C[:, cb + c:cb + c + 1], bias=C[:, cb + 8 + c:cb + 9 + c])),
                   reads=[xnn, f"cols{l}"] + list(extra_reads), writes=[ht_res])

    def resid_update(self, l, sub, t0, n, Y, y_res, ssbank):
        sc = self.sc
        XT = self.XT
        tbs = [f"xT{tb}" for tb in range(t0 // 256, (t0 + n) // 256)]
        ps = self.psb(ssbank)
        C = self.COLS
        cb = l * 64 + (16 if sub == 0 else 40)
        rs = self.RSTD[1]
        sc.add("act", (lambda e: e.activation(out=rs[:, 0:n], in_=ps[:, 0:n], func=AF.Ln, bias=self.EPSC[:, 0:1], scale=1.0)),
               reads=[f"ps{ssbank}", "epsc"], writes=["rstd1p", "rstd1"])
        sc.add("act", (lambda e: e.activation(out=rs[:, 0:n], in_=rs[:, 0:n], func=AF.Exp, scale=-0.5)),
               reads=["rstd1p"], writes=["rstd1", "rstd1p"])
        if t0 == 0 and sub == 1:
            self.dump("rstd1", rs, ["rstd1"])
            self.dump("ysb", Y, [y_res(c) for c in range(8)])
        for c in range(8):
            sc.add("dve", (lambda e, c=c: e.scalar_tensor_tensor(out=Y[:, c, 0:n], in0=Y[:, c, 0:n], scalar=C[:, cb + c:cb + c + 1], in1=rs[:, 0:n],
                                                                 op0=ALU.mult, op1=ALU.mult)),
                   reads=[y_res(c), "rstd1", f"cols{l}"], writes=[y_res(c)])
            sc.add("dve", (lambda e, c=c: e.tensor_tensor(out=XT[:, c, t0:t0 + n], in0=XT[:, c, t0:t0 + n], in1=Y[:, c, 0:n], op=ALU.add)),
                   reads=[y_res(c)] + tbs, writes=tbs)

    def ffn_phase(self, l):
        sc = self.sc
        view = self.view
        base = self.phase_base
        o = base
        HID = view(o, 32 * 1024 // 2, BF16).rearrange("p (m t) -> p m t", m=32); o += 16384
        rgn = o; o += 8192
        HT = view(rgn, 4096, BF16).rearrange("p (c t) -> p c t", c=8)
        W1B = [view(rgn + 4096 + i * 1024, 1024, BF16).rearrange("p (k n) -> p k n", k=8) for i in range(3)]
        RL = [view(rgn + 4096 + 3072 + i * 512, 512) for i in range(2)]
        YSB = view(rgn, 8192).rearrange("p (c t) -> p c t", c=8)
        W2B = [view(o + i * 1024, 1024, BF16).rearrange("p (k n) -> p k n", k=16) for i in range(3)]; o += 3072
        assert o <= 53200, o
        B1C, B2C = self.B1C, self.B2C
        w1cnt = 0
        w2cnt = 0
        for H in range(2):
            T0 = H * 1024
            region_users = ["rgnA_ok"]
            for tg in range(2):
                self.rmsnorm_in(l, 1, T0 + tg * 512, 512, HT[:, :, tg * 512:(tg + 1) * 512], f"ht{tg}", 6,
                                extra_reads=region_users)
            if H == 0:
                self.dump("ht", HT, ["ht0", "ht1"])
            psi = 0
            for g in range(16):
                self.run_side(1)
                wb = W1B[w1cnt % 3]; wn = f"w1b{w1cnt % 3}"; w1cnt += 1
                src = self.d_w1r[l, g]
                sc.add("pool", (lambda e, wb=wb, src=src: e.dma_start(out=wb, in_=src)), reads=region_users, writes=[wn], dma=wn)
                for mm_ in range(2):
                    m = 2 * g + mm_
                    for tg in range(2):
                        bank = psi % 4; psi += 1
                        ps = self.psb(bank)

                        def mm(e, wb=wb, mm_=mm_, tg=tg, ps=ps):
                            last = None
                            for c in range(8):
                                last = e.matmul(ps, wb[:, c, mm_ * 128:(mm_ + 1) * 128], HT[:, c, tg * 512:(tg + 1) * 512],
                                                start=(c == 0), stop=(c == 7))
                            return last
                        sc.add("pe", mm, reads=[wn, f"ht{tg}"], writes=[f"ps{bank}"])
                        rl = RL[psi % 2]; rln = f"rl{psi % 2}"
                        sc.add("act", (lambda e, rl=rl, ps=ps, m=m: e.activation(out=rl, in_=ps, func=AF.Relu,
                                                                                 bias=B1C[:, l * 32 + m:l * 32 + m + 1])),
                               reads=[f"ps{bank}", "b1c"] + region_users, writes=[rln])
                        sc.add("dve", (lambda e, rl=rl, m=m, tg=tg: e.tensor_tensor(out=HID[:, m, tg * 512:(tg + 1) * 512], in0=rl, in1=rl, op=ALU.mult)),
                               reads=[rln], writes=[f"hid{m}_{tg}"])
            if H == 0:
                self.dump("hid", HID, [f"hid{m}_{tg}" for m in range(32) for tg in range(2)])
            sc.marker(writes=["ht0", "ht1", "w1b0", "w1b1", "w1b2", "rl0", "rl1", "rgnB_ok"])
            ht_users = ["rgnB_ok"]
            for o_ in range(8):
                wbs = []
                for kh in range(2):
                    wb = W2B[w2cnt % 3]; wn = f"w2b{w2cnt % 3}"; w2cnt += 1
                    src = self.d_w2r[l, o_, kh]
                    sc.add("pool", (lambda e, wb=wb, src=src: e.dma_start(out=wb, in_=src)), writes=[wn], dma=wn)
                    wbs.append((wb, wn))
                banks = [(o_ % 2) * 2, (o_ % 2) * 2 + 1]
                for kh in range(2):
                    wb, wn = wbs[kh]
                    for tg in range(2):
                        ps = self.psb(banks[tg])

                        def mm(e, wb=wb, kh=kh, tg=tg, ps=ps):
                            last = None
                            for kk in range(16):
                                m = kh * 16 + kk
                                last = e.matmul(ps, wb[:, kk, :], HID[:, m, tg * 512:(tg + 1) * 512],
                                                start=(m == 0), stop=(m == 31))
                            return last
                        sc.add("pe", mm, reads=[wn] + [f"hid{kh * 16 + kk}_{tg}" for kk in range(16)], writes=[f"ps{banks[tg]}"])
                for tg in range(2):
                    ps = self.psb(banks[tg])
                    ysl = YSB[:, o_, tg * 512:(tg + 1) * 512]
                    sc.add("act", (lambda e, ps=ps, ysl=ysl, o_=o_: e.activation(out=ysl, in_=ps, func=AF.Identity,
                                                                                  bias=B2C[:, l * 8 + o_:l * 8 + o_ + 1])),
                           reads=[f"ps{banks[tg]}", "b2c"] + ht_users, writes=[f"ysb{o_}t{tg}"])
                    sq = self.SQ[tg][:, 0, :]
                    sc.add("dve", (lambda e, ysl=ysl, sq=sq: e.tensor_tensor(out=sq, in0=ysl, in1=ysl, op=ALU.mult)),
                           reads=[f"ysb{o_}t{tg}"], writes=[f"sq{tg}"])
                    ssb = 4 + tg
                    sc.add("pe", (lambda e, sq=sq, ssb=ssb, o_=o_: e.matmul(self.psb(ssb), self.ONES, sq, start=(o_ == 0), stop=(o_ == 7))),
                           reads=[f"sq{tg}", "ones"], writes=[f"ps{ssb}"])
            for tg in range(2):
                self.resid_update(l, 1, T0 + tg * 512, 512, YSB[:, :, tg * 512:(tg + 1) * 512],
                                  (lambda c, tg=tg: f"ysb{c}t{tg}"), 4 + tg)
            sc.marker(writes=[f"ysb{c}t{tg}" for c in range(8) for tg in range(2)] + ["rgnA_ok"])

    def attn_phase(self, l):
        sc = self.sc
        view = self.view
        XT = self.XT
        o = self.phase_base
        KT = view(o, 5120, BF16).rearrange("p (c t) -> p c t", c=5); o += 5120
        VA = view(o, 5200, BF16).rearrange("p (t h d) -> p t h d", t=16, h=10); o += 5200
        WIN = view(o, 9216, BF16).rearrange("p (k n) -> p k n", k=8); o += 9216
        WOUT = view(o, 4096, BF16).rearrange("p (k n) -> p k n", k=8); o += 4096
        DT = view(o, 2048, BF16).rearrange("p (h t q) -> p h t q", h=16, t=2); o += 2048
        rA = o; o += 1024
        rC = o; o += 1024
        rB = o; o += 1024
        HTG = view(rA, 1024, BF16).rearrange("p (c t) -> p c t", c=8)
        OG = view(rA, 1024, BF16).rearrange("p (q f) -> p q f", q=2)
        QTG = view(rC, 1024, BF16).rearrange("p (c t) -> p c t", c=8)
        QSQ = view(rB, 1024, BF16).rearrange("p (c t) -> p c t", c=8)
        OTG = view(rB, 1024, BF16).rearrange("p (c t) -> p c t", c=8)
        YSBA = view(rA, 2048).rearrange("p (c t) -> p c t", c=8)
        AUGT1 = [view(o + i * 128, 128, BF16) for i in range(3)]; o += 384
        IND = view(o, 512, BF16).rearrange("p (j k) -> p j k", j=8); o += 512
        HIND = view(o, 8, BF16)[:, 0:2]; o += 8
        KMEANT = view(o, 32, BF16).rearrange("p (c r j) -> p c r j", c=4, r=2); o += 32
        KSUM = view(o, 8, F32); o += 8
        KMAX2 = view(o, 8, F32); o += 8
        KMXG = view(o, 8, F32); o += 8
        KM16 = view(o, 16, F32); o += 16
        QMXG = view(o, 8, F32); o += 8
        QM16 = view(o, 16, F32); o += 16
        SINKS = view(o, 8, F32); o += 8
        BM8 = view(o, 16, F32); o += 16
        RB = self.XN[0].rearrange("p (h b) -> p h b", h=16)
        B31 = view(o, 16, F32); o += 16
        KSQ = view(rB, 640, BF16).rearrange("p (c t) -> p c t", c=5)
        RDEN = view(o, 8, F32); o += 8
        assert o <= 53200, o
        wsc = self.wada_off
        CMP = view(wsc, 1024).rearrange("p (a j k) -> p a j k", a=16, j=8)
        AUGB = view(wsc + 1024, 128, BF16).rearrange("p (q s j) -> p q s j", q=2, s=16)
        BMASK = view(wsc + 1600, 192).rearrange("p (k b j) -> p k b j", k=3, b=8)
        GM = view(wsc + 1792, 128).rearrange("p (a j) -> p a j", a=16)
        SEL = view(wsc + 1920, 128).rearrange("p (a j) -> p a j", a=16)
        rs_off = self.rstd_off
        SELF = view(rs_off + 256, 256).rearrange("p (q s j) -> p q s j", q=2, s=16)
        SH8 = view(rs_off + 512 + 256, 32).rearrange("p (q s) -> p q s", q=2)
        SINKT = view(rs_off + 512 + 288, 16).rearrange("p (q s) -> p q s", q=2)
        SQRT_T = view(rs_off + 512 + 304, 32).rearrange("p (q s) -> p q s", q=2)
        TMPS = [self.XN[0], self.XN[1]]
        PTS = [self.SQ[0].rearrange("p a t -> p (a t)")[:, 0:512], self.SQ[0].rearrange("p a t -> p (a t)")[:, 512:1024],
               self.SQ[1].rearrange("p a t -> p (a t)")[:, 0:512], self.SQ[1].rearrange("p a t -> p (a t)")[:, 512:1024]]
        PTN = ["sq0", "sq0b", "sq1", "sq1b"]
        IDENT = self.IDENT.rearrange("p (a b) -> p a b", a=1)[:, 0, :]

        sc.marker(writes=["wada0", "wada1", "rstd0", "rstd1", "rstd0p", "rstd1p", "wadafree"])
        sc.add("pool", (lambda e: e.dma_start(out=WIN, in_=self.d_winr[l])), writes=["win"], dma="win")
        sc.add("pool", (lambda e: e.dma_start(out=DT.rearrange("p h t q -> p (h t q)"), in_=self.d_dtile[:, :])), writes=["dt"], dma="dt")
        sc.add("pool", (lambda e: e.dma_start(out=IND.rearrange("p j k -> p (j k)")[0:72, :], in_=self.d_indall[:, :])), writes=["ind"], dma="ind")
        sc.add("pool", (lambda e: e.dma_start(out=HIND, in_=self.d_hind[:, :])), writes=["hind"], dma="hind")
        sc.add("sp", (lambda e: e.dma_start(out=BMASK.rearrange("p k b j -> p (k b j)"), in_=self.d_bmask[:, :])), reads=["wadafree"], writes=["bmask"], dma="bmask")
        sc.add("sp", (lambda e: e.dma_start(out=SINKS, in_=self.d_sinks[l:l + 1, :].partition_broadcast(128))), writes=["sinks"], dma="sinks")
        sc.add("sp", (lambda e: e.dma_start(out=RB.rearrange("p h b -> p (h b)"), in_=self.d_rbT[0:1, :].partition_broadcast(128))), writes=["xn0"], dma="rb")
        sc.add("pool", (lambda e: e.dma_start(out=WOUT, in_=self.d_woutr[l])), writes=["wout"], dma="wout")

        def init1(e):
            e.memset(KMEANT, 0.0)
            e.memset(KMAX2, 0.0)
            e.memset(VA[:, :, :, 64:65], 1.0)
            e.memset(AUGB, 0.0)
            e.memset(SELF, 1.0)
            e.tensor_reduce(out=BM8, in_=RB, axis=AX.X, op=ALU.max)
            e.tensor_copy(out=B31, in_=RB[:, :, 31])
            return e.memset(KM16, 0.0)
        sc.add("dve", init1, reads=["xn0", "wadafree"], writes=["kmeant", "kmax2", "va_ones", "augb_s", "augb_m", "selfm", "bm8raw", "b31", "km16"])

        sc.add("dve", (lambda e: e.tensor_tensor(out=BM8[:, 8:16], in0=BM8[:, 8:16], in1=SINKS, op=ALU.max)),
               reads=["bm8raw", "sinks"], writes=["bm8raw"])
        sc.add("dve", (lambda e: e.tensor_scalar(out=BM8, in0=BM8, scalar1=8.0, scalar2=None, op0=ALU.mult)),
               reads=["bm8raw"], writes=["bm8", "bm8raw"])

        QCOL, KCOL, VCOL, QBCOL, KBCOL, VBCOL = 0, 512, 1024, 1536, 2048, 2176
        sbank = [0]
        ptc = [0]

        def group(g):
            t0 = g * 256
            b = g
            xres = [f"xT{g}"]
            self.rmsnorm_in(l, 0, t0, 256, HTG, "rA", 7, sq_names=(["sq0", "sq0b"], ["sq1", "sq1b"]))
            pbank = [0]

            def nextbank():
                bk = 4 + pbank[0] % 4
                pbank[0] += 1
                return bk
            for ci in range(8):
                col = QCOL + ci * 128 if ci < 4 else QBCOL + (ci - 4) * 128
                bk = nextbank()
                ps = self.psb(bk)

                def mm(e, col=col, ps=ps):
                    last = None
                    for kc in range(8):
                        last = e.matmul(ps[:, 0:256], WIN[:, kc, col:col + 128], HTG[:, kc, :], start=(kc == 0), stop=(kc == 7))
                    return last
                sc.add("pe", mm, reads=["win", "rA"], writes=[f"ps{bk}"])
                sc.add("dve", (lambda e, ci=ci, ps=ps: e.tensor_copy(out=QTG[:, ci, :], in_=ps[:, 0:256])),
                       reads=[f"ps{bk}"], writes=["rC"])
            sc.add("dve", (lambda e: e.memset(KSUM, 0.0)), writes=[f"ksum{c}" for c in range(4)])
            for ci in range(5):
                col = KCOL + ci * 128 if ci < 4 else KBCOL
                bk = nextbank()
                ps = self.psb(bk)

                def mm(e, col=col, ps=ps):
                    last = None
                    for kc in range(8):
                        last = e.matmul(ps[:, 0:256], WIN[:, kc, col:col + 128], HTG[:, kc, :], start=(kc == 0), stop=(kc == 7))
                    return last
                sc.add("pe", mm, reads=["win", "rA"], writes=[f"ps{bk}"])
                if ci < 4:
                    sc.add("act", (lambda e, ci=ci, ps=ps: e.activation(out=KT[:, ci, t0:t0 + 256], in_=ps[:, 0:256], func=AF.Copy,
                                                                           accum_out=KSUM[:, ci:ci + 1])),
                           reads=[f"ps{bk}"], writes=[f"kt{ci}_{g}", f"ksum{ci}"])
                else:
                    sc.add("act", (lambda e, ci=ci, ps=ps: e.activation(out=KT[:, ci, t0:t0 + 256], in_=ps[:, 0:256], func=AF.Copy)),
                           reads=[f"ps{bk}"], writes=[f"kt{ci}_{g}"])
            for qt in range(2):
                tile_i = g * 2 + qt
                bk = nextbank()
                ps = self.psb(bk)

                def mmv(e, qt=qt, ps=ps):
                    last = None
                    for kc in range(8):
                        last = e.matmul(ps[:, 0:512], HTG[:, kc, qt * 128:(qt + 1) * 128], WIN[:, kc, VCOL:VCOL + 512], start=(kc == 0), stop=(kc == 7))
                    return last
                sc.add("pe", mmv, reads=["win", "rA"], writes=[f"ps{bk}"])
                sc.add("act", (lambda e, tile_i=tile_i, ps=ps: e.activation(out=VA[:, tile_i, 0:8, 0:64],
                                                                               in_=ps[:, 0:512].rearrange("p (h d) -> p h d", h=8), func=AF.Copy)),
                       reads=[f"ps{bk}", "va_ones"], writes=[f"va{g}_{qt}a"])
                bk2 = nextbank()
                ps2 = self.psb(bk2)

                def mmv2(e, qt=qt, ps2=ps2):
                    last = None
                    for kc in range(8):
                        last = e.matmul(ps2[:, 0:128], HTG[:, kc, qt * 128:(qt + 1) * 128], WIN[:, kc, VBCOL:VBCOL + 128], start=(kc == 0), stop=(kc == 7))
                    return last
                sc.add("pe", mmv2, reads=["win", "rA"], writes=[f"ps{bk2}"])
                sc.add("dve", (lambda e, tile_i=tile_i, ps2=ps2: e.tensor_copy(out=VA[:, tile_i, 8:10, 0:64],
                                                                                 in_=ps2[:, 0:128].rearrange("p (h d) -> p h d", h=2))),
                       reads=[f"ps{bk2}", "va_ones"], writes=[f"va{g}_{qt}b"])
            if getattr(self, 'stop', 99) <= 2:
                return
            def kmw(e, b=b):
                e.tensor_scalar(out=KMEANT[0:64, :, 0, b], in0=KSUM[0:64, 0:4], scalar1=1.0 / 256.0, scalar2=None, op0=ALU.mult)
                return e.tensor_scalar(out=KMEANT[64:128, :, 1, b], in0=KSUM[64:128, 0:4], scalar1=1.0 / 256.0, scalar2=None, op0=ALU.mult)
            sc.add("dve", kmw, reads=[f"ksum{c}" for c in range(4)], writes=["kmeant"])
            sc.add("dve", (lambda e: e.tensor_tensor(out=KSQ, in0=KT[:, 0:5, t0:t0 + 256], in1=KT[:, 0:5, t0:t0 + 256], op=ALU.mult)),
                   reads=[f"kt{c}_{g}" for c in range(5)], writes=["rB", "rBb"])
            if getattr(self, 'stop', 99) <= 2.2:
                return
            for bi, cs in enumerate([(0, 1), (2, 3), (4,)]):
                bk = 4 + bi
                ps = self.psb(bk).rearrange("p (a t) -> p a t", a=2)

                def mmk(e, cs=cs, ps=ps):
                    last = None
                    for a, c in enumerate(cs):
                        last = e.matmul(ps[:, a, :], self.ONES, KSQ[:, c, :], start=True, stop=True)
                    return last
                sc.add("pe", mmk, reads=["rB", "ones"], writes=[f"ps{bk}"])
                sc.add("dve", (lambda e, cs=cs, ps=ps: e.tensor_reduce(out=KMXG[:, cs[0]:cs[0] + len(cs)], in_=ps[:, 0:len(cs), :], axis=AX.X, op=ALU.max)),
                       reads=[f"ps{bk}"], writes=["kmxg"])

            if getattr(self, 'stop', 99) <= 2.4:
                return
            sc.add("dve", (lambda e: e.tensor_tensor(out=KMAX2[:, 0:5], in0=KMAX2[:, 0:5], in1=KMXG[:, 0:5], op=ALU.max)),
                   reads=["kmxg", "kmax2"], writes=["kmax2"])

            def kmax(e):
                e.tensor_copy(out=KM16[:, 0:8].rearrange("p (c r) -> p c r", r=2), in_=KMAX2[:, 0:4].unsqueeze(2).to_broadcast([128, 4, 2]))
                return e.tensor_copy(out=KM16[:, 8:16], in_=KMAX2[:, 4:5].to_broadcast([128, 8]))
            sc.add("dve", kmax, reads=["kmax2"], writes=["km16"])
            if getattr(self, 'stop', 99) <= 2.6:
                return
            sc.add("dve", (lambda e: e.tensor_tensor(out=QSQ, in0=QTG, in1=QTG, op=ALU.mult)), reads=["rC"], writes=["rB", "rBb"])
            ps7 = self.psb(7)
            GATE = ps7[:, 0:128].rearrange("p (a j) -> p a j", a=16)

            for bi in range(4):
                psq = self.psb(bi).rearrange("p (a t) -> p a t", a=2)

                def mmq(e, bi=bi, psq=psq):
                    last = None
                    for a in range(2):
                        last = e.matmul(psq[:, a, :], self.ONES, QSQ[:, 2 * bi + a, :], start=True, stop=True)
                    return last
                sc.add("pe", mmq, reads=["rB", "ones"], writes=[f"ps{bi}"])
                sc.add("dve", (lambda e, bi=bi, psq=psq: e.tensor_reduce(out=QMXG[:, 2 * bi:2 * bi + 2], in_=psq, axis=AX.X, op=ALU.max)),
                       reads=[f"ps{bi}"], writes=["qmxg"])
            sc.add("dve", (lambda e: e.tensor_copy(out=QM16.rearrange("p (c r) -> p c r", r=2), in_=QMXG.unsqueeze(2).to_broadcast([128, 8, 2]))),
                   reads=["qmxg"], writes=["qm16"])

            def mmg(e):
                last = None
                for qt in range(2):
                    for c in range(4):
                        last = e.matmul(ps7[:, (qt * 8 + 2 * c) * 8:(qt * 8 + 2 * c + 2) * 8], QTG[:, c, qt * 128:(qt + 1) * 128],
                                        KMEANT[:, c, :, :].rearrange("p r j -> p (r j)"), start=True, stop=True)
                return last
            sc.add("pe", mmg, reads=["rC", "kmeant"], writes=["ps7"])
            if getattr(self, 'stop', 99) <= 3:
                return
            NEGM = BMASK[:, 0, b, :]
            ELIG = BMASK[:, 1, b, :]
            OWN = BMASK[:, 2, b, :]

            sc.add("dve", (lambda e: e.tensor_tensor(out=SQRT_T, in0=QM16.unsqueeze(1).to_broadcast([128, 2, 16]),
                                                     in1=KM16.unsqueeze(1).to_broadcast([128, 2, 16]), op=ALU.mult)),
                   reads=["qm16", "km16"], writes=["sqrt_t"])
            sc.add("act", (lambda e: e.activation(out=SQRT_T, in_=SQRT_T, func=AF.Ln, bias=self.EPSC[:, 0:1], scale=1.0)),
                   reads=["sqrt_t", "epsc"], writes=["sqrt_t"])
            sc.add("act", (lambda e: e.activation(out=SQRT_T, in_=SQRT_T, func=AF.Exp, scale=0.5)),
                   reads=["sqrt_t"], writes=["sqrt_t"])
            AUGBv = AUGB
            sc.add("dve", (lambda e: e.tensor_tensor(out=SH8, in0=SQRT_T, in1=BM8.unsqueeze(1).to_broadcast([128, 2, 16]), op=ALU.add)),
                   reads=["sqrt_t", "bm8"], writes=["sh8"])
            sc.add("dve", (lambda e: e.tensor_scalar(out=AUGBv[:, :, 8:16, :], in0=SH8[:, :, 8:16].unsqueeze(3).to_broadcast([128, 2, 8, 8]),
                                                     scalar1=-1.0, scalar2=None, op0=ALU.mult)),
                   reads=["sh8"], writes=["augb_s"])
            sc.add("dve", (lambda e: e.scalar_tensor_tensor(out=SINKT, in0=AUGBv[:, :, 8:16, 0], scalar=0.125, in1=SINKS.unsqueeze(1).to_broadcast([128, 2, 8]),
                                                            op0=ALU.mult, op1=ALU.add)),
                   reads=["augb_s", "sinks"], writes=["sinkt"])
            sc.add("act", (lambda e: e.activation(out=SINKT, in_=SINKT, func=AF.Exp)), reads=["sinkt"], writes=["sinkt"])
            SELM = SELF[:, :, 0:8, :]
            sel_jobs = [
                lambda: sc.add("dve", (lambda e: e.tensor_tensor(out=GM, in0=GATE, in1=NEGM.unsqueeze(1).to_broadcast([128, 16, 8]), op=ALU.add)),
                               reads=["ps7", "bmask"], writes=["gm"]),
                lambda: sc.add("dve", (lambda e: e.tensor_tensor(out=CMP, in0=GM.unsqueeze(2).to_broadcast([128, 16, 8, 8]),
                                                                 in1=GM.unsqueeze(3).to_broadcast([128, 16, 8, 8]), op=ALU.is_gt)),
                               reads=["gm"], writes=["cmp"]),
                lambda: sc.add("dve", (lambda e: e.tensor_reduce(out=SEL, in_=CMP, axis=AX.X, op=ALU.add)), reads=["cmp"], writes=["selr"]),
                lambda: sc.add("dve", (lambda e: e.scalar_tensor_tensor(out=SEL, in0=SEL, scalar=3.0, in1=ELIG.unsqueeze(1).to_broadcast([128, 16, 8]),
                                                                        op0=ALU.is_lt, op1=ALU.mult)),
                               reads=["selr", "bmask"], writes=["selr"]),
                lambda: sc.add("dve", (lambda e: e.tensor_tensor(out=SELM, in0=SEL.rearrange("p (q h) j -> p q h j", q=2),
                                                                 in1=OWN.unsqueeze(1).unsqueeze(1).to_broadcast([128, 2, 8, 8]), op=ALU.add)),
                               reads=["selr", "bmask", "selfm"], writes=["selfm"]),
                lambda: sc.add("dve", (lambda e: e.tensor_scalar(out=SELM, in0=SELM, scalar1=BIG, scalar2=-BIG, op0=ALU.mult, op1=ALU.add)),
                               reads=["selfm"], writes=["selfm"]),
                lambda: sc.add("dve", (lambda e: e.tensor_tensor(out=AUGBv[:, :, 0:8, :], in0=SELM,
                                                                 in1=SH8[:, :, 0:8].unsqueeze(3).to_broadcast([128, 2, 8, 8]), op=ALU.subtract)),
                               reads=["sh8", "selfm"], writes=["augb_m"]),
            ]
            if getattr(self, 'stop', 99) <= 4:
                return
            order = list(range(8, 16)) + list(range(8))
            sel_jobs.pop(0)()
            for grp in (1, 0):
                pvq = []

                def flush(keep):
                    while len(pvq) > keep:
                        a_, k_ = pvq.pop(0)
                        sc.add(*a_, **k_)

                def prep(s16):
                    ps7b = self.psb(7, BF16)
                    pos_ = order.index(s16)
                    AT = AUGT1[pos_ % 3]
                    ares = "augb_s" if s16 >= 8 else "augb_m"

                    def tr(e):
                        last = None
                        for qt in range(2):
                            last = e.transpose(ps7b[0:8, qt * 128:(qt + 1) * 128], AUGB[:, qt, s16, :], IDENT)
                        return last
                    sc.add("pe", tr, reads=[ares, "ident"], writes=["ps7"])
                    sc.add("act", (lambda e: e.activation(out=AT[0:8, 0:256], in_=ps7b[0:8, 0:256], func=AF.Copy)),
                           reads=["ps7"], writes=[f"augt{pos_ % 3}"])
                if grp == 1:
                    prep(order[0])
                    prep(order[1])

                def head(hs8):
                    s16 = grp * 8 + hs8
                    pos = order.index(s16)
                    if grp == 1 and sel_jobs:
                        sel_jobs.pop(0)()
                    if pos + 2 < 16:
                        if order[pos + 2] < 8:
                            while sel_jobs:
                                sel_jobs.pop(0)()
                        prep(order[pos + 2])
                    AT = AUGT1[pos % 3]
                    pb = 0
                    if grp == 0:
                        ck, r0, cq, vh = hs8 // 2, (hs8 % 2) * 64, hs8 // 2, hs8
                    else:
                        i_, r_ = hs8 // 2, hs8 % 2
                        ck, r0, cq, vh = 4, r_ * 64, 4 + i_, 8 + r_
                    quad, hsl = hs8 // 4, hs8 % 4
                    augres = f"augt{pos % 3}"
                    far = []
                    if grp == 0 and b >= 1:
                        for kc in range(0, 2 * b - 1):
                            far.append((kc, 0, 256))
                        far.append((2 * b - 1, 128, 128))
                    near = [(2 * b - 1, 0, 0), (2 * b, 0, 1), (2 * b, 1, 0), (2 * b + 1, 1, 1)]
                    if b == 0:
                        near = near[1:]
                    n_qt = [0, 0]
                    for (kc, q0, qn) in far:
                        for sub in range(qn // 128):
                            n_qt[(q0 + sub * 128) // 128] += 1
                    for (kc, qt, _) in near:
                        n_qt[qt] += 1
                    done_qt = [0, 0]
                    banks = []
                    cur, tot = [], 0
                    for p_ in far:
                        if tot + p_[2] > 512:
                            banks.append(cur)
                            cur, tot = [], 0
                        cur.append(p_)
                        tot += p_[2]
                    if cur:
                        banks.append(cur)
                    for pieces in banks:
                        bk = 4 + sbank[0] % 3
                        sbank[0] += 1
                        ps = self.psb(bk)
                        pti = ptc[0] % 4
                        ptc[0] += 1
                        PT = PTS[pti]
                        offs = []
                        off = 0
                        for p_ in pieces:
                            offs.append(off)
                            off += p_[2]
                        tot = off

                        def mms(e, pieces=pieces, offs=offs, ps=ps):
                            last = None
                            for (kc, q0, qn), of in zip(pieces, offs):
                                e.matmul(ps[:, of:of + qn], KT[r0:r0 + 64, ck, kc * 128:(kc + 1) * 128], QTG[r0:r0 + 64, cq, q0:q0 + qn],
                                         start=True, stop=False)
                                last = e.matmul(ps[:, of:of + qn], IND[pb:pb + 8, kc // 2, :], AT[0:8, q0:q0 + qn],
                                                start=False, stop=True)
                            return last
                        flush(1)
                        sc.add("pe", mms, reads=[f"kt{ck}_{p_[0] // 2}" for p_ in pieces] + ["rC", "ind", augres], writes=[f"ps{bk}"])
                        sc.add("act", (lambda e, PT=PT, ps=ps, tot=tot: e.activation(out=PT[:, 0:tot], in_=ps[:, 0:tot], func=AF.Exp,
                                                                                       scale=0.125, bias=B31[:, s16:s16 + 1])),
                               reads=[f"ps{bk}", "b31"], writes=[PTN[pti]])
                        flags = []
                        for (kc, q0, qn), of in zip(pieces, offs):
                            for sub in range(qn // 128):
                                qt = (q0 + sub * 128) // 128
                                st = done_qt[qt] == 0
                                done_qt[qt] += 1
                                sp_ = done_qt[qt] == n_qt[qt]
                                flags.append((kc, qt, of + sub * 128, st, sp_))

                        def pv(e, flags=flags, PT=PT):
                            last = None
                            for (kc, qt, of, st, sp_) in flags:
                                last = e.matmul(self.psb(qt * 2 + quad).rearrange("p (h d) -> p h d", d=65)[:, hsl, 0:65] if False else
                                                self.oacc(qt * 2 + quad)[:, hsl, :], PT[:, of:of + 128], VA[:, kc, vh, :], start=st, stop=sp_)
                            return last
                        pvq.append((("pe", pv), dict(reads=[PTN[pti]] + [f"va{p_[0] // 2}_{p_[0] % 2}{'a' if grp == 0 else 'b'}" for p_ in pieces],
                                                     writes=[f"ps{quad}", f"ps{2 + quad}"])))
                    bk = 4 + sbank[0] % 3
                    sbank[0] += 1
                    ps = self.psb(bk).rearrange("p (a t) -> p a t", a=4)
                    pti = ptc[0] % 4
                    ptc[0] += 1
                    PT = PTS[pti].rearrange("p (a t) -> p a t", a=4)
                    TMP = TMPS[pti % 2].rearrange("p (a t) -> p a t", a=4)
                    tmpn = f"xn{pti % 2}"
                    ti0 = 4 - len(near)

                    def mmn(e, near=near, ps=ps, ti0=ti0):
                        last = None
                        for k_, (kc, qt, di) in enumerate(near):
                            ti = ti0 + k_
                            e.matmul(ps[:, ti, :], KT[r0:r0 + 64, ck, kc * 128:(kc + 1) * 128], QTG[r0:r0 + 64, cq, qt * 128:(qt + 1) * 128],
                                     start=True, stop=False)
                            last = e.matmul(ps[:, ti, :], IND[pb:pb + 8, kc // 2, :], AT[0:8, qt * 128:(qt + 1) * 128],
                                            start=False, stop=True)
                        return last
                    flush(1)
                    sc.add("pe", mmn, reads=[f"kt{ck}_{kc // 2}" for (kc, _, _) in near] + ["rC", "ind", augres], writes=[f"ps{bk}"])

                    def biasadd(e, ps=ps, TMP=TMP, ti0=ti0):
                        if ti0 == 0:
                            e.scalar_tensor_tensor(out=TMP[:, 0:2, :], in0=ps[:, 0:2, :], scalar=0.125, in1=DT[:, s16, 0:2, :], op0=ALU.mult, op1=ALU.add)
                        else:
                            e.scalar_tensor_tensor(out=TMP[:, 1:2, :], in0=ps[:, 1:2, :], scalar=0.125, in1=DT[:, s16, 1:2, :], op0=ALU.mult, op1=ALU.add)
                        return e.scalar_tensor_tensor(out=TMP[:, 2:4, :], in0=ps[:, 2:4, :], scalar=0.125, in1=DT[:, s16, 0:2, :], op0=ALU.mult, op1=ALU.add)
                    sc.add("dve", biasadd, reads=[f"ps{bk}", "dt"], writes=[tmpn])
                    sc.add("act", (lambda e, PT=PT, TMP=TMP, ti0=ti0: e.activation(out=PT[:, ti0:4, :], in_=TMP[:, ti0:4, :], func=AF.Exp)),
                           reads=[tmpn], writes=[PTN[pti]])
                    flags = []
                    for k_, (kc, qt, di) in enumerate(near):
                        st = done_qt[qt] == 0
                        done_qt[qt] += 1
                        sp_ = done_qt[qt] == n_qt[qt]
                        flags.append((kc, qt, ti0 + k_, st, sp_))

                    def pvn(e, flags=flags, PT=PT):
                        last = None
                        for (kc, qt, ti, st, sp_) in flags:
                            last = e.matmul(self.oacc(qt * 2 + quad)[:, hsl, :], PT[:, ti, :], VA[:, kc, vh, :], start=st, stop=sp_)
                        return last
                    pvq.append((("pe", pvn), dict(reads=[PTN[pti]] + [f"va{kc // 2}_{kc % 2}{'a' if grp == 0 else 'b'}" for (kc, _, _) in near],
                                                  writes=[f"ps{quad}", f"ps{2 + quad}"])))
                for hs8 in range(8):
                    head(hs8)
                flush(0)
                for qt in range(2):
                    for quad in range(2):
                        bk = qt * 2 + quad
                        oa = self.oacc(bk)
                        if grp == 0:
                            outv = OG[:, qt, quad * 256:(quad + 1) * 256].rearrange("p (h d) -> p h d", h=4)
                            inv = oa[:, :, 0:64]
                        else:
                            outv = OG[:, qt, 512:1024].rearrange("p (r i d) -> p i r d", r=2, i=4)[:, 2 * quad:2 * quad + 2, :, :]
                            inv = oa[:, :, 0:64].rearrange("p (i r) d -> p i r d", r=2)

                        def nrm(e, oa=oa, outv=outv, inv=inv, qt=qt, quad=quad, grp=grp):
                            if grp == 0:
                                e.reciprocal(out=RDEN[:, 0:4], in_=oa[:, :, 64])
                            else:
                                e.tensor_tensor(out=RDEN[:, 0:4], in0=oa[:, :, 64], in1=SINKT[:, qt, 4 * quad:4 * quad + 4], op=ALU.add)
                                e.reciprocal(out=RDEN[:, 0:4], in_=RDEN[:, 0:4])
                            if grp == 0:
                                rb_ = RDEN[:, 0:4].unsqueeze(2).to_broadcast([128, 4, 64])
                            else:
                                rb_ = RDEN[:, 0:4].rearrange("p (i r) -> p i r", r=2).unsqueeze(3).to_broadcast([128, 2, 2, 64])
                            return e.tensor_tensor(out=outv, in0=inv, in1=rb_, op=ALU.mult)
                        def nrm1(e, oa=oa, qt=qt, quad=quad, grp=grp):
                            if grp == 0:
                                return e.reciprocal(out=RDEN[:, 4 * (bk % 2):4 * (bk % 2) + 4], in_=oa[:, :, 64])
                            return e.tensor_tensor(out=RDEN[:, 4 * (bk % 2):4 * (bk % 2) + 4], in0=oa[:, :, 64], in1=SINKT[:, qt, 4 * quad:4 * quad + 4], op=ALU.add)
                        rdn = f"rden{bk % 2}"
                        RD = RDEN[:, 4 * (bk % 2):4 * (bk % 2) + 4]
                        if grp == 0:
                            sc.add("dve", (lambda e, oa=oa, RD=RD: e.reciprocal(out=RD, in_=oa[:, :, 64])), reads=[f"ps{bk}"], writes=[rdn])
                        else:
                            sc.add("dve", (lambda e, oa=oa, RD=RD, qt=qt, quad=quad: e.tensor_tensor(out=RD, in0=oa[:, :, 64], in1=SINKT[:, qt, 4 * quad:4 * quad + 4], op=ALU.add)),
                                   reads=[f"ps{bk}", "sinkt"], writes=[rdn])
                            sc.add("dve", (lambda e, RD=RD: e.reciprocal(out=RD, in_=RD)), reads=[rdn], writes=[rdn])
                        if grp == 0:
                            rb_ = RD.unsqueeze(2).to_broadcast([128, 4, 64])
                        else:
                            rb_ = RD.rearrange("p (i r) -> p i r", r=2).unsqueeze(3).to_broadcast([128, 2, 2, 64])
                        sc.add("dve", (lambda e, outv=outv, inv=inv, rb_=rb_: e.tensor_tensor(out=outv, in0=inv, in1=rb_, op=ALU.mult)),
                               reads=[f"ps{bk}", rdn], writes=["rA"])
            if getattr(self, 'stop', 99) <= 7:
                return
            for half in range(2):
                bk = 4 + half
                psT = self.psb(bk, BF16).rearrange("p (c t) -> p c t", c=4)

                def trO(e, half=half, psT=psT):
                    last = None
                    for cc in range(4):
                        c = half * 4 + cc
                        for qt in range(2):
                            last = e.transpose(psT[:, cc, qt * 128:(qt + 1) * 128], OG[:, qt, c * 128:(c + 1) * 128], IDENT)
                    return last
                sc.add("pe", trO, reads=["rA", "ident"], writes=[f"ps{bk}"])
                if half == 0:
                    sc.add("act", (lambda e, psT=psT: e.activation(out=OTG[:, 0:4, :], in_=psT, func=AF.Copy)), reads=[f"ps{bk}"], writes=["rB"])
                else:
                    sc.add("dve", (lambda e, psT=psT: e.tensor_copy(out=OTG[:, 4:8, :], in_=psT)), reads=[f"ps{bk}"], writes=["rBb"])
            if getattr(self, 'stop', 99) <= 8:
                return
            for o_ in range(8):
                bk = 4 + o_ % 3
                ps = self.psb(bk)

                def mmo(e, o_=o_, ps=ps):
                    last = None
                    for c in range(8):
                        last = e.matmul(ps[:, 0:256], WOUT[:, c, o_ * 128:(o_ + 1) * 128], OTG[:, c, :], start=(c == 0), stop=(c == 7))
                    return last
                sc.add("pe", mmo, reads=["wout", "rB", "rBb"], writes=[f"ps{bk}"])
                ysl = YSBA[:, o_, :]
                sc.add("act", (lambda e, ps=ps, ysl=ysl: e.activation(out=ysl, in_=ps[:, 0:256], func=AF.Copy)),
                       reads=[f"ps{bk}", "rA", "rC"], writes=[f"ysa{o_}"])
                sqb = PTS[o_ % 4]
                sc.add("dve", (lambda e, ysl=ysl, sqb=sqb: e.tensor_tensor(out=sqb[:, 0:256], in0=ysl, in1=ysl, op=ALU.mult)),
                       reads=[f"ysa{o_}"], writes=[PTN[o_ % 4]])
                sc.add("pe", (lambda e, sqb=sqb, o_=o_: e.matmul(self.psb(7)[:, 0:256], self.ONES, sqb[:, 0:256], start=(o_ == 0), stop=(o_ == 7))),
                       reads=[PTN[o_ % 4], "ones"], writes=["ps7"])
            if getattr(self, 'stop', 99) <= 9:
                return
            self.resid_update(l, 0, t0, 256, YSBA, (lambda c: f"ysa{c}"), 7)
            sc.marker(reads=[f"ysa{c}" for c in range(8)], writes=["rA", "rC"])

        for g in range(getattr(self, 'ngroups', 8)):
            group(g)
        sc.marker(writes=["bmask", "gm", "cmp", "selr", "augb_s", "augb_m", "selfm", "sh8", "sinkt", "sqrt_t", "wada_ok"])

    def oacc(self, bk):
        return self.PS[bk][:, 0:260].rearrange("p (h d) -> p h d", d=65)


def _host_prep(inputs):
    f = np.float32
    w_ada = np.asarray(inputs["w_ada"], f)
    w1 = np.asarray(inputs["w1"], f)
    w2 = np.asarray(inputs["w2"], f)
    w_in = np.asarray(inputs["w_in"], f)
    w_out = np.asarray(inputs["w_out"], f)
    sh = {}
    sh["wada"] = np.ascontiguousarray(w_ada.reshape(DEPTH, 8, 128, 24, 256).transpose(0, 3, 2, 1, 4))
    sh["badac"] = np.ascontiguousarray(np.asarray(inputs["b_ada"], f).reshape(DEPTH, 48, 128).transpose(2, 0, 1).reshape(128, DEPTH * 48))
    sh["gainc"] = np.ascontiguousarray(np.asarray(inputs["norm_gains"], f).reshape(DEPTH, 4, 8, 128).transpose(3, 0, 1, 2).reshape(128, 128))
    sh["b1c"] = np.ascontiguousarray(np.asarray(inputs["b1"], f).reshape(DEPTH, 32, 128).transpose(2, 0, 1).reshape(128, 128))
    sh["b2c"] = np.ascontiguousarray(np.asarray(inputs["b2"], f).reshape(DEPTH, 8, 128).transpose(2, 0, 1).reshape(128, 32))
    sh["w1r"] = np.ascontiguousarray(w1.reshape(DEPTH, 8, 128, 16, 256).transpose(0, 3, 2, 1, 4))
    sh["w2r"] = np.ascontiguousarray(w2.reshape(DEPTH, 2, 16, 128, 8, 128).transpose(0, 4, 1, 3, 2, 5))
    perm = [(k // 2) + 4 * (k % 2) for k in range(8)]
    colidx = np.arange(DIN)
    qb = colidx[1536:2048].reshape(8, 64)[perm].reshape(-1)
    colidx = np.concatenate([colidx[:1536], qb, colidx[2048:]])
    w_in_p = w_in[:, :, colidx]
    sh["winr"] = np.ascontiguousarray(w_in_p.reshape(DEPTH, 8, 128, DIN).transpose(0, 2, 1, 3))
    sh["woutr"] = np.ascontiguousarray(w_out.reshape(DEPTH, 8, 128, D).transpose(0, 2, 1, 3))
    sh["sinks"] = np.ascontiguousarray(np.asarray(inputs["sinks"], f)[:, perm])
    rb = np.asarray(inputs["rel_bias"], f)
    hperm = list(range(8)) + [8 + p for p in perm]
    rb = rb[:, hperm]
    sh["rbT"] = np.ascontiguousarray(rb.T).reshape(1, 16 * 32)
    tab = np.concatenate([rb, np.full((1, 16), NEG, f)], axis=0)
    idx = _dtile_index()
    dt = np.zeros((128, 16, 2, 128), f)
    for h in range(16):
        hg = 0 if h < 8 else 1
        for t in range(2):
            dt[:, h, t, :] = tab[idx[hg, t], h]
    sh["dtile"] = dt.reshape(128, 16 * 2 * 128)
    sh["ident"] = np.eye(128, dtype=f)
    hsel = np.zeros((128, 2, 128), f)
    hsel[0:64, 0, :] = 1.0
    hsel[64:128, 1, :] = 1.0
    sh["hsel"] = hsel.reshape(128, 256)
    hind = np.zeros((128, 2), f)
    hind[0:64, 0] = 1.0
    hind[64:128, 1] = 1.0
    sh["hind"] = hind
    ind = np.zeros((72, 8, 128), f)
    for j in range(8):
        for pb in (0, 32, 64):
            ind[pb + j, j, :] = 1.0
    sh["indall"] = ind.reshape(72, 1024)
    bm = np.zeros((128, 3, 8, 8), f)
    for b in range(8):
        for j in range(8):
            bm[:, 0, b, j] = 0.0 if j < b else -1e30
            bm[:, 1, b, j] = 1.0 if j < b else 0.0
            bm[:, 2, b, j] = 1.0 if j == b else 0.0
    sh["bmask"] = bm.reshape(128, 192)
    x = np.asarray(inputs["x"], f)
    c = np.asarray(inputs["c"], f)
    per = []
    for b in range(x.shape[0]):
        m = dict(sh)
        m["xT"] = np.ascontiguousarray(x[b].T)
        m["cT"] = np.ascontiguousarray(c[b].reshape(8, 128).T)
        per.append(m)
    return per


_PROG_CACHE = {}


def _get_prog(phases, debug=False, ngroups=8, stop=99):
    key = (tuple(phases), debug, ngroups, stop)
    if key not in _PROG_CACHE:
        _PROG_CACHE[key] = Prog(list(phases), debug=debug, ngroups=ngroups, stop=stop)
    return _PROG_CACHE[key]


def run_phases(inputs, phases, n_cores=8, trace=False, debug=False, ngroups=8, stop=99):
    per = _host_prep(inputs)[:n_cores]
    prog = _get_prog(phases, debug, ngroups, stop)
    res = run_bass_kernel_spmd(prog.nc, per, core_ids=list(range(n_cores)), trace=trace)
    outs = [np.ascontiguousarray(r["outT"].T) for r in res.results]
    return np.stack(outs, axis=0), res


def kernel(**inputs):
    phases = []
    for l in range(DEPTH):
        phases += [("attn", l), ("ffn", l)]
    out, _ = run_phases(inputs, phases)
    return out.astype(np.float32)
```

```python
import math
import numpy as np
import concourse.bass as bass
import concourse.mybir as mybir
from concourse.bass_utils import run_bass_kernel_spmd

F32 = mybir.dt.float32
BF16 = mybir.dt.bfloat16
AF = mybir.ActivationFunctionType
ALU = mybir.AluOpType
AX = mybir.AxisListType

D = 1024
S = 2048
DEPTH = 4
DFF = 4096
DIN = 2304
EPS = 1e-6
NEG = -30000.0
BIG = 1024.0
MOBA_ROUND = "trunc"
SWA_ROUND = "trunc"


class Sched:
    def __init__(self):
        self.ops = []
        self.last_w = {}
        self.readers = {}

    def marker(self, reads=(), writes=()):
        k = getattr(self, "_mk", 0)
        self._mk = k + 1
        col = k % 8
        dm = self.dummy
        self.add("dve", (lambda e: e.memset(dm[:, col:col + 1], 0.0)), reads=reads, writes=list(writes) + [f"dummy{col}"])

    def barrier(self):
        names = set(self.last_w) | set(self.readers)
        names.add("__bar__")
        self.marker(writes=sorted(names))

    def add(self, eng, fn, reads=(), writes=(), dma=None, ndma=1, total=False):
        idx = len(self.ops)
        reads = tuple(reads) + ("__bar__",)
        writes = tuple(writes)
        deps = set()
        for r in reads:
            w = self.last_w.get(r)
            if w is not None:
                deps.add(w)
        for w_ in writes:
            w = self.last_w.get(w_)
            if w is not None:
                deps.add(w)
            for rd in self.readers.get(w_, ()):
                deps.add(rd)
        for r in reads:
            self.readers.setdefault(r, []).append(idx)
        for w_ in writes:
            self.last_w[w_] = idx
            self.readers[w_] = []
        self.ops.append(dict(eng=eng, fn=fn, deps=deps, dma=dma, ndma=ndma, total=total,
                             reads=set(reads), writes=set(writes)))
        return idx

    def finalize(self, nc, semctx):
        ops = self.ops
        need = [False] * len(ops)
        for i, o in enumerate(ops):
            keep = set()
            for d in o["deps"]:
                p = ops[d]
                if p["dma"] is not None or o["dma"] is not None:
                    keep.add(d)
                elif p["eng"] != o["eng"]:
                    keep.add(d)
                else:
                    if o["eng"] != "pe" and (p["writes"] & (o["reads"] | o["writes"])):
                        keep.add(d)
            o["deps"] = keep
            for d in keep:
                need[d] = True
        sems = {}

        def getsem(name):
            if name not in sems:
                sems[name] = semctx(name)
            return sems[name]

        cnt = {}
        totals = {}
        for i, o in enumerate(ops):
            if o["dma"] is not None:
                key = "d_" + o["dma"]
                cnt[key] = cnt.get(key, 0) + 16 * o["ndma"]
                o["sig"] = (key, cnt[key])
                if o["total"]:
                    totals[key] = True
            elif need[i]:
                key = "e_" + o["eng"]
                cnt[key] = cnt.get(key, 0) + 1
                o["sig"] = (key, cnt[key])
            else:
                o["sig"] = None
        for o in ops:
            if o["dma"] is not None and o["total"]:
                o["sig"] = (o["sig"][0], cnt[o["sig"][0]])
        for o in ops:
            w = {}
            for d in o["deps"]:
                k, v = ops[d]["sig"]
                if w.get(k, 0) < v:
                    w[k] = v
            o["waits"] = w
        for k in cnt:
            getsem(k)
        self.sems = sems
        self.cnt = cnt

    def emit(self, eng, e):
        waited = {}
        n = 0
        for o in self.ops:
            if o["eng"] != eng:
                continue
            for k in sorted(o["waits"]):
                v = o["waits"][k]
                if waited.get(k, 0) < v:
                    e.wait_ge(self.sems[k], v)
                    waited[k] = v
            ins = o["fn"](e)
            n += 1
            if o["dma"] is not None:
                if not isinstance(ins, (list, tuple)):
                    ins = [ins]
                assert len(ins) == o["ndma"], (len(ins), o["ndma"])
                for i_ in ins:
                    i_.then_inc(self.sems[o["sig"][0]], 16)
            elif o["sig"] is not None:
                if isinstance(ins, (list, tuple)):
                    ins = ins[-1]
                ins.then_inc(self.sems[o["sig"][0]], 1)
        return n


def _t5_bucket_np(dist, mode):
    n = np.maximum(dist, 0).astype(np.int32)
    nf = np.maximum(n, 1).astype(np.float32)
    val = (np.log(nf / np.float32(16)) / np.float32(math.log(128 / 16)) * np.float32(16)).astype(np.float32)
    if mode == "trunc":
        li = val.astype(np.int32)
    else:
        li = np.rint(val).astype(np.int32)
    large = np.minimum(16 + li, 31)
    return np.where(n < 16, n, large)


def _dtile_index():
    k = np.arange(128)[:, None]
    q = np.arange(128)[None, :]
    out = np.zeros((2, 2, 128, 128), np.int64)
    for hg, mode in ((0, MOBA_ROUND), (1, SWA_ROUND)):
        d0 = q - k
        b0 = _t5_bucket_np(d0, mode)
        out[hg, 1] = np.where(d0 >= 0, b0, 32)
        d1 = 128 + q - k
        b1 = _t5_bucket_np(d1, mode)
        if hg == 0:
            out[hg, 0] = b1
        else:
            out[hg, 0] = np.where(d1 < 128, b1, 32)
    return out


class Prog:
    def __init__(self, phases, debug=False, ngroups=8, stop=99):
        self.phases = phases
        self.stop = stop
        self.ngroups = ngroups
        self.debug = debug
        self.dbg_names = []
        self.nc = bass.Bass("TRN2", target_bir_lowering=False)
        self.sc = Sched()
        self.build()

    def dram_in(self, name, shape, dt=F32):
        return self.nc.dram_tensor(name, list(shape), dt, kind="ExternalInput").ap()

    def build(self):
        nc = self.nc
        sc = self.sc
        self.d_xT = self.dram_in("xT", [D, S])
        self.d_cT = self.dram_in("cT", [128, 8])
        self.d_wada = self.dram_in("wada", [DEPTH, 24, 128, 8, 256])
        self.d_badac = self.dram_in("badac", [128, DEPTH * 48])
        self.d_gainc = self.dram_in("gainc", [128, 128])
        self.d_b1c = self.dram_in("b1c", [128, 128])
        self.d_b2c = self.dram_in("b2c", [128, 32])
        self.d_w1r = self.dram_in("w1r", [DEPTH, 16, 128, 8, 256])
        self.d_w2r = self.dram_in("w2r", [DEPTH, 8, 2, 128, 16, 128])
        self.d_winr = self.dram_in("winr", [DEPTH, 128, 8, DIN])
        self.d_woutr = self.dram_in("woutr", [DEPTH, 128, 8, D])
        self.d_sinks = self.dram_in("sinks", [DEPTH, 8])
        self.d_rbT = self.dram_in("rbT", [1, 16 * 32])
        self.d_dtile = self.dram_in("dtile", [128, 16 * 2 * 128])
        self.d_ident = self.dram_in("ident", [128, 128])
        self.d_hsel = self.dram_in("hsel", [128, 2 * 128])
        self.d_hind = self.dram_in("hind", [128, 2])
        self.d_indall = self.dram_in("indall", [72, 8 * 128])
        self.d_bmask = self.dram_in("bmask", [128, 3 * 64])
        self.d_out = nc.dram_tensor("outT", [D, S], F32, kind="ExternalOutput").ap()

        total_words = 53200
        self.pool = nc.alloc_sbuf_tensor("pool", [128, total_words], F32)
        self.off = 0

        def alloc(words):
            o = self.off
            self.off += (words + 7) // 8 * 8
            assert self.off <= total_words, (self.off, total_words)
            return o

        def view(o, words, dt=F32):
            v = self.pool[:, o:o + words]
            if dt != F32:
                v = v.bitcast(dt)
            return v

        self.view = view
        o_x = alloc(8 * S)
        self.XT = view(o_x, 8 * S).rearrange("p (c t) -> p c t", c=8)
        self.COLS = view(alloc(320), 320)
        self.GAINC = view(alloc(128), 128)
        self.B1C = view(alloc(128), 128)
        self.B2C = view(alloc(32), 32)
        self.BADAC = view(alloc(192), 192)
        self.MODC = view(alloc(48), 48)
        self.CT = view(alloc(8), 8)
        self.CACT = view(alloc(8), 8, BF16)[:, 0:8]
        self.IDENT = view(alloc(64), 64, BF16)
        self.ONES = view(alloc(64), 64, BF16)
        self.SQ = [view(alloc(512), 512, BF16).rearrange("p (a t) -> p a t", a=2) for _ in range(2)]
        self.rstd_off = self.off
        self.RSTD = [view(alloc(512), 512) for _ in range(2)]
        self.XN = [view(alloc(512), 512) for _ in range(2)]
        self.wada_off = self.off
        self.WADA = [view(alloc(1024), 1024, BF16).rearrange("p (k n) -> p k n", k=8) for _ in range(2)]
        self.DUMMY = view(alloc(8), 8)
        sc.dummy = self.DUMMY
        self.EPSC = view(alloc(8), 8)
        self.phase_base = self.off

        self.PS = [nc.alloc_psum_tensor(f"psb{i}", [128, 512], F32) for i in range(8)]

        self.preamble()
        done_mod = set()
        self.side = []
        for pi, (kind, l) in enumerate(self.phases):
            if l not in done_mod:
                self.mod_layer(l)
                done_mod.add(l)
            sc.barrier()
            if kind == "attn":
                self.attn_phase(l)
            else:
                nxt = [ll for (_, ll) in self.phases[pi + 1:] if ll not in done_mod]
                if nxt:
                    self.side = self.mod_jobs(nxt[0])
                    done_mod.add(nxt[0])
                self.ffn_phase(l)
                self.run_side(100)
        sc.barrier()
        self.epilogue()

        class _SemCtx:
            pass
        semlist = []

        def semctx(name):
            cm = nc.semaphore(name)
            h = cm.__enter__()
            semlist.append(cm)
            return h

        sc.finalize(nc, semctx)
        with nc.Block() as block:
            @block.tensor
            def _(e):
                sc.emit("pe", e)

            @block.scalar
            def _(e):
                sc.emit("act", e)

            @block.vector
            def _(e):
                sc.emit("dve", e)

            @block.gpsimd
            def _(e):
                sc.emit("pool", e)

            @block.sync
            def _(e):
                sc.emit("sp", e)

    def dump(self, name, ap, reads):
        if not getattr(self, "debug", False):
            return
        shp = list(ap.shape)
        d = self.nc.dram_tensor("dbg_" + name, shp, ap.dtype, kind="ExternalOutput").ap()
        self.sc.add("sp", (lambda e: e.dma_start(out=d, in_=ap)), reads=list(reads), writes=["dbg_" + name], dma="dbg_" + name)
        self.dbg_names.append("dbg_" + name)

    def psb(self, i, dt=F32):
        v = self.PS[i][:, :]
        if dt != F32:
            v = v.bitcast(dt)
        return v

    def preamble(self):
        sc = self.sc
        XT = self.XT
        xs = self.d_xT.rearrange("(c p) t -> p c t", p=128)
        for c in range(8):
            sc.add("sp", (lambda e, c=c: e.dma_start(out=XT[:, c, :], in_=xs[:, c, :])),
                   writes=[f"xTc{c}"], dma=f"xin{c}")
        small = [(self.GAINC, self.d_gainc, "gainc"), (self.B1C, self.d_b1c, "b1c"), (self.B2C, self.d_b2c, "b2c"),
                 (self.BADAC, self.d_badac, "badac"), (self.CT, self.d_cT, "ct")]
        for (dst, src, nm) in small:
            sc.add("sp", (lambda e, dst=dst, src=src: e.dma_start(out=dst, in_=src[:, :])),
                   writes=[nm], dma="c_" + nm)
        sc.add("pool", (lambda e: e.dma_start(out=self.IDENT, in_=self.d_ident[:, :])), writes=["ident"], dma="c_ident")
        sc.add("dve", (lambda e: e.memset(self.ONES, 1.0)), writes=["ones"])
        sc.add("dve", (lambda e: e.memset(self.EPSC, float(D * EPS))), writes=["epsc"])
        sc.add("act", (lambda e: e.activation(out=self.CACT, in_=self.CT, func=AF.Silu)), reads=["ct"], writes=["cact"])
        sc.marker(reads=[f"xTc{c}" for c in range(8)], writes=[f"xT{tb}" for tb in range(8)] + ["rgnA_ok", "rgnB_ok"])

    def epilogue(self):
        sc = self.sc
        XT = self.XT
        od = self.d_out.rearrange("(c p) t -> p c t", p=128)
        for c in range(8):
            sc.add("sp", (lambda e, c=c: e.dma_start(out=od[:, c, :], in_=XT[:, c, :])),
                   reads=[f"xT{tb}" for tb in range(8)], writes=[f"out{c}"], dma=f"xout{c}")
        sc.add("sp", (lambda e: e.nop()), reads=[f"out{c}" for c in range(8)])

    def mod_layer(self, l):
        for j in self.mod_jobs(l):
            j()

    def run_side(self, n=1):
        for _ in range(n):
            if self.side:
                self.side.pop(0)()

    def mod_jobs(self, l):
        jobs = []
        for pc in range(24):
            jobs.append(lambda pc=pc: self.mod_piece(l, pc))
        jobs.append(lambda: self.mod_finish(l))
        return jobs

    def mod_piece(self, l, pc):
        sc = self.sc
        ps = self.psb(7)
        if True:
            buf = self.WADA[pc % 2]
            bn = f"wada{pc % 2}"
            src = self.d_wada[l, pc]
            sc.add("pool", (lambda e, buf=buf, src=src: e.dma_start(out=buf, in_=src)), reads=["wada_ok"], writes=[bn], dma=bn)

            def mm(e, buf=buf, pc=pc):
                last = None
                for j in range(2):
                    col = pc * 2 + j
                    for kc in range(8):
                        last = e.matmul(ps[:, col:col + 1], buf[:, kc, j * 128:(j + 1) * 128],
                                        self.CACT[:, kc:kc + 1], start=(kc == 0), stop=(kc == 7))
                return last
            sc.add("pe", mm, reads=[bn, "cact"], writes=["ps7"])
    def mod_finish(self, l):
        sc = self.sc
        ps = self.psb(7)
        MODC = self.MODC
        sc.add("dve", (lambda e: e.tensor_tensor(out=MODC, in0=ps[:, 0:48], in1=self.BADAC[:, l * 48:(l + 1) * 48], op=ALU.add)),
               reads=["ps7", "badac"], writes=["modc"])
        C = self.COLS
        b = l * 64
        G = self.GAINC
        g0 = (l * 4) * 8

        def cols(e):
            e.scalar_tensor_tensor(out=C[:, b + 0:b + 8], in0=MODC[:, 8:16], scalar=1.0, in1=G[:, g0 + 0:g0 + 8], op0=ALU.add, op1=ALU.mult)
            e.tensor_copy(out=C[:, b + 8:b + 16], in_=MODC[:, 0:8])
            e.tensor_tensor(out=C[:, b + 16:b + 24], in0=MODC[:, 16:24], in1=G[:, g0 + 8:g0 + 16], op=ALU.mult)
            e.scalar_tensor_tensor(out=C[:, b + 24:b + 32], in0=MODC[:, 32:40], scalar=1.0, in1=G[:, g0 + 16:g0 + 24], op0=ALU.add, op1=ALU.mult)
            e.tensor_copy(out=C[:, b + 32:b + 40], in_=MODC[:, 24:32])
            return e.tensor_tensor(out=C[:, b + 40:b + 48], in0=MODC[:, 40:48], in1=G[:, g0 + 24:g0 + 32], op=ALU.mult)
        sc.add("dve", cols, reads=["modc", "gainc"], writes=[f"colsraw{l}"])

        def cols2(e):
            e.tensor_scalar(out=C[:, b + 0:b + 8], in0=C[:, b + 0:b + 8], scalar1=32.0, scalar2=None, op0=ALU.mult)
            e.tensor_scalar(out=C[:, b + 16:b + 32], in0=C[:, b + 16:b + 32], scalar1=32.0, scalar2=None, op0=ALU.mult)
            return e.tensor_scalar(out=C[:, b + 40:b + 48], in0=C[:, b + 40:b + 48], scalar1=32.0, scalar2=None, op0=ALU.mult)
        sc.add("dve", cols2, reads=[f"colsraw{l}"], writes=[f"cols{l}"])
        self.dump(f"cols{l}", C[:, b:b + 48], [f"cols{l}"])
        self.dump(f"modc{l}", MODC, [f"cols{l}"])

    def rmsnorm_in(self, l, sub, t0, n, HT, ht_res, psbank, extra_reads=(), sq_names=None):
        sc = self.sc
        XT = self.XT
        tbs = [f"xT{tb}" for tb in range(t0 // 256, (t0 + n) // 256)]
        ps = self.psb(psbank)
        cb = l * 64 + (0 if sub == 0 else 24)
        C = self.COLS
        for cp in range(4):
            sq = self.SQ[cp % 2]
            sqn = f"sq{cp % 2}"
            sqw = [sqn] if sq_names is None else sq_names[cp % 2]
            sc.add("act", (lambda e, cp=cp, sq=sq: e.activation(out=sq[:, :, 0:n], in_=XT[:, 2 * cp:2 * cp + 2, t0:t0 + n], func=AF.Square)),
                   reads=tbs, writes=sqw)

            def mm(e, cp=cp, sq=sq):
                last = None
                for j in range(2):
                    c = 2 * cp + j
                    last = e.matmul(ps[:, 0:n], self.ONES, sq[:, j, 0:n], start=(c == 0), stop=(c == 7))
                return last
            sc.add("pe", mm, reads=[sqn, "ones"], writes=[f"ps{psbank}"])
        rs = self.RSTD[0]
        sc.add("act", (lambda e: e.activation(out=rs[:, 0:n], in_=ps[:, 0:n], func=AF.Ln, bias=self.EPSC[:, 0:1], scale=1.0)),
               reads=[f"ps{psbank}", "epsc"], writes=["rstd0p", "rstd0"])
        sc.add("act", (lambda e: e.activation(out=rs[:, 0:n], in_=rs[:, 0:n], func=AF.Exp, scale=-0.5)),
               reads=["rstd0p"], writes=["rstd0", "rstd0p"])
        for c in range(8):
            xn = self.XN[c % 2]
            xnn = f"xn{c % 2}"
            sc.add("dve", (lambda e, c=c, xn=xn: e.tensor_tensor(out=xn[:, 0:n], in0=XT[:, c, t0:t0 + n], in1=rs[:, 0:n], op=ALU.mult)),
                   reads=tbs + ["rstd0"], writes=[xnn])
            sc.add("act", (lambda e, c=c, xn=xn: e.activation(out=HT[:, c, 0:n], in_=xn[:, 0:n], func=AF.Identity,
                                                               scale=C[:, cb + c:cb + c + 1], bias=C[:, cb + 8 + c:cb + 9 + c])),
                   reads=[xnn, f"cols{l}"] + list(extra_reads), writes=[ht_res])

    def resid_update(self, l, sub, t0, n, Y, y_res, ssbank):
        sc = self.sc
        XT = self.XT
        tbs = [f"xT{tb}" for tb in range(t0 // 256, (t0 + n) // 256)]
        ps = self.psb(ssbank)
        C = self.COLS
        cb = l * 64 + (16 if sub == 0 else 40)
        rs = self.RSTD[1]
        sc.add("act", (lambda e: e.activation(out=rs[:, 0:n], in_=ps[:, 0:n], func=AF.Ln, bias=self.EPSC[:, 0:1], scale=1.0)),
               reads=[f"ps{ssbank}", "epsc"], writes=["rstd1p", "rstd1"])
        sc.add("act", (lambda e: e.activation(out=rs[:, 0:n], in_=rs[:, 0:n], func=AF.Exp, scale=-0.5)),
               reads=["rstd1p"], writes=["rstd1", "rstd1p"])
        if t0 == 0 and sub == 1:
            self.dump("rstd1", rs, ["rstd1"])
            self.dump("ysb", Y, [y_res(c) for c in range(8)])
        for c in range(8):
            sc.add("dve", (lambda e, c=c: e.scalar_tensor_tensor(out=Y[:, c, 0:n], in0=Y[:, c, 0:n], scalar=C[:, cb + c:cb + c + 1], in1=rs[:, 0:n],
                                                                 op0=ALU.mult, op1=ALU.mult)),
                   reads=[y_res(c), "rstd1", f"cols{l}"], writes=[y_res(c)])
            sc.add("dve", (lambda e, c=c: e.tensor_tensor(out=XT[:, c, t0:t0 + n], in0=XT[:, c, t0:t0 + n], in1=Y[:, c, 0:n], op=ALU.add)),
                   reads=[y_res(c)] + tbs, writes=tbs)

    def ffn_phase(self, l):
        sc = self.sc
        view = self.view
        base = self.phase_base
        o = base
        HID = view(o, 32 * 1024 // 2, BF16).rearrange("p (m t) -> p m t", m=32); o += 16384
        rgn = o; o += 8192
        HT = view(rgn, 4096, BF16).rearrange("p (c t) -> p c t", c=8)
        W1B = [view(rgn + 4096 + i * 1024, 1024, BF16).rearrange("p (k n) -> p k n", k=8) for i in range(3)]
        RL = [view(rgn + 4096 + 3072 + i * 512, 512) for i in range(2)]
        YSB = view(rgn, 8192).rearrange("p (c t) -> p c t", c=8)
        W2B = [view(o + i * 1024, 1024, BF16).rearrange("p (k n) -> p k n", k=16) for i in range(3)]; o += 3072
        assert o <= 53200, o
        B1C, B2C = self.B1C, self.B2C
        w1cnt = 0
        w2cnt = 0
        for H in range(2):
            T0 = H * 1024
            region_users = ["rgnA_ok"]
            for tg in range(2):
                self.rmsnorm_in(l, 1, T0 + tg * 512, 512, HT[:, :, tg * 512:(tg + 1) * 512], f"ht{tg}", 6,
                                extra_reads=region_users)
            if H == 0:
                self.dump("ht", HT, ["ht0", "ht1"])
            psi = 0
            for g in range(16):
                self.run_side(1)
                wb = W1B[w1cnt % 3]; wn = f"w1b{w1cnt % 3}"; w1cnt += 1
                src = self.d_w1r[l, g]
                sc.add("pool", (lambda e, wb=wb, src=src: e.dma_start(out=wb, in_=src)), reads=region_users, writes=[wn], dma=wn)
                for mm_ in range(2):
                    m = 2 * g + mm_
                    for tg in range(2):
                        bank = psi % 4; psi += 1
                        ps = self.psb(bank)

                        def mm(e, wb=wb, mm_=mm_, tg=tg, ps=ps):
                            last = None
                            for c in range(8):
                                last = e.matmul(ps, wb[:, c, mm_ * 128:(mm_ + 1) * 128], HT[:, c, tg * 512:(tg + 1) * 512],
                                                start=(c == 0), stop=(c == 7))
                            return last
                        sc.add("pe", mm, reads=[wn, f"ht{tg}"], writes=[f"ps{bank}"])
                        rl = RL[psi % 2]; rln = f"rl{psi % 2}"
                        sc.add("act", (lambda e, rl=rl, ps=ps, m=m: e.activation(out=rl, in_=ps, func=AF.Relu,
                                                                                 bias=B1C[:, l * 32 + m:l * 32 + m + 1])),
                               reads=[f"ps{bank}", "b1c"] + region_users, writes=[rln])
                        sc.add("dve", (lambda e, rl=rl, m=m, tg=tg: e.tensor_tensor(out=HID[:, m, tg * 512:(tg + 1) * 512], in0=rl, in1=rl, op=ALU.mult)),
                               reads=[rln], writes=[f"hid{m}_{tg}"])
            if H == 0:
                self.dump("hid", HID, [f"hid{m}_{tg}" for m in range(32) for tg in range(2)])
            sc.marker(writes=["ht0", "ht1", "w1b0", "w1b1", "w1b2", "rl0", "rl1", "rgnB_ok"])
            ht_users = ["rgnB_ok"]
            for o_ in range(8):
                wbs = []
                for kh in range(2):
                    wb = W2B[w2cnt % 3]; wn = f"w2b{w2cnt % 3}"; w2cnt += 1
                    src = self.d_w2r[l, o_, kh]
                    sc.add("pool", (lambda e, wb=wb, src=src: e.dma_start(out=wb, in_=src)), writes=[wn], dma=wn)
                    wbs.append((wb, wn))
                banks = [(o_ % 2) * 2, (o_ % 2) * 2 + 1]
                for kh in range(2):
                    wb, wn = wbs[kh]
                    for tg in range(2):
                        ps = self.psb(banks[tg])

                        def mm(e, wb=wb, kh=kh, tg=tg, ps=ps):
                            last = None
                            for kk in range(16):
                                m = kh * 16 + kk
                                last = e.matmul(ps, wb[:, kk, :], HID[:, m, tg * 512:(tg + 1) * 512],
                                                start=(m == 0), stop=(m == 31))
                            return last
                        sc.add("pe", mm, reads=[wn] + [f"hid{kh * 16 + kk}_{tg}" for kk in range(16)], writes=[f"ps{banks[tg]}"])
                for tg in range(2):
                    ps = self.psb(banks[tg])
                    ysl = YSB[:, o_, tg * 512:(tg + 1) * 512]
                    sc.add("act", (lambda e, ps=ps, ysl=ysl, o_=o_: e.activation(out=ysl, in_=ps, func=AF.Identity,
                                                                                  bias=B2C[:, l * 8 + o_:l * 8 + o_ + 1])),
                           reads=[f"ps{banks[tg]}", "b2c"] + ht_users, writes=[f"ysb{o_}t{tg}"])
                    sq = self.SQ[tg][:, 0, :]
                    sc.add("dve", (lambda e, ysl=ysl, sq=sq: e.tensor_tensor(out=sq, in0=ysl, in1=ysl, op=ALU.mult)),
                           reads=[f"ysb{o_}t{tg}"], writes=[f"sq{tg}"])
                    ssb = 4 + tg
                    sc.add("pe", (lambda e, sq=sq, ssb=ssb, o_=o_: e.matmul(self.psb(ssb), self.ONES, sq, start=(o_ == 0), stop=(o_ == 7))),
                           reads=[f"sq{tg}", "ones"], writes=[f"ps{ssb}"])
            for tg in range(2):
                self.resid_update(l, 1, T0 + tg * 512, 512, YSB[:, :, tg * 512:(tg + 1) * 512],
                                  (lambda c, tg=tg: f"ysb{c}t{tg}"), 4 + tg)
            sc.marker(writes=[f"ysb{c}t{tg}" for c in range(8) for tg in range(2)] + ["rgnA_ok"])

    def attn_phase(self, l):
        sc = self.sc
        view = self.view
        XT = self.XT
        o = self.phase_base
        KT = view(o, 5120, BF16).rearrange("p (c t) -> p c t", c=5); o += 5120
        VA = view(o, 5200, BF16).rearrange("p (t h d) -> p t h d", t=16, h=10); o += 5200
        WIN = view(o, 9216, BF16).rearrange("p (k n) -> p k n", k=8); o += 9216
        WOUT = view(o, 4096, BF16).rearrange("p (k n) -> p k n", k=8); o += 4096
        DT = view(o, 2048, BF16).rearrange("p (h t q) -> p h t q", h=16, t=2); o += 2048
        rA = o; o += 1024
        rC = o; o += 1024
        rB = o; o += 1024
        HTG = view(rA, 1024, BF16).rearrange("p (c t) -> p c t", c=8)
        OG = view(rA, 1024, BF16).rearrange("p (q f) -> p q f", q=2)
        QTG = view(rC, 1024, BF16).rearrange("p (c t) -> p c t", c=8)
        QSQ = view(rB, 1024, BF16).rearrange("p (c t) -> p c t", c=8)
        OTG = view(rB, 1024, BF16).rearrange("p (c t) -> p c t", c=8)
        YSBA = view(rA, 2048).rearrange("p (c t) -> p c t", c=8)
        AUGT1 = [view(o + i * 128, 128, BF16) for i in range(3)]; o += 384
        IND = view(o, 512, BF16).rearrange("p (j k) -> p j k", j=8); o += 512
        HIND = view(o, 8, BF16)[:, 0:2]; o += 8
        KMEANT = view(o, 32, BF16).rearrange("p (c r j) -> p c r j", c=4, r=2); o += 32
        KSUM = view(o, 8, F32); o += 8
        KMAX2 = view(o, 8, F32); o += 8
        KMXG = view(o, 8, F32); o += 8
        KM16 = view(o, 16, F32); o += 16
        QMXG = view(o, 8, F32); o += 8
        NB = view(o, 16, F32); o += 16
        FB = view(o, 16, F32); o += 16
        QM16 = view(o, 16, F32); o += 16
        SINKS = view(o, 8, F32); o += 8
        BM8 = view(o, 16, F32); o += 16
        RB = self.XN[0].rearrange("p (h b) -> p h b", h=16)
        B31 = view(o, 16, F32); o += 16
        KSQ = view(rB, 640, BF16).rearrange("p (c t) -> p c t", c=5)
        RDEN = view(o, 8, F32); o += 8
        assert o <= 53200, o
        wsc = self.wada_off
        CMP = view(wsc, 1024).rearrange("p (a j k) -> p a j k", a=16, j=8)
        AUGB = view(wsc + 1024, 128, BF16).rearrange("p (q s j) -> p q s j", q=2, s=16)
        BMASK = view(wsc + 1600, 192).rearrange("p (k b j) -> p k b j", k=3, b=8)
        GM = view(wsc + 1792, 128).rearrange("p (a j) -> p a j", a=16)
        SEL = view(wsc + 1920, 128).rearrange("p (a j) -> p a j", a=16)
        rs_off = self.rstd_off
        SELF = view(rs_off + 256, 256).rearrange("p (q s j) -> p q s j", q=2, s=16)
        SH8 = view(rs_off + 512 + 256, 32).rearrange("p (q s) -> p q s", q=2)
        SINKT = view(rs_off + 512 + 288, 16).rearrange("p (q s) -> p q s", q=2)
        SQRT_T = view(rs_off + 512 + 304, 32).rearrange("p (q s) -> p q s", q=2)
        TMPS = [self.XN[0], self.XN[1]]
        PTS = [self.SQ[0].rearrange("p a t -> p (a t)")[:, 0:512], self.SQ[0].rearrange("p a t -> p (a t)")[:, 512:1024],
               self.SQ[1].rearrange("p a t -> p (a t)")[:, 0:512], self.SQ[1].rearrange("p a t -> p (a t)")[:, 512:1024]]
        PTN = ["sq0", "sq0b", "sq1", "sq1b"]
        IDENT = self.IDENT.rearrange("p (a b) -> p a b", a=1)[:, 0, :]

        sc.marker(writes=["wada0", "wada1", "rstd0", "rstd1", "rstd0p", "rstd1p", "wadafree"])
        sc.add("pool", (lambda e: e.dma_start(out=WIN, in_=self.d_winr[l])), writes=["win"], dma="win")
        sc.add("pool", (lambda e: e.dma_start(out=DT.rearrange("p h t q -> p (h t q)"), in_=self.d_dtile[:, :])), writes=["dt"], dma="dt")
        sc.add("pool", (lambda e: e.dma_start(out=IND.rearrange("p j k -> p (j k)")[0:72, :], in_=self.d_indall[:, :])), writes=["ind"], dma="ind")
        sc.add("pool", (lambda e: e.dma_start(out=HIND, in_=self.d_hind[:, :])), writes=["hind"], dma="hind")
        sc.add("sp", (lambda e: e.dma_start(out=BMASK.rearrange("p k b j -> p (k b j)"), in_=self.d_bmask[:, :])), reads=["wadafree"], writes=["bmask"], dma="bmask")
        sc.add("sp", (lambda e: e.dma_start(out=SINKS, in_=self.d_sinks[l:l + 1, :].partition_broadcast(128))), writes=["sinks"], dma="sinks")
        sc.add("sp", (lambda e: e.dma_start(out=RB.rearrange("p h b -> p (h b)"), in_=self.d_rbT[0:1, :].partition_broadcast(128))), writes=["xn0"], dma="rb")
        sc.add("pool", (lambda e: e.dma_start(out=WOUT, in_=self.d_woutr[l])), writes=["wout"], dma="wout")

        def init1(e):
            e.memset(KMEANT, 0.0)
            e.memset(KMAX2, 0.0)
            e.memset(VA[:, :, :, 64:65], 1.0)
            e.memset(AUGB, 0.0)
            e.memset(SELF, 1.0)
            e.tensor_reduce(out=BM8, in_=RB, axis=AX.X, op=ALU.max)
            e.tensor_copy(out=B31, in_=RB[:, :, 31])
            return e.memset(KM16, 0.0)
        sc.add("dve", init1, reads=["xn0", "wadafree"], writes=["kmeant", "kmax2", "va_ones", "augb_s", "augb_m", "selfm", "bm8raw", "b31", "km16"])

        sc.add("dve", (lambda e: e.tensor_tensor(out=BM8[:, 8:16], in0=BM8[:, 8:16], in1=SINKS, op=ALU.max)),
               reads=["bm8raw", "sinks"], writes=["bm8raw"])
        sc.add("dve", (lambda e: e.tensor_scalar(out=BM8, in0=BM8, scalar1=8.0, scalar2=None, op0=ALU.mult)),
               reads=["bm8raw"], writes=["bm8", "bm8raw"])

        QCOL, KCOL, VCOL, QBCOL, KBCOL, VBCOL = 0, 512, 1024, 1536, 2048, 2176
        sbank = [0]
        ptc = [0]

        def group(g):
            t0 = g * 256
            b = g
            xres = [f"xT{g}"]
            self.rmsnorm_in(l, 0, t0, 256, HTG, "rA", 7, sq_names=(["sq0", "sq0b"], ["sq1", "sq1b"]))
            pbank = [0]

            def nextbank():
                bk = 4 + pbank[0] % 4
                pbank[0] += 1
                return bk
            for ci in range(8):
                col = QCOL + ci * 128 if ci < 4 else QBCOL + (ci - 4) * 128
                bk = nextbank()
                ps = self.psb(bk)

                def mm(e, col=col, ps=ps):
                    last = None
                    for kc in range(8):
                        last = e.matmul(ps[:, 0:256], WIN[:, kc, col:col + 128], HTG[:, kc, :], start=(kc == 0), stop=(kc == 7))
                    return last
                sc.add("pe", mm, reads=["win", "rA"], writes=[f"ps{bk}"])
                sc.add("dve", (lambda e, ci=ci, ps=ps: e.tensor_copy(out=QTG[:, ci, :], in_=ps[:, 0:256])),
                       reads=[f"ps{bk}"], writes=["rC"])
            sc.add("dve", (lambda e: e.memset(KSUM, 0.0)), writes=[f"ksum{c}" for c in range(4)])
            for ci in range(5):
                col = KCOL + ci * 128 if ci < 4 else KBCOL
                bk = nextbank()
                ps = self.psb(bk)

                def mm(e, col=col, ps=ps):
                    last = None
                    for kc in range(8):
                        last = e.matmul(ps[:, 0:256], WIN[:, kc, col:col + 128], HTG[:, kc, :], start=(kc == 0), stop=(kc == 7))
                    return last
                sc.add("pe", mm, reads=["win", "rA"], writes=[f"ps{bk}"])
                if ci < 4:
                    sc.add("act", (lambda e, ci=ci, ps=ps: e.activation(out=KT[:, ci, t0:t0 + 256], in_=ps[:, 0:256], func=AF.Copy,
                                                                           accum_out=KSUM[:, ci:ci + 1])),
                           reads=[f"ps{bk}"], writes=[f"kt{ci}_{g}", f"ksum{ci}"])
                else:
                    sc.add("act", (lambda e, ci=ci, ps=ps: e.activation(out=KT[:, ci, t0:t0 + 256], in_=ps[:, 0:256], func=AF.Copy)),
                           reads=[f"ps{bk}"], writes=[f"kt{ci}_{g}"])
            for qt in range(2):
                tile_i = g * 2 + qt
                bk = nextbank()
                ps = self.psb(bk)

                def mmv(e, qt=qt, ps=ps):
                    last = None
                    for kc in range(8):
                        last = e.matmul(ps[:, 0:512], HTG[:, kc, qt * 128:(qt + 1) * 128], WIN[:, kc, VCOL:VCOL + 512], start=(kc == 0), stop=(kc == 7))
                    return last
                sc.add("pe", mmv, reads=["win", "rA"], writes=[f"ps{bk}"])
                sc.add("act", (lambda e, tile_i=tile_i, ps=ps: e.activation(out=VA[:, tile_i, 0:8, 0:64],
                                                                               in_=ps[:, 0:512].rearrange("p (h d) -> p h d", h=8), func=AF.Copy)),
                       reads=[f"ps{bk}", "va_ones"], writes=[f"va{g}_{qt}a"])
                bk2 = nextbank()
                ps2 = self.psb(bk2)

                def mmv2(e, qt=qt, ps2=ps2):
                    last = None
                    for kc in range(8):
                        last = e.matmul(ps2[:, 0:128], HTG[:, kc, qt * 128:(qt + 1) * 128], WIN[:, kc, VBCOL:VBCOL + 128], start=(kc == 0), stop=(kc == 7))
                    return last
                sc.add("pe", mmv2, reads=["win", "rA"], writes=[f"ps{bk2}"])
                sc.add("dve", (lambda e, tile_i=tile_i, ps2=ps2: e.tensor_copy(out=VA[:, tile_i, 8:10, 0:64],
                                                                                 in_=ps2[:, 0:128].rearrange("p (h d) -> p h d", h=2))),
                       reads=[f"ps{bk2}", "va_ones"], writes=[f"va{g}_{qt}b"])
            if getattr(self, 'stop', 99) <= 2:
                return
            def kmw(e, b=b):
                e.tensor_scalar(out=KMEANT[0:64, :, 0, b], in0=KSUM[0:64, 0:4], scalar1=1.0 / 256.0, scalar2=None, op0=ALU.mult)
                return e.tensor_scalar(out=KMEANT[64:128, :, 1, b], in0=KSUM[64:128, 0:4], scalar1=1.0 / 256.0, scalar2=None, op0=ALU.mult)
            sc.add("dve", kmw, reads=[f"ksum{c}" for c in range(4)], writes=["kmeant"])
            sc.add("dve", (lambda e: e.tensor_tensor(out=KSQ, in0=KT[:, 0:5, t0:t0 + 256], in1=KT[:, 0:5, t0:t0 + 256], op=ALU.mult)),
                   reads=[f"kt{c}_{g}" for c in range(5)], writes=["rB", "rBb"])
            if getattr(self, 'stop', 99) <= 2.2:
                return
            for bi, cs in enumerate([(0, 1), (2, 3), (4,)]):
                bk = 4 + bi
                ps = self.psb(bk).rearrange("p (a t) -> p a t", a=2)

                def mmk(e, cs=cs, ps=ps):
                    last = None
                    for a, c in enumerate(cs):
                        last = e.matmul(ps[:, a, :], self.ONES, KSQ[:, c, :], start=True, stop=True)
                    return last
                sc.add("pe", mmk, reads=["rB", "ones"], writes=[f"ps{bk}"])
                sc.add("dve", (lambda e, cs=cs, ps=ps: e.tensor_reduce(out=KMXG[:, cs[0]:cs[0] + len(cs)], in_=ps[:, 0:len(cs), :], axis=AX.X, op=ALU.max)),
                       reads=[f"ps{bk}"], writes=["kmxg"])

            if getattr(self, 'stop', 99) <= 2.4:
                return
            sc.add("dve", (lambda e: e.tensor_tensor(out=KMAX2[:, 0:5], in0=KMAX2[:, 0:5], in1=KMXG[:, 0:5], op=ALU.max)),
                   reads=["kmxg", "kmax2"], writes=["kmax2"])

            def kmax(e):
                e.tensor_copy(out=KM16[:, 0:8].rearrange("p (c r) -> p c r", r=2), in_=KMAX2[:, 0:4].unsqueeze(2).to_broadcast([128, 4, 2]))
                return e.tensor_copy(out=KM16[:, 8:16], in_=KMAX2[:, 4:5].to_broadcast([128, 8]))
            sc.add("dve", kmax, reads=["kmax2"], writes=["km16"])
            if getattr(self, 'stop', 99) <= 2.6:
                return
            sc.add("dve", (lambda e: e.tensor_tensor(out=QSQ, in0=QTG, in1=QTG, op=ALU.mult)), reads=["rC"], writes=["rB", "rBb"])
            ps7 = self.psb(7)
            GATE = ps7[:, 0:128].rearrange("p (a j) -> p a j", a=16)

            for bi in range(4):
                psq = self.psb(bi).rearrange("p (a t) -> p a t", a=2)

                def mmq(e, bi=bi, psq=psq):
                    last = None
                    for a in range(2):
                        last = e.matmul(psq[:, a, :], self.ONES, QSQ[:, 2 * bi + a, :], start=True, stop=True)
                    return last
                sc.add("pe", mmq, reads=["rB", "ones"], writes=[f"ps{bi}"])
                sc.add("dve", (lambda e, bi=bi, psq=psq: e.tensor_reduce(out=QMXG[:, 2 * bi:2 * bi + 2], in_=psq, axis=AX.X, op=ALU.max)),
                       reads=[f"ps{bi}"], writes=["qmxg"])
            sc.add("dve", (lambda e: e.tensor_copy(out=QM16.rearrange("p (c r) -> p c r", r=2), in_=QMXG.unsqueeze(2).to_broadcast([128, 8, 2]))),
                   reads=["qmxg"], writes=["qm16"])

            def mmg(e):
                last = None
                for qt in range(2):
                    for c in range(4):
                        last = e.matmul(ps7[:, (qt * 8 + 2 * c) * 8:(qt * 8 + 2 * c + 2) * 8], QTG[:, c, qt * 128:(qt + 1) * 128],
                                        KMEANT[:, c, :, :].rearrange("p r j -> p (r j)"), start=True, stop=True)
                return last
            if b >= 4:
                sc.add("pe", mmg, reads=["rC", "kmeant"], writes=["ps7"])
            if getattr(self, 'stop', 99) <= 3:
                return
            NEGM = BMASK[:, 0, b, :]
            ELIG = BMASK[:, 1, b, :]
            OWN = BMASK[:, 2, b, :]

            sc.add("dve", (lambda e: e.tensor_tensor(out=SQRT_T, in0=QM16.unsqueeze(1).to_broadcast([128, 2, 16]),
                                                     in1=KM16.unsqueeze(1).to_broadcast([128, 2, 16]), op=ALU.mult)),
                   reads=["qm16", "km16"], writes=["sqrt_t"])
            sc.add("act", (lambda e: e.activation(out=SQRT_T, in_=SQRT_T, func=AF.Ln, bias=self.EPSC[:, 0:1], scale=1.0)),
                   reads=["sqrt_t", "epsc"], writes=["sqrt_t"])
            sc.add("act", (lambda e: e.activation(out=SQRT_T, in_=SQRT_T, func=AF.Exp, scale=0.5)),
                   reads=["sqrt_t"], writes=["sqrt_t"])
            AUGBv = AUGB
            sc.add("dve", (lambda e: e.tensor_tensor(out=SH8, in0=SQRT_T, in1=BM8.unsqueeze(1).to_broadcast([128, 2, 16]), op=ALU.add)),
                   reads=["sqrt_t", "bm8"], writes=["sh8"])
            sc.add("dve", (lambda e: e.tensor_scalar(out=NB, in0=SH8[:, 0, :], scalar1=-0.125, scalar2=None, op0=ALU.mult)),
                   reads=["sh8"], writes=["nb"])
            sc.add("dve", (lambda e: e.tensor_tensor(out=FB, in0=NB, in1=B31, op=ALU.add)), reads=["nb", "b31"], writes=["fb"])
            sc.add("dve", (lambda e: e.tensor_tensor(out=SINKT, in0=SINKS.unsqueeze(1).to_broadcast([128, 2, 8]),
                                                     in1=NB[:, 8:16].unsqueeze(1).to_broadcast([128, 2, 8]), op=ALU.add)),
                   reads=["nb", "sinks"], writes=["sinkt"])
            sc.add("act", (lambda e: e.activation(out=SINKT, in_=SINKT, func=AF.Exp)), reads=["sinkt"], writes=["sinkt"])
            SELM = SELF[:, :, 0:8, :]
            sel_jobs = [
                lambda: sc.add("dve", (lambda e: e.tensor_tensor(out=GM, in0=GATE, in1=NEGM.unsqueeze(1).to_broadcast([128, 16, 8]), op=ALU.add)),
                               reads=["ps7", "bmask"], writes=["gm"]),
                lambda: sc.add("dve", (lambda e: e.tensor_tensor(out=CMP, in0=GM.unsqueeze(2).to_broadcast([128, 16, 8, 8]),
                                                                 in1=GM.unsqueeze(3).to_broadcast([128, 16, 8, 8]), op=ALU.is_gt)),
                               reads=["gm"], writes=["cmp"]),
                lambda: sc.add("dve", (lambda e: e.tensor_reduce(out=SEL, in_=CMP, axis=AX.X, op=ALU.add)), reads=["cmp"], writes=["selr"]),
                lambda: sc.add("dve", (lambda e: e.scalar_tensor_tensor(out=SEL, in0=SEL, scalar=3.0, in1=ELIG.unsqueeze(1).to_broadcast([128, 16, 8]),
                                                                        op0=ALU.is_lt, op1=ALU.mult)),
                               reads=["selr", "bmask"], writes=["selr"]),
                lambda: sc.add("dve", (lambda e: e.tensor_tensor(out=SELM, in0=SEL.rearrange("p (q h) j -> p q h j", q=2),
                                                                 in1=OWN.unsqueeze(1).unsqueeze(1).to_broadcast([128, 2, 8, 8]), op=ALU.add)),
                               reads=["selr", "bmask", "selfm"], writes=["selfm"]),
                lambda: sc.add("dve", (lambda e: e.tensor_scalar(out=AUGBv[:, :, 0:8, :], in0=SELM, scalar1=BIG, scalar2=-BIG, op0=ALU.mult, op1=ALU.add)),
                               reads=["selfm"], writes=["augb_m"]),
            ]
            if b < 4:
                sel_jobs = []
            if getattr(self, 'stop', 99) <= 4:
                return
            order = list(range(8, 16)) + list(range(8))
            if sel_jobs:
                sel_jobs.pop(0)()
            for grp in (1, 0):
                pvq = []

                def flush(keep):
                    while len(pvq) > keep:
                        a_, k_ = pvq.pop(0)
                        sc.add(*a_, **k_)

                def prep(s16):
                    ps7b = self.psb(7, BF16)
                    pos_ = order.index(s16)
                    AT = AUGT1[pos_ % 3]
                    ares = "augb_s" if s16 >= 8 else "augb_m"

                    def tr(e):
                        last = None
                        for qt in range(2):
                            last = e.transpose(ps7b[0:8, qt * 128:(qt + 1) * 128], AUGB[:, qt, s16, :], IDENT)
                        return last
                    sc.add("pe", tr, reads=[ares, "ident"], writes=["ps7"])
                    sc.add("act", (lambda e: e.activation(out=AT[0:8, 0:256], in_=ps7b[0:8, 0:256], func=AF.Copy)),
                           reads=["ps7"], writes=[f"augt{pos_ % 3}"])

                def head(hs8):
                    s16 = grp * 8 + hs8
                    pos = order.index(s16)
                    if grp == 1 and sel_jobs:
                        sel_jobs.pop(0)()
                    if pos + 2 < 16 and order[pos + 2] < 8 and b >= 4:
                        while sel_jobs:
                            sel_jobs.pop(0)()
                        prep(order[pos + 2])
                    AT = AUGT1[pos % 3]
                    use_aug = (grp == 0 and b >= 4)
                    pb = 0
                    if grp == 0:
                        ck, r0, cq, vh = hs8 // 2, (hs8 % 2) * 64, hs8 // 2, hs8
                    else:
                        i_, r_ = hs8 // 2, hs8 % 2
                        ck, r0, cq, vh = 4, r_ * 64, 4 + i_, 8 + r_
                    quad, hsl = hs8 // 4, hs8 % 4
                    augres = f"augt{pos % 3}"
                    far = []
                    if grp == 0 and b >= 1:
                        for kc in range(0, 2 * b - 1):
                            far.append((kc, 0, 256))
                        far.append((2 * b - 1, 128, 128))
                    near = [(2 * b - 1, 0, 0), (2 * b, 0, 1), (2 * b, 1, 0), (2 * b + 1, 1, 1)]
                    if b == 0:
                        near = near[1:]
                    n_qt = [0, 0]
                    for (kc, q0, qn) in far:
                        for sub in range(qn // 128):
                            n_qt[(q0 + sub * 128) // 128] += 1
                    for (kc, qt, _) in near:
                        n_qt[qt] += 1
                    done_qt = [0, 0]
                    banks = []
                    cur, tot = [], 0
                    for p_ in far:
                        if tot + p_[2] > 512:
                            banks.append(cur)
                            cur, tot = [], 0
                        cur.append(p_)
                        tot += p_[2]
                    if cur:
                        banks.append(cur)
                    for pieces in banks:
                        bk = 4 + sbank[0] % 3
                        sbank[0] += 1
                        ps = self.psb(bk)
                        pti = ptc[0] % 4
                        ptc[0] += 1
                        PT = PTS[pti]
                        offs = []
                        off = 0
                        for p_ in pieces:
                            offs.append(off)
                            off += p_[2]
                        tot = off

                        def mms(e, pieces=pieces, offs=offs, ps=ps):
                            last = None
                            for (kc, q0, qn), of in zip(pieces, offs):
                                last = e.matmul(ps[:, of:of + qn], KT[r0:r0 + 64, ck, kc * 128:(kc + 1) * 128], QTG[r0:r0 + 64, cq, q0:q0 + qn],
                                                start=True, stop=not use_aug)
                                if use_aug:
                                    last = e.matmul(ps[:, of:of + qn], IND[pb:pb + 8, kc // 2, :], AT[0:8, q0:q0 + qn],
                                                    start=False, stop=True)
                            return last
                        flush(1)
                        sc.add("pe", mms, reads=[f"kt{ck}_{p_[0] // 2}" for p_ in pieces] + ["rC", "ind"] + ([augres] if use_aug else []), writes=[f"ps{bk}"])
                        sc.add("act", (lambda e, PT=PT, ps=ps, tot=tot: e.activation(out=PT[:, 0:tot], in_=ps[:, 0:tot], func=AF.Exp,
                                                                                       scale=0.125, bias=FB[:, s16:s16 + 1])),
                               reads=[f"ps{bk}", "fb"], writes=[PTN[pti]])
                        flags = []
                        for (kc, q0, qn), of in zip(pieces, offs):
                            for sub in range(qn // 128):
                                qt = (q0 + sub * 128) // 128
                                st = done_qt[qt] == 0
                                done_qt[qt] += 1
                                sp_ = done_qt[qt] == n_qt[qt]
                                flags.append((kc, qt, of + sub * 128, st, sp_))

                        def pv(e, flags=flags, PT=PT):
                            last = None
                            for (kc, qt, of, st, sp_) in flags:
                                last = e.matmul(self.psb(qt * 2 + quad).rearrange("p (h d) -> p h d", d=65)[:, hsl, 0:65] if False else
                                                self.oacc(qt * 2 + quad)[:, hsl, :], PT[:, of:of + 128], VA[:, kc, vh, :], start=st, stop=sp_)
                            return last
                        pvq.append((("pe", pv), dict(reads=[PTN[pti]] + [f"va{p_[0] // 2}_{p_[0] % 2}{'a' if grp == 0 else 'b'}" for p_ in pieces],
                                                     writes=[f"ps{quad}", f"ps{2 + quad}"])))
                    bk = 4 + sbank[0] % 3
                    sbank[0] += 1
                    ps = self.psb(bk).rearrange("p (a t) -> p a t", a=4)
                    pti = ptc[0] % 4
                    ptc[0] += 1
                    PT = PTS[pti].rearrange("p (a t) -> p a t", a=4)
                    TMP = TMPS[pti % 2].rearrange("p (a t) -> p a t", a=4)
                    tmpn = f"xn{pti % 2}"
                    ti0 = 4 - len(near)

                    def mmn(e, near=near, ps=ps, ti0=ti0):
                        last = None
                        for k_, (kc, qt, di) in enumerate(near):
                            ti = ti0 + k_
                            ua = use_aug and (kc // 2) < b
                            last = e.matmul(ps[:, ti, :], KT[r0:r0 + 64, ck, kc * 128:(kc + 1) * 128], QTG[r0:r0 + 64, cq, qt * 128:(qt + 1) * 128],
                                            start=True, stop=not ua)
                            if ua:
                                last = e.matmul(ps[:, ti, :], IND[pb:pb + 8, kc // 2, :], AT[0:8, qt * 128:(qt + 1) * 128],
                                                start=False, stop=True)
                        return last
                    flush(1)
                    sc.add("pe", mmn, reads=[f"kt{ck}_{kc // 2}" for (kc, _, _) in near] + ["rC", "ind"] + ([augres] if use_aug else []), writes=[f"ps{bk}"])

                    def biasadd(e, ps=ps, TMP=TMP, ti0=ti0):
                        if ti0 == 0:
                            e.scalar_tensor_tensor(out=TMP[:, 0:2, :], in0=ps[:, 0:2, :], scalar=0.125, in1=DT[:, s16, 0:2, :], op0=ALU.mult, op1=ALU.add)
                        else:
                            e.scalar_tensor_tensor(out=TMP[:, 1:2, :], in0=ps[:, 1:2, :], scalar=0.125, in1=DT[:, s16, 1:2, :], op0=ALU.mult, op1=ALU.add)
                        return e.scalar_tensor_tensor(out=TMP[:, 2:4, :], in0=ps[:, 2:4, :], scalar=0.125, in1=DT[:, s16, 0:2, :], op0=ALU.mult, op1=ALU.add)
                    sc.add("dve", biasadd, reads=[f"ps{bk}", "dt"], writes=[tmpn])
                    sc.add("act", (lambda e, PT=PT, TMP=TMP, ti0=ti0: e.activation(out=PT[:, ti0:4, :], in_=TMP[:, ti0:4, :], func=AF.Exp,
                                                                                       bias=NB[:, s16:s16 + 1])),
                           reads=[tmpn, "nb"], writes=[PTN[pti]])
                    flags = []
                    for k_, (kc, qt, di) in enumerate(near):
                        st = done_qt[qt] == 0
                        done_qt[qt] += 1
                        sp_ = done_qt[qt] == n_qt[qt]
                        flags.append((kc, qt, ti0 + k_, st, sp_))

                    def pvn(e, flags=flags, PT=PT):
                        last = None
                        for (kc, qt, ti, st, sp_) in flags:
                            last = e.matmul(self.oacc(qt * 2 + quad)[:, hsl, :], PT[:, ti, :], VA[:, kc, vh, :], start=st, stop=sp_)
                        return last
                    pvq.append((("pe", pvn), dict(reads=[PTN[pti]] + [f"va{kc // 2}_{kc % 2}{'a' if grp == 0 else 'b'}" for (kc, _, _) in near],
                                                  writes=[f"ps{quad}", f"ps{2 + quad}"])))
                for hs8 in range(8):
                    head(hs8)
                flush(0)
                for qt in range(2):
                    for quad in range(2):
                        bk = qt * 2 + quad
                        oa = self.oacc(bk)
                        if grp == 0:
                            outv = OG[:, qt, quad * 256:(quad + 1) * 256].rearrange("p (h d) -> p h d", h=4)
                            inv = oa[:, :, 0:64]
                        else:
                            outv = OG[:, qt, 512:1024].rearrange("p (r i d) -> p i r d", r=2, i=4)[:, 2 * quad:2 * quad + 2, :, :]
                            inv = oa[:, :, 0:64].rearrange("p (i r) d -> p i r d", r=2)

                        def nrm(e, oa=oa, outv=outv, inv=inv, qt=qt, quad=quad, grp=grp):
                            if grp == 0:
                                e.reciprocal(out=RDEN[:, 0:4], in_=oa[:, :, 64])
                            else:
                                e.tensor_tensor(out=RDEN[:, 0:4], in0=oa[:, :, 64], in1=SINKT[:, qt, 4 * quad:4 * quad + 4], op=ALU.add)
                                e.reciprocal(out=RDEN[:, 0:4], in_=RDEN[:, 0:4])
                            if grp == 0:
                                rb_ = RDEN[:, 0:4].unsqueeze(2).to_broadcast([128, 4, 64])
                            else:
                                rb_ = RDEN[:, 0:4].rearrange("p (i r) -> p i r", r=2).unsqueeze(3).to_broadcast([128, 2, 2, 64])
                            return e.tensor_tensor(out=outv, in0=inv, in1=rb_, op=ALU.mult)
                        def nrm1(e, oa=oa, qt=qt, quad=quad, grp=grp):
                            if grp == 0:
                                return e.reciprocal(out=RDEN[:, 4 * (bk % 2):4 * (bk % 2) + 4], in_=oa[:, :, 64])
                            return e.tensor_tensor(out=RDEN[:, 4 * (bk % 2):4 * (bk % 2) + 4], in0=oa[:, :, 64], in1=SINKT[:, qt, 4 * quad:4 * quad + 4], op=ALU.add)
                        rdn = f"rden{bk % 2}"
                        RD = RDEN[:, 4 * (bk % 2):4 * (bk % 2) + 4]
                        if grp == 0:
                            sc.add("dve", (lambda e, oa=oa, RD=RD: e.reciprocal(out=RD, in_=oa[:, :, 64])), reads=[f"ps{bk}"], writes=[rdn])
                        else:
                            sc.add("dve", (lambda e, oa=oa, RD=RD, qt=qt, quad=quad: e.tensor_tensor(out=RD, in0=oa[:, :, 64], in1=SINKT[:, qt, 4 * quad:4 * quad + 4], op=ALU.add)),
                                   reads=[f"ps{bk}", "sinkt"], writes=[rdn])
                            sc.add("dve", (lambda e, RD=RD: e.reciprocal(out=RD, in_=RD)), reads=[rdn], writes=[rdn])
                        if grp == 0:
                            rb_ = RD.unsqueeze(2).to_broadcast([128, 4, 64])
                        else:
                            rb_ = RD.rearrange("p (i r) -> p i r", r=2).unsqueeze(3).to_broadcast([128, 2, 2, 64])
                        sc.add("dve", (lambda e, outv=outv, inv=inv, rb_=rb_: e.tensor_tensor(out=outv, in0=inv, in1=rb_, op=ALU.mult)),
                               reads=[f"ps{bk}", rdn], writes=["rA"])
            if getattr(self, 'stop', 99) <= 7:
                return
            for half in range(2):
                bk = 4 + half
                psT = self.psb(bk, BF16).rearrange("p (c t) -> p c t", c=4)

                def trO(e, half=half, psT=psT):
                    last = None
                    for cc in range(4):
                        c = half * 4 + cc
                        for qt in range(2):
                            last = e.transpose(psT[:, cc, qt * 128:(qt + 1) * 128], OG[:, qt, c * 128:(c + 1) * 128], IDENT)
                    return last
                sc.add("pe", trO, reads=["rA", "ident"], writes=[f"ps{bk}"])
                if half == 0:
                    sc.add("act", (lambda e, psT=psT: e.activation(out=OTG[:, 0:4, :], in_=psT, func=AF.Copy)), reads=[f"ps{bk}"], writes=["rB"])
                else:
                    sc.add("dve", (lambda e, psT=psT: e.tensor_copy(out=OTG[:, 4:8, :], in_=psT)), reads=[f"ps{bk}"], writes=["rBb"])
            if getattr(self, 'stop', 99) <= 8:
                return
            for o_ in range(8):
                bk = 4 + o_ % 3
                ps = self.psb(bk)

                def mmo(e, o_=o_, ps=ps):
                    last = None
                    for c in range(8):
                        last = e.matmul(ps[:, 0:256], WOUT[:, c, o_ * 128:(o_ + 1) * 128], OTG[:, c, :], start=(c == 0), stop=(c == 7))
                    return last
                sc.add("pe", mmo, reads=["wout", "rB", "rBb"], writes=[f"ps{bk}"])
                ysl = YSBA[:, o_, :]
                sc.add("act", (lambda e, ps=ps, ysl=ysl: e.activation(out=ysl, in_=ps[:, 0:256], func=AF.Copy)),
                       reads=[f"ps{bk}", "rA", "rC"], writes=[f"ysa{o_}"])
                sqb = PTS[o_ % 4]
                sc.add("dve", (lambda e, ysl=ysl, sqb=sqb: e.tensor_tensor(out=sqb[:, 0:256], in0=ysl, in1=ysl, op=ALU.mult)),
                       reads=[f"ysa{o_}"], writes=[PTN[o_ % 4]])
                sc.add("pe", (lambda e, sqb=sqb, o_=o_: e.matmul(self.psb(7)[:, 0:256], self.ONES, sqb[:, 0:256], start=(o_ == 0), stop=(o_ == 7))),
                       reads=[PTN[o_ % 4], "ones"], writes=["ps7"])
            if getattr(self, 'stop', 99) <= 9:
                return
            self.resid_update(l, 0, t0, 256, YSBA, (lambda c: f"ysa{c}"), 7)
            sc.marker(reads=[f"ysa{c}" for c in range(8)], writes=["rA", "rC"])

        for g in range(getattr(self, 'ngroups', 8)):
            group(g)
        sc.marker(writes=["bmask", "gm", "cmp", "selr", "augb_s", "augb_m", "selfm", "sh8", "sinkt", "sqrt_t", "nb", "fb", "wada_ok"])

    def oacc(self, bk):
        return self.PS[bk][:, 0:260].rearrange("p (h d) -> p h d", d=65)


def _host_prep(inputs):
    f = np.float32
    w_ada = np.asarray(inputs["w_ada"], f)
    w1 = np.asarray(inputs["w1"], f)
    w2 = np.asarray(inputs["w2"], f)
    w_in = np.asarray(inputs["w_in"], f)
    w_out = np.asarray(inputs["w_out"], f)
    sh = {}
    sh["wada"] = np.ascontiguousarray(w_ada.reshape(DEPTH, 8, 128, 24, 256).transpose(0, 3, 2, 1, 4))
    sh["badac"] = np.ascontiguousarray(np.asarray(inputs["b_ada"], f).reshape(DEPTH, 48, 128).transpose(2, 0, 1).reshape(128, DEPTH * 48))
    sh["gainc"] = np.ascontiguousarray(np.asarray(inputs["norm_gains"], f).reshape(DEPTH, 4, 8, 128).transpose(3, 0, 1, 2).reshape(128, 128))
    sh["b1c"] = np.ascontiguousarray(np.asarray(inputs["b1"], f).reshape(DEPTH, 32, 128).transpose(2, 0, 1).reshape(128, 128))
    sh["b2c"] = np.ascontiguousarray(np.asarray(inputs["b2"], f).reshape(DEPTH, 8, 128).transpose(2, 0, 1).reshape(128, 32))
    sh["w1r"] = np.ascontiguousarray(w1.reshape(DEPTH, 8, 128, 16, 256).transpose(0, 3, 2, 1, 4))
    sh["w2r"] = np.ascontiguousarray(w2.reshape(DEPTH, 2, 16, 128, 8, 128).transpose(0, 4, 1, 3, 2, 5))
    perm = [(k // 2) + 4 * (k % 2) for k in range(8)]
    colidx = np.arange(DIN)
    qb = colidx[1536:2048].reshape(8, 64)[perm].reshape(-1)
    colidx = np.concatenate([colidx[:1536], qb, colidx[2048:]])
    w_in_p = w_in[:, :, colidx]
    sh["winr"] = np.ascontiguousarray(w_in_p.reshape(DEPTH, 8, 128, DIN).transpose(0, 2, 1, 3))
    sh["woutr"] = np.ascontiguousarray(w_out.reshape(DEPTH, 8, 128, D).transpose(0, 2, 1, 3))
    sh["sinks"] = np.ascontiguousarray(np.asarray(inputs["sinks"], f)[:, perm])
    rb = np.asarray(inputs["rel_bias"], f)
    hperm = list(range(8)) + [8 + p for p in perm]
    rb = rb[:, hperm]
    sh["rbT"] = np.ascontiguousarray(rb.T).reshape(1, 16 * 32)
    tab = np.concatenate([rb, np.full((1, 16), NEG, f)], axis=0)
    idx = _dtile_index()
    dt = np.zeros((128, 16, 2, 128), f)
    for h in range(16):
        hg = 0 if h < 8 else 1
        for t in range(2):
            dt[:, h, t, :] = tab[idx[hg, t], h]
    sh["dtile"] = dt.reshape(128, 16 * 2 * 128)
    sh["ident"] = np.eye(128, dtype=f)
    hsel = np.zeros((128, 2, 128), f)
    hsel[0:64, 0, :] = 1.0
    hsel[64:128, 1, :] = 1.0
    sh["hsel"] = hsel.reshape(128, 256)
    hind = np.zeros((128, 2), f)
    hind[0:64, 0] = 1.0
    hind[64:128, 1] = 1.0
    sh["hind"] = hind
    ind = np.zeros((72, 8, 128), f)
    for j in range(8):
        for pb in (0, 32, 64):
            ind[pb + j, j, :] = 1.0
    sh["indall"] = ind.reshape(72, 1024)
    bm = np.zeros((128, 3, 8, 8), f)
    for b in range(8):
        for j in range(8):
            bm[:, 0, b, j] = 0.0 if j < b else -1e30
            bm[:, 1, b, j] = 1.0 if j < b else 0.0
            bm[:, 2, b, j] = 1.0 if j == b else 0.0
    sh["bmask"] = bm.reshape(128, 192)
    x = np.asarray(inputs["x"], f)
    c = np.asarray(inputs["c"], f)
    per = []
    for b in range(x.shape[0]):
        m = dict(sh)
        m["xT"] = np.ascontiguousarray(x[b].T)
        m["cT"] = np.ascontiguousarray(c[b].reshape(8, 128).T)
        per.append(m)
    return per


_PROG_CACHE = {}


def _get_prog(phases, debug=False, ngroups=8, stop=99):
    key = (tuple(phases), debug, ngroups, stop)
    if key not in _PROG_CACHE:
        _PROG_CACHE[key] = Prog(list(phases), debug=debug, ngroups=ngroups, stop=stop)
    return _PROG_CACHE[key]


def run_phases(inputs, phases, n_cores=8, trace=False, debug=False, ngroups=8, stop=99):
    per = _host_prep(inputs)[:n_cores]
    prog = _get_prog(phases, debug, ngroups, stop)
    res = run_bass_kernel_spmd(prog.nc, per, core_ids=list(range(n_cores)), trace=trace)
    outs = [np.ascontiguousarray(r["outT"].T) for r in res.results]
    return np.stack(outs, axis=0), res


def kernel(**inputs):
    phases = []
    for l in range(DEPTH):
        phases += [("attn", l), ("ffn", l)]
    out, _ = run_phases(inputs, phases)
    return out.astype(np.float32)
```

```python
import math
import numpy as np
import concourse.bass as bass
import concourse.mybir as mybir
from concourse.bass_utils import run_bass_kernel_spmd

F32 = mybir.dt.float32
BF16 = mybir.dt.bfloat16
AF = mybir.ActivationFunctionType
ALU = mybir.AluOpType
AX = mybir.AxisListType

D = 1024
S = 2048
DEPTH = 4
DFF = 4096
DIN = 2304
EPS = 1e-6
NEG = -30000.0
BIG = 1024.0
MOBA_ROUND = "trunc"
SWA_ROUND = "trunc"


class Sched:
    def __init__(self):
        self.ops = []
        self.last_w = {}
        self.readers = {}

    def marker(self, reads=(), writes=()):
        k = getattr(self, "_mk", 0)
        self._mk = k + 1
        col = k % 8
        dm = self.dummy
        self.add("dve", (lambda e: e.memset(dm[:, col:col + 1], 0.0)), reads=reads, writes=list(writes) + [f"dummy{col}"])

    def barrier(self):
        names = set(self.last_w) | set(self.readers)
        names.add("__bar__")
        self.marker(writes=sorted(names))

    def add(self, eng, fn, reads=(), writes=(), dma=None, ndma=1, total=False):
        idx = len(self.ops)
        reads = tuple(reads) + ("__bar__",)
        writes = tuple(writes)
        deps = set()
        for r in reads:
            w = self.last_w.get(r)
            if w is not None:
                deps.add(w)
        for w_ in writes:
            w = self.last_w.get(w_)
            if w is not None:
                deps.add(w)
            for rd in self.readers.get(w_, ()):
                deps.add(rd)
        for r in reads:
            self.readers.setdefault(r, []).append(idx)
        for w_ in writes:
            self.last_w[w_] = idx
            self.readers[w_] = []
        self.ops.append(dict(eng=eng, fn=fn, deps=deps, dma=dma, ndma=ndma, total=total,
                             reads=set(reads), writes=set(writes)))
        return idx

    def finalize(self, nc, semctx):
        ops = self.ops
        need = [False] * len(ops)
        for i, o in enumerate(ops):
            keep = set()
            for d in o["deps"]:
                p = ops[d]
                if p["dma"] is not None or o["dma"] is not None:
                    keep.add(d)
                elif p["eng"] != o["eng"]:
                    keep.add(d)
                else:
                    if o["eng"] != "pe" and (p["writes"] & (o["reads"] | o["writes"])):
                        keep.add(d)
            o["deps"] = keep
            for d in keep:
                need[d] = True
        sems = {}

        def getsem(name):
            if name not in sems:
                sems[name] = semctx(name)
            return sems[name]

        cnt = {}
        totals = {}
        for i, o in enumerate(ops):
            if o["dma"] is not None:
                key = "d_" + o["dma"]
                cnt[key] = cnt.get(key, 0) + 16 * o["ndma"]
                o["sig"] = (key, cnt[key])
                if o["total"]:
                    totals[key] = True
            elif need[i]:
                key = "e_" + o["eng"]
                cnt[key] = cnt.get(key, 0) + 1
                o["sig"] = (key, cnt[key])
            else:
                o["sig"] = None
        for o in ops:
            if o["dma"] is not None and o["total"]:
                o["sig"] = (o["sig"][0], cnt[o["sig"][0]])
        for o in ops:
            w = {}
            for d in o["deps"]:
                k, v = ops[d]["sig"]
                if w.get(k, 0) < v:
                    w[k] = v
            o["waits"] = w
        for k in cnt:
            getsem(k)
        self.sems = sems
        self.cnt = cnt

    def emit(self, eng, e):
        waited = {}
        n = 0
        for o in self.ops:
            if o["eng"] != eng:
                continue
            for k in sorted(o["waits"]):
                v = o["waits"][k]
                if waited.get(k, 0) < v:
                    e.wait_ge(self.sems[k], v)
                    waited[k] = v
            ins = o["fn"](e)
            n += 1
            if o["dma"] is not None:
                if not isinstance(ins, (list, tuple)):
                    ins = [ins]
                assert len(ins) == o["ndma"], (len(ins), o["ndma"])
                for i_ in ins:
                    i_.then_inc(self.sems[o["sig"][0]], 16)
            elif o["sig"] is not None:
                if isinstance(ins, (list, tuple)):
                    ins = ins[-1]
                ins.then_inc(self.sems[o["sig"][0]], 1)
        return n


def _t5_bucket_np(dist, mode):
    n = np.maximum(dist, 0).astype(np.int32)
    nf = np.maximum(n, 1).astype(np.float32)
    val = (np.log(nf / np.float32(16)) / np.float32(math.log(128 / 16)) * np.float32(16)).astype(np.float32)
    if mode == "trunc":
        li = val.astype(np.int32)
    else:
        li = np.rint(val).astype(np.int32)
    large = np.minimum(16 + li, 31)
    return np.where(n < 16, n, large)


def _dtile_index():
    k = np.arange(128)[:, None]
    q = np.arange(128)[None, :]
    out = np.zeros((2, 2, 128, 128), np.int64)
    for hg, mode in ((0, MOBA_ROUND), (1, SWA_ROUND)):
        d0 = q - k
        b0 = _t5_bucket_np(d0, mode)
        out[hg, 1] = np.where(d0 >= 0, b0, 32)
        d1 = 128 + q - k
        b1 = _t5_bucket_np(d1, mode)
        if hg == 0:
            out[hg, 0] = b1
        else:
            out[hg, 0] = np.where(d1 < 128, b1, 32)
    return out


class Prog:
    def __init__(self, phases, debug=False, ngroups=8, stop=99):
        self.phases = phases
        self.stop = stop
        self.ngroups = ngroups
        self.debug = debug
        self.dbg_names = []
        self.nc = bass.Bass("TRN2", target_bir_lowering=False)
        self.sc = Sched()
        self.build()

    def dram_in(self, name, shape, dt=F32):
        return self.nc.dram_tensor(name, list(shape), dt, kind="ExternalInput").ap()

    def build(self):
        nc = self.nc
        sc = self.sc
        self.d_xT = self.dram_in("xT", [D, S])
        self.d_cT = self.dram_in("cT", [128, 8])
        self.d_wada = self.dram_in("wada", [DEPTH, 24, 128, 8, 256])
        self.d_badac = self.dram_in("badac", [128, DEPTH * 48])
        self.d_gainc = self.dram_in("gainc", [128, 128])
        self.d_b1c = self.dram_in("b1c", [128, 128])
        self.d_b2c = self.dram_in("b2c", [128, 32])
        self.d_w1r = self.dram_in("w1r", [DEPTH, 16, 128, 8, 256])
        self.d_w2r = self.dram_in("w2r", [DEPTH, 8, 2, 128, 16, 128])
        self.d_winr = self.dram_in("winr", [DEPTH, 128, 8, DIN])
        self.d_woutr = self.dram_in("woutr", [DEPTH, 128, 8, D])
        self.d_sinks = self.dram_in("sinks", [DEPTH, 8])
        self.d_rbT = self.dram_in("rbT", [1, 16 * 32])
        self.d_dtile = self.dram_in("dtile", [128, 16 * 2 * 128])
        self.d_ident = self.dram_in("ident", [128, 128])
        self.d_hsel = self.dram_in("hsel", [128, 2 * 128])
        self.d_hind = self.dram_in("hind", [128, 2])
        self.d_indall = self.dram_in("indall", [72, 8 * 128])
        self.d_bmask = self.dram_in("bmask", [128, 3 * 64])
        self.d_out = nc.dram_tensor("outT", [D, S], F32, kind="ExternalOutput").ap()

        total_words = 53200
        self.pool = nc.alloc_sbuf_tensor("pool", [128, total_words], F32)
        self.off = 0

        def alloc(words):
            o = self.off
            self.off += (words + 7) // 8 * 8
            assert self.off <= total_words, (self.off, total_words)
            return o

        def view(o, words, dt=F32):
            v = self.pool[:, o:o + words]
            if dt != F32:
                v = v.bitcast(dt)
            return v

        self.view = view
        o_x = alloc(8 * S)
        self.XT = view(o_x, 8 * S).rearrange("p (c t) -> p c t", c=8)
        self.COLS = view(alloc(320), 320)
        self.GAINC = view(alloc(128), 128)
        self.B1C = view(alloc(128), 128)
        self.B2C = view(alloc(32), 32)
        self.BADAC = view(alloc(192), 192)
        self.MODC = view(alloc(48), 48)
        self.CT = view(alloc(8), 8)
        self.CACT = view(alloc(8), 8, BF16)[:, 0:8]
        self.IDENT = view(alloc(64), 64, BF16)
        self.ONES = view(alloc(64), 64, BF16)
        self.SQ = [view(alloc(512), 512, BF16).rearrange("p (a t) -> p a t", a=2) for _ in range(2)]
        self.rstd_off = self.off
        self.RSTD = [view(alloc(512), 512) for _ in range(2)]
        self.XN = [view(alloc(512), 512) for _ in range(2)]
        self.wada_off = self.off
        self.WADA = [view(alloc(1024), 1024, BF16).rearrange("p (k n) -> p k n", k=8) for _ in range(2)]
        self.DUMMY = view(alloc(8), 8)
        sc.dummy = self.DUMMY
        self.EPSC = view(alloc(8), 8)
        self.phase_base = self.off

        self.PS = [nc.alloc_psum_tensor(f"psb{i}", [128, 512], F32) for i in range(8)]

        self.preamble()
        done_mod = set()
        self.side = []
        for pi, (kind, l) in enumerate(self.phases):
            if l not in done_mod:
                self.mod_layer(l)
                done_mod.add(l)
            sc.barrier()
            if kind == "attn":
                self.attn_phase(l)
            else:
                nxt = [ll for (_, ll) in self.phases[pi + 1:] if ll not in done_mod]
                if nxt:
                    self.side = self.mod_jobs(nxt[0])
                    done_mod.add(nxt[0])
                self.ffn_phase(l)
                self.run_side(100)
        sc.barrier()
        self.epilogue()

        class _SemCtx:
            pass
        semlist = []

        def semctx(name):
            cm = nc.semaphore(name)
            h = cm.__enter__()
            semlist.append(cm)
            return h

        sc.finalize(nc, semctx)
        with nc.Block() as block:
            @block.tensor
            def _(e):
                sc.emit("pe", e)

            @block.scalar
            def _(e):
                sc.emit("act", e)

            @block.vector
            def _(e):
                sc.emit("dve", e)

            @block.gpsimd
            def _(e):
                sc.emit("pool", e)

            @block.sync
            def _(e):
                sc.emit("sp", e)

    def dump(self, name, ap, reads):
        if not getattr(self, "debug", False):
            return
        shp = list(ap.shape)
        d = self.nc.dram_tensor("dbg_" + name, shp, ap.dtype, kind="ExternalOutput").ap()
        self.sc.add("sp", (lambda e: e.dma_start(out=d, in_=ap)), reads=list(reads), writes=["dbg_" + name], dma="dbg_" + name)
        self.dbg_names.append("dbg_" + name)

    def psb(self, i, dt=F32):
        v = self.PS[i][:, :]
        if dt != F32:
            v = v.bitcast(dt)
        return v

    def preamble(self):
        sc = self.sc
        XT = self.XT
        xs = self.d_xT.rearrange("(c p) t -> p c t", p=128)
        for c in range(8):
            sc.add("sp", (lambda e, c=c: e.dma_start(out=XT[:, c, :], in_=xs[:, c, :])),
                   writes=[f"xTc{c}"], dma=f"xin{c}")
        small = [(self.GAINC, self.d_gainc, "gainc"), (self.B1C, self.d_b1c, "b1c"), (self.B2C, self.d_b2c, "b2c"),
                 (self.BADAC, self.d_badac, "badac"), (self.CT, self.d_cT, "ct")]
        for (dst, src, nm) in small:
            sc.add("sp", (lambda e, dst=dst, src=src: e.dma_start(out=dst, in_=src[:, :])),
                   writes=[nm], dma="c_" + nm)
        sc.add("pool", (lambda e: e.dma_start(out=self.IDENT, in_=self.d_ident[:, :])), writes=["ident"], dma="c_ident")
        sc.add("dve", (lambda e: e.memset(self.ONES, 1.0)), writes=["ones"])
        sc.add("dve", (lambda e: e.memset(self.EPSC, float(D * EPS))), writes=["epsc"])
        sc.add("act", (lambda e: e.activation(out=self.CACT, in_=self.CT, func=AF.Silu)), reads=["ct"], writes=["cact"])
        sc.marker(reads=[f"xTc{c}" for c in range(8)], writes=[f"xT{tb}" for tb in range(8)] + ["rgnA_ok", "rgnB_ok"])

    def epilogue(self):
        sc = self.sc
        XT = self.XT
        od = self.d_out.rearrange("(c p) t -> p c t", p=128)
        for c in range(8):
            sc.add("sp", (lambda e, c=c: e.dma_start(out=od[:, c, :], in_=XT[:, c, :])),
                   reads=[f"xT{tb}" for tb in range(8)], writes=[f"out{c}"], dma=f"xout{c}")
        sc.add("sp", (lambda e: e.nop()), reads=[f"out{c}" for c in range(8)])

    def mod_layer(self, l):
        for j in self.mod_jobs(l):
            j()

    def run_side(self, n=1):
        for _ in range(n):
            if self.side:
                self.side.pop(0)()

    def mod_jobs(self, l):
        jobs = []
        for pc in range(24):
            jobs.append(lambda pc=pc: self.mod_piece(l, pc))
        jobs.append(lambda: self.mod_finish(l))
        return jobs

    def mod_piece(self, l, pc):
        sc = self.sc
        ps = self.psb(7)
        if True:
            buf = self.WADA[pc % 2]
            bn = f"wada{pc % 2}"
            src = self.d_wada[l, pc]
            sc.add("pool", (lambda e, buf=buf, src=src: e.dma_start(out=buf, in_=src)), reads=["wada_ok"], writes=[bn], dma=bn)

            def mm(e, buf=buf, pc=pc):
                last = None
                for j in range(2):
                    col = pc * 2 + j
                    for kc in range(8):
                        last = e.matmul(ps[:, col:col + 1], buf[:, kc, j * 128:(j + 1) * 128],
                                        self.CACT[:, kc:kc + 1], start=(kc == 0), stop=(kc == 7))
                return last
            sc.add("pe", mm, reads=[bn, "cact"], writes=["ps7"])
    def mod_finish(self, l):
        sc = self.sc
        ps = self.psb(7)
        MODC = self.MODC
        sc.add("dve", (lambda e: e.tensor_tensor(out=MODC, in0=ps[:, 0:48], in1=self.BADAC[:, l * 48:(l + 1) * 48], op=ALU.add)),
               reads=["ps7", "badac"], writes=["modc"])
        C = self.COLS
        b = l * 64
        G = self.GAINC
        g0 = (l * 4) * 8

        def cols(e):
            e.scalar_tensor_tensor(out=C[:, b + 0:b + 8], in0=MODC[:, 8:16], scalar=1.0, in1=G[:, g0 + 0:g0 + 8], op0=ALU.add, op1=ALU.mult)
            e.tensor_copy(out=C[:, b + 8:b + 16], in_=MODC[:, 0:8])
            e.tensor_tensor(out=C[:, b + 16:b + 24], in0=MODC[:, 16:24], in1=G[:, g0 + 8:g0 + 16], op=ALU.mult)
            e.scalar_tensor_tensor(out=C[:, b + 24:b + 32], in0=MODC[:, 32:40], scalar=1.0, in1=G[:, g0 + 16:g0 + 24], op0=ALU.add, op1=ALU.mult)
            e.tensor_copy(out=C[:, b + 32:b + 40], in_=MODC[:, 24:32])
            return e.tensor_tensor(out=C[:, b + 40:b + 48], in0=MODC[:, 40:48], in1=G[:, g0 + 24:g0 + 32], op=ALU.mult)
        sc.add("dve", cols, reads=["modc", "gainc"], writes=[f"colsraw{l}"])

        def cols2(e):
            e.tensor_scalar(out=C[:, b + 0:b + 8], in0=C[:, b + 0:b + 8], scalar1=32.0, scalar2=None, op0=ALU.mult)
            e.tensor_scalar(out=C[:, b + 16:b + 32], in0=C[:, b + 16:b + 32], scalar1=32.0, scalar2=None, op0=ALU.mult)
            return e.tensor_scalar(out=C[:, b + 40:b + 48], in0=C[:, b + 40:b + 48], scalar1=32.0, scalar2=None, op0=ALU.mult)
        sc.add("dve", cols2, reads=[f"colsraw{l}"], writes=[f"cols{l}"])
        self.dump(f"cols{l}", C[:, b:b + 48], [f"cols{l}"])
        self.dump(f"modc{l}", MODC, [f"cols{l}"])

    def rmsnorm_in(self, l, sub, t0, n, HT, ht_res, psbank, extra_reads=(), sq_names=None):
        sc = self.sc
        XT = self.XT
        tbs = [f"xT{tb}" for tb in range(t0 // 256, (t0 + n) // 256)]
        ps = self.psb(psbank)
        cb = l * 64 + (0 if sub == 0 else 24)
        C = self.COLS
        for cp in range(4):
            sq = self.SQ[cp % 2]
            sqn = f"sq{cp % 2}"
            sqw = [sqn] if sq_names is None else sq_names[cp % 2]
            sc.add("act", (lambda e, cp=cp, sq=sq: e.activation(out=sq[:, :, 0:n], in_=XT[:, 2 * cp:2 * cp + 2, t0:t0 + n], func=AF.Square)),
                   reads=tbs, writes=sqw)

            def mm(e, cp=cp, sq=sq):
                last = None
                for j in range(2):
                    c = 2 * cp + j
                    last = e.matmul(ps[:, 0:n], self.ONES, sq[:, j, 0:n], start=(c == 0), stop=(c == 7))
                return last
            sc.add("pe", mm, reads=[sqn, "ones"], writes=[f"ps{psbank}"])
        rs = self.RSTD[0]
        sc.add("act", (lambda e: e.activation(out=rs[:, 0:n], in_=ps[:, 0:n], func=AF.Ln, bias=self.EPSC[:, 0:1], scale=1.0)),
               reads=[f"ps{psbank}", "epsc"], writes=["rstd0p", "rstd0"])
        sc.add("act", (lambda e: e.activation(out=rs[:, 0:n], in_=rs[:, 0:n], func=AF.Exp, scale=-0.5)),
               reads=["rstd0p"], writes=["rstd0", "rstd0p"])
        for c in range(8):
            xn = self.XN[c % 2]
            xnn = f"xn{c % 2}"
            sc.add("dve", (lambda e, c=c, xn=xn: e.tensor_tensor(out=xn[:, 0:n], in0=XT[:, c, t0:t0 + n], in1=rs[:, 0:n], op=ALU.mult)),
                   reads=tbs + ["rstd0"], writes=[xnn])
            sc.add("act", (lambda e, c=c, xn=xn: e.activation(out=HT[:, c, 0:n], in_=xn[:, 0:n], func=AF.Identity,
                                                               scale=C[:, cb + c:cb + c + 1], bias=C[:, cb + 8 + c:cb + 9 + c])),
                   reads=[xnn, f"cols{l}"] + list(extra_reads), writes=[ht_res])

    def resid_update(self, l, sub, t0, n, Y, y_res, ssbank):
        sc = self.sc
        XT = self.XT
        tbs = [f"xT{tb}" for tb in range(t0 // 256, (t0 + n) // 256)]
        ps = self.psb(ssbank)
        C = self.COLS
        cb = l * 64 + (16 if sub == 0 else 40)
        rs = self.RSTD[1]
        sc.add("act", (lambda e: e.activation(out=rs[:, 0:n], in_=ps[:, 0:n], func=AF.Ln, bias=self.EPSC[:, 0:1], scale=1.0)),
               reads=[f"ps{ssbank}", "epsc"], writes=["rstd1p", "rstd1"])
        sc.add("act", (lambda e: e.activation(out=rs[:, 0:n], in_=rs[:, 0:n], func=AF.Exp, scale=-0.5)),
               reads=["rstd1p"], writes=["rstd1", "rstd1p"])
        if t0 == 0 and sub == 1:
            self.dump("rstd1", rs, ["rstd1"])
            self.dump("ysb", Y, [y_res(c) for c in range(8)])
        for c in range(8):
            sc.add("dve", (lambda e, c=c: e.scalar_tensor_tensor(out=Y[:, c, 0:n], in0=Y[:, c, 0:n], scalar=C[:, cb + c:cb + c + 1], in1=rs[:, 0:n],
                                                                 op0=ALU.mult, op1=ALU.mult)),
                   reads=[y_res(c), "rstd1", f"cols{l}"], writes=[y_res(c)])
            sc.add("dve", (lambda e, c=c: e.tensor_tensor(out=XT[:, c, t0:t0 + n], in0=XT[:, c, t0:t0 + n], in1=Y[:, c, 0:n], op=ALU.add)),
                   reads=[y_res(c)] + tbs, writes=tbs)

    def ffn_phase(self, l):
        sc = self.sc
        view = self.view
        base = self.phase_base
        o = base
        HID = view(o, 32 * 1024 // 2, BF16).rearrange("p (m t) -> p m t", m=32); o += 16384
        rgn = o; o += 8192
        HT = view(rgn, 4096, BF16).rearrange("p (c t) -> p c t", c=8)
        W1B = [view(rgn + 4096 + i * 1024, 1024, BF16).rearrange("p (k n) -> p k n", k=8) for i in range(3)]
        RL = [view(rgn + 4096 + 3072 + i * 512, 512) for i in range(2)]
        YSB = view(rgn, 8192).rearrange("p (c t) -> p c t", c=8)
        W2B = [view(o + i * 1024, 1024, BF16).rearrange("p (k n) -> p k n", k=16) for i in range(3)]; o += 3072
        assert o <= 53200, o
        B1C, B2C = self.B1C, self.B2C
        w1cnt = 0
        w2cnt = 0
        for H in range(2):
            T0 = H * 1024
            region_users = ["rgnA_ok"]
            for tg in range(2):
                self.rmsnorm_in(l, 1, T0 + tg * 512, 512, HT[:, :, tg * 512:(tg + 1) * 512], f"ht{tg}", 6,
                                extra_reads=region_users)
            if H == 0:
                self.dump("ht", HT, ["ht0", "ht1"])
            psi = 0
            for g in range(16):
                self.run_side(1)
                wb = W1B[w1cnt % 3]; wn = f"w1b{w1cnt % 3}"; w1cnt += 1
                src = self.d_w1r[l, g]
                sc.add("pool", (lambda e, wb=wb, src=src: e.dma_start(out=wb, in_=src)), reads=region_users, writes=[wn], dma=wn)
                for mm_ in range(2):
                    m = 2 * g + mm_
                    for tg in range(2):
                        bank = psi % 4; psi += 1
                        ps = self.psb(bank)

                        def mm(e, wb=wb, mm_=mm_, tg=tg, ps=ps):
                            last = None
                            for c in range(8):
                                last = e.matmul(ps, wb[:, c, mm_ * 128:(mm_ + 1) * 128], HT[:, c, tg * 512:(tg + 1) * 512],
                                                start=(c == 0), stop=(c == 7))
                            return last
                        sc.add("pe", mm, reads=[wn, f"ht{tg}"], writes=[f"ps{bank}"])
                        rl = RL[psi % 2]; rln = f"rl{psi % 2}"
                        sc.add("act", (lambda e, rl=rl, ps=ps, m=m: e.activation(out=rl, in_=ps, func=AF.Relu,
                                                                                 bias=B1C[:, l * 32 + m:l * 32 + m + 1])),
                               reads=[f"ps{bank}", "b1c"] + region_users, writes=[rln])
                        sc.add("dve", (lambda e, rl=rl, m=m, tg=tg: e.tensor_tensor(out=HID[:, m, tg * 512:(tg + 1) * 512], in0=rl, in1=rl, op=ALU.mult)),
                               reads=[rln], writes=[f"hid{m}_{tg}"])
            if H == 0:
                self.dump("hid", HID, [f"hid{m}_{tg}" for m in range(32) for tg in range(2)])
            sc.marker(writes=["ht0", "ht1", "w1b0", "w1b1", "w1b2", "rl0", "rl1", "rgnB_ok"])
            ht_users = ["rgnB_ok"]
            for o_ in range(8):
                wbs = []
                for kh in range(2):
                    wb = W2B[w2cnt % 3]; wn = f"w2b{w2cnt % 3}"; w2cnt += 1
                    src = self.d_w2r[l, o_, kh]
                    sc.add("pool", (lambda e, wb=wb, src=src: e.dma_start(out=wb, in_=src)), writes=[wn], dma=wn)
                    wbs.append((wb, wn))
                banks = [(o_ % 2) * 2, (o_ % 2) * 2 + 1]
                for kh in range(2):
                    wb, wn = wbs[kh]
                    for tg in range(2):
                        ps = self.psb(banks[tg])

                        def mm(e, wb=wb, kh=kh, tg=tg, ps=ps):
                            last = None
                            for kk in range(16):
                                m = kh * 16 + kk
                                last = e.matmul(ps, wb[:, kk, :], HID[:, m, tg * 512:(tg + 1) * 512],
                                                start=(m == 0), stop=(m == 31))
                            return last
                        sc.add("pe", mm, reads=[wn] + [f"hid{kh * 16 + kk}_{tg}" for kk in range(16)], writes=[f"ps{banks[tg]}"])
                for tg in range(2):
                    ps = self.psb(banks[tg])
                    ysl = YSB[:, o_, tg * 512:(tg + 1) * 512]
                    sc.add("act", (lambda e, ps=ps, ysl=ysl, o_=o_: e.activation(out=ysl, in_=ps, func=AF.Identity,
                                                                                  bias=B2C[:, l * 8 + o_:l * 8 + o_ + 1])),
                           reads=[f"ps{banks[tg]}", "b2c"] + ht_users, writes=[f"ysb{o_}t{tg}"])
                    sq = self.SQ[tg][:, 0, :]
                    sc.add("dve", (lambda e, ysl=ysl, sq=sq: e.tensor_tensor(out=sq, in0=ysl, in1=ysl, op=ALU.mult)),
                           reads=[f"ysb{o_}t{tg}"], writes=[f"sq{tg}"])
                    ssb = 4 + tg
                    sc.add("pe", (lambda e, sq=sq, ssb=ssb, o_=o_: e.matmul(self.psb(ssb), self.ONES, sq, start=(o_ == 0), stop=(o_ == 7))),
                           reads=[f"sq{tg}", "ones"], writes=[f"ps{ssb}"])
            for tg in range(2):
                self.resid_update(l, 1, T0 + tg * 512, 512, YSB[:, :, tg * 512:(tg + 1) * 512],
                                  (lambda c, tg=tg: f"ysb{c}t{tg}"), 4 + tg)
            sc.marker(writes=[f"ysb{c}t{tg}" for c in range(8) for tg in range(2)] + ["rgnA_ok"])

    def attn_phase(self, l):
        sc = self.sc
        view = self.view
        XT = self.XT
        o = self.phase_base
        KT = view(o, 5120, BF16).rearrange("p (c t) -> p c t", c=5); o += 5120
        VA = view(o, 5200, BF16).rearrange("p (t h d) -> p t h d", t=16, h=10); o += 5200
        WIN = view(o, 9216, BF16).rearrange("p (k n) -> p k n", k=8); o += 9216
        WOUT = view(o, 4096, BF16).rearrange("p (k n) -> p k n", k=8); o += 4096
        DT = view(o, 2048, BF16).rearrange("p (h t q) -> p h t q", h=16, t=2); o += 2048
        rA = o; o += 1024
        rC = o; o += 1024
        rB = o; o += 1024
        HTG = view(rA, 1024, BF16).rearrange("p (c t) -> p c t", c=8)
        OG = view(rA, 1024, BF16).rearrange("p (q f) -> p q f", q=2)
        QTG = view(rC, 1024, BF16).rearrange("p (c t) -> p c t", c=8)
        QSQ = view(rB, 1024, BF16).rearrange("p (c t) -> p c t", c=8)
        OTG = view(rB, 1024, BF16).rearrange("p (c t) -> p c t", c=8)
        YSBA = view(rA, 2048).rearrange("p (c t) -> p c t", c=8)
        AUGT1 = [view(o + i * 128, 128, BF16) for i in range(3)]; o += 384
        IND = view(o, 512, BF16).rearrange("p (j k) -> p j k", j=8); o += 512
        HIND = view(o, 8, BF16)[:, 0:2]; o += 8
        KMEANT = view(o, 32, BF16).rearrange("p (c r j) -> p c r j", c=4, r=2); o += 32
        KSUM = view(o, 8, F32); o += 8
        KMAX2 = view(o, 8, F32); o += 8
        KMXG = view(o, 8, F32); o += 8
        KM16 = view(o, 16, F32); o += 16
        QMXG = view(o, 8, F32); o += 8
        NB = view(o, 16, F32); o += 16
        FB = view(o, 16, F32); o += 16
        QM16 = view(o, 16, F32); o += 16
        SINKS = view(o, 8, F32); o += 8
        BM8 = view(o, 16, F32); o += 16
        RB = self.XN[0].rearrange("p (h b) -> p h b", h=16)
        B31 = view(o, 16, F32); o += 16
        KSQ = view(rB, 640, BF16).rearrange("p (c t) -> p c t", c=5)
        RDEN = view(o, 8, F32); o += 8
        assert o <= 53200, o
        wsc = self.wada_off
        CMP = view(wsc, 1024).rearrange("p (a j k) -> p a j k", a=16, j=8)
        AUGB = view(wsc + 1024, 128, BF16).rearrange("p (q s j) -> p q s j", q=2, s=16)
        BMASK = view(wsc + 1600, 192).rearrange("p (k b j) -> p k b j", k=3, b=8)
        GM = view(wsc + 1792, 128).rearrange("p (a j) -> p a j", a=16)
        SEL = view(wsc + 1920, 128).rearrange("p (a j) -> p a j", a=16)
        rs_off = self.rstd_off
        SELF = view(rs_off + 256, 256).rearrange("p (q s j) -> p q s j", q=2, s=16)
        SH8 = view(rs_off + 512 + 256, 32).rearrange("p (q s) -> p q s", q=2)
        SINKT = view(rs_off + 512 + 288, 16).rearrange("p (q s) -> p q s", q=2)
        SQRT_T = view(rs_off + 512 + 304, 32).rearrange("p (q s) -> p q s", q=2)
        TMPS = [self.XN[0], self.XN[1]]
        PTS = [self.SQ[0].rearrange("p a t -> p (a t)")[:, 0:512], self.SQ[0].rearrange("p a t -> p (a t)")[:, 512:1024],
               self.SQ[1].rearrange("p a t -> p (a t)")[:, 0:512], self.SQ[1].rearrange("p a t -> p (a t)")[:, 512:1024]]
        PTN = ["sq0", "sq0b", "sq1", "sq1b"]
        IDENT = self.IDENT.rearrange("p (a b) -> p a b", a=1)[:, 0, :]

        sc.marker(writes=["wada0", "wada1", "rstd0", "rstd1", "rstd0p", "rstd1p", "wadafree"])
        sc.add("pool", (lambda e: e.dma_start(out=WIN, in_=self.d_winr[l])), writes=["win"], dma="win")
        sc.add("pool", (lambda e: e.dma_start(out=DT.rearrange("p h t q -> p (h t q)"), in_=self.d_dtile[:, :])), writes=["dt"], dma="dt")
        sc.add("pool", (lambda e: e.dma_start(out=IND.rearrange("p j k -> p (j k)")[0:72, :], in_=self.d_indall[:, :])), writes=["ind"], dma="ind")
        sc.add("pool", (lambda e: e.dma_start(out=HIND, in_=self.d_hind[:, :])), writes=["hind"], dma="hind")
        sc.add("sp", (lambda e: e.dma_start(out=BMASK.rearrange("p k b j -> p (k b j)"), in_=self.d_bmask[:, :])), reads=["wadafree"], writes=["bmask"], dma="bmask")
        sc.add("sp", (lambda e: e.dma_start(out=SINKS, in_=self.d_sinks[l:l + 1, :].partition_broadcast(128))), writes=["sinks"], dma="sinks")
        sc.add("sp", (lambda e: e.dma_start(out=RB.rearrange("p h b -> p (h b)"), in_=self.d_rbT[0:1, :].partition_broadcast(128))), writes=["xn0"], dma="rb")
        sc.add("pool", (lambda e: e.dma_start(out=WOUT, in_=self.d_woutr[l])), writes=["wout"], dma="wout")

        def init1(e):
            e.memset(KMEANT, 0.0)
            e.memset(KMAX2, 0.0)
            e.memset(VA[:, :, :, 64:65], 1.0)
            e.memset(AUGB, 0.0)
            e.memset(SELF, 1.0)
            e.tensor_reduce(out=BM8, in_=RB, axis=AX.X, op=ALU.max)
            e.tensor_copy(out=B31, in_=RB[:, :, 31])
            return e.memset(KM16, 0.0)
        sc.add("dve", init1, reads=["xn0", "wadafree"], writes=["kmeant", "kmax2", "va_ones", "augb_s", "augb_m", "selfm", "bm8raw", "b31", "km16"])

        sc.add("dve", (lambda e: e.tensor_tensor(out=BM8[:, 8:16], in0=BM8[:, 8:16], in1=SINKS, op=ALU.max)),
               reads=["bm8raw", "sinks"], writes=["bm8raw"])
        sc.add("dve", (lambda e: e.tensor_scalar(out=BM8, in0=BM8, scalar1=8.0, scalar2=None, op0=ALU.mult)),
               reads=["bm8raw"], writes=["bm8", "bm8raw"])

        QCOL, KCOL, VCOL, QBCOL, KBCOL, VBCOL = 0, 512, 1024, 1536, 2048, 2176
        sbank = [0]
        ptc = [0]

        def group(g):
            t0 = g * 256
            b = g
            xres = [f"xT{g}"]
            self.rmsnorm_in(l, 0, t0, 256, HTG, "rA", 7, sq_names=(["sq0", "sq0b"], ["sq1", "sq1b"]))
            pbank = [0]

            def nextbank():
                bk = 4 + pbank[0] % 4
                pbank[0] += 1
                return bk
            for ci in range(8):
                col = QCOL + ci * 128 if ci < 4 else QBCOL + (ci - 4) * 128
                bk = nextbank()
                ps = self.psb(bk)

                def mm(e, col=col, ps=ps):
                    last = None
                    for kc in range(8):
                        last = e.matmul(ps[:, 0:256], WIN[:, kc, col:col + 128], HTG[:, kc, :], start=(kc == 0), stop=(kc == 7))
                    return last
                sc.add("pe", mm, reads=["win", "rA"], writes=[f"ps{bk}"])
                sc.add("dve", (lambda e, ci=ci, ps=ps: e.tensor_copy(out=QTG[:, ci, :], in_=ps[:, 0:256])),
                       reads=[f"ps{bk}"], writes=["rC"])
            sc.add("dve", (lambda e: e.memset(KSUM, 0.0)), writes=[f"ksum{c}" for c in range(4)])
            for ci in range(5):
                col = KCOL + ci * 128 if ci < 4 else KBCOL
                bk = nextbank()
                ps = self.psb(bk)

                def mm(e, col=col, ps=ps):
                    last = None
                    for kc in range(8):
                        last = e.matmul(ps[:, 0:256], WIN[:, kc, col:col + 128], HTG[:, kc, :], start=(kc == 0), stop=(kc == 7))
                    return last
                sc.add("pe", mm, reads=["win", "rA"], writes=[f"ps{bk}"])
                if ci < 4:
                    sc.add("act", (lambda e, ci=ci, ps=ps: e.activation(out=KT[:, ci, t0:t0 + 256], in_=ps[:, 0:256], func=AF.Copy,
                                                                           accum_out=KSUM[:, ci:ci + 1])),
                           reads=[f"ps{bk}"], writes=[f"kt{ci}_{g}", f"ksum{ci}"])
                else:
                    sc.add("act", (lambda e, ci=ci, ps=ps: e.activation(out=KT[:, ci, t0:t0 + 256], in_=ps[:, 0:256], func=AF.Copy)),
                           reads=[f"ps{bk}"], writes=[f"kt{ci}_{g}"])
            for qt in range(2):
                tile_i = g * 2 + qt
                bk = nextbank()
                ps = self.psb(bk)

                def mmv(e, qt=qt, ps=ps):
                    last = None
                    for kc in range(8):
                        last = e.matmul(ps[:, 0:512], HTG[:, kc, qt * 128:(qt + 1) * 128], WIN[:, kc, VCOL:VCOL + 512], start=(kc == 0), stop=(kc == 7))
                    return last
                sc.add("pe", mmv, reads=["win", "rA"], writes=[f"ps{bk}"])
                sc.add("act", (lambda e, tile_i=tile_i, ps=ps: e.activation(out=VA[:, tile_i, 0:8, 0:64],
                                                                               in_=ps[:, 0:512].rearrange("p (h d) -> p h d", h=8), func=AF.Copy)),
                       reads=[f"ps{bk}", "va_ones"], writes=[f"va{g}_{qt}a"])
                bk2 = nextbank()
                ps2 = self.psb(bk2)

                def mmv2(e, qt=qt, ps2=ps2):
                    last = None
                    for kc in range(8):
                        last = e.matmul(ps2[:, 0:128], HTG[:, kc, qt * 128:(qt + 1) * 128], WIN[:, kc, VBCOL:VBCOL + 128], start=(kc == 0), stop=(kc == 7))
                    return last
                sc.add("pe", mmv2, reads=["win", "rA"], writes=[f"ps{bk2}"])
                sc.add("dve", (lambda e, tile_i=tile_i, ps2=ps2: e.tensor_copy(out=VA[:, tile_i, 8:10, 0:64],
                                                                                 in_=ps2[:, 0:128].rearrange("p (h d) -> p h d", h=2))),
                       reads=[f"ps{bk2}", "va_ones"], writes=[f"va{g}_{qt}b"])
            if getattr(self, 'stop', 99) <= 2:
                return
            def kmw(e, b=b):
                e.tensor_scalar(out=KMEANT[0:64, :, 0, b], in0=KSUM[0:64, 0:4], scalar1=1.0 / 256.0, scalar2=None, op0=ALU.mult)
                return e.tensor_scalar(out=KMEANT[64:128, :, 1, b], in0=KSUM[64:128, 0:4], scalar1=1.0 / 256.0, scalar2=None, op0=ALU.mult)
            sc.add("dve", kmw, reads=[f"ksum{c}" for c in range(4)], writes=["kmeant"])
            sc.add("dve", (lambda e: e.tensor_tensor(out=KSQ, in0=KT[:, 0:5, t0:t0 + 256], in1=KT[:, 0:5, t0:t0 + 256], op=ALU.mult)),
                   reads=[f"kt{c}_{g}" for c in range(5)], writes=["rB", "rBb"])
            if getattr(self, 'stop', 99) <= 2.2:
                return
            for bi, cs in enumerate([(0, 1), (2, 3), (4,)]):
                bk = 4 + bi
                ps = self.psb(bk).rearrange("p (a t) -> p a t", a=2)

                def mmk(e, cs=cs, ps=ps):
                    last = None
                    for a, c in enumerate(cs):
                        last = e.matmul(ps[:, a, :], self.ONES, KSQ[:, c, :], start=True, stop=True)
                    return last
                sc.add("pe", mmk, reads=["rB", "ones"], writes=[f"ps{bk}"])
                sc.add("dve", (lambda e, cs=cs, ps=ps: e.tensor_reduce(out=KMXG[:, cs[0]:cs[0] + len(cs)], in_=ps[:, 0:len(cs), :], axis=AX.X, op=ALU.max)),
                       reads=[f"ps{bk}"], writes=["kmxg"])

            if getattr(self, 'stop', 99) <= 2.4:
                return
            sc.add("dve", (lambda e: e.tensor_tensor(out=KMAX2[:, 0:5], in0=KMAX2[:, 0:5], in1=KMXG[:, 0:5], op=ALU.max)),
                   reads=["kmxg", "kmax2"], writes=["kmax2"])

            def kmax(e):
                e.tensor_copy(out=KM16[:, 0:8].rearrange("p (c r) -> p c r", r=2), in_=KMAX2[:, 0:4].unsqueeze(2).to_broadcast([128, 4, 2]))
                return e.tensor_copy(out=KM16[:, 8:16], in_=KMAX2[:, 4:5].to_broadcast([128, 8]))
            sc.add("dve", kmax, reads=["kmax2"], writes=["km16"])
            if getattr(self, 'stop', 99) <= 2.6:
                return
            sc.add("dve", (lambda e: e.tensor_tensor(out=QSQ, in0=QTG, in1=QTG, op=ALU.mult)), reads=["rC"], writes=["rB", "rBb"])
            ps7 = self.psb(7)
            GATE = ps7[:, 0:128].rearrange("p (a j) -> p a j", a=16)

            for bi in range(4):
                psq = self.psb(bi).rearrange("p (a t) -> p a t", a=2)

                def mmq(e, bi=bi, psq=psq):
                    last = None
                    for a in range(2):
                        last = e.matmul(psq[:, a, :], self.ONES, QSQ[:, 2 * bi + a, :], start=True, stop=True)
                    return last
                sc.add("pe", mmq, reads=["rB", "ones"], writes=[f"ps{bi}"])
                sc.add("dve", (lambda e, bi=bi, psq=psq: e.tensor_reduce(out=QMXG[:, 2 * bi:2 * bi + 2], in_=psq, axis=AX.X, op=ALU.max)),
                       reads=[f"ps{bi}"], writes=["qmxg"])
            sc.add("dve", (lambda e: e.tensor_copy(out=QM16.rearrange("p (c r) -> p c r", r=2), in_=QMXG.unsqueeze(2).to_broadcast([128, 8, 2]))),
                   reads=["qmxg"], writes=["qm16"])

            def mmg(e):
                last = None
                for qt in range(2):
                    for c in range(4):
                        last = e.matmul(ps7[:, (qt * 8 + 2 * c) * 8:(qt * 8 + 2 * c + 2) * 8], QTG[:, c, qt * 128:(qt + 1) * 128],
                                        KMEANT[:, c, :, :].rearrange("p r j -> p (r j)"), start=True, stop=True)
                return last
            if b >= 4:
                sc.add("pe", mmg, reads=["rC", "kmeant"], writes=["ps7"])
            if getattr(self, 'stop', 99) <= 3:
                return
            NEGM = BMASK[:, 0, b, :]
            ELIG = BMASK[:, 1, b, :]
            OWN = BMASK[:, 2, b, :]

            sc.add("dve", (lambda e: e.tensor_tensor(out=SQRT_T, in0=QM16.unsqueeze(1).to_broadcast([128, 2, 16]),
                                                     in1=KM16.unsqueeze(1).to_broadcast([128, 2, 16]), op=ALU.mult)),
                   reads=["qm16", "km16"], writes=["sqrt_t"])
            sc.add("act", (lambda e: e.activation(out=SQRT_T, in_=SQRT_T, func=AF.Ln, bias=self.EPSC[:, 0:1], scale=1.0)),
                   reads=["sqrt_t", "epsc"], writes=["sqrt_t"])
            sc.add("act", (lambda e: e.activation(out=SQRT_T, in_=SQRT_T, func=AF.Exp, scale=0.5)),
                   reads=["sqrt_t"], writes=["sqrt_t"])
            AUGBv = AUGB
            sc.add("dve", (lambda e: e.tensor_tensor(out=SH8, in0=SQRT_T, in1=BM8.unsqueeze(1).to_broadcast([128, 2, 16]), op=ALU.add)),
                   reads=["sqrt_t", "bm8"], writes=["sh8"])
            sc.add("dve", (lambda e: e.tensor_scalar(out=NB, in0=SH8[:, 0, :], scalar1=-0.125, scalar2=None, op0=ALU.mult)),
                   reads=["sh8"], writes=["nb"])
            sc.add("dve", (lambda e: e.tensor_tensor(out=FB, in0=NB, in1=B31, op=ALU.add)), reads=["nb", "b31"], writes=["fb"])
            sc.add("dve", (lambda e: e.tensor_tensor(out=SINKT, in0=SINKS.unsqueeze(1).to_broadcast([128, 2, 8]),
                                                     in1=NB[:, 8:16].unsqueeze(1).to_broadcast([128, 2, 8]), op=ALU.add)),
                   reads=["nb", "sinks"], writes=["sinkt"])
            sc.add("act", (lambda e: e.activation(out=SINKT, in_=SINKT, func=AF.Exp)), reads=["sinkt"], writes=["sinkt"])
            SELM = SELF[:, :, 0:8, :]
            sel_jobs = [
                lambda: sc.add("dve", (lambda e: e.tensor_tensor(out=GM, in0=GATE, in1=NEGM.unsqueeze(1).to_broadcast([128, 16, 8]), op=ALU.add)),
                               reads=["ps7", "bmask"], writes=["gm"]),
                lambda: sc.add("dve", (lambda e: e.tensor_tensor(out=CMP, in0=GM.unsqueeze(2).to_broadcast([128, 16, 8, 8]),
                                                                 in1=GM.unsqueeze(3).to_broadcast([128, 16, 8, 8]), op=ALU.is_gt)),
                               reads=["gm"], writes=["cmp"]),
                lambda: sc.add("dve", (lambda e: e.tensor_reduce(out=SEL, in_=CMP, axis=AX.X, op=ALU.add)), reads=["cmp"], writes=["selr"]),
                lambda: sc.add("dve", (lambda e: e.scalar_tensor_tensor(out=SEL, in0=SEL, scalar=3.0, in1=ELIG.unsqueeze(1).to_broadcast([128, 16, 8]),
                                                                        op0=ALU.is_lt, op1=ALU.mult)),
                               reads=["selr", "bmask"], writes=["selr"]),
                lambda: sc.add("dve", (lambda e: e.tensor_tensor(out=SELM, in0=SEL.rearrange("p (q h) j -> p q h j", q=2),
                                                                 in1=OWN.unsqueeze(1).unsqueeze(1).to_broadcast([128, 2, 8, 8]), op=ALU.add)),
                               reads=["selr", "bmask", "selfm"], writes=["selfm"]),
                lambda: sc.add("dve", (lambda e: e.tensor_scalar(out=AUGBv[:, :, 0:8, :], in0=SELM, scalar1=BIG, scalar2=-BIG, op0=ALU.mult, op1=ALU.add)),
                               reads=["selfm"], writes=["augb_m"]),
            ]
            if b < 4:
                sel_jobs = []
            if getattr(self, 'stop', 99) <= 4:
                return
            order = list(range(8, 16)) + list(range(8))
            if sel_jobs:
                sel_jobs.pop(0)()
            for grp in (1, 0):
                pvq = []

                def flush(keep):
                    while len(pvq) > keep:
                        a_, k_ = pvq.pop(0)
                        sc.add(*a_, **k_)

                def prep(s16):
                    ps7b = self.psb(7, BF16)
                    pos_ = order.index(s16)
                    AT = AUGT1[pos_ % 3]
                    ares = "augb_s" if s16 >= 8 else "augb_m"

                    def tr(e):
                        last = None
                        for qt in range(2):
                            last = e.transpose(ps7b[0:8, qt * 128:(qt + 1) * 128], AUGB[:, qt, s16, :], IDENT)
                        return last
                    sc.add("pe", tr, reads=[ares, "ident"], writes=["ps7"])
                    sc.add("act", (lambda e: e.activation(out=AT[0:8, 0:256], in_=ps7b[0:8, 0:256], func=AF.Copy)),
                           reads=["ps7"], writes=[f"augt{pos_ % 3}"])

                def head(hs8):
                    s16 = grp * 8 + hs8
                    pos = order.index(s16)
                    if grp == 1 and sel_jobs:
                        sel_jobs.pop(0)()
                    if pos + 2 < 16 and order[pos + 2] < 8 and b >= 4:
                        while sel_jobs:
                            sel_jobs.pop(0)()
                        prep(order[pos + 2])
                    AT = AUGT1[pos % 3]
                    use_aug = (grp == 0 and b >= 4)
                    pb = 0
                    if grp == 0:
                        ck, r0, cq, vh = hs8 // 2, (hs8 % 2) * 64, hs8 // 2, hs8
                    else:
                        i_, r_ = hs8 // 2, hs8 % 2
                        ck, r0, cq, vh = 4, r_ * 64, 4 + i_, 8 + r_
                    quad, hsl = hs8 // 4, hs8 % 4
                    augres = f"augt{pos % 3}"
                    far = []
                    if grp == 0 and b >= 1:
                        for kc in range(0, 2 * b - 1):
                            far.append((kc, 0, 256))
                        far.append((2 * b - 1, 128, 128))
                    near = [(2 * b - 1, 0, 0), (2 * b, 0, 1), (2 * b, 1, 0), (2 * b + 1, 1, 1)]
                    if b == 0:
                        near = near[1:]
                    n_qt = [0, 0]
                    for (kc, q0, qn) in far:
                        for sub in range(qn // 128):
                            n_qt[(q0 + sub * 128) // 128] += 1
                    for (kc, qt, _) in near:
                        n_qt[qt] += 1
                    done_qt = [0, 0]
                    banks = []
                    cur, tot = [], 0
                    for p_ in far:
                        if tot + p_[2] > 512:
                            banks.append(cur)
                            cur, tot = [], 0
                        cur.append(p_)
                        tot += p_[2]
                    if cur:
                        banks.append(cur)
                    for pieces in banks:
                        bk = 4 + sbank[0] % 3
                        sbank[0] += 1
                        ps = self.psb(bk)
                        pti = ptc[0] % 4
                        ptc[0] += 1
                        PT = PTS[pti]
                        offs = []
                        off = 0
                        for p_ in pieces:
                            offs.append(off)
                            off += p_[2]
                        tot = off

                        def mms(e, pieces=pieces, offs=offs, ps=ps):
                            last = None
                            for (kc, q0, qn), of in zip(pieces, offs):
                                last = e.matmul(ps[:, of:of + qn], KT[r0:r0 + 64, ck, kc * 128:(kc + 1) * 128], QTG[r0:r0 + 64, cq, q0:q0 + qn],
                                                start=True, stop=not use_aug)
                                if use_aug:
                                    last = e.matmul(ps[:, of:of + qn], IND[pb:pb + 8, kc // 2, :], AT[0:8, q0:q0 + qn],
                                                    start=False, stop=True)
                            return last
                        flush(2)
                        sc.add("pe", mms, reads=[f"kt{ck}_{p_[0] // 2}" for p_ in pieces] + ["rC", "ind"] + ([augres] if use_aug else []), writes=[f"ps{bk}"])
                        sc.add("act", (lambda e, PT=PT, ps=ps, tot=tot: e.activation(out=PT[:, 0:tot], in_=ps[:, 0:tot], func=AF.Exp,
                                                                                       scale=0.125, bias=FB[:, s16:s16 + 1])),
                               reads=[f"ps{bk}", "fb"], writes=[PTN[pti]])
                        flags = []
                        for (kc, q0, qn), of in zip(pieces, offs):
                            for sub in range(qn // 128):
                                qt = (q0 + sub * 128) // 128
                                st = done_qt[qt] == 0
                                done_qt[qt] += 1
                                sp_ = done_qt[qt] == n_qt[qt]
                                flags.append((kc, qt, of + sub * 128, st, sp_))

                        def pv(e, flags=flags, PT=PT):
                            last = None
                            for (kc, qt, of, st, sp_) in flags:
                                last = e.matmul(self.psb(qt * 2 + quad).rearrange("p (h d) -> p h d", d=65)[:, hsl, 0:65] if False else
                                                self.oacc(qt * 2 + quad)[:, hsl, :], PT[:, of:of + 128], VA[:, kc, vh, :], start=st, stop=sp_)
                            return last
                        pvq.append((("pe", pv), dict(reads=[PTN[pti]] + [f"va{p_[0] // 2}_{p_[0] % 2}{'a' if grp == 0 else 'b'}" for p_ in pieces],
                                                     writes=[f"ps{quad}", f"ps{2 + quad}"])))
                    bk = 4 + sbank[0] % 3
                    sbank[0] += 1
                    ps = self.psb(bk).rearrange("p (a t) -> p a t", a=4)
                    pti = ptc[0] % 4
                    ptc[0] += 1
                    PT = PTS[pti].rearrange("p (a t) -> p a t", a=4)
                    TMP = TMPS[pti % 2].rearrange("p (a t) -> p a t", a=4)
                    tmpn = f"xn{pti % 2}"
                    ti0 = 4 - len(near)

                    def mmn(e, near=near, ps=ps, ti0=ti0):
                        last = None
                        for k_, (kc, qt, di) in enumerate(near):
                            ti = ti0 + k_
                            ua = use_aug and (kc // 2) < b
                            last = e.matmul(ps[:, ti, :], KT[r0:r0 + 64, ck, kc * 128:(kc + 1) * 128], QTG[r0:r0 + 64, cq, qt * 128:(qt + 1) * 128],
                                            start=True, stop=not ua)
                            if ua:
                                last = e.matmul(ps[:, ti, :], IND[pb:pb + 8, kc // 2, :], AT[0:8, qt * 128:(qt + 1) * 128],
                                                start=False, stop=True)
                        return last
                    flush(2)
                    sc.add("pe", mmn, reads=[f"kt{ck}_{kc // 2}" for (kc, _, _) in near] + ["rC", "ind"] + ([augres] if use_aug else []), writes=[f"ps{bk}"])

                    def biasadd(e, ps=ps, TMP=TMP, ti0=ti0):
                        if ti0 == 0:
                            e.scalar_tensor_tensor(out=TMP[:, 0:2, :], in0=ps[:, 0:2, :], scalar=0.125, in1=DT[:, s16, 0:2, :], op0=ALU.mult, op1=ALU.add)
                        else:
                            e.scalar_tensor_tensor(out=TMP[:, 1:2, :], in0=ps[:, 1:2, :], scalar=0.125, in1=DT[:, s16, 1:2, :], op0=ALU.mult, op1=ALU.add)
                        return e.scalar_tensor_tensor(out=TMP[:, 2:4, :], in0=ps[:, 2:4, :], scalar=0.125, in1=DT[:, s16, 0:2, :], op0=ALU.mult, op1=ALU.add)
                    sc.add("dve", biasadd, reads=[f"ps{bk}", "dt"], writes=[tmpn])
                    sc.add("act", (lambda e, PT=PT, TMP=TMP, ti0=ti0: e.activation(out=PT[:, ti0:4, :], in_=TMP[:, ti0:4, :], func=AF.Exp,
                                                                                       bias=NB[:, s16:s16 + 1])),
                           reads=[tmpn, "nb"], writes=[PTN[pti]])
                    flags = []
                    for k_, (kc, qt, di) in enumerate(near):
                        st = done_qt[qt] == 0
                        done_qt[qt] += 1
                        sp_ = done_qt[qt] == n_qt[qt]
                        flags.append((kc, qt, ti0 + k_, st, sp_))

                    def pvn(e, flags=flags, PT=PT):
                        last = None
                        for (kc, qt, ti, st, sp_) in flags:
                            last = e.matmul(self.oacc(qt * 2 + quad)[:, hsl, :], PT[:, ti, :], VA[:, kc, vh, :], start=st, stop=sp_)
                        return last
                    pvq.append((("pe", pvn), dict(reads=[PTN[pti]] + [f"va{kc // 2}_{kc % 2}{'a' if grp == 0 else 'b'}" for (kc, _, _) in near],
                                                  writes=[f"ps{quad}", f"ps{2 + quad}"])))
                for hs8 in range(8):
                    head(hs8)
                flush(0)
                for qt in range(2):
                    for quad in range(2):
                        bk = qt * 2 + quad
                        oa = self.oacc(bk)
                        if grp == 0:
                            outv = OG[:, qt, quad * 256:(quad + 1) * 256].rearrange("p (h d) -> p h d", h=4)
                            inv = oa[:, :, 0:64]
                        else:
                            outv = OG[:, qt, 512:1024].rearrange("p (r i d) -> p i r d", r=2, i=4)[:, 2 * quad:2 * quad + 2, :, :]
                            inv = oa[:, :, 0:64].rearrange("p (i r) d -> p i r d", r=2)

                        def nrm(e, oa=oa, outv=outv, inv=inv, qt=qt, quad=quad, grp=grp):
                            if grp == 0:
                                e.reciprocal(out=RDEN[:, 0:4], in_=oa[:, :, 64])
                            else:
                                e.tensor_tensor(out=RDEN[:, 0:4], in0=oa[:, :, 64], in1=SINKT[:, qt, 4 * quad:4 * quad + 4], op=ALU.add)
                                e.reciprocal(out=RDEN[:, 0:4], in_=RDEN[:, 0:4])
                            if grp == 0:
                                rb_ = RDEN[:, 0:4].unsqueeze(2).to_broadcast([128, 4, 64])
                            else:
                                rb_ = RDEN[:, 0:4].rearrange("p (i r) -> p i r", r=2).unsqueeze(3).to_broadcast([128, 2, 2, 64])
                            return e.tensor_tensor(out=outv, in0=inv, in1=rb_, op=ALU.mult)
                        def nrm1(e, oa=oa, qt=qt, quad=quad, grp=grp):
                            if grp == 0:
                                return e.reciprocal(out=RDEN[:, 4 * (bk % 2):4 * (bk % 2) + 4], in_=oa[:, :, 64])
                            return e.tensor_tensor(out=RDEN[:, 4 * (bk % 2):4 * (bk % 2) + 4], in0=oa[:, :, 64], in1=SINKT[:, qt, 4 * quad:4 * quad + 4], op=ALU.add)
                        rdn = f"rden{bk % 2}"
                        RD = RDEN[:, 4 * (bk % 2):4 * (bk % 2) + 4]
                        if grp == 0:
                            sc.add("dve", (lambda e, oa=oa, RD=RD: e.reciprocal(out=RD, in_=oa[:, :, 64])), reads=[f"ps{bk}"], writes=[rdn])
                        else:
                            sc.add("dve", (lambda e, oa=oa, RD=RD, qt=qt, quad=quad: e.tensor_tensor(out=RD, in0=oa[:, :, 64], in1=SINKT[:, qt, 4 * quad:4 * quad + 4], op=ALU.add)),
                                   reads=[f"ps{bk}", "sinkt"], writes=[rdn])
                            sc.add("dve", (lambda e, RD=RD: e.reciprocal(out=RD, in_=RD)), reads=[rdn], writes=[rdn])
                        if grp == 0:
                            rb_ = RD.unsqueeze(2).to_broadcast([128, 4, 64])
                        else:
                            rb_ = RD.rearrange("p (i r) -> p i r", r=2).unsqueeze(3).to_broadcast([128, 2, 2, 64])
                        sc.add("dve", (lambda e, outv=outv, inv=inv, rb_=rb_: e.tensor_tensor(out=outv, in0=inv, in1=rb_, op=ALU.mult)),
                               reads=[f"ps{bk}", rdn], writes=["rA"])
            if getattr(self, 'stop', 99) <= 7:
                return
            for half in range(2):
                bk = 4 + half
                psT = self.psb(bk, BF16).rearrange("p (c t) -> p c t", c=4)

                def trO(e, half=half, psT=psT):
                    last = None
                    for cc in range(4):
                        c = half * 4 + cc
                        for qt in range(2):
                            last = e.transpose(psT[:, cc, qt * 128:(qt + 1) * 128], OG[:, qt, c * 128:(c + 1) * 128], IDENT)
                    return last
                sc.add("pe", trO, reads=["rA", "ident"], writes=[f"ps{bk}"])
                if half == 0:
                    sc.add("act", (lambda e, psT=psT: e.activation(out=OTG[:, 0:4, :], in_=psT, func=AF.Copy)), reads=[f"ps{bk}"], writes=["rB"])
                else:
                    sc.add("dve", (lambda e, psT=psT: e.tensor_copy(out=OTG[:, 4:8, :], in_=psT)), reads=[f"ps{bk}"], writes=["rBb"])
            if getattr(self, 'stop', 99) <= 8:
                return
            for o_ in range(8):
                bk = 4 + o_ % 3
                ps = self.psb(bk)

                def mmo(e, o_=o_, ps=ps):
                    last = None
                    for c in range(8):
                        last = e.matmul(ps[:, 0:256], WOUT[:, c, o_ * 128:(o_ + 1) * 128], OTG[:, c, :], start=(c == 0), stop=(c == 7))
                    return last
                sc.add("pe", mmo, reads=["wout", "rB", "rBb"], writes=[f"ps{bk}"])
                ysl = YSBA[:, o_, :]
                sc.add("act", (lambda e, ps=ps, ysl=ysl: e.activation(out=ysl, in_=ps[:, 0:256], func=AF.Copy)),
                       reads=[f"ps{bk}", "rA", "rC"], writes=[f"ysa{o_}"])
                sqb = PTS[o_ % 4]
                sc.add("dve", (lambda e, ysl=ysl, sqb=sqb: e.tensor_tensor(out=sqb[:, 0:256], in0=ysl, in1=ysl, op=ALU.mult)),
                       reads=[f"ysa{o_}"], writes=[PTN[o_ % 4]])
                sc.add("pe", (lambda e, sqb=sqb, o_=o_: e.matmul(self.psb(7)[:, 0:256], self.ONES, sqb[:, 0:256], start=(o_ == 0), stop=(o_ == 7))),
                       reads=[PTN[o_ % 4], "ones"], writes=["ps7"])
            if getattr(self, 'stop', 99) <= 9:
                return
            self.resid_update(l, 0, t0, 256, YSBA, (lambda c: f"ysa{c}"), 7)
            sc.marker(reads=[f"ysa{c}" for c in range(8)], writes=["rA", "rC"])

        for g in range(getattr(self, 'ngroups', 8)):
            group(g)
        sc.marker(writes=["bmask", "gm", "cmp", "selr", "augb_s", "augb_m", "selfm", "sh8", "sinkt", "sqrt_t", "nb", "fb", "wada_ok"])

    def oacc(self, bk):
        return self.PS[bk][:, 0:260].rearrange("p (h d) -> p h d", d=65)


def _host_prep(inputs):
    f = np.float32
    w_ada = np.asarray(inputs["w_ada"], f)
    w1 = np.asarray(inputs["w1"], f)
    w2 = np.asarray(inputs["w2"], f)
    w_in = np.asarray(inputs["w_in"], f)
    w_out = np.asarray(inputs["w_out"], f)
    sh = {}
    sh["wada"] = np.ascontiguousarray(w_ada.reshape(DEPTH, 8, 128, 24, 256).transpose(0, 3, 2, 1, 4))
    sh["badac"] = np.ascontiguousarray(np.asarray(inputs["b_ada"], f).reshape(DEPTH, 48, 128).transpose(2, 0, 1).reshape(128, DEPTH * 48))
    sh["gainc"] = np.ascontiguousarray(np.asarray(inputs["norm_gains"], f).reshape(DEPTH, 4, 8, 128).transpose(3, 0, 1, 2).reshape(128, 128))
    sh["b1c"] = np.ascontiguousarray(np.asarray(inputs["b1"], f).reshape(DEPTH, 32, 128).transpose(2, 0, 1).reshape(128, 128))
    sh["b2c"] = np.ascontiguousarray(np.asarray(inputs["b2"], f).reshape(DEPTH, 8, 128).transpose(2, 0, 1).reshape(128, 32))
    sh["w1r"] = np.ascontiguousarray(w1.reshape(DEPTH, 8, 128, 16, 256).transpose(0, 3, 2, 1, 4))
    sh["w2r"] = np.ascontiguousarray(w2.reshape(DEPTH, 2, 16, 128, 8, 128).transpose(0, 4, 1, 3, 2, 5))
    perm = [(k // 2) + 4 * (k % 2) for k in range(8)]
    colidx = np.arange(DIN)
    qb = colidx[1536:2048].reshape(8, 64)[perm].reshape(-1)
    colidx = np.concatenate([colidx[:1536], qb, colidx[2048:]])
    w_in_p = w_in[:, :, colidx]
    sh["winr"] = np.ascontiguousarray(w_in_p.reshape(DEPTH, 8, 128, DIN).transpose(0, 2, 1, 3))
    sh["woutr"] = np.ascontiguousarray(w_out.reshape(DEPTH, 8, 128, D).transpose(0, 2, 1, 3))
    sh["sinks"] = np.ascontiguousarray(np.asarray(inputs["sinks"], f)[:, perm])
    rb = np.asarray(inputs["rel_bias"], f)
    hperm = list(range(8)) + [8 + p for p in perm]
    rb = rb[:, hperm]
    sh["rbT"] = np.ascontiguousarray(rb.T).reshape(1, 16 * 32)
    tab = np.concatenate([rb, np.full((1, 16), NEG, f)], axis=0)
    idx = _dtile_index()
    dt = np.zeros((128, 16, 2, 128), f)
    for h in range(16):
        hg = 0 if h < 8 else 1
        for t in range(2):
            dt[:, h, t, :] = tab[idx[hg, t], h]
    sh["dtile"] = dt.reshape(128, 16 * 2 * 128)
    sh["ident"] = np.eye(128, dtype=f)
    hsel = np.zeros((128, 2, 128), f)
    hsel[0:64, 0, :] = 1.0
    hsel[64:128, 1, :] = 1.0
    sh["hsel"] = hsel.reshape(128, 256)
    hind = np.zeros((128, 2), f)
    hind[0:64, 0] = 1.0
    hind[64:128, 1] = 1.0
    sh["hind"] = hind
    ind = np.zeros((72, 8, 128), f)
    for j in range(8):
        for pb in (0, 32, 64):
            ind[pb + j, j, :] = 1.0
    sh["indall"] = ind.reshape(72, 1024)
    bm = np.zeros((128, 3, 8, 8), f)
    for b in range(8):
        for j in range(8):
            bm[:, 0, b, j] = 0.0 if j < b else -1e30
            bm[:, 1, b, j] = 1.0 if j < b else 0.0
            bm[:, 2, b, j] = 1.0 if j == b else 0.0
    sh["bmask"] = bm.reshape(128, 192)
    x = np.asarray(inputs["x"], f)
    c = np.asarray(inputs["c"], f)
    per = []
    for b in range(x.shape[0]):
        m = dict(sh)
        m["xT"] = np.ascontiguousarray(x[b].T)
        m["cT"] = np.ascontiguousarray(c[b].reshape(8, 128).T)
        per.append(m)
    return per


_PROG_CACHE = {}


def _get_prog(phases, debug=False, ngroups=8, stop=99):
    key = (tuple(phases), debug, ngroups, stop)
    if key not in _PROG_CACHE:
        _PROG_CACHE[key] = Prog(list(phases), debug=debug, ngroups=ngroups, stop=stop)
    return _PROG_CACHE[key]


def run_phases(inputs, phases, n_cores=8, trace=False, debug=False, ngroups=8, stop=99):
    per = _host_prep(inputs)[:n_cores]
    prog = _get_prog(phases, debug, ngroups, stop)
    res = run_bass_kernel_spmd(prog.nc, per, core_ids=list(range(n_cores)), trace=trace)
    outs = [np.ascontiguousarray(r["outT"].T) for r in res.results]
    return np.stack(outs, axis=0), res


def kernel(**inputs):
    phases = []
    for l in range(DEPTH):
        phases += [("attn", l), ("ffn", l)]
    out, _ = run_phases(inputs, phases)
    return out.astype(np.float32)
```

```python
import math
import numpy as np
import concourse.bass as bass
import concourse.mybir as mybir
from concourse.bass_utils import run_bass_kernel_spmd

F32 = mybir.dt.float32
BF16 = mybir.dt.bfloat16
AF = mybir.ActivationFunctionType
ALU = mybir.AluOpType
AX = mybir.AxisListType

D = 1024
S = 2048
DEPTH = 4
DFF = 4096
DIN = 2304
EPS = 1e-6
NEG = -30000.0
BIG = 1024.0
MOBA_ROUND = "trunc"
SWA_ROUND = "trunc"


class Sched:
    def __init__(self):
        self.ops = []
        self.last_w = {}
        self.readers = {}

    def marker(self, reads=(), writes=()):
        k = getattr(self, "_mk", 0)
        self._mk = k + 1
        col = k % 8
        dm = self.dummy
        self.add("dve", (lambda e: e.memset(dm[:, col:col + 1], 0.0)), reads=reads, writes=list(writes) + [f"dummy{col}"])

    def barrier(self):
        names = set(self.last_w) | set(self.readers)
        names.add("__bar__")
        self.marker(writes=sorted(names))

    def add(self, eng, fn, reads=(), writes=(), dma=None, ndma=1, total=False):
        idx = len(self.ops)
        reads = tuple(reads) + ("__bar__",)
        writes = tuple(writes)
        deps = set()
        for r in reads:
            w = self.last_w.get(r)
            if w is not None:
                deps.add(w)
        for w_ in writes:
            w = self.last_w.get(w_)
            if w is not None:
                deps.add(w)
            for rd in self.readers.get(w_, ()):
                deps.add(rd)
        for r in reads:
            self.readers.setdefault(r, []).append(idx)
        for w_ in writes:
            self.last_w[w_] = idx
            self.readers[w_] = []
        self.ops.append(dict(eng=eng, fn=fn, deps=deps, dma=dma, ndma=ndma, total=total,
                             reads=set(reads), writes=set(writes)))
        return idx

    def finalize(self, nc, semctx):
        ops = self.ops
        need = [False] * len(ops)
        for i, o in enumerate(ops):
            keep = set()
            for d in o["deps"]:
                p = ops[d]
                if p["dma"] is not None or o["dma"] is not None:
                    keep.add(d)
                elif p["eng"] != o["eng"]:
                    keep.add(d)
                else:
                    if o["eng"] != "pe":
                        keep.add(d)
            o["deps"] = keep
            for d in keep:
                need[d] = True
        sems = {}

        def getsem(name):
            if name not in sems:
                sems[name] = semctx(name)
            return sems[name]

        cnt = {}
        totals = {}
        for i, o in enumerate(ops):
            if o["dma"] is not None:
                key = "d_" + o["dma"]
                cnt[key] = cnt.get(key, 0) + 16 * o["ndma"]
                o["sig"] = (key, cnt[key])
                if o["total"]:
                    totals[key] = True
            elif need[i]:
                key = "e_" + o["eng"]
                cnt[key] = cnt.get(key, 0) + 1
                o["sig"] = (key, cnt[key])
            else:
                o["sig"] = None
        for o in ops:
            if o["dma"] is not None and o["total"]:
                o["sig"] = (o["sig"][0], cnt[o["sig"][0]])
        for o in ops:
            w = {}
            for d in o["deps"]:
                k, v = ops[d]["sig"]
                if w.get(k, 0) < v:
                    w[k] = v
            o["waits"] = w
        for k in cnt:
            getsem(k)
        self.sems = sems
        self.cnt = cnt

    def emit(self, eng, e):
        waited = {}
        n = 0
        for o in self.ops:
            if o["eng"] != eng:
                continue
            for k in sorted(o["waits"]):
                v = o["waits"][k]
                if waited.get(k, 0) < v:
                    e.wait_ge(self.sems[k], v)
                    waited[k] = v
            ins = o["fn"](e)
            n += 1
            if o["dma"] is not None:
                if not isinstance(ins, (list, tuple)):
                    ins = [ins]
                assert len(ins) == o["ndma"], (len(ins), o["ndma"])
                for i_ in ins:
                    i_.then_inc(self.sems[o["sig"][0]], 16)
            elif o["sig"] is not None:
                if isinstance(ins, (list, tuple)):
                    ins = ins[-1]
                ins.then_inc(self.sems[o["sig"][0]], 1)
        return n


def _t5_bucket_np(dist, mode):
    n = np.maximum(dist, 0).astype(np.int32)
    nf = np.maximum(n, 1).astype(np.float32)
    val = (np.log(nf / np.float32(16)) / np.float32(math.log(128 / 16)) * np.float32(16)).astype(np.float32)
    if mode == "trunc":
        li = val.astype(np.int32)
    else:
        li = np.rint(val).astype(np.int32)
    large = np.minimum(16 + li, 31)
    return np.where(n < 16, n, large)


def _dtile_index():
    k = np.arange(128)[:, None]
    q = np.arange(128)[None, :]
    out = np.zeros((2, 2, 128, 128), np.int64)
    for hg, mode in ((0, MOBA_ROUND), (1, SWA_ROUND)):
        d0 = q - k
        b0 = _t5_bucket_np(d0, mode)
        out[hg, 1] = np.where(d0 >= 0, b0, 32)
        d1 = 128 + q - k
        b1 = _t5_bucket_np(d1, mode)
        if hg == 0:
            out[hg, 0] = b1
        else:
            out[hg, 0] = np.where(d1 < 128, b1, 32)
    return out


class Prog:
    def __init__(self, phases, debug=False, ngroups=8, stop=99):
        self.phases = phases
        self.stop = stop
        self.ngroups = ngroups
        self.debug = debug
        self.dbg_names = []
        self.nc = bass.Bass("TRN2", target_bir_lowering=False)
        self.sc = Sched()
        self.build()

    def dram_in(self, name, shape, dt=F32):
        return self.nc.dram_tensor(name, list(shape), dt, kind="ExternalInput").ap()

    def build(self):
        nc = self.nc
        sc = self.sc
        self.d_xT = self.dram_in("xT", [D, S])
        self.d_cT = self.dram_in("cT", [128, 8])
        self.d_wada = self.dram_in("wada", [DEPTH, 24, 128, 8, 256])
        self.d_badac = self.dram_in("badac", [128, DEPTH * 48])
        self.d_gainc = self.dram_in("gainc", [128, 128])
        self.d_b1c = self.dram_in("b1c", [128, 128])
        self.d_b2c = self.dram_in("b2c", [128, 32])
        self.d_w1r = self.dram_in("w1r", [DEPTH, 16, 128, 8, 256])
        self.d_w2r = self.dram_in("w2r", [DEPTH, 8, 2, 128, 16, 128])
        self.d_winr = self.dram_in("winr", [DEPTH, 128, 8, DIN])
        self.d_woutr = self.dram_in("woutr", [DEPTH, 128, 8, D])
        self.d_sinks = self.dram_in("sinks", [DEPTH, 8])
        self.d_rbT = self.dram_in("rbT", [1, 16 * 32])
        self.d_dtile = self.dram_in("dtile", [128, 16 * 2 * 128])
        self.d_ident = self.dram_in("ident", [128, 128])
        self.d_hsel = self.dram_in("hsel", [128, 2 * 128])
        self.d_hind = self.dram_in("hind", [128, 2])
        self.d_indall = self.dram_in("indall", [72, 8 * 128])
        self.d_bmask = self.dram_in("bmask", [128, 3 * 64])
        self.d_out = nc.dram_tensor("outT", [D, S], F32, kind="ExternalOutput").ap()

        total_words = 53200
        self.pool = nc.alloc_sbuf_tensor("pool", [128, total_words], F32)
        self.off = 0

        def alloc(words):
            o = self.off
            self.off += (words + 7) // 8 * 8
            assert self.off <= total_words, (self.off, total_words)
            return o

        def view(o, words, dt=F32):
            v = self.pool[:, o:o + words]
            if dt != F32:
                v = v.bitcast(dt)
            return v

        self.view = view
        o_x = alloc(8 * S)
        self.XT = view(o_x, 8 * S).rearrange("p (c t) -> p c t", c=8)
        self.COLS = view(alloc(320), 320)
        self.GAINC = view(alloc(128), 128)
        self.B1C = view(alloc(128), 128)
        self.B2C = view(alloc(32), 32)
        self.BADAC = view(alloc(192), 192)
        self.MODC = view(alloc(48), 48)
        self.CT = view(alloc(8), 8)
        self.CACT = view(alloc(8), 8, BF16)[:, 0:8]
        self.IDENT = view(alloc(64), 64, BF16)
        self.ONES = view(alloc(64), 64, BF16)
        self.SQ = [view(alloc(512), 512, BF16).rearrange("p (a t) -> p a t", a=2) for _ in range(2)]
        self.rstd_off = self.off
        self.RSTD = [view(alloc(512), 512) for _ in range(2)]
        self.XN = [view(alloc(512), 512) for _ in range(2)]
        self.wada_off = self.off
        self.WADA = [view(alloc(1024), 1024, BF16).rearrange("p (k n) -> p k n", k=8) for _ in range(2)]
        self.DUMMY = view(alloc(8), 8)
        sc.dummy = self.DUMMY
        self.EPSC = view(alloc(8), 8)
        self.phase_base = self.off

        self.PS = [nc.alloc_psum_tensor(f"psb{i}", [128, 512], F32) for i in range(8)]

        self.preamble()
        done_mod = set()
        self.side = []
        for pi, (kind, l) in enumerate(self.phases):
            if l not in done_mod:
                self.mod_layer(l)
                done_mod.add(l)
            sc.barrier()
            if kind == "attn":
                self.attn_phase(l)
            else:
                nxt = [ll for (_, ll) in self.phases[pi + 1:] if ll not in done_mod]
                if nxt:
                    self.side = self.mod_jobs(nxt[0])
                    done_mod.add(nxt[0])
                self.ffn_phase(l)
                self.run_side(100)
        sc.barrier()
        self.epilogue()

        class _SemCtx:
            pass
        semlist = []

        def semctx(name):
            cm = nc.semaphore(name)
            h = cm.__enter__()
            semlist.append(cm)
            return h

        sc.finalize(nc, semctx)
        with nc.Block() as block:
            @block.tensor
            def _(e):
                sc.emit("pe", e)

            @block.scalar
            def _(e):
                sc.emit("act", e)

            @block.vector
            def _(e):
                sc.emit("dve", e)

            @block.gpsimd
            def _(e):
                sc.emit("pool", e)

            @block.sync
            def _(e):
                sc.emit("sp", e)

    def dump(self, name, ap, reads):
        if not getattr(self, "debug", False):
            return
        shp = list(ap.shape)
        d = self.nc.dram_tensor("dbg_" + name, shp, ap.dtype, kind="ExternalOutput").ap()
        self.sc.add("sp", (lambda e: e.dma_start(out=d, in_=ap)), reads=list(reads), writes=["dbg_" + name], dma="dbg_" + name)
        self.dbg_names.append("dbg_" + name)

    def psb(self, i, dt=F32):
        v = self.PS[i][:, :]
        if dt != F32:
            v = v.bitcast(dt)
        return v

    def preamble(self):
        sc = self.sc
        XT = self.XT
        xs = self.d_xT.rearrange("(c p) t -> p c t", p=128)
        for c in range(8):
            sc.add("sp", (lambda e, c=c: e.dma_start(out=XT[:, c, :], in_=xs[:, c, :])),
                   writes=[f"xTc{c}"], dma=f"xin{c}")
        small = [(self.GAINC, self.d_gainc, "gainc"), (self.B1C, self.d_b1c, "b1c"), (self.B2C, self.d_b2c, "b2c"),
                 (self.BADAC, self.d_badac, "badac"), (self.CT, self.d_cT, "ct")]
        for (dst, src, nm) in small:
            sc.add("sp", (lambda e, dst=dst, src=src: e.dma_start(out=dst, in_=src[:, :])),
                   writes=[nm], dma="c_" + nm)
        sc.add("pool", (lambda e: e.dma_start(out=self.IDENT, in_=self.d_ident[:, :])), writes=["ident"], dma="c_ident")
        sc.add("dve", (lambda e: e.memset(self.ONES, 1.0)), writes=["ones"])
        sc.add("dve", (lambda e: e.memset(self.EPSC, float(D * EPS))), writes=["epsc"])
        sc.add("act", (lambda e: e.activation(out=self.CACT, in_=self.CT, func=AF.Silu)), reads=["ct"], writes=["cact"])
        sc.marker(reads=[f"xTc{c}" for c in range(8)], writes=[f"xT{tb}" for tb in range(8)] + ["rgnA_ok", "rgnB_ok"])

    def epilogue(self):
        sc = self.sc
        XT = self.XT
        od = self.d_out.rearrange("(c p) t -> p c t", p=128)
        for c in range(8):
            sc.add("sp", (lambda e, c=c: e.dma_start(out=od[:, c, :], in_=XT[:, c, :])),
                   reads=[f"xT{tb}" for tb in range(8)], writes=[f"out{c}"], dma=f"xout{c}")
        sc.add("sp", (lambda e: e.nop()), reads=[f"out{c}" for c in range(8)])

    def mod_layer(self, l):
        for j in self.mod_jobs(l):
            j()

    def run_side(self, n=1):
        for _ in range(n):
            if self.side:
                self.side.pop(0)()

    def mod_jobs(self, l):
        jobs = []
        for pc in range(24):
            jobs.append(lambda pc=pc: self.mod_piece(l, pc))
        jobs.append(lambda: self.mod_finish(l))
        return jobs

    def mod_piece(self, l, pc):
        sc = self.sc
        ps = self.psb(7)
        if True:
            buf = self.WADA[pc % 2]
            bn = f"wada{pc % 2}"
            src = self.d_wada[l, pc]
            sc.add("pool", (lambda e, buf=buf, src=src: e.dma_start(out=buf, in_=src)), reads=["wada_ok"], writes=[bn], dma=bn)

            def mm(e, buf=buf, pc=pc):
                last = None
                for j in range(2):
                    col = pc * 2 + j
                    for kc in range(8):
                        last = e.matmul(ps[:, col:col + 1], buf[:, kc, j * 128:(j + 1) * 128],
                                        self.CACT[:, kc:kc + 1], start=(kc == 0), stop=(kc == 7))
                return last
            sc.add("pe", mm, reads=[bn, "cact"], writes=["ps7"])
    def mod_finish(self, l):
        sc = self.sc
        ps = self.psb(7)
        MODC = self.MODC
        sc.add("dve", (lambda e: e.tensor_tensor(out=MODC, in0=ps[:, 0:48], in1=self.BADAC[:, l * 48:(l + 1) * 48], op=ALU.add)),
               reads=["ps7", "badac"], writes=["modc"])
        C = self.COLS
        b = l * 64
        G = self.GAINC
        g0 = (l * 4) * 8

        def cols(e):
            e.scalar_tensor_tensor(out=C[:, b + 0:b + 8], in0=MODC[:, 8:16], scalar=1.0, in1=G[:, g0 + 0:g0 + 8], op0=ALU.add, op1=ALU.mult)
            e.tensor_copy(out=C[:, b + 8:b + 16], in_=MODC[:, 0:8])
            e.tensor_tensor(out=C[:, b + 16:b + 24], in0=MODC[:, 16:24], in1=G[:, g0 + 8:g0 + 16], op=ALU.mult)
            e.scalar_tensor_tensor(out=C[:, b + 24:b + 32], in0=MODC[:, 32:40], scalar=1.0, in1=G[:, g0 + 16:g0 + 24], op0=ALU.add, op1=ALU.mult)
            e.tensor_copy(out=C[:, b + 32:b + 40], in_=MODC[:, 24:32])
            return e.tensor_tensor(out=C[:, b + 40:b + 48], in0=MODC[:, 40:48], in1=G[:, g0 + 24:g0 + 32], op=ALU.mult)
        sc.add("dve", cols, reads=["modc", "gainc"], writes=[f"colsraw{l}"])

        def cols2(e):
            e.tensor_scalar(out=C[:, b + 0:b + 8], in0=C[:, b + 0:b + 8], scalar1=32.0, scalar2=None, op0=ALU.mult)
            e.tensor_scalar(out=C[:, b + 16:b + 32], in0=C[:, b + 16:b + 32], scalar1=32.0, scalar2=None, op0=ALU.mult)
            return e.tensor_scalar(out=C[:, b + 40:b + 48], in0=C[:, b + 40:b + 48], scalar1=32.0, scalar2=None, op0=ALU.mult)
        sc.add("dve", cols2, reads=[f"colsraw{l}"], writes=[f"cols{l}"])
        self.dump(f"cols{l}", C[:, b:b + 48], [f"cols{l}"])
        self.dump(f"modc{l}", MODC, [f"cols{l}"])

    def rmsnorm_in(self, l, sub, t0, n, HT, ht_res, psbank, extra_reads=(), sq_names=None):
        sc = self.sc
        XT = self.XT
        tbs = [f"xT{tb}" for tb in range(t0 // 256, (t0 + n) // 256)]
        ps = self.psb(psbank)
        cb = l * 64 + (0 if sub == 0 else 24)
        C = self.COLS
        for cp in range(4):
            sq = self.SQ[cp % 2]
            sqn = f"sq{cp % 2}"
            sqw = [sqn] if sq_names is None else sq_names[cp % 2]
            sc.add("act", (lambda e, cp=cp, sq=sq: e.activation(out=sq[:, :, 0:n], in_=XT[:, 2 * cp:2 * cp + 2, t0:t0 + n], func=AF.Square)),
                   reads=tbs, writes=sqw)

            def mm(e, cp=cp, sq=sq):
                last = None
                for j in range(2):
                    c = 2 * cp + j
                    last = e.matmul(ps[:, 0:n], self.ONES, sq[:, j, 0:n], start=(c == 0), stop=(c == 7))
                return last
            sc.add("pe", mm, reads=[sqn, "ones"], writes=[f"ps{psbank}"])
        rs = self.RSTD[0]
        sc.add("act", (lambda e: e.activation(out=rs[:, 0:n], in_=ps[:, 0:n], func=AF.Ln, bias=self.EPSC[:, 0:1], scale=1.0)),
               reads=[f"ps{psbank}", "epsc"], writes=["rstd0p", "rstd0"])
        sc.add("act", (lambda e: e.activation(out=rs[:, 0:n], in_=rs[:, 0:n], func=AF.Exp, scale=-0.5)),
               reads=["rstd0p"], writes=["rstd0", "rstd0p"])
        nn = 1 if n > 256 else 2
        for ci, c0 in enumerate(range(0, 8, nn)):
            xn = self.XN[ci % 2]
            xnn = f"xn{ci % 2}"
            xv = xn[:, 0:nn * n].rearrange("p (a t) -> p a t", a=nn)
            sc.add("dve", (lambda e, c0=c0, xv=xv: e.tensor_tensor(out=xv, in0=XT[:, c0:c0 + nn, t0:t0 + n],
                                                                    in1=rs[:, 0:n].unsqueeze(1).to_broadcast([128, nn, n]), op=ALU.mult)),
                   reads=tbs + ["rstd0"], writes=[xnn])
            for a in range(nn):
                c = c0 + a
                sc.add("act", (lambda e, c=c, a=a, xv=xv: e.activation(out=HT[:, c, 0:n], in_=xv[:, a, :], func=AF.Identity,
                                                                       scale=C[:, cb + c:cb + c + 1], bias=C[:, cb + 8 + c:cb + 9 + c])),
                       reads=[xnn, f"cols{l}"] + list(extra_reads), writes=[ht_res])

    def resid_update(self, l, sub, t0, n, Y, y_res, ssbank):
        sc = self.sc
        XT = self.XT
        tbs = [f"xT{tb}" for tb in range(t0 // 256, (t0 + n) // 256)]
        ps = self.psb(ssbank)
        C = self.COLS
        cb = l * 64 + (16 if sub == 0 else 40)
        rs = self.RSTD[1]
        sc.add("act", (lambda e: e.activation(out=rs[:, 0:n], in_=ps[:, 0:n], func=AF.Ln, bias=self.EPSC[:, 0:1], scale=1.0)),
               reads=[f"ps{ssbank}", "epsc"], writes=["rstd1p", "rstd1"])
        sc.add("act", (lambda e: e.activation(out=rs[:, 0:n], in_=rs[:, 0:n], func=AF.Exp, scale=-0.5)),
               reads=["rstd1p"], writes=["rstd1", "rstd1p"])
        if t0 == 0 and sub == 1:
            self.dump("rstd1", rs, ["rstd1"])
            self.dump("ysb", Y, [y_res(c) for c in range(8)])
        yall = [y_res(c) for c in range(8)]
        sc.add("dve", (lambda e: e.tensor_tensor(out=Y[:, :, 0:n], in0=Y[:, :, 0:n], in1=C[:, cb:cb + 8].unsqueeze(2).to_broadcast([128, 8, n]), op=ALU.mult)),
               reads=yall + [f"cols{l}"], writes=yall)
        sc.add("dve", (lambda e: e.tensor_tensor(out=Y[:, :, 0:n], in0=Y[:, :, 0:n], in1=rs[:, 0:n].unsqueeze(1).to_broadcast([128, 8, n]), op=ALU.mult)),
               reads=yall + ["rstd1"], writes=yall)
        sc.add("dve", (lambda e: e.tensor_tensor(out=XT[:, :, t0:t0 + n], in0=XT[:, :, t0:t0 + n], in1=Y[:, :, 0:n], op=ALU.add)),
               reads=yall + tbs, writes=tbs)

    def ffn_phase(self, l):
        sc = self.sc
        view = self.view
        base = self.phase_base
        o = base
        HID = view(o, 32 * 1024 // 2, BF16).rearrange("p (m t) -> p m t", m=32); o += 16384
        rgn = o; o += 8192
        HT = view(rgn, 4096, BF16).rearrange("p (c t) -> p c t", c=8)
        W1B = [view(rgn + 4096 + i * 1024, 1024, BF16).rearrange("p (k n) -> p k n", k=8) for i in range(3)]
        RL = [view(rgn + 4096 + 3072 + i * 512, 512) for i in range(2)]
        YSB = view(rgn, 8192).rearrange("p (c t) -> p c t", c=8)
        W2B = [view(o + i * 1024, 1024, BF16).rearrange("p (k n) -> p k n", k=16) for i in range(3)]; o += 3072
        assert o <= 53200, o
        B1C, B2C = self.B1C, self.B2C
        w1cnt = 0
        w2cnt = 0
        for H in range(2):
            T0 = H * 1024
            region_users = ["rgnA_ok"]
            for tg in range(2):
                self.rmsnorm_in(l, 1, T0 + tg * 512, 512, HT[:, :, tg * 512:(tg + 1) * 512], f"ht{tg}", 6,
                                extra_reads=region_users)
            if H == 0:
                self.dump("ht", HT, ["ht0", "ht1"])
            psi = 0
            for g in range(16):
                self.run_side(1)
                wb = W1B[w1cnt % 3]; wn = f"w1b{w1cnt % 3}"; w1cnt += 1
                src = self.d_w1r[l, g]
                sc.add("pool", (lambda e, wb=wb, src=src: e.dma_start(out=wb, in_=src)), reads=region_users, writes=[wn], dma=wn)
                for mm_ in range(2):
                    m = 2 * g + mm_
                    for tg in range(2):
                        bank = psi % 4; psi += 1
                        ps = self.psb(bank)

                        def mm(e, wb=wb, mm_=mm_, tg=tg, ps=ps):
                            last = None
                            for c in range(8):
                                last = e.matmul(ps, wb[:, c, mm_ * 128:(mm_ + 1) * 128], HT[:, c, tg * 512:(tg + 1) * 512],
                                                start=(c == 0), stop=(c == 7))
                            return last
                        sc.add("pe", mm, reads=[wn, f"ht{tg}"], writes=[f"ps{bank}"])
                        rl = RL[psi % 2]; rln = f"rl{psi % 2}"
                        sc.add("act", (lambda e, rl=rl, ps=ps, m=m: e.activation(out=rl, in_=ps, func=AF.Relu,
                                                                                 bias=B1C[:, l * 32 + m:l * 32 + m + 1])),
                               reads=[f"ps{bank}", "b1c"] + region_users, writes=[rln])
                        sc.add("dve", (lambda e, rl=rl, m=m, tg=tg: e.tensor_tensor(out=HID[:, m, tg * 512:(tg + 1) * 512], in0=rl, in1=rl, op=ALU.mult)),
                               reads=[rln], writes=[f"hid{m}_{tg}"])
            if H == 0:
                self.dump("hid", HID, [f"hid{m}_{tg}" for m in range(32) for tg in range(2)])
            sc.marker(writes=["ht0", "ht1", "w1b0", "w1b1", "w1b2", "rl0", "rl1", "rgnB_ok"])
            ht_users = ["rgnB_ok"]
            for o_ in range(8):
                wbs = []
                for kh in range(2):
                    wb = W2B[w2cnt % 3]; wn = f"w2b{w2cnt % 3}"; w2cnt += 1
                    src = self.d_w2r[l, o_, kh]
                    sc.add("pool", (lambda e, wb=wb, src=src: e.dma_start(out=wb, in_=src)), writes=[wn], dma=wn)
                    wbs.append((wb, wn))
                banks = [(o_ % 2) * 2, (o_ % 2) * 2 + 1]
                for kh in range(2):
                    wb, wn = wbs[kh]
                    for tg in range(2):
                        ps = self.psb(banks[tg])

                        def mm(e, wb=wb, kh=kh, tg=tg, ps=ps):
                            last = None
                            for kk in range(16):
                                m = kh * 16 + kk
                                last = e.matmul(ps, wb[:, kk, :], HID[:, m, tg * 512:(tg + 1) * 512],
                                                start=(m == 0), stop=(m == 31))
                            return last
                        sc.add("pe", mm, reads=[wn] + [f"hid{kh * 16 + kk}_{tg}" for kk in range(16)], writes=[f"ps{banks[tg]}"])
                for tg in range(2):
                    ps = self.psb(banks[tg])
                    ysl = YSB[:, o_, tg * 512:(tg + 1) * 512]
                    sc.add("act", (lambda e, ps=ps, ysl=ysl, o_=o_: e.activation(out=ysl, in_=ps, func=AF.Identity,
                                                                                  bias=B2C[:, l * 8 + o_:l * 8 + o_ + 1])),
                           reads=[f"ps{banks[tg]}", "b2c"] + ht_users, writes=[f"ysb{o_}t{tg}"])
                    sq = self.SQ[tg][:, 0, :]
                    sc.add("dve", (lambda e, ysl=ysl, sq=sq: e.tensor_tensor(out=sq, in0=ysl, in1=ysl, op=ALU.mult)),
                           reads=[f"ysb{o_}t{tg}"], writes=[f"sq{tg}"])
                    ssb = 4 + tg
                    sc.add("pe", (lambda e, sq=sq, ssb=ssb, o_=o_: e.matmul(self.psb(ssb), self.ONES, sq, start=(o_ == 0), stop=(o_ == 7))),
                           reads=[f"sq{tg}", "ones"], writes=[f"ps{ssb}"])
            for tg in range(2):
                self.resid_update(l, 1, T0 + tg * 512, 512, YSB[:, :, tg * 512:(tg + 1) * 512],
                                  (lambda c, tg=tg: f"ysb{c}t{tg}"), 4 + tg)
            sc.marker(writes=[f"ysb{c}t{tg}" for c in range(8) for tg in range(2)] + ["rgnA_ok"])

    def attn_phase(self, l):
        sc = self.sc
        view = self.view
        XT = self.XT
        o = self.phase_base
        KT = view(o, 5120, BF16).rearrange("p (c t) -> p c t", c=5); o += 5120
        VA = view(o, 5200, BF16).rearrange("p (t h d) -> p t h d", t=16, h=10); o += 5200
        WIN = view(o, 9216, BF16).rearrange("p (k n) -> p k n", k=8); o += 9216
        WOUT = view(o, 4096, BF16).rearrange("p (k n) -> p k n", k=8); o += 4096
        DT = view(o, 2048, BF16).rearrange("p (h t q) -> p h t q", h=16, t=2); o += 2048
        rA = o; o += 1024
        rC = o; o += 1024
        rB = o; o += 1024
        HTG = view(rA, 1024, BF16).rearrange("p (c t) -> p c t", c=8)
        OG = view(rA, 1024, BF16).rearrange("p (q f) -> p q f", q=2)
        QTG = view(rC, 1024, BF16).rearrange("p (c t) -> p c t", c=8)
        QSQ = view(rB, 1024, BF16).rearrange("p (c t) -> p c t", c=8)
        OTG = view(rB, 1024, BF16).rearrange("p (c t) -> p c t", c=8)
        YSBA = view(rA, 2048).rearrange("p (c t) -> p c t", c=8)
        AUGT1 = [view(o + i * 128, 128, BF16) for i in range(3)]; o += 384
        IND = view(o, 512, BF16).rearrange("p (j k) -> p j k", j=8); o += 512
        HIND = view(o, 8, BF16)[:, 0:2]; o += 8
        KMEANT = view(o, 32, BF16).rearrange("p (c r j) -> p c r j", c=4, r=2); o += 32
        KSUM = view(o, 8, F32); o += 8
        KMAX2 = view(o, 8, F32); o += 8
        KMXG = view(o, 8, F32); o += 8
        KM16 = view(o, 16, F32); o += 16
        QMXG = view(o, 8, F32); o += 8
        NB = view(o, 16, F32); o += 16
        FB = view(o, 16, F32); o += 16
        QM16 = view(o, 16, F32); o += 16
        SINKS = view(o, 8, F32); o += 8
        BM8 = view(o, 16, F32); o += 16
        RB = self.XN[0].rearrange("p (h b) -> p h b", h=16)
        B31 = view(o, 16, F32); o += 16
        KSQ = view(rB, 640, BF16).rearrange("p (c t) -> p c t", c=5)
        RDEN = view(o, 8, F32); o += 8
        assert o <= 53200, o
        wsc = self.wada_off
        CMP = view(wsc, 1024).rearrange("p (a j k) -> p a j k", a=16, j=8)
        AUGB = view(wsc + 1024, 128, BF16).rearrange("p (q s j) -> p q s j", q=2, s=16)
        BMASK = view(wsc + 1600, 192).rearrange("p (k b j) -> p k b j", k=3, b=8)
        GM = view(wsc + 1792, 128).rearrange("p (a j) -> p a j", a=16)
        SEL = view(wsc + 1920, 128).rearrange("p (a j) -> p a j", a=16)
        rs_off = self.rstd_off
        SELF = view(rs_off + 256, 256).rearrange("p (q s j) -> p q s j", q=2, s=16)
        SH8 = view(rs_off + 512 + 256, 32).rearrange("p (q s) -> p q s", q=2)
        SINKT = view(rs_off + 512 + 288, 16).rearrange("p (q s) -> p q s", q=2)
        SQRT_T = view(rs_off + 512 + 304, 32).rearrange("p (q s) -> p q s", q=2)
        TMPS = [self.XN[0], self.XN[1]]
        PTS = [self.SQ[0].rearrange("p a t -> p (a t)")[:, 0:512], self.SQ[0].rearrange("p a t -> p (a t)")[:, 512:1024],
               self.SQ[1].rearrange("p a t -> p (a t)")[:, 0:512], self.SQ[1].rearrange("p a t -> p (a t)")[:, 512:1024]]
        PTN = ["sq0", "sq0b", "sq1", "sq1b"]
        IDENT = self.IDENT.rearrange("p (a b) -> p a b", a=1)[:, 0, :]

        sc.marker(writes=["wada0", "wada1", "rstd0", "rstd1", "rstd0p", "rstd1p", "wadafree"])
        sc.add("pool", (lambda e: e.dma_start(out=WIN, in_=self.d_winr[l])), writes=["win"], dma="win")
        sc.add("pool", (lambda e: e.dma_start(out=DT.rearrange("p h t q -> p (h t q)"), in_=self.d_dtile[:, :])), writes=["dt"], dma="dt")
        sc.add("pool", (lambda e: e.dma_start(out=IND.rearrange("p j k -> p (j k)")[0:72, :], in_=self.d_indall[:, :])), writes=["ind"], dma="ind")
        sc.add("pool", (lambda e: e.dma_start(out=HIND, in_=self.d_hind[:, :])), writes=["hind"], dma="hind")
        sc.add("sp", (lambda e: e.dma_start(out=BMASK.rearrange("p k b j -> p (k b j)"), in_=self.d_bmask[:, :])), reads=["wadafree"], writes=["bmask"], dma="bmask")
        sc.add("sp", (lambda e: e.dma_start(out=SINKS, in_=self.d_sinks[l:l + 1, :].partition_broadcast(128))), writes=["sinks"], dma="sinks")
        sc.add("sp", (lambda e: e.dma_start(out=RB.rearrange("p h b -> p (h b)"), in_=self.d_rbT[0:1, :].partition_broadcast(128))), writes=["xn0"], dma="rb")
        sc.add("pool", (lambda e: e.dma_start(out=WOUT, in_=self.d_woutr[l])), writes=["wout"], dma="wout")

        def init1(e):
            e.memset(KMEANT, 0.0)
            e.memset(KMAX2, 0.0)
            e.memset(VA[:, :, :, 64:65], 1.0)
            e.memset(AUGB, 0.0)
            e.memset(SELF, 1.0)
            e.tensor_reduce(out=BM8, in_=RB, axis=AX.X, op=ALU.max)
            e.tensor_copy(out=B31, in_=RB[:, :, 31])
            return e.memset(KM16, 0.0)
        sc.add("dve", init1, reads=["xn0", "wadafree"], writes=["kmeant", "kmax2", "va_ones", "augb_s", "augb_m", "selfm", "bm8raw", "b31", "km16"])

        sc.add("dve", (lambda e: e.tensor_tensor(out=BM8[:, 8:16], in0=BM8[:, 8:16], in1=SINKS, op=ALU.max)),
               reads=["bm8raw", "sinks"], writes=["bm8raw"])
        sc.add("dve", (lambda e: e.tensor_scalar(out=BM8, in0=BM8, scalar1=8.0, scalar2=None, op0=ALU.mult)),
               reads=["bm8raw"], writes=["bm8", "bm8raw"])

        QCOL, KCOL, VCOL, QBCOL, KBCOL, VBCOL = 0, 512, 1024, 1536, 2048, 2176
        sbank = [0]
        ptc = [0]

        def group(g):
            t0 = g * 256
            b = g
            xres = [f"xT{g}"]
            self.rmsnorm_in(l, 0, t0, 256, HTG, "rA", 7, sq_names=(["sq0", "sq0b"], ["sq1", "sq1b"]))
            pbank = [0]

            def nextbank():
                bk = 4 + pbank[0] % 4
                pbank[0] += 1
                return bk
            def qproj():
              for ci in range(8):
                col = QCOL + ci * 128 if ci < 4 else QBCOL + (ci - 4) * 128
                bk = nextbank()
                ps = self.psb(bk)

                def mm(e, col=col, ps=ps):
                    last = None
                    for kc in range(8):
                        last = e.matmul(ps[:, 0:256], WIN[:, kc, col:col + 128], HTG[:, kc, :], start=(kc == 0), stop=(kc == 7))
                    return last
                sc.add("pe", mm, reads=["win", "rA"], writes=[f"ps{bk}"])
                sc.add("dve", (lambda e, ci=ci, ps=ps: e.tensor_copy(out=QTG[:, ci, :], in_=ps[:, 0:256])),
                       reads=[f"ps{bk}"], writes=["rC"])
            sc.add("dve", (lambda e: e.memset(KSUM, 0.0)), writes=[f"ksum{c}" for c in range(4)])
            for ci in range(5):
                col = KCOL + ci * 128 if ci < 4 else KBCOL
                bk = nextbank()
                ps = self.psb(bk)

                def mm(e, col=col, ps=ps):
                    last = None
                    for kc in range(8):
                        last = e.matmul(ps[:, 0:256], WIN[:, kc, col:col + 128], HTG[:, kc, :], start=(kc == 0), stop=(kc == 7))
                    return last
                sc.add("pe", mm, reads=["win", "rA"], writes=[f"ps{bk}"])
                if ci < 4:
                    sc.add("act", (lambda e, ci=ci, ps=ps: e.activation(out=KT[:, ci, t0:t0 + 256], in_=ps[:, 0:256], func=AF.Copy,
                                                                           accum_out=KSUM[:, ci:ci + 1])),
                           reads=[f"ps{bk}"], writes=[f"kt{ci}_{g}", f"ksum{ci}"])
                else:
                    sc.add("act", (lambda e, ci=ci, ps=ps: e.activation(out=KT[:, ci, t0:t0 + 256], in_=ps[:, 0:256], func=AF.Copy)),
                           reads=[f"ps{bk}"], writes=[f"kt{ci}_{g}"])
            qproj()

            def vproj():
              for qt in range(2):
                tile_i = g * 2 + qt
                bk = nextbank()
                ps = self.psb(bk)

                def mmv(e, qt=qt, ps=ps):
                    last = None
                    for kc in range(8):
                        last = e.matmul(ps[:, 0:512], HTG[:, kc, qt * 128:(qt + 1) * 128], WIN[:, kc, VCOL:VCOL + 512], start=(kc == 0), stop=(kc == 7))
                    return last
                sc.add("pe", mmv, reads=["win", "rA"], writes=[f"ps{bk}"])
                sc.add("act", (lambda e, tile_i=tile_i, ps=ps: e.activation(out=VA[:, tile_i, 0:8, 0:64],
                                                                               in_=ps[:, 0:512].rearrange("p (h d) -> p h d", h=8), func=AF.Copy)),
                       reads=[f"ps{bk}", "va_ones"], writes=[f"va{g}_{qt}a"])
                bk2 = nextbank()
                ps2 = self.psb(bk2)

                def mmv2(e, qt=qt, ps2=ps2):
                    last = None
                    for kc in range(8):
                        last = e.matmul(ps2[:, 0:128], HTG[:, kc, qt * 128:(qt + 1) * 128], WIN[:, kc, VBCOL:VBCOL + 128], start=(kc == 0), stop=(kc == 7))
                    return last
                sc.add("pe", mmv2, reads=["win", "rA"], writes=[f"ps{bk2}"])
                sc.add("dve", (lambda e, tile_i=tile_i, ps2=ps2: e.tensor_copy(out=VA[:, tile_i, 8:10, 0:64],
                                                                                 in_=ps2[:, 0:128].rearrange("p (h d) -> p h d", h=2))),
                       reads=[f"ps{bk2}", "va_ones"], writes=[f"va{g}_{qt}b"])
            if getattr(self, 'stop', 99) <= 2:
                return
            def kmw(e, b=b):
                e.tensor_scalar(out=KMEANT[0:64, :, 0, b], in0=KSUM[0:64, 0:4], scalar1=1.0 / 256.0, scalar2=None, op0=ALU.mult)
                return e.tensor_scalar(out=KMEANT[64:128, :, 1, b], in0=KSUM[64:128, 0:4], scalar1=1.0 / 256.0, scalar2=None, op0=ALU.mult)
            sc.add("dve", kmw, reads=[f"ksum{c}" for c in range(4)], writes=["kmeant"])
            sc.add("dve", (lambda e: e.tensor_tensor(out=KSQ, in0=KT[:, 0:5, t0:t0 + 256], in1=KT[:, 0:5, t0:t0 + 256], op=ALU.mult)),
                   reads=[f"kt{c}_{g}" for c in range(5)], writes=["rB", "rBb"])
            if getattr(self, 'stop', 99) <= 2.2:
                return
            for bi, cs in enumerate([(0, 1), (2, 3), (4,)]):
                bk = 4 + bi
                ps = self.psb(bk).rearrange("p (a t) -> p a t", a=2)

                def mmk(e, cs=cs, ps=ps):
                    last = None
                    for a, c in enumerate(cs):
                        last = e.matmul(ps[:, a, :], self.ONES, KSQ[:, c, :], start=True, stop=True)
                    return last
                sc.add("pe", mmk, reads=["rB", "ones"], writes=[f"ps{bk}"])
                sc.add("dve", (lambda e, cs=cs, ps=ps: e.tensor_reduce(out=KMXG[:, cs[0]:cs[0] + len(cs)], in_=ps[:, 0:len(cs), :], axis=AX.X, op=ALU.max)),
                       reads=[f"ps{bk}"], writes=["kmxg"])

            if getattr(self, 'stop', 99) <= 2.4:
                return
            sc.add("dve", (lambda e: e.tensor_tensor(out=KMAX2[:, 0:5], in0=KMAX2[:, 0:5], in1=KMXG[:, 0:5], op=ALU.max)),
                   reads=["kmxg", "kmax2"], writes=["kmax2"])

            def kmax(e):
                e.tensor_copy(out=KM16[:, 0:8].rearrange("p (c r) -> p c r", r=2), in_=KMAX2[:, 0:4].unsqueeze(2).to_broadcast([128, 4, 2]))
                return e.tensor_copy(out=KM16[:, 8:16], in_=KMAX2[:, 4:5].to_broadcast([128, 8]))
            sc.add("dve", kmax, reads=["kmax2"], writes=["km16"])
            if getattr(self, 'stop', 99) <= 2.6:
                return
            sc.add("dve", (lambda e: e.tensor_tensor(out=QSQ, in0=QTG, in1=QTG, op=ALU.mult)), reads=["rC"], writes=["rB", "rBb"])
            ps7 = self.psb(7)
            GATE = ps7[:, 0:128].rearrange("p (a j) -> p a j", a=16)

            for bi in range(4):
                psq = self.psb(bi).rearrange("p (a t) -> p a t", a=2)

                def mmq(e, bi=bi, psq=psq):
                    last = None
                    for a in range(2):
                        last = e.matmul(psq[:, a, :], self.ONES, QSQ[:, 2 * bi + a, :], start=True, stop=True)
                    return last
                sc.add("pe", mmq, reads=["rB", "ones"], writes=[f"ps{bi}"])
                sc.add("dve", (lambda e, bi=bi, psq=psq: e.tensor_reduce(out=QMXG[:, 2 * bi:2 * bi + 2], in_=psq, axis=AX.X, op=ALU.max)),
                       reads=[f"ps{bi}"], writes=["qmxg"])
            sc.add("dve", (lambda e: e.tensor_copy(out=QM16.rearrange("p (c r) -> p c r", r=2), in_=QMXG.unsqueeze(2).to_broadcast([128, 8, 2]))),
                   reads=["qmxg"], writes=["qm16"])

            def mmg(e):
                last = None
                for qt in range(2):
                    for c in range(4):
                        last = e.matmul(ps7[:, (qt * 8 + 2 * c) * 8:(qt * 8 + 2 * c + 2) * 8], QTG[:, c, qt * 128:(qt + 1) * 128],
                                        KMEANT[:, c, :, :].rearrange("p r j -> p (r j)"), start=True, stop=True)
                return last
            if b >= 4:
                sc.add("pe", mmg, reads=["rC", "kmeant"], writes=["ps7"])
            if getattr(self, 'stop', 99) <= 3:
                return
            NEGM = BMASK[:, 0, b, :]
            ELIG = BMASK[:, 1, b, :]
            OWN = BMASK[:, 2, b, :]

            sc.add("dve", (lambda e: e.tensor_tensor(out=SQRT_T, in0=QM16.unsqueeze(1).to_broadcast([128, 2, 16]),
                                                     in1=KM16.unsqueeze(1).to_broadcast([128, 2, 16]), op=ALU.mult)),
                   reads=["qm16", "km16"], writes=["sqrt_t"])
            sc.add("act", (lambda e: e.activation(out=SQRT_T, in_=SQRT_T, func=AF.Ln, bias=self.EPSC[:, 0:1], scale=1.0)),
                   reads=["sqrt_t", "epsc"], writes=["sqrt_t"])
            sc.add("act", (lambda e: e.activation(out=SQRT_T, in_=SQRT_T, func=AF.Exp, scale=0.5)),
                   reads=["sqrt_t"], writes=["sqrt_t"])
            AUGBv = AUGB
            sc.add("dve", (lambda e: e.tensor_tensor(out=SH8, in0=SQRT_T, in1=BM8.unsqueeze(1).to_broadcast([128, 2, 16]), op=ALU.add)),
                   reads=["sqrt_t", "bm8"], writes=["sh8"])
            sc.add("dve", (lambda e: e.tensor_scalar(out=NB, in0=SH8[:, 0, :], scalar1=-0.125, scalar2=None, op0=ALU.mult)),
                   reads=["sh8"], writes=["nb"])
            sc.add("dve", (lambda e: e.tensor_tensor(out=FB, in0=NB, in1=B31, op=ALU.add)), reads=["nb", "b31"], writes=["fb"])
            sc.add("dve", (lambda e: e.tensor_tensor(out=SINKT, in0=SINKS.unsqueeze(1).to_broadcast([128, 2, 8]),
                                                     in1=NB[:, 8:16].unsqueeze(1).to_broadcast([128, 2, 8]), op=ALU.add)),
                   reads=["nb", "sinks"], writes=["sinkt"])
            sc.add("act", (lambda e: e.activation(out=SINKT, in_=SINKT, func=AF.Exp)), reads=["sinkt"], writes=["sinkt"])
            SELM = SELF[:, :, 0:8, :]
            sel_jobs = [
                lambda: sc.add("dve", (lambda e: e.tensor_tensor(out=GM, in0=GATE, in1=NEGM.unsqueeze(1).to_broadcast([128, 16, 8]), op=ALU.add)),
                               reads=["ps7", "bmask"], writes=["gm"]),
                lambda: sc.add("dve", (lambda e: e.tensor_tensor(out=CMP, in0=GM.unsqueeze(2).to_broadcast([128, 16, 8, 8]),
                                                                 in1=GM.unsqueeze(3).to_broadcast([128, 16, 8, 8]), op=ALU.is_gt)),
                               reads=["gm"], writes=["cmp"]),
                lambda: sc.add("dve", (lambda e: e.tensor_reduce(out=SEL, in_=CMP, axis=AX.X, op=ALU.add)), reads=["cmp"], writes=["selr"]),
                lambda: sc.add("dve", (lambda e: e.scalar_tensor_tensor(out=SEL, in0=SEL, scalar=3.0, in1=ELIG.unsqueeze(1).to_broadcast([128, 16, 8]),
                                                                        op0=ALU.is_lt, op1=ALU.mult)),
                               reads=["selr", "bmask"], writes=["selr"]),
                lambda: sc.add("dve", (lambda e: e.tensor_tensor(out=SELM, in0=SEL.rearrange("p (q h) j -> p q h j", q=2),
                                                                 in1=OWN.unsqueeze(1).unsqueeze(1).to_broadcast([128, 2, 8, 8]), op=ALU.add)),
                               reads=["selr", "bmask", "selfm"], writes=["selfm"]),
                lambda: sc.add("dve", (lambda e: e.tensor_scalar(out=AUGBv[:, :, 0:8, :], in0=SELM, scalar1=BIG, scalar2=-BIG, op0=ALU.mult, op1=ALU.add)),
                               reads=["selfm"], writes=["augb_m"]),
            ]
            if b < 4:
                sel_jobs = []
            if getattr(self, 'stop', 99) <= 4:
                return
            order = list(range(8, 16)) + list(range(8))
            if sel_jobs:
                sel_jobs.pop(0)()
            vproj()
            for grp in (1, 0):
                pvq = []

                def flush(keep):
                    while len(pvq) > keep:
                        a_, k_ = pvq.pop(0)
                        sc.add(*a_, **k_)

                def prep(s16):
                    ps7b = self.psb(7, BF16)
                    pos_ = order.index(s16)
                    AT = AUGT1[pos_ % 3]
                    ares = "augb_s" if s16 >= 8 else "augb_m"

                    def tr(e):
                        last = None
                        for qt in range(2):
                            last = e.transpose(ps7b[0:8, qt * 128:(qt + 1) * 128], AUGB[:, qt, s16, :], IDENT)
                        return last
                    sc.add("pe", tr, reads=[ares, "ident"], writes=["ps7"])
                    sc.add("act", (lambda e: e.activation(out=AT[0:8, 0:256], in_=ps7b[0:8, 0:256], func=AF.Copy)),
                           reads=["ps7"], writes=[f"augt{pos_ % 3}"])

                def head(hs8):
                    s16 = grp * 8 + hs8
                    pos = order.index(s16)
                    if grp == 1 and sel_jobs:
                        sel_jobs.pop(0)()
                    if pos + 2 < 16 and order[pos + 2] < 8 and b >= 4:
                        while sel_jobs:
                            sel_jobs.pop(0)()
                        prep(order[pos + 2])
                    AT = AUGT1[pos % 3]
                    use_aug = (grp == 0 and b >= 4)
                    pb = 0
                    if grp == 0:
                        ck, r0, cq, vh = hs8 // 2, (hs8 % 2) * 64, hs8 // 2, hs8
                    else:
                        i_, r_ = hs8 // 2, hs8 % 2
                        ck, r0, cq, vh = 4, r_ * 64, 4 + i_, 8 + r_
                    quad, hsl = hs8 // 4, hs8 % 4
                    augres = f"augt{pos % 3}"
                    far = []
                    if grp == 0 and b >= 1:
                        for kc in range(0, 2 * b - 1):
                            far.append((kc, 0, 256))
                        far.append((2 * b - 1, 128, 128))
                    near = [(2 * b - 1, 0, 0), (2 * b, 0, 1), (2 * b, 1, 0), (2 * b + 1, 1, 1)]
                    if b == 0:
                        near = near[1:]
                    n_qt = [0, 0]
                    for (kc, q0, qn) in far:
                        for sub in range(qn // 128):
                            n_qt[(q0 + sub * 128) // 128] += 1
                    for (kc, qt, _) in near:
                        n_qt[qt] += 1
                    done_qt = [0, 0]
                    banks = []
                    cur, tot = [], 0
                    for p_ in far:
                        if tot + p_[2] > 512:
                            banks.append(cur)
                            cur, tot = [], 0
                        cur.append(p_)
                        tot += p_[2]
                    if cur:
                        banks.append(cur)
                    for pieces in banks:
                        bk = 4 + sbank[0] % 3
                        sbank[0] += 1
                        ps = self.psb(bk)
                        pti = ptc[0] % 4
                        ptc[0] += 1
                        PT = PTS[pti]
                        offs = []
                        off = 0
                        for p_ in pieces:
                            offs.append(off)
                            off += p_[2]
                        tot = off

                        def mms(e, pieces=pieces, offs=offs, ps=ps):
                            last = None
                            for (kc, q0, qn), of in zip(pieces, offs):
                                last = e.matmul(ps[:, of:of + qn], KT[r0:r0 + 64, ck, kc * 128:(kc + 1) * 128], QTG[r0:r0 + 64, cq, q0:q0 + qn],
                                                start=True, stop=not use_aug)
                                if use_aug:
                                    last = e.matmul(ps[:, of:of + qn], IND[pb:pb + 8, kc // 2, :], AT[0:8, q0:q0 + qn],
                                                    start=False, stop=True)
                            return last
                        flush(2)
                        sc.add("pe", mms, reads=[f"kt{ck}_{p_[0] // 2}" for p_ in pieces] + ["rC", "ind"] + ([augres] if use_aug else []), writes=[f"ps{bk}"])
                        sc.add("act", (lambda e, PT=PT, ps=ps, tot=tot: e.activation(out=PT[:, 0:tot], in_=ps[:, 0:tot], func=AF.Exp,
                                                                                       scale=0.125, bias=FB[:, s16:s16 + 1])),
                               reads=[f"ps{bk}", "fb"], writes=[PTN[pti]])
                        flags = []
                        for (kc, q0, qn), of in zip(pieces, offs):
                            for sub in range(qn // 128):
                                qt = (q0 + sub * 128) // 128
                                st = done_qt[qt] == 0
                                done_qt[qt] += 1
                                sp_ = done_qt[qt] == n_qt[qt]
                                flags.append((kc, qt, of + sub * 128, st, sp_))

                        def pv(e, flags=flags, PT=PT):
                            last = None
                            for (kc, qt, of, st, sp_) in flags:
                                last = e.matmul(self.psb(qt * 2 + quad).rearrange("p (h d) -> p h d", d=65)[:, hsl, 0:65] if False else
                                                self.oacc(qt * 2 + quad)[:, hsl, :], PT[:, of:of + 128], VA[:, kc, vh, :], start=st, stop=sp_)
                            return last
                        pvq.append((("pe", pv), dict(reads=[PTN[pti]] + [f"va{p_[0] // 2}_{p_[0] % 2}{'a' if grp == 0 else 'b'}" for p_ in pieces],
                                                     writes=[f"ps{quad}", f"ps{2 + quad}"])))
                    bk = 4 + sbank[0] % 3
                    sbank[0] += 1
                    ps = self.psb(bk).rearrange("p (a t) -> p a t", a=4)
                    pti = ptc[0] % 4
                    ptc[0] += 1
                    PT = PTS[pti].rearrange("p (a t) -> p a t", a=4)
                    TMP = TMPS[pti % 2].rearrange("p (a t) -> p a t", a=4)
                    tmpn = f"xn{pti % 2}"
                    ti0 = 4 - len(near)

                    def mmn(e, near=near, ps=ps, ti0=ti0):
                        last = None
                        for k_, (kc, qt, di) in enumerate(near):
                            ti = ti0 + k_
                            ua = use_aug and (kc // 2) < b
                            last = e.matmul(ps[:, ti, :], KT[r0:r0 + 64, ck, kc * 128:(kc + 1) * 128], QTG[r0:r0 + 64, cq, qt * 128:(qt + 1) * 128],
                                            start=True, stop=not ua)
                            if ua:
                                last = e.matmul(ps[:, ti, :], IND[pb:pb + 8, kc // 2, :], AT[0:8, qt * 128:(qt + 1) * 128],
                                                start=False, stop=True)
                        return last
                    flush(2)
                    sc.add("pe", mmn, reads=[f"kt{ck}_{kc // 2}" for (kc, _, _) in near] + ["rC", "ind"] + ([augres] if use_aug else []), writes=[f"ps{bk}"])

                    def biasadd(e, ps=ps, TMP=TMP, ti0=ti0):
                        if ti0 == 0:
                            e.scalar_tensor_tensor(out=TMP[:, 0:2, :], in0=ps[:, 0:2, :], scalar=0.125, in1=DT[:, s16, 0:2, :], op0=ALU.mult, op1=ALU.add)
                        else:
                            e.scalar_tensor_tensor(out=TMP[:, 1:2, :], in0=ps[:, 1:2, :], scalar=0.125, in1=DT[:, s16, 1:2, :], op0=ALU.mult, op1=ALU.add)
                        return e.scalar_tensor_tensor(out=TMP[:, 2:4, :], in0=ps[:, 2:4, :], scalar=0.125, in1=DT[:, s16, 0:2, :], op0=ALU.mult, op1=ALU.add)
                    sc.add("dve", biasadd, reads=[f"ps{bk}", "dt"], writes=[tmpn])
                    sc.add("act", (lambda e, PT=PT, TMP=TMP, ti0=ti0: e.activation(out=PT[:, ti0:4, :], in_=TMP[:, ti0:4, :], func=AF.Exp,
                                                                                       bias=NB[:, s16:s16 + 1])),
                           reads=[tmpn, "nb"], writes=[PTN[pti]])
                    flags = []
                    for k_, (kc, qt, di) in enumerate(near):
                        st = done_qt[qt] == 0
                        done_qt[qt] += 1
                        sp_ = done_qt[qt] == n_qt[qt]
                        flags.append((kc, qt, ti0 + k_, st, sp_))

                    def pvn(e, flags=flags, PT=PT):
                        last = None
                        for (kc, qt, ti, st, sp_) in flags:
                            last = e.matmul(self.oacc(qt * 2 + quad)[:, hsl, :], PT[:, ti, :], VA[:, kc, vh, :], start=st, stop=sp_)
                        return last
                    pvq.append((("pe", pvn), dict(reads=[PTN[pti]] + [f"va{kc // 2}_{kc % 2}{'a' if grp == 0 else 'b'}" for (kc, _, _) in near],
                                                  writes=[f"ps{quad}", f"ps{2 + quad}"])))
                for hs8 in range(8):
                    head(hs8)
                flush(0)
                for qt in range(2):
                    for quad in range(2):
                        bk = qt * 2 + quad
                        oa = self.oacc(bk)
                        if grp == 0:
                            outv = OG[:, qt, quad * 256:(quad + 1) * 256].rearrange("p (h d) -> p h d", h=4)
                            inv = oa[:, :, 0:64]
                        else:
                            outv = OG[:, qt, 512:1024].rearrange("p (r i d) -> p i r d", r=2, i=4)[:, 2 * quad:2 * quad + 2, :, :]
                            inv = oa[:, :, 0:64].rearrange("p (i r) d -> p i r d", r=2)

                        def nrm(e, oa=oa, outv=outv, inv=inv, qt=qt, quad=quad, grp=grp):
                            if grp == 0:
                                e.reciprocal(out=RDEN[:, 0:4], in_=oa[:, :, 64])
                            else:
                                e.tensor_tensor(out=RDEN[:, 0:4], in0=oa[:, :, 64], in1=SINKT[:, qt, 4 * quad:4 * quad + 4], op=ALU.add)
                                e.reciprocal(out=RDEN[:, 0:4], in_=RDEN[:, 0:4])
                            if grp == 0:
                                rb_ = RDEN[:, 0:4].unsqueeze(2).to_broadcast([128, 4, 64])
                            else:
                                rb_ = RDEN[:, 0:4].rearrange("p (i r) -> p i r", r=2).unsqueeze(3).to_broadcast([128, 2, 2, 64])
                            return e.tensor_tensor(out=outv, in0=inv, in1=rb_, op=ALU.mult)
                        def nrm1(e, oa=oa, qt=qt, quad=quad, grp=grp):
                            if grp == 0:
                                return e.reciprocal(out=RDEN[:, 4 * (bk % 2):4 * (bk % 2) + 4], in_=oa[:, :, 64])
                            return e.tensor_tensor(out=RDEN[:, 4 * (bk % 2):4 * (bk % 2) + 4], in0=oa[:, :, 64], in1=SINKT[:, qt, 4 * quad:4 * quad + 4], op=ALU.add)
                        rdn = f"rden{bk % 2}"
                        RD = RDEN[:, 4 * (bk % 2):4 * (bk % 2) + 4]
                        if grp == 0:
                            sc.add("dve", (lambda e, oa=oa, RD=RD: e.reciprocal(out=RD, in_=oa[:, :, 64])), reads=[f"ps{bk}"], writes=[rdn])
                        else:
                            sc.add("dve", (lambda e, oa=oa, RD=RD, qt=qt, quad=quad: e.tensor_tensor(out=RD, in0=oa[:, :, 64], in1=SINKT[:, qt, 4 * quad:4 * quad + 4], op=ALU.add)),
                                   reads=[f"ps{bk}", "sinkt"], writes=[rdn])
                            sc.add("dve", (lambda e, RD=RD: e.reciprocal(out=RD, in_=RD)), reads=[rdn], writes=[rdn])
                        if grp == 0:
                            rb_ = RD.unsqueeze(2).to_broadcast([128, 4, 64])
                        else:
                            rb_ = RD.rearrange("p (i r) -> p i r", r=2).unsqueeze(3).to_broadcast([128, 2, 2, 64])
                        sc.add("dve", (lambda e, outv=outv, inv=inv, rb_=rb_: e.tensor_tensor(out=outv, in0=inv, in1=rb_, op=ALU.mult)),
                               reads=[f"ps{bk}", rdn], writes=["rA"])
            if getattr(self, 'stop', 99) <= 7:
                return
            for half in range(2):
                bk = 4 + half
                psT = self.psb(bk, BF16).rearrange("p (c t) -> p c t", c=4)

                def trO(e, half=half, psT=psT):
                    last = None
                    for cc in range(4):
                        c = half * 4 + cc
                        for qt in range(2):
                            last = e.transpose(psT[:, cc, qt * 128:(qt + 1) * 128], OG[:, qt, c * 128:(c + 1) * 128], IDENT)
                    return last
                sc.add("pe", trO, reads=["rA", "ident"], writes=[f"ps{bk}"])
                if half == 0:
                    sc.add("act", (lambda e, psT=psT: e.activation(out=OTG[:, 0:4, :], in_=psT, func=AF.Copy)), reads=[f"ps{bk}"], writes=["rB"])
                else:
                    sc.add("dve", (lambda e, psT=psT: e.tensor_copy(out=OTG[:, 4:8, :], in_=psT)), reads=[f"ps{bk}"], writes=["rBb"])
            if getattr(self, 'stop', 99) <= 8:
                return
            for o_ in range(8):
                bk = 4 + o_ % 3
                ps = self.psb(bk)

                def mmo(e, o_=o_, ps=ps):
                    last = None
                    for c in range(8):
                        last = e.matmul(ps[:, 0:256], WOUT[:, c, o_ * 128:(o_ + 1) * 128], OTG[:, c, :], start=(c == 0), stop=(c == 7))
                    return last
                sc.add("pe", mmo, reads=["wout", "rB", "rBb"], writes=[f"ps{bk}"])
                ysl = YSBA[:, o_, :]
                sc.add("act", (lambda e, ps=ps, ysl=ysl: e.activation(out=ysl, in_=ps[:, 0:256], func=AF.Copy)),
                       reads=[f"ps{bk}", "rA", "rC"], writes=[f"ysa{o_}"])
                sqb = PTS[o_ % 4]
                sc.add("dve", (lambda e, ysl=ysl, sqb=sqb: e.tensor_tensor(out=sqb[:, 0:256], in0=ysl, in1=ysl, op=ALU.mult)),
                       reads=[f"ysa{o_}"], writes=[PTN[o_ % 4]])
                sc.add("pe", (lambda e, sqb=sqb, o_=o_: e.matmul(self.psb(7)[:, 0:256], self.ONES, sqb[:, 0:256], start=(o_ == 0), stop=(o_ == 7))),
                       reads=[PTN[o_ % 4], "ones"], writes=["ps7"])
            if getattr(self, 'stop', 99) <= 9:
                return
            self.resid_update(l, 0, t0, 256, YSBA, (lambda c: f"ysa{c}"), 7)
            sc.marker(reads=[f"ysa{c}" for c in range(8)], writes=["rA", "rC"])

        for g in range(getattr(self, 'ngroups', 8)):
            group(g)
        sc.marker(writes=["bmask", "gm", "cmp", "selr", "augb_s", "augb_m", "selfm", "sh8", "sinkt", "sqrt_t", "nb", "fb", "wada_ok"])

    def oacc(self, bk):
        return self.PS[bk][:, 0:260].rearrange("p (h d) -> p h d", d=65)


def _host_prep(inputs):
    f = np.float32
    w_ada = np.asarray(inputs["w_ada"], f)
    w1 = np.asarray(inputs["w1"], f)
    w2 = np.asarray(inputs["w2"], f)
    w_in = np.asarray(inputs["w_in"], f)
    w_out = np.asarray(inputs["w_out"], f)
    sh = {}
    sh["wada"] = np.ascontiguousarray(w_ada.reshape(DEPTH, 8, 128, 24, 256).transpose(0, 3, 2, 1, 4))
    sh["badac"] = np.ascontiguousarray(np.asarray(inputs["b_ada"], f).reshape(DEPTH, 48, 128).transpose(2, 0, 1).reshape(128, DEPTH * 48))
    sh["gainc"] = np.ascontiguousarray(np.asarray(inputs["norm_gains"], f).reshape(DEPTH, 4, 8, 128).transpose(3, 0, 1, 2).reshape(128, 128))
    sh["b1c"] = np.ascontiguousarray(np.asarray(inputs["b1"], f).reshape(DEPTH, 32, 128).transpose(2, 0, 1).reshape(128, 128))
    sh["b2c"] = np.ascontiguousarray(np.asarray(inputs["b2"], f).reshape(DEPTH, 8, 128).transpose(2, 0, 1).reshape(128, 32))
    sh["w1r"] = np.ascontiguousarray(w1.reshape(DEPTH, 8, 128, 16, 256).transpose(0, 3, 2, 1, 4))
    sh["w2r"] = np.ascontiguousarray(w2.reshape(DEPTH, 2, 16, 128, 8, 128).transpose(0, 4, 1, 3, 2, 5))
    perm = [(k // 2) + 4 * (k % 2) for k in range(8)]
    colidx = np.arange(DIN)
    qb = colidx[1536:2048].reshape(8, 64)[perm].reshape(-1)
    colidx = np.concatenate([colidx[:1536], qb, colidx[2048:]])
    w_in_p = w_in[:, :, colidx]
    sh["winr"] = np.ascontiguousarray(w_in_p.reshape(DEPTH, 8, 128, DIN).transpose(0, 2, 1, 3))
    sh["woutr"] = np.ascontiguousarray(w_out.reshape(DEPTH, 8, 128, D).transpose(0, 2, 1, 3))
    sh["sinks"] = np.ascontiguousarray(np.asarray(inputs["sinks"], f)[:, perm])
    rb = np.asarray(inputs["rel_bias"], f)
    hperm = list(range(8)) + [8 + p for p in perm]
    rb = rb[:, hperm]
    sh["rbT"] = np.ascontiguousarray(rb.T).reshape(1, 16 * 32)
    tab = np.concatenate([rb, np.full((1, 16), NEG, f)], axis=0)
    idx = _dtile_index()
    dt = np.zeros((128, 16, 2, 128), f)
    for h in range(16):
        hg = 0 if h < 8 else 1
        for t in range(2):
            dt[:, h, t, :] = tab[idx[hg, t], h]
    sh["dtile"] = dt.reshape(128, 16 * 2 * 128)
    sh["ident"] = np.eye(128, dtype=f)
    hsel = np.zeros((128, 2, 128), f)
    hsel[0:64, 0, :] = 1.0
    hsel[64:128, 1, :] = 1.0
    sh["hsel"] = hsel.reshape(128, 256)
    hind = np.zeros((128, 2), f)
    hind[0:64, 0] = 1.0
    hind[64:128, 1] = 1.0
    sh["hind"] = hind
    ind = np.zeros((72, 8, 128), f)
    for j in range(8):
        for pb in (0, 32, 64):
            ind[pb + j, j, :] = 1.0
    sh["indall"] = ind.reshape(72, 1024)
    bm = np.zeros((128, 3, 8, 8), f)
    for b in range(8):
        for j in range(8):
            bm[:, 0, b, j] = 0.0 if j < b else -1e30
            bm[:, 1, b, j] = 1.0 if j < b else 0.0
            bm[:, 2, b, j] = 1.0 if j == b else 0.0
    sh["bmask"] = bm.reshape(128, 192)
    x = np.asarray(inputs["x"], f)
    c = np.asarray(inputs["c"], f)
    per = []
    for b in range(x.shape[0]):
        m = dict(sh)
        m["xT"] = np.ascontiguousarray(x[b].T)
        m["cT"] = np.ascontiguousarray(c[b].reshape(8, 128).T)
        per.append(m)
    return per


_PROG_CACHE = {}


def _get_prog(phases, debug=False, ngroups=8, stop=99):
    key = (tuple(phases), debug, ngroups, stop)
    if key not in _PROG_CACHE:
        _PROG_CACHE[key] = Prog(list(phases), debug=debug, ngroups=ngroups, stop=stop)
    return _PROG_CACHE[key]


def run_phases(inputs, phases, n_cores=8, trace=False, debug=False, ngroups=8, stop=99):
    per = _host_prep(inputs)[:n_cores]
    prog = _get_prog(phases, debug, ngroups, stop)
    res = run_bass_kernel_spmd(prog.nc, per, core_ids=list(range(n_cores)), trace=trace)
    outs = [np.ascontiguousarray(r["outT"].T) for r in res.results]
    return np.stack(outs, axis=0), res


def kernel(**inputs):
    phases = []
    for l in range(DEPTH):
        phases += [("attn", l), ("ffn", l)]
    out, _ = run_phases(inputs, phases)
    return out.astype(np.float32)
```

```python
import math
import numpy as np
import concourse.bass as bass
import concourse.mybir as mybir
from concourse.bass_utils import run_bass_kernel_spmd

F32 = mybir.dt.float32
BF16 = mybir.dt.bfloat16
AF = mybir.ActivationFunctionType
ALU = mybir.AluOpType
AX = mybir.AxisListType

D = 1024
S = 2048
DEPTH = 4
DFF = 4096
DIN = 2304
EPS = 1e-6
NEG = -30000.0
BIG = 1024.0
MOBA_ROUND = "trunc"
SWA_ROUND = "trunc"


class Sched:
    def __init__(self):
        self.ops = []
        self.last_w = {}
        self.readers = {}

    def marker(self, reads=(), writes=()):
        k = getattr(self, "_mk", 0)
        self._mk = k + 1
        col = k % 8
        dm = self.dummy
        self.add("dve", (lambda e: e.memset(dm[:, col:col + 1], 0.0)), reads=reads, writes=list(writes) + [f"dummy{col}"])

    def barrier(self):
        names = set(self.last_w) | set(self.readers)
        names.add("__bar__")
        self.marker(writes=sorted(names))

    def add(self, eng, fn, reads=(), writes=(), dma=None, ndma=1, total=False):
        idx = len(self.ops)
        reads = tuple(reads) + ("__bar__",)
        writes = tuple(writes)
        deps = set()
        for r in reads:
            w = self.last_w.get(r)
            if w is not None:
                deps.add(w)
        for w_ in writes:
            w = self.last_w.get(w_)
            if w is not None:
                deps.add(w)
            for rd in self.readers.get(w_, ()):
                deps.add(rd)
        for r in reads:
            self.readers.setdefault(r, []).append(idx)
        for w_ in writes:
            self.last_w[w_] = idx
            self.readers[w_] = []
        self.ops.append(dict(eng=eng, fn=fn, deps=deps, dma=dma, ndma=ndma, total=total,
                             reads=set(reads), writes=set(writes)))
        return idx

    def finalize(self, nc, semctx):
        ops = self.ops
        need = [False] * len(ops)
        for i, o in enumerate(ops):
            keep = set()
            for d in o["deps"]:
                p = ops[d]
                if p["dma"] is not None or o["dma"] is not None:
                    keep.add(d)
                elif p["eng"] != o["eng"]:
                    keep.add(d)
                else:
                    if o["eng"] != "pe":
                        keep.add(d)
            o["deps"] = keep
            for d in keep:
                need[d] = True
        sems = {}

        def getsem(name):
            if name not in sems:
                sems[name] = semctx(name)
            return sems[name]

        cnt = {}
        totals = {}
        for i, o in enumerate(ops):
            if o["dma"] is not None:
                key = "d_" + o["dma"]
                cnt[key] = cnt.get(key, 0) + 16 * o["ndma"]
                o["sig"] = (key, cnt[key])
                if o["total"]:
                    totals[key] = True
            elif need[i]:
                key = "e_" + o["eng"]
                cnt[key] = cnt.get(key, 0) + 1
                o["sig"] = (key, cnt[key])
            else:
                o["sig"] = None
        for o in ops:
            if o["dma"] is not None and o["total"]:
                o["sig"] = (o["sig"][0], cnt[o["sig"][0]])
        for o in ops:
            w = {}
            for d in o["deps"]:
                k, v = ops[d]["sig"]
                if w.get(k, 0) < v:
                    w[k] = v
            o["waits"] = w
        for k in cnt:
            getsem(k)
        self.sems = sems
        self.cnt = cnt

    def emit(self, eng, e):
        waited = {}
        n = 0
        for o in self.ops:
            if o["eng"] != eng:
                continue
            for k in sorted(o["waits"]):
                v = o["waits"][k]
                if waited.get(k, 0) < v:
                    e.wait_ge(self.sems[k], v)
                    waited[k] = v
            ins = o["fn"](e)
            n += 1
            if o["dma"] is not None:
                if not isinstance(ins, (list, tuple)):
                    ins = [ins]
                assert len(ins) == o["ndma"], (len(ins), o["ndma"])
                for i_ in ins:
                    i_.then_inc(self.sems[o["sig"][0]], 16)
            elif o["sig"] is not None:
                if isinstance(ins, (list, tuple)):
                    ins = ins[-1]
                ins.then_inc(self.sems[o["sig"][0]], 1)
        return n


def _t5_bucket_np(dist, mode):
    n = np.maximum(dist, 0).astype(np.int32)
    nf = np.maximum(n, 1).astype(np.float32)
    val = (np.log(nf / np.float32(16)) / np.float32(math.log(128 / 16)) * np.float32(16)).astype(np.float32)
    if mode == "trunc":
        li = val.astype(np.int32)
    else:
        li = np.rint(val).astype(np.int32)
    large = np.minimum(16 + li, 31)
    return np.where(n < 16, n, large)


def _dtile_index():
    k = np.arange(128)[:, None]
    q = np.arange(128)[None, :]
    out = np.zeros((2, 2, 128, 128), np.int64)
    for hg, mode in ((0, MOBA_ROUND), (1, SWA_ROUND)):
        d0 = q - k
        b0 = _t5_bucket_np(d0, mode)
        out[hg, 1] = np.where(d0 >= 0, b0, 32)
        d1 = 128 + q - k
        b1 = _t5_bucket_np(d1, mode)
        if hg == 0:
            out[hg, 0] = b1
        else:
            out[hg, 0] = np.where(d1 < 128, b1, 32)
    return out


class Prog:
    def __init__(self, phases, debug=False, ngroups=8, stop=99):
        self.phases = phases
        self.stop = stop
        self.ngroups = ngroups
        self.debug = debug
        self.dbg_names = []
        self.nc = bass.Bass("TRN2", target_bir_lowering=False)
        self.sc = Sched()
        self.build()

    def dram_in(self, name, shape, dt=F32):
        return self.nc.dram_tensor(name, list(shape), dt, kind="ExternalInput").ap()

    def build(self):
        nc = self.nc
        sc = self.sc
        self.d_xT = self.dram_in("xT", [D, S])
        self.d_cT = self.dram_in("cT", [128, 8])
        self.d_wada = self.dram_in("wada", [DEPTH, 24, 128, 8, 256])
        self.d_badac = self.dram_in("badac", [128, DEPTH * 48])
        self.d_gainc = self.dram_in("gainc", [128, 128])
        self.d_b1c = self.dram_in("b1c", [128, 128])
        self.d_b2c = self.dram_in("b2c", [128, 32])
        self.d_w1r = self.dram_in("w1r", [DEPTH, 16, 128, 8, 256])
        self.d_w2r = self.dram_in("w2r", [DEPTH, 8, 2, 128, 16, 128])
        self.d_winr = self.dram_in("winr", [DEPTH, 128, 8, DIN])
        self.d_woutr = self.dram_in("woutr", [DEPTH, 128, 8, D])
        self.d_sinks = self.dram_in("sinks", [DEPTH, 8])
        self.d_rbT = self.dram_in("rbT", [1, 16 * 32])
        self.d_dtile = self.dram_in("dtile", [128, 16 * 2 * 128])
        self.d_ident = self.dram_in("ident", [128, 128])
        self.d_hsel = self.dram_in("hsel", [128, 2 * 128])
        self.d_hind = self.dram_in("hind", [128, 2])
        self.d_indall = self.dram_in("indall", [72, 8 * 128])
        self.d_bmask = self.dram_in("bmask", [128, 3 * 64])
        self.d_out = nc.dram_tensor("outT", [D, S], F32, kind="ExternalOutput").ap()

        total_words = 53200
        self.pool = nc.alloc_sbuf_tensor("pool", [128, total_words], F32)
        self.off = 0

        def alloc(words):
            o = self.off
            self.off += (words + 7) // 8 * 8
            assert self.off <= total_words, (self.off, total_words)
            return o

        def view(o, words, dt=F32):
            v = self.pool[:, o:o + words]
            if dt != F32:
                v = v.bitcast(dt)
            return v

        self.view = view
        o_x = alloc(8 * S)
        self.XT = view(o_x, 8 * S).rearrange("p (c t) -> p c t", c=8)
        self.COLS = view(alloc(320), 320)
        self.GAINC = view(alloc(128), 128)
        self.B1C = view(alloc(128), 128)
        self.B2C = view(alloc(32), 32)
        self.BADAC = view(alloc(192), 192)
        self.MODC = view(alloc(48), 48)
        self.CT = view(alloc(8), 8)
        self.CACT = view(alloc(8), 8, BF16)[:, 0:8]
        self.IDENT = view(alloc(64), 64, BF16)
        self.ONES = view(alloc(64), 64, BF16)
        self.SQ = [view(alloc(512), 512, BF16).rearrange("p (a t) -> p a t", a=2) for _ in range(2)]
        self.rstd_off = self.off
        self.RSTD = [view(alloc(512), 512) for _ in range(2)]
        self.XN = [view(alloc(512), 512) for _ in range(2)]
        self.wada_off = self.off
        self.WADA = [view(alloc(1024), 1024, BF16).rearrange("p (k n) -> p k n", k=8) for _ in range(2)]
        self.DUMMY = view(alloc(8), 8)
        sc.dummy = self.DUMMY
        self.EPSC = view(alloc(8), 8)
        self.phase_base = self.off

        self.PS = [nc.alloc_psum_tensor(f"psb{i}", [128, 512], F32) for i in range(8)]

        self.preamble()
        done_mod = set()
        self.side = []
        for pi, (kind, l) in enumerate(self.phases):
            if l not in done_mod:
                self.mod_layer(l)
                done_mod.add(l)
            sc.barrier()
            if kind == "attn":
                self.attn_phase(l)
            else:
                nxt = [ll for (_, ll) in self.phases[pi + 1:] if ll not in done_mod]
                if nxt:
                    self.side = self.mod_jobs(nxt[0])
                    done_mod.add(nxt[0])
                self.ffn_phase(l)
                self.run_side(100)
        sc.barrier()
        self.epilogue()

        class _SemCtx:
            pass
        semlist = []

        def semctx(name):
            cm = nc.semaphore(name)
            h = cm.__enter__()
            semlist.append(cm)
            return h

        sc.finalize(nc, semctx)
        with nc.Block() as block:
            @block.tensor
            def _(e):
                sc.emit("pe", e)

            @block.scalar
            def _(e):
                sc.emit("act", e)

            @block.vector
            def _(e):
                sc.emit("dve", e)

            @block.gpsimd
            def _(e):
                sc.emit("pool", e)

            @block.sync
            def _(e):
                sc.emit("sp", e)

    def dump(self, name, ap, reads):
        if not getattr(self, "debug", False):
            return
        shp = list(ap.shape)
        d = self.nc.dram_tensor("dbg_" + name, shp, ap.dtype, kind="ExternalOutput").ap()
        self.sc.add("sp", (lambda e: e.dma_start(out=d, in_=ap)), reads=list(reads), writes=["dbg_" + name], dma="dbg_" + name)
        self.dbg_names.append("dbg_" + name)

    def psb(self, i, dt=F32):
        v = self.PS[i][:, :]
        if dt != F32:
            v = v.bitcast(dt)
        return v

    def preamble(self):
        sc = self.sc
        XT = self.XT
        xs = self.d_xT.rearrange("(c p) t -> p c t", p=128)
        for c in range(8):
            sc.add("sp", (lambda e, c=c: e.dma_start(out=XT[:, c, :], in_=xs[:, c, :])),
                   writes=[f"xTc{c}"], dma=f"xin{c}")
        small = [(self.GAINC, self.d_gainc, "gainc"), (self.B1C, self.d_b1c, "b1c"), (self.B2C, self.d_b2c, "b2c"),
                 (self.BADAC, self.d_badac, "badac"), (self.CT, self.d_cT, "ct")]
        for (dst, src, nm) in small:
            sc.add("sp", (lambda e, dst=dst, src=src: e.dma_start(out=dst, in_=src[:, :])),
                   writes=[nm], dma="c_" + nm)
        sc.add("pool", (lambda e: e.dma_start(out=self.IDENT, in_=self.d_ident[:, :])), writes=["ident"], dma="c_ident")
        sc.add("dve", (lambda e: e.memset(self.ONES, 1.0)), writes=["ones"])
        sc.add("dve", (lambda e: e.memset(self.EPSC, float(D * EPS))), writes=["epsc"])
        sc.add("act", (lambda e: e.activation(out=self.CACT, in_=self.CT, func=AF.Silu)), reads=["ct"], writes=["cact"])
        sc.marker(reads=[f"xTc{c}" for c in range(8)], writes=[f"xT{tb}" for tb in range(8)] + ["rgnA_ok", "rgnB_ok"])

    def epilogue(self):
        sc = self.sc
        XT = self.XT
        od = self.d_out.rearrange("(c p) t -> p c t", p=128)
        for c in range(8):
            sc.add("sp", (lambda e, c=c: e.dma_start(out=od[:, c, :], in_=XT[:, c, :])),
                   reads=[f"xT{tb}" for tb in range(8)], writes=[f"out{c}"], dma=f"xout{c}")
        sc.add("sp", (lambda e: e.nop()), reads=[f"out{c}" for c in range(8)])

    def mod_layer(self, l):
        for j in self.mod_jobs(l):
            j()

    def run_side(self, n=1):
        for _ in range(n):
            if self.side:
                self.side.pop(0)()

    def mod_jobs(self, l):
        jobs = []
        for pc in range(24):
            jobs.append(lambda pc=pc: self.mod_piece(l, pc))
        jobs.append(lambda: self.mod_finish(l))
        return jobs

    def mod_piece(self, l, pc):
        sc = self.sc
        ps = self.psb(7)
        if True:
            buf = self.WADA[pc % 2]
            bn = f"wada{pc % 2}"
            src = self.d_wada[l, pc]
            sc.add("pool", (lambda e, buf=buf, src=src: e.dma_start(out=buf, in_=src)), reads=["wada_ok"], writes=[bn], dma=bn)

            def mm(e, buf=buf, pc=pc):
                last = None
                for j in range(2):
                    col = pc * 2 + j
                    for kc in range(8):
                        last = e.matmul(ps[:, col:col + 1], buf[:, kc, j * 128:(j + 1) * 128],
                                        self.CACT[:, kc:kc + 1], start=(kc == 0), stop=(kc == 7))
                return last
            sc.add("pe", mm, reads=[bn, "cact"], writes=["ps7"])
    def mod_finish(self, l):
        sc = self.sc
        ps = self.psb(7)
        MODC = self.MODC
        sc.add("dve", (lambda e: e.tensor_tensor(out=MODC, in0=ps[:, 0:48], in1=self.BADAC[:, l * 48:(l + 1) * 48], op=ALU.add)),
               reads=["ps7", "badac"], writes=["modc"])
        C = self.COLS
        b = l * 64
        G = self.GAINC
        g0 = (l * 4) * 8

        def cols(e):
            e.scalar_tensor_tensor(out=C[:, b + 0:b + 8], in0=MODC[:, 8:16], scalar=1.0, in1=G[:, g0 + 0:g0 + 8], op0=ALU.add, op1=ALU.mult)
            e.tensor_copy(out=C[:, b + 8:b + 16], in_=MODC[:, 0:8])
            e.tensor_tensor(out=C[:, b + 16:b + 24], in0=MODC[:, 16:24], in1=G[:, g0 + 8:g0 + 16], op=ALU.mult)
            e.scalar_tensor_tensor(out=C[:, b + 24:b + 32], in0=MODC[:, 32:40], scalar=1.0, in1=G[:, g0 + 16:g0 + 24], op0=ALU.add, op1=ALU.mult)
            e.tensor_copy(out=C[:, b + 32:b + 40], in_=MODC[:, 24:32])
            return e.tensor_tensor(out=C[:, b + 40:b + 48], in0=MODC[:, 40:48], in1=G[:, g0 + 24:g0 + 32], op=ALU.mult)
        sc.add("dve", cols, reads=["modc", "gainc"], writes=[f"colsraw{l}"])

        def cols2(e):
            e.tensor_scalar(out=C[:, b + 0:b + 8], in0=C[:, b + 0:b + 8], scalar1=32.0, scalar2=None, op0=ALU.mult)
            e.tensor_scalar(out=C[:, b + 16:b + 32], in0=C[:, b + 16:b + 32], scalar1=32.0, scalar2=None, op0=ALU.mult)
            return e.tensor_scalar(out=C[:, b + 40:b + 48], in0=C[:, b + 40:b + 48], scalar1=32.0, scalar2=None, op0=ALU.mult)
        sc.add("dve", cols2, reads=[f"colsraw{l}"], writes=[f"cols{l}"])
        self.dump(f"cols{l}", C[:, b:b + 48], [f"cols{l}"])
        self.dump(f"modc{l}", MODC, [f"cols{l}"])

    def rmsnorm_in(self, l, sub, t0, n, HT, ht_res, psbank, extra_reads=(), sq_names=None):
        sc = self.sc
        XT = self.XT
        tbs = [f"xT{tb}" for tb in range(t0 // 256, (t0 + n) // 256)]
        ps = self.psb(psbank)
        cb = l * 64 + (0 if sub == 0 else 24)
        C = self.COLS
        for cp in range(4):
            sq = self.SQ[cp % 2]
            sqn = f"sq{cp % 2}"
            sqw = [sqn] if sq_names is None else sq_names[cp % 2]
            sc.add("act", (lambda e, cp=cp, sq=sq: e.activation(out=sq[:, :, 0:n], in_=XT[:, 2 * cp:2 * cp + 2, t0:t0 + n], func=AF.Square)),
                   reads=tbs, writes=sqw)

            def mm(e, cp=cp, sq=sq):
                last = None
                for j in range(2):
                    c = 2 * cp + j
                    last = e.matmul(ps[:, 0:n], self.ONES, sq[:, j, 0:n], start=(c == 0), stop=(c == 7))
                return last
            sc.add("pe", mm, reads=[sqn, "ones"], writes=[f"ps{psbank}"])
        rs = self.RSTD[0]
        sc.add("act", (lambda e: e.activation(out=rs[:, 0:n], in_=ps[:, 0:n], func=AF.Ln, bias=self.EPSC[:, 0:1], scale=1.0)),
               reads=[f"ps{psbank}", "epsc"], writes=["rstd0p", "rstd0"])
        sc.add("act", (lambda e: e.activation(out=rs[:, 0:n], in_=rs[:, 0:n], func=AF.Exp, scale=-0.5)),
               reads=["rstd0p"], writes=["rstd0", "rstd0p"])
        nn = 1 if n > 256 else 2
        for ci, c0 in enumerate(range(0, 8, nn)):
            xn = self.XN[ci % 2]
            xnn = f"xn{ci % 2}"
            xv = xn[:, 0:nn * n].rearrange("p (a t) -> p a t", a=nn)
            sc.add("dve", (lambda e, c0=c0, xv=xv: e.tensor_tensor(out=xv, in0=XT[:, c0:c0 + nn, t0:t0 + n],
                                                                    in1=rs[:, 0:n].unsqueeze(1).to_broadcast([128, nn, n]), op=ALU.mult)),
                   reads=tbs + ["rstd0"], writes=[xnn])
            for a in range(nn):
                c = c0 + a
                sc.add("act", (lambda e, c=c, a=a, xv=xv: e.activation(out=HT[:, c, 0:n], in_=xv[:, a, :], func=AF.Identity,
                                                                       scale=C[:, cb + c:cb + c + 1], bias=C[:, cb + 8 + c:cb + 9 + c])),
                       reads=[xnn, f"cols{l}"] + list(extra_reads), writes=[ht_res])

    def resid_update(self, l, sub, t0, n, Y, y_res, ssbank):
        sc = self.sc
        XT = self.XT
        tbs = [f"xT{tb}" for tb in range(t0 // 256, (t0 + n) // 256)]
        ps = self.psb(ssbank)
        C = self.COLS
        cb = l * 64 + (16 if sub == 0 else 40)
        rs = self.RSTD[1]
        sc.add("act", (lambda e: e.activation(out=rs[:, 0:n], in_=ps[:, 0:n], func=AF.Ln, bias=self.EPSC[:, 0:1], scale=1.0)),
               reads=[f"ps{ssbank}", "epsc"], writes=["rstd1p", "rstd1"])
        sc.add("act", (lambda e: e.activation(out=rs[:, 0:n], in_=rs[:, 0:n], func=AF.Exp, scale=-0.5)),
               reads=["rstd1p"], writes=["rstd1", "rstd1p"])
        if t0 == 0 and sub == 1:
            self.dump("rstd1", rs, ["rstd1"])
            self.dump("ysb", Y, [y_res(c) for c in range(8)])
        yall = [y_res(c) for c in range(8)]
        sc.add("dve", (lambda e: e.tensor_tensor(out=Y[:, :, 0:n], in0=Y[:, :, 0:n], in1=C[:, cb:cb + 8].unsqueeze(2).to_broadcast([128, 8, n]), op=ALU.mult)),
               reads=yall + [f"cols{l}"], writes=yall)
        sc.add("dve", (lambda e: e.tensor_tensor(out=Y[:, :, 0:n], in0=Y[:, :, 0:n], in1=rs[:, 0:n].unsqueeze(1).to_broadcast([128, 8, n]), op=ALU.mult)),
               reads=yall + ["rstd1"], writes=yall)
        sc.add("dve", (lambda e: e.tensor_tensor(out=XT[:, :, t0:t0 + n], in0=XT[:, :, t0:t0 + n], in1=Y[:, :, 0:n], op=ALU.add)),
               reads=yall + tbs, writes=tbs)

    def ffn_phase(self, l):
        sc = self.sc
        view = self.view
        base = self.phase_base
        o = base
        HID = view(o, 32 * 1024 // 2, BF16).rearrange("p (m t) -> p m t", m=32); o += 16384
        rgn = o; o += 8192
        HT = view(rgn, 4096, BF16).rearrange("p (c t) -> p c t", c=8)
        W1B = [view(rgn + 4096 + i * 1024, 1024, BF16).rearrange("p (k n) -> p k n", k=8) for i in range(3)]
        RL = [view(rgn + 4096 + 3072 + i * 512, 512) for i in range(2)]
        YSB = view(rgn, 8192).rearrange("p (c t) -> p c t", c=8)
        W2B = [view(o + i * 1024, 1024, BF16).rearrange("p (k n) -> p k n", k=16) for i in range(3)]; o += 3072
        assert o <= 53200, o
        B1C, B2C = self.B1C, self.B2C
        w1cnt = 0
        w2cnt = 0
        for H in range(2):
            T0 = H * 1024
            region_users = ["rgnA_ok"]
            for tg in range(2):
                self.rmsnorm_in(l, 1, T0 + tg * 512, 512, HT[:, :, tg * 512:(tg + 1) * 512], f"ht{tg}", 6,
                                extra_reads=region_users)
            if H == 0:
                self.dump("ht", HT, ["ht0", "ht1"])
            psi = 0
            for g in range(16):
                self.run_side(1)
                wb = W1B[w1cnt % 3]; wn = f"w1b{w1cnt % 3}"; w1cnt += 1
                src = self.d_w1r[l, g]
                sc.add("pool", (lambda e, wb=wb, src=src: e.dma_start(out=wb, in_=src)), reads=region_users, writes=[wn], dma=wn)
                for mm_ in range(2):
                    m = 2 * g + mm_
                    for tg in range(2):
                        bank = psi % 4; psi += 1
                        ps = self.psb(bank)

                        def mm(e, wb=wb, mm_=mm_, tg=tg, ps=ps):
                            last = None
                            for c in range(8):
                                last = e.matmul(ps, wb[:, c, mm_ * 128:(mm_ + 1) * 128], HT[:, c, tg * 512:(tg + 1) * 512],
                                                start=(c == 0), stop=(c == 7))
                            return last
                        sc.add("pe", mm, reads=[wn, f"ht{tg}"], writes=[f"ps{bank}"])
                        rl = RL[psi % 2]; rln = f"rl{psi % 2}"
                        sc.add("act", (lambda e, rl=rl, ps=ps, m=m: e.activation(out=rl, in_=ps, func=AF.Relu,
                                                                                 bias=B1C[:, l * 32 + m:l * 32 + m + 1])),
                               reads=[f"ps{bank}", "b1c"] + region_users, writes=[rln])
                        sc.add("dve", (lambda e, rl=rl, m=m, tg=tg: e.tensor_tensor(out=HID[:, m, tg * 512:(tg + 1) * 512], in0=rl, in1=rl, op=ALU.mult)),
                               reads=[rln], writes=[f"hid{m}_{tg}"])
            if H == 0:
                self.dump("hid", HID, [f"hid{m}_{tg}" for m in range(32) for tg in range(2)])
            sc.marker(writes=["ht0", "ht1", "w1b0", "w1b1", "w1b2", "rl0", "rl1", "rgnB_ok"])
            ht_users = ["rgnB_ok"]
            for o_ in range(8):
                wbs = []
                for kh in range(2):
                    wb = W2B[w2cnt % 3]; wn = f"w2b{w2cnt % 3}"; w2cnt += 1
                    src = self.d_w2r[l, o_, kh]
                    sc.add("pool", (lambda e, wb=wb, src=src: e.dma_start(out=wb, in_=src)), writes=[wn], dma=wn)
                    wbs.append((wb, wn))
                banks = [(o_ % 2) * 2, (o_ % 2) * 2 + 1]
                for kh in range(2):
                    wb, wn = wbs[kh]
                    for tg in range(2):
                        ps = self.psb(banks[tg])

                        def mm(e, wb=wb, kh=kh, tg=tg, ps=ps):
                            last = None
                            for kk in range(16):
                                m = kh * 16 + kk
                                last = e.matmul(ps, wb[:, kk, :], HID[:, m, tg * 512:(tg + 1) * 512],
                                                start=(m == 0), stop=(m == 31))
                            return last
                        sc.add("pe", mm, reads=[wn] + [f"hid{kh * 16 + kk}_{tg}" for kk in range(16)], writes=[f"ps{banks[tg]}"])
                for tg in range(2):
                    ps = self.psb(banks[tg])
                    ysl = YSB[:, o_, tg * 512:(tg + 1) * 512]
                    sc.add("act", (lambda e, ps=ps, ysl=ysl, o_=o_: e.activation(out=ysl, in_=ps, func=AF.Identity,
                                                                                  bias=B2C[:, l * 8 + o_:l * 8 + o_ + 1])),
                           reads=[f"ps{banks[tg]}", "b2c"] + ht_users, writes=[f"ysb{o_}t{tg}"])
                    sq = self.SQ[tg][:, 0, :]
                    sc.add("dve", (lambda e, ysl=ysl, sq=sq: e.tensor_tensor(out=sq, in0=ysl, in1=ysl, op=ALU.mult)),
                           reads=[f"ysb{o_}t{tg}"], writes=[f"sq{tg}"])
                    ssb = 4 + tg
                    sc.add("pe", (lambda e, sq=sq, ssb=ssb, o_=o_: e.matmul(self.psb(ssb), self.ONES, sq, start=(o_ == 0), stop=(o_ == 7))),
                           reads=[f"sq{tg}", "ones"], writes=[f"ps{ssb}"])
            for tg in range(2):
                self.resid_update(l, 1, T0 + tg * 512, 512, YSB[:, :, tg * 512:(tg + 1) * 512],
                                  (lambda c, tg=tg: f"ysb{c}t{tg}"), 4 + tg)
            sc.marker(writes=[f"ysb{c}t{tg}" for c in range(8) for tg in range(2)] + ["rgnA_ok"])

    def attn_phase(self, l):
        sc = self.sc
        view = self.view
        XT = self.XT
        o = self.phase_base
        KT = view(o, 5120, BF16).rearrange("p (c t) -> p c t", c=5); o += 5120
        VA = view(o, 5200, BF16).rearrange("p (t h d) -> p t h d", t=16, h=10); o += 5200
        WIN = view(o, 9216, BF16).rearrange("p (k n) -> p k n", k=8); o += 9216
        WOUT = view(o, 4096, BF16).rearrange("p (k n) -> p k n", k=8); o += 4096
        DT = view(o, 2048, BF16).rearrange("p (h t q) -> p h t q", h=16, t=2); o += 2048
        rA = o; o += 1024
        rC = o; o += 1024
        rB = o; o += 1024
        HTG = view(rA, 1024, BF16).rearrange("p (c t) -> p c t", c=8)
        OG = view(rA, 1024, BF16).rearrange("p (q f) -> p q f", q=2)
        QTG = view(rC, 1024, BF16).rearrange("p (c t) -> p c t", c=8)
        QSQ = view(rB, 1024, BF16).rearrange("p (c t) -> p c t", c=8)
        OTG = view(rB, 1024, BF16).rearrange("p (c t) -> p c t", c=8)
        YSBA = view(rA, 2048).rearrange("p (c t) -> p c t", c=8)
        AUGT1 = [view(o + i * 128, 128, BF16) for i in range(3)]; o += 384
        IND = view(o, 512, BF16).rearrange("p (j k) -> p j k", j=8); o += 512
        HIND = view(o, 8, BF16)[:, 0:2]; o += 8
        KMEANT = view(o, 32, BF16).rearrange("p (c r j) -> p c r j", c=4, r=2); o += 32
        KSUM = view(o, 8, F32); o += 8
        KMAX2 = view(o, 8, F32); o += 8
        KMXG = view(o, 8, F32); o += 8
        KM16 = view(o, 16, F32); o += 16
        QMXG = view(o, 8, F32); o += 8
        NB = view(o, 16, F32); o += 16
        FB = view(o, 16, F32); o += 16
        QM16 = view(o, 16, F32); o += 16
        SINKS = view(o, 8, F32); o += 8
        BM8 = view(o, 16, F32); o += 16
        RB = self.XN[0].rearrange("p (h b) -> p h b", h=16)
        B31 = view(o, 16, F32); o += 16
        KSQ = view(rB, 640, BF16).rearrange("p (c t) -> p c t", c=5)
        RDEN = view(o, 8, F32); o += 8
        assert o <= 53200, o
        wsc = self.wada_off
        CMP = view(wsc, 1024).rearrange("p (a j k) -> p a j k", a=16, j=8)
        AUGB = view(wsc + 1024, 128, BF16).rearrange("p (q s j) -> p q s j", q=2, s=16)
        BMASK = view(wsc + 1600, 192).rearrange("p (k b j) -> p k b j", k=3, b=8)
        GM = view(wsc + 1792, 128).rearrange("p (a j) -> p a j", a=16)
        SEL = view(wsc + 1920, 128).rearrange("p (a j) -> p a j", a=16)
        rs_off = self.rstd_off
        SELF = view(rs_off + 256, 256).rearrange("p (q s j) -> p q s j", q=2, s=16)
        SH8 = view(rs_off + 512 + 256, 32).rearrange("p (q s) -> p q s", q=2)
        SINKT = view(rs_off + 512 + 288, 16).rearrange("p (q s) -> p q s", q=2)
        SQRT_T = view(rs_off + 512 + 304, 32).rearrange("p (q s) -> p q s", q=2)
        TMPS = [self.XN[0], self.XN[1]]
        PTS = [self.SQ[0].rearrange("p a t -> p (a t)")[:, 0:512], self.SQ[0].rearrange("p a t -> p (a t)")[:, 512:1024],
               self.SQ[1].rearrange("p a t -> p (a t)")[:, 0:512], self.SQ[1].rearrange("p a t -> p (a t)")[:, 512:1024]]
        PTN = ["sq0", "sq0b", "sq1", "sq1b"]
        IDENT = self.IDENT.rearrange("p (a b) -> p a b", a=1)[:, 0, :]

        sc.marker(writes=["wada0", "wada1", "rstd0", "rstd1", "rstd0p", "rstd1p", "wadafree"])
        for (nm, c0, c1) in (("ka", 512, 1024), ("kb", 2048, 2176), ("qa", 0, 512), ("qb", 1536, 2048), ("va", 1024, 1536), ("vb", 2176, 2304)):
            sc.add("pool", (lambda e, c0=c0, c1=c1: e.dma_start(out=WIN[:, :, c0:c1], in_=self.d_winr[l][:, :, c0:c1])),
                   writes=["win_" + nm], dma="win_" + nm)
        sc.add("pool", (lambda e: e.dma_start(out=DT.rearrange("p h t q -> p (h t q)"), in_=self.d_dtile[:, :])), writes=["dt"], dma="dt")
        sc.add("pool", (lambda e: e.dma_start(out=IND.rearrange("p j k -> p (j k)")[0:72, :], in_=self.d_indall[:, :])), writes=["ind"], dma="ind")
        sc.add("pool", (lambda e: e.dma_start(out=HIND, in_=self.d_hind[:, :])), writes=["hind"], dma="hind")
        sc.add("sp", (lambda e: e.dma_start(out=BMASK.rearrange("p k b j -> p (k b j)"), in_=self.d_bmask[:, :])), reads=["wadafree"], writes=["bmask"], dma="bmask")
        sc.add("sp", (lambda e: e.dma_start(out=SINKS, in_=self.d_sinks[l:l + 1, :].partition_broadcast(128))), writes=["sinks"], dma="sinks")
        sc.add("sp", (lambda e: e.dma_start(out=RB.rearrange("p h b -> p (h b)"), in_=self.d_rbT[0:1, :].partition_broadcast(128))), writes=["xn0"], dma="rb")
        sc.add("pool", (lambda e: e.dma_start(out=WOUT, in_=self.d_woutr[l])), writes=["wout"], dma="wout")

        def init1(e):
            e.memset(KMEANT, 0.0)
            e.memset(KMAX2, 0.0)
            e.memset(VA[:, :, :, 64:65], 1.0)
            e.memset(AUGB, 0.0)
            e.memset(SELF, 1.0)
            e.tensor_reduce(out=BM8, in_=RB, axis=AX.X, op=ALU.max)
            e.tensor_copy(out=B31, in_=RB[:, :, 31])
            return e.memset(KM16, 0.0)
        sc.add("dve", init1, reads=["xn0", "wadafree"], writes=["kmeant", "kmax2", "va_ones", "augb_s", "augb_m", "selfm", "bm8raw", "b31", "km16"])

        sc.add("dve", (lambda e: e.tensor_tensor(out=BM8[:, 8:16], in0=BM8[:, 8:16], in1=SINKS, op=ALU.max)),
               reads=["bm8raw", "sinks"], writes=["bm8raw"])
        sc.add("dve", (lambda e: e.tensor_scalar(out=BM8, in0=BM8, scalar1=8.0, scalar2=None, op0=ALU.mult)),
               reads=["bm8raw"], writes=["bm8", "bm8raw"])

        QCOL, KCOL, VCOL, QBCOL, KBCOL, VBCOL = 0, 512, 1024, 1536, 2048, 2176
        sbank = [0]
        ptc = [0]

        def group(g):
            t0 = g * 256
            b = g
            xres = [f"xT{g}"]
            self.rmsnorm_in(l, 0, t0, 256, HTG, "rA", 7, sq_names=(["sq0", "sq0b"], ["sq1", "sq1b"]))
            pbank = [0]

            def nextbank():
                bk = 4 + pbank[0] % 4
                pbank[0] += 1
                return bk
            def qproj():
              for ci in range(8):
                col = QCOL + ci * 128 if ci < 4 else QBCOL + (ci - 4) * 128
                bk = nextbank()
                ps = self.psb(bk)

                def mm(e, col=col, ps=ps):
                    last = None
                    for kc in range(8):
                        last = e.matmul(ps[:, 0:256], WIN[:, kc, col:col + 128], HTG[:, kc, :], start=(kc == 0), stop=(kc == 7))
                    return last
                sc.add("pe", mm, reads=["win_qa" if ci < 4 else "win_qb", "rA"], writes=[f"ps{bk}"])
                sc.add("dve", (lambda e, ci=ci, ps=ps: e.tensor_copy(out=QTG[:, ci, :], in_=ps[:, 0:256])),
                       reads=[f"ps{bk}"], writes=["rC"])
            sc.add("dve", (lambda e: e.memset(KSUM, 0.0)), writes=[f"ksum{c}" for c in range(4)])
            for ci in range(5):
                col = KCOL + ci * 128 if ci < 4 else KBCOL
                bk = nextbank()
                ps = self.psb(bk)

                def mm(e, col=col, ps=ps):
                    last = None
                    for kc in range(8):
                        last = e.matmul(ps[:, 0:256], WIN[:, kc, col:col + 128], HTG[:, kc, :], start=(kc == 0), stop=(kc == 7))
                    return last
                sc.add("pe", mm, reads=["win_ka" if ci < 4 else "win_kb", "rA"], writes=[f"ps{bk}"])
                if ci < 4:
                    sc.add("act", (lambda e, ci=ci, ps=ps: e.activation(out=KT[:, ci, t0:t0 + 256], in_=ps[:, 0:256], func=AF.Copy,
                                                                           accum_out=KSUM[:, ci:ci + 1])),
                           reads=[f"ps{bk}"], writes=[f"kt{ci}_{g}", f"ksum{ci}"])
                else:
                    sc.add("act", (lambda e, ci=ci, ps=ps: e.activation(out=KT[:, ci, t0:t0 + 256], in_=ps[:, 0:256], func=AF.Copy)),
                           reads=[f"ps{bk}"], writes=[f"kt{ci}_{g}"])
            qproj()

            def vproj():
              for qt in range(2):
                tile_i = g * 2 + qt
                bk = nextbank()
                ps = self.psb(bk)

                def mmv(e, qt=qt, ps=ps):
                    last = None
                    for kc in range(8):
                        last = e.matmul(ps[:, 0:512], HTG[:, kc, qt * 128:(qt + 1) * 128], WIN[:, kc, VCOL:VCOL + 512], start=(kc == 0), stop=(kc == 7))
                    return last
                sc.add("pe", mmv, reads=["win_va", "rA"], writes=[f"ps{bk}"])
                sc.add("act", (lambda e, tile_i=tile_i, ps=ps: e.activation(out=VA[:, tile_i, 0:8, 0:64],
                                                                               in_=ps[:, 0:512].rearrange("p (h d) -> p h d", h=8), func=AF.Copy)),
                       reads=[f"ps{bk}", "va_ones"], writes=[f"va{g}_{qt}a"])
                bk2 = nextbank()
                ps2 = self.psb(bk2)

                def mmv2(e, qt=qt, ps2=ps2):
                    last = None
                    for kc in range(8):
                        last = e.matmul(ps2[:, 0:128], HTG[:, kc, qt * 128:(qt + 1) * 128], WIN[:, kc, VBCOL:VBCOL + 128], start=(kc == 0), stop=(kc == 7))
                    return last
                sc.add("pe", mmv2, reads=["win_vb", "rA"], writes=[f"ps{bk2}"])
                sc.add("dve", (lambda e, tile_i=tile_i, ps2=ps2: e.tensor_copy(out=VA[:, tile_i, 8:10, 0:64],
                                                                                 in_=ps2[:, 0:128].rearrange("p (h d) -> p h d", h=2))),
                       reads=[f"ps{bk2}", "va_ones"], writes=[f"va{g}_{qt}b"])
            if getattr(self, 'stop', 99) <= 2:
                return
            def kmw(e, b=b):
                e.tensor_scalar(out=KMEANT[0:64, :, 0, b], in0=KSUM[0:64, 0:4], scalar1=1.0 / 256.0, scalar2=None, op0=ALU.mult)
                return e.tensor_scalar(out=KMEANT[64:128, :, 1, b], in0=KSUM[64:128, 0:4], scalar1=1.0 / 256.0, scalar2=None, op0=ALU.mult)
            sc.add("dve", kmw, reads=[f"ksum{c}" for c in range(4)], writes=["kmeant"])
            sc.add("dve", (lambda e: e.tensor_tensor(out=KSQ, in0=KT[:, 0:5, t0:t0 + 256], in1=KT[:, 0:5, t0:t0 + 256], op=ALU.mult)),
                   reads=[f"kt{c}_{g}" for c in range(5)], writes=["rB", "rBb"])
            if getattr(self, 'stop', 99) <= 2.2:
                return
            for bi, cs in enumerate([(0, 1), (2, 3), (4,)]):
                bk = 4 + bi
                ps = self.psb(bk).rearrange("p (a t) -> p a t", a=2)

                def mmk(e, cs=cs, ps=ps):
                    last = None
                    for a, c in enumerate(cs):
                        last = e.matmul(ps[:, a, :], self.ONES, KSQ[:, c, :], start=True, stop=True)
                    return last
                sc.add("pe", mmk, reads=["rB", "ones"], writes=[f"ps{bk}"])
                sc.add("dve", (lambda e, cs=cs, ps=ps: e.tensor_reduce(out=KMXG[:, cs[0]:cs[0] + len(cs)], in_=ps[:, 0:len(cs), :], axis=AX.X, op=ALU.max)),
                       reads=[f"ps{bk}"], writes=["kmxg"])

            if getattr(self, 'stop', 99) <= 2.4:
                return
            sc.add("dve", (lambda e: e.tensor_tensor(out=KMAX2[:, 0:5], in0=KMAX2[:, 0:5], in1=KMXG[:, 0:5], op=ALU.max)),
                   reads=["kmxg", "kmax2"], writes=["kmax2"])

            def kmax(e):
                e.tensor_copy(out=KM16[:, 0:8].rearrange("p (c r) -> p c r", r=2), in_=KMAX2[:, 0:4].unsqueeze(2).to_broadcast([128, 4, 2]))
                return e.tensor_copy(out=KM16[:, 8:16], in_=KMAX2[:, 4:5].to_broadcast([128, 8]))
            sc.add("dve", kmax, reads=["kmax2"], writes=["km16"])
            if getattr(self, 'stop', 99) <= 2.6:
                return
            sc.add("dve", (lambda e: e.tensor_tensor(out=QSQ, in0=QTG, in1=QTG, op=ALU.mult)), reads=["rC"], writes=["rB", "rBb"])
            ps7 = self.psb(7)
            GATE = ps7[:, 0:128].rearrange("p (a j) -> p a j", a=16)

            for bi in range(4):
                psq = self.psb(bi).rearrange("p (a t) -> p a t", a=2)

                def mmq(e, bi=bi, psq=psq):
                    last = None
                    for a in range(2):
                        last = e.matmul(psq[:, a, :], self.ONES, QSQ[:, 2 * bi + a, :], start=True, stop=True)
                    return last
                sc.add("pe", mmq, reads=["rB", "ones"], writes=[f"ps{bi}"])
                sc.add("dve", (lambda e, bi=bi, psq=psq: e.tensor_reduce(out=QMXG[:, 2 * bi:2 * bi + 2], in_=psq, axis=AX.X, op=ALU.max)),
                       reads=[f"ps{bi}"], writes=["qmxg"])
            sc.add("dve", (lambda e: e.tensor_copy(out=QM16.rearrange("p (c r) -> p c r", r=2), in_=QMXG.unsqueeze(2).to_broadcast([128, 8, 2]))),
                   reads=["qmxg"], writes=["qm16"])

            def mmg(e):
                last = None
                for qt in range(2):
                    for c in range(4):
                        last = e.matmul(ps7[:, (qt * 8 + 2 * c) * 8:(qt * 8 + 2 * c + 2) * 8], QTG[:, c, qt * 128:(qt + 1) * 128],
                                        KMEANT[:, c, :, :].rearrange("p r j -> p (r j)"), start=True, stop=True)
                return last
            if b >= 4:
                sc.add("pe", mmg, reads=["rC", "kmeant"], writes=["ps7"])
            if getattr(self, 'stop', 99) <= 3:
                return
            NEGM = BMASK[:, 0, b, :]
            ELIG = BMASK[:, 1, b, :]
            OWN = BMASK[:, 2, b, :]

            sc.add("dve", (lambda e: e.tensor_tensor(out=SQRT_T, in0=QM16.unsqueeze(1).to_broadcast([128, 2, 16]),
                                                     in1=KM16.unsqueeze(1).to_broadcast([128, 2, 16]), op=ALU.mult)),
                   reads=["qm16", "km16"], writes=["sqrt_t"])
            sc.add("act", (lambda e: e.activation(out=SQRT_T, in_=SQRT_T, func=AF.Ln, bias=self.EPSC[:, 0:1], scale=1.0)),
                   reads=["sqrt_t", "epsc"], writes=["sqrt_t"])
            sc.add("act", (lambda e: e.activation(out=SQRT_T, in_=SQRT_T, func=AF.Exp, scale=0.5)),
                   reads=["sqrt_t"], writes=["sqrt_t"])
            AUGBv = AUGB
            sc.add("dve", (lambda e: e.tensor_tensor(out=SH8, in0=SQRT_T, in1=BM8.unsqueeze(1).to_broadcast([128, 2, 16]), op=ALU.add)),
                   reads=["sqrt_t", "bm8"], writes=["sh8"])
            sc.add("dve", (lambda e: e.tensor_scalar(out=NB, in0=SH8[:, 0, :], scalar1=-0.125, scalar2=None, op0=ALU.mult)),
                   reads=["sh8"], writes=["nb"])
            sc.add("dve", (lambda e: e.tensor_tensor(out=FB, in0=NB, in1=B31, op=ALU.add)), reads=["nb", "b31"], writes=["fb"])
            sc.add("dve", (lambda e: e.tensor_tensor(out=SINKT, in0=SINKS.unsqueeze(1).to_broadcast([128, 2, 8]),
                                                     in1=NB[:, 8:16].unsqueeze(1).to_broadcast([128, 2, 8]), op=ALU.add)),
                   reads=["nb", "sinks"], writes=["sinkt"])
            sc.add("act", (lambda e: e.activation(out=SINKT, in_=SINKT, func=AF.Exp)), reads=["sinkt"], writes=["sinkt"])
            SELM = SELF[:, :, 0:8, :]
            sel_jobs = [
                lambda: sc.add("dve", (lambda e: e.tensor_tensor(out=GM, in0=GATE, in1=NEGM.unsqueeze(1).to_broadcast([128, 16, 8]), op=ALU.add)),
                               reads=["ps7", "bmask"], writes=["gm"]),
                lambda: sc.add("dve", (lambda e: e.tensor_tensor(out=CMP, in0=GM.unsqueeze(2).to_broadcast([128, 16, 8, 8]),
                                                                 in1=GM.unsqueeze(3).to_broadcast([128, 16, 8, 8]), op=ALU.is_gt)),
                               reads=["gm"], writes=["cmp"]),
                lambda: sc.add("dve", (lambda e: e.tensor_reduce(out=SEL, in_=CMP, axis=AX.X, op=ALU.add)), reads=["cmp"], writes=["selr"]),
                lambda: sc.add("dve", (lambda e: e.scalar_tensor_tensor(out=SEL, in0=SEL, scalar=3.0, in1=ELIG.unsqueeze(1).to_broadcast([128, 16, 8]),
                                                                        op0=ALU.is_lt, op1=ALU.mult)),
                               reads=["selr", "bmask"], writes=["selr"]),
                lambda: sc.add("dve", (lambda e: e.tensor_tensor(out=SELM, in0=SEL.rearrange("p (q h) j -> p q h j", q=2),
                                                                 in1=OWN.unsqueeze(1).unsqueeze(1).to_broadcast([128, 2, 8, 8]), op=ALU.add)),
                               reads=["selr", "bmask", "selfm"], writes=["selfm"]),
                lambda: sc.add("dve", (lambda e: e.tensor_scalar(out=AUGBv[:, :, 0:8, :], in0=SELM, scalar1=BIG, scalar2=-BIG, op0=ALU.mult, op1=ALU.add)),
                               reads=["selfm"], writes=["augb_m"]),
            ]
            if b < 4:
                sel_jobs = []
            if getattr(self, 'stop', 99) <= 4:
                return
            order = list(range(8, 16)) + list(range(8))
            if sel_jobs:
                sel_jobs.pop(0)()
            vproj()
            for grp in (1, 0):
                pvq = []

                def flush(keep):
                    while len(pvq) > keep:
                        a_, k_ = pvq.pop(0)
                        sc.add(*a_, **k_)

                def prep(s16):
                    ps7b = self.psb(7, BF16)
                    pos_ = order.index(s16)
                    AT = AUGT1[pos_ % 3]
                    ares = "augb_s" if s16 >= 8 else "augb_m"

                    def tr(e):
                        last = None
                        for qt in range(2):
                            last = e.transpose(ps7b[0:8, qt * 128:(qt + 1) * 128], AUGB[:, qt, s16, :], IDENT)
                        return last
                    sc.add("pe", tr, reads=[ares, "ident"], writes=["ps7"])
                    sc.add("act", (lambda e: e.activation(out=AT[0:8, 0:256], in_=ps7b[0:8, 0:256], func=AF.Copy)),
                           reads=["ps7"], writes=[f"augt{pos_ % 3}"])

                def head(hs8):
                    s16 = grp * 8 + hs8
                    pos = order.index(s16)
                    if grp == 1 and sel_jobs:
                        sel_jobs.pop(0)()
                    if pos + 2 < 16 and order[pos + 2] < 8 and b >= 4:
                        while sel_jobs:
                            sel_jobs.pop(0)()
                        prep(order[pos + 2])
                    AT = AUGT1[pos % 3]
                    use_aug = (grp == 0 and b >= 4)
                    pb = 0
                    if grp == 0:
                        ck, r0, cq, vh = hs8 // 2, (hs8 % 2) * 64, hs8 // 2, hs8
                    else:
                        i_, r_ = hs8 // 2, hs8 % 2
                        ck, r0, cq, vh = 4, r_ * 64, 4 + i_, 8 + r_
                    quad, hsl = hs8 // 4, hs8 % 4
                    augres = f"augt{pos % 3}"
                    far = []
                    if grp == 0 and b >= 1:
                        for kc in range(0, 2 * b - 1):
                            far.append((kc, 0, 256))
                        far.append((2 * b - 1, 128, 128))
                    near = [(2 * b - 1, 0, 0), (2 * b, 0, 1), (2 * b, 1, 0), (2 * b + 1, 1, 1)]
                    if b == 0:
                        near = near[1:]
                    n_qt = [0, 0]
                    for (kc, q0, qn) in far:
                        for sub in range(qn // 128):
                            n_qt[(q0 + sub * 128) // 128] += 1
                    for (kc, qt, _) in near:
                        n_qt[qt] += 1
                    done_qt = [0, 0]
                    banks = []
                    cur, tot = [], 0
                    for p_ in far:
                        if tot + p_[2] > 512:
                            banks.append(cur)
                            cur, tot = [], 0
                        cur.append(p_)
                        tot += p_[2]
                    if cur:
                        banks.append(cur)
                    for pieces in banks:
                        bk = 4 + sbank[0] % 3
                        sbank[0] += 1
                        ps = self.psb(bk)
                        pti = ptc[0] % 4
                        ptc[0] += 1
                        PT = PTS[pti]
                        offs = []
                        off = 0
                        for p_ in pieces:
                            offs.append(off)
                            off += p_[2]
                        tot = off

                        def mms(e, pieces=pieces, offs=offs, ps=ps):
                            last = None
                            for (kc, q0, qn), of in zip(pieces, offs):
                                last = e.matmul(ps[:, of:of + qn], KT[r0:r0 + 64, ck, kc * 128:(kc + 1) * 128], QTG[r0:r0 + 64, cq, q0:q0 + qn],
                                                start=True, stop=not use_aug)
                                if use_aug:
                                    last = e.matmul(ps[:, of:of + qn], IND[pb:pb + 8, kc // 2, :], AT[0:8, q0:q0 + qn],
                                                    start=False, stop=True)
                            return last
                        flush(2)
                        sc.add("pe", mms, reads=[f"kt{ck}_{p_[0] // 2}" for p_ in pieces] + ["rC", "ind"] + ([augres] if use_aug else []), writes=[f"ps{bk}"])
                        sc.add("act", (lambda e, PT=PT, ps=ps, tot=tot: e.activation(out=PT[:, 0:tot], in_=ps[:, 0:tot], func=AF.Exp,
                                                                                       scale=0.125, bias=FB[:, s16:s16 + 1])),
                               reads=[f"ps{bk}", "fb"], writes=[PTN[pti]])
                        flags = []
                        for (kc, q0, qn), of in zip(pieces, offs):
                            for sub in range(qn // 128):
                                qt = (q0 + sub * 128) // 128
                                st = done_qt[qt] == 0
                                done_qt[qt] += 1
                                sp_ = done_qt[qt] == n_qt[qt]
                                flags.append((kc, qt, of + sub * 128, st, sp_))

                        def pv(e, flags=flags, PT=PT):
                            last = None
                            for (kc, qt, of, st, sp_) in flags:
                                last = e.matmul(self.psb(qt * 2 + quad).rearrange("p (h d) -> p h d", d=65)[:, hsl, 0:65] if False else
                                                self.oacc(qt * 2 + quad)[:, hsl, :], PT[:, of:of + 128], VA[:, kc, vh, :], start=st, stop=sp_)
                            return last
                        pvq.append((("pe", pv), dict(reads=[PTN[pti]] + [f"va{p_[0] // 2}_{p_[0] % 2}{'a' if grp == 0 else 'b'}" for p_ in pieces],
                                                     writes=[f"ps{quad}", f"ps{2 + quad}"])))
                    bk = 4 + sbank[0] % 3
                    sbank[0] += 1
                    ps = self.psb(bk).rearrange("p (a t) -> p a t", a=4)
                    pti = ptc[0] % 4
                    ptc[0] += 1
                    PT = PTS[pti].rearrange("p (a t) -> p a t", a=4)
                    TMP = TMPS[pti % 2].rearrange("p (a t) -> p a t", a=4)
                    tmpn = f"xn{pti % 2}"
                    ti0 = 4 - len(near)

                    def mmn(e, near=near, ps=ps, ti0=ti0):
                        last = None
                        for k_, (kc, qt, di) in enumerate(near):
                            ti = ti0 + k_
                            ua = use_aug and (kc // 2) < b
                            last = e.matmul(ps[:, ti, :], KT[r0:r0 + 64, ck, kc * 128:(kc + 1) * 128], QTG[r0:r0 + 64, cq, qt * 128:(qt + 1) * 128],
                                            start=True, stop=not ua)
                            if ua:
                                last = e.matmul(ps[:, ti, :], IND[pb:pb + 8, kc // 2, :], AT[0:8, qt * 128:(qt + 1) * 128],
                                                start=False, stop=True)
                        return last
                    flush(2)
                    sc.add("pe", mmn, reads=[f"kt{ck}_{kc // 2}" for (kc, _, _) in near] + ["rC", "ind"] + ([augres] if use_aug else []), writes=[f"ps{bk}"])

                    def biasadd(e, ps=ps, TMP=TMP, ti0=ti0):
                        if ti0 == 0:
                            e.scalar_tensor_tensor(out=TMP[:, 0:2, :], in0=ps[:, 0:2, :], scalar=0.125, in1=DT[:, s16, 0:2, :], op0=ALU.mult, op1=ALU.add)
                        else:
                            e.scalar_tensor_tensor(out=TMP[:, 1:2, :], in0=ps[:, 1:2, :], scalar=0.125, in1=DT[:, s16, 1:2, :], op0=ALU.mult, op1=ALU.add)
                        return e.scalar_tensor_tensor(out=TMP[:, 2:4, :], in0=ps[:, 2:4, :], scalar=0.125, in1=DT[:, s16, 0:2, :], op0=ALU.mult, op1=ALU.add)
                    sc.add("dve", biasadd, reads=[f"ps{bk}", "dt"], writes=[tmpn])
                    sc.add("act", (lambda e, PT=PT, TMP=TMP, ti0=ti0: e.activation(out=PT[:, ti0:4, :], in_=TMP[:, ti0:4, :], func=AF.Exp,
                                                                                       bias=NB[:, s16:s16 + 1])),
                           reads=[tmpn, "nb"], writes=[PTN[pti]])
                    flags = []
                    for k_, (kc, qt, di) in enumerate(near):
                        st = done_qt[qt] == 0
                        done_qt[qt] += 1
                        sp_ = done_qt[qt] == n_qt[qt]
                        flags.append((kc, qt, ti0 + k_, st, sp_))

                    def pvn(e, flags=flags, PT=PT):
                        last = None
                        for (kc, qt, ti, st, sp_) in flags:
                            last = e.matmul(self.oacc(qt * 2 + quad)[:, hsl, :], PT[:, ti, :], VA[:, kc, vh, :], start=st, stop=sp_)
                        return last
                    pvq.append((("pe", pvn), dict(reads=[PTN[pti]] + [f"va{kc // 2}_{kc % 2}{'a' if grp == 0 else 'b'}" for (kc, _, _) in near],
                                                  writes=[f"ps{quad}", f"ps{2 + quad}"])))
                for hs8 in range(8):
                    head(hs8)
                flush(0)
                for qt in range(2):
                    for quad in range(2):
                        bk = qt * 2 + quad
                        oa = self.oacc(bk)
                        if grp == 0:
                            outv = OG[:, qt, quad * 256:(quad + 1) * 256].rearrange("p (h d) -> p h d", h=4)
                            inv = oa[:, :, 0:64]
                        else:
                            outv = OG[:, qt, 512:1024].rearrange("p (r i d) -> p i r d", r=2, i=4)[:, 2 * quad:2 * quad + 2, :, :]
                            inv = oa[:, :, 0:64].rearrange("p (i r) d -> p i r d", r=2)

                        def nrm(e, oa=oa, outv=outv, inv=inv, qt=qt, quad=quad, grp=grp):
                            if grp == 0:
                                e.reciprocal(out=RDEN[:, 0:4], in_=oa[:, :, 64])
                            else:
                                e.tensor_tensor(out=RDEN[:, 0:4], in0=oa[:, :, 64], in1=SINKT[:, qt, 4 * quad:4 * quad + 4], op=ALU.add)
                                e.reciprocal(out=RDEN[:, 0:4], in_=RDEN[:, 0:4])
                            if grp == 0:
                                rb_ = RDEN[:, 0:4].unsqueeze(2).to_broadcast([128, 4, 64])
                            else:
                                rb_ = RDEN[:, 0:4].rearrange("p (i r) -> p i r", r=2).unsqueeze(3).to_broadcast([128, 2, 2, 64])
                            return e.tensor_tensor(out=outv, in0=inv, in1=rb_, op=ALU.mult)
                        def nrm1(e, oa=oa, qt=qt, quad=quad, grp=grp):
                            if grp == 0:
                                return e.reciprocal(out=RDEN[:, 4 * (bk % 2):4 * (bk % 2) + 4], in_=oa[:, :, 64])
                            return e.tensor_tensor(out=RDEN[:, 4 * (bk % 2):4 * (bk % 2) + 4], in0=oa[:, :, 64], in1=SINKT[:, qt, 4 * quad:4 * quad + 4], op=ALU.add)
                        rdn = f"rden{bk % 2}"
                        RD = RDEN[:, 4 * (bk % 2):4 * (bk % 2) + 4]
                        if grp == 0:
                            sc.add("dve", (lambda e, oa=oa, RD=RD: e.reciprocal(out=RD, in_=oa[:, :, 64])), reads=[f"ps{bk}"], writes=[rdn])
                        else:
                            sc.add("dve", (lambda e, oa=oa, RD=RD, qt=qt, quad=quad: e.tensor_tensor(out=RD, in0=oa[:, :, 64], in1=SINKT[:, qt, 4 * quad:4 * quad + 4], op=ALU.add)),
                                   reads=[f"ps{bk}", "sinkt"], writes=[rdn])
                            sc.add("dve", (lambda e, RD=RD: e.reciprocal(out=RD, in_=RD)), reads=[rdn], writes=[rdn])
                        if grp == 0:
                            rb_ = RD.unsqueeze(2).to_broadcast([128, 4, 64])
                        else:
                            rb_ = RD.rearrange("p (i r) -> p i r", r=2).unsqueeze(3).to_broadcast([128, 2, 2, 64])
                        sc.add("dve", (lambda e, outv=outv, inv=inv, rb_=rb_: e.tensor_tensor(out=outv, in0=inv, in1=rb_, op=ALU.mult)),
                               reads=[f"ps{bk}", rdn], writes=["rA"])
            if getattr(self, 'stop', 99) <= 7:
                return
            for half in range(2):
                bk = 4 + half
                psT = self.psb(bk, BF16).rearrange("p (c t) -> p c t", c=4)

                def trO(e, half=half, psT=psT):
                    last = None
                    for cc in range(4):
                        c = half * 4 + cc
                        for qt in range(2):
                            last = e.transpose(psT[:, cc, qt * 128:(qt + 1) * 128], OG[:, qt, c * 128:(c + 1) * 128], IDENT)
                    return last
                sc.add("pe", trO, reads=["rA", "ident"], writes=[f"ps{bk}"])
                if half == 0:
                    sc.add("act", (lambda e, psT=psT: e.activation(out=OTG[:, 0:4, :], in_=psT, func=AF.Copy)), reads=[f"ps{bk}"], writes=["rB"])
                else:
                    sc.add("dve", (lambda e, psT=psT: e.tensor_copy(out=OTG[:, 4:8, :], in_=psT)), reads=[f"ps{bk}"], writes=["rBb"])
            if getattr(self, 'stop', 99) <= 8:
                return
            for o_ in range(8):
                bk = 4 + o_ % 3
                ps = self.psb(bk)

                def mmo(e, o_=o_, ps=ps):
                    last = None
                    for c in range(8):
                        last = e.matmul(ps[:, 0:256], WOUT[:, c, o_ * 128:(o_ + 1) * 128], OTG[:, c, :], start=(c == 0), stop=(c == 7))
                    return last
                sc.add("pe", mmo, reads=["wout", "rB", "rBb"], writes=[f"ps{bk}"])
                ysl = YSBA[:, o_, :]
                sc.add("act", (lambda e, ps=ps, ysl=ysl: e.activation(out=ysl, in_=ps[:, 0:256], func=AF.Copy)),
                       reads=[f"ps{bk}", "rA", "rC"], writes=[f"ysa{o_}"])
                sqb = PTS[o_ % 4]
                sc.add("dve", (lambda e, ysl=ysl, sqb=sqb: e.tensor_tensor(out=sqb[:, 0:256], in0=ysl, in1=ysl, op=ALU.mult)),
                       reads=[f"ysa{o_}"], writes=[PTN[o_ % 4]])
                sc.add("pe", (lambda e, sqb=sqb, o_=o_: e.matmul(self.psb(7)[:, 0:256], self.ONES, sqb[:, 0:256], start=(o_ == 0), stop=(o_ == 7))),
                       reads=[PTN[o_ % 4], "ones"], writes=["ps7"])
            if getattr(self, 'stop', 99) <= 9:
                return
            self.resid_update(l, 0, t0, 256, YSBA, (lambda c: f"ysa{c}"), 7)
            sc.marker(reads=[f"ysa{c}" for c in range(8)], writes=["rA", "rC"])

        for g in range(getattr(self, 'ngroups', 8)):
            group(g)
        sc.marker(writes=["bmask", "gm", "cmp", "selr", "augb_s", "augb_m", "selfm", "sh8", "sinkt", "sqrt_t", "nb", "fb", "wada_ok"])

    def oacc(self, bk):
        return self.PS[bk][:, 0:260].rearrange("p (h d) -> p h d", d=65)


def _host_prep(inputs):
    f = np.float32
    w_ada = np.asarray(inputs["w_ada"], f)
    w1 = np.asarray(inputs["w1"], f)
    w2 = np.asarray(inputs["w2"], f)
    w_in = np.asarray(inputs["w_in"], f)
    w_out = np.asarray(inputs["w_out"], f)
    sh = {}
    sh["wada"] = np.ascontiguousarray(w_ada.reshape(DEPTH, 8, 128, 24, 256).transpose(0, 3, 2, 1, 4))
    sh["badac"] = np.ascontiguousarray(np.asarray(inputs["b_ada"], f).reshape(DEPTH, 48, 128).transpose(2, 0, 1).reshape(128, DEPTH * 48))
    sh["gainc"] = np.ascontiguousarray(np.asarray(inputs["norm_gains"], f).reshape(DEPTH, 4, 8, 128).transpose(3, 0, 1, 2).reshape(128, 128))
    sh["b1c"] = np.ascontiguousarray(np.asarray(inputs["b1"], f).reshape(DEPTH, 32, 128).transpose(2, 0, 1).reshape(128, 128))
    sh["b2c"] = np.ascontiguousarray(np.asarray(inputs["b2"], f).reshape(DEPTH, 8, 128).transpose(2, 0, 1).reshape(128, 32))
    sh["w1r"] = np.ascontiguousarray(w1.reshape(DEPTH, 8, 128, 16, 256).transpose(0, 3, 2, 1, 4))
    sh["w2r"] = np.ascontiguousarray(w2.reshape(DEPTH, 2, 16, 128, 8, 128).transpose(0, 4, 1, 3, 2, 5))
    perm = [(k // 2) + 4 * (k % 2) for k in range(8)]
    colidx = np.arange(DIN)
    qb = colidx[1536:2048].reshape(8, 64)[perm].reshape(-1)
    colidx = np.concatenate([colidx[:1536], qb, colidx[2048:]])
    w_in_p = w_in[:, :, colidx]
    sh["winr"] = np.ascontiguousarray(w_in_p.reshape(DEPTH, 8, 128, DIN).transpose(0, 2, 1, 3))
    sh["woutr"] = np.ascontiguousarray(w_out.reshape(DEPTH, 8, 128, D).transpose(0, 2, 1, 3))
    sh["sinks"] = np.ascontiguousarray(np.asarray(inputs["sinks"], f)[:, perm])
    rb = np.asarray(inputs["rel_bias"], f)
    hperm = list(range(8)) + [8 + p for p in perm]
    rb = rb[:, hperm]
    sh["rbT"] = np.ascontiguousarray(rb.T).reshape(1, 16 * 32)
    tab = np.concatenate([rb, np.full((1, 16), NEG, f)], axis=0)
    idx = _dtile_index()
    dt = np.zeros((128, 16, 2, 128), f)
    for h in range(16):
        hg = 0 if h < 8 else 1
        for t in range(2):
            dt[:, h, t, :] = tab[idx[hg, t], h]
    sh["dtile"] = dt.reshape(128, 16 * 2 * 128)
    sh["ident"] = np.eye(128, dtype=f)
    hsel = np.zeros((128, 2, 128), f)
    hsel[0:64, 0, :] = 1.0
    hsel[64:128, 1, :] = 1.0
    sh["hsel"] = hsel.reshape(128, 256)
    hind = np.zeros((128, 2), f)
    hind[0:64, 0] = 1.0
    hind[64:128, 1] = 1.0
    sh["hind"] = hind
    ind = np.zeros((72, 8, 128), f)
    for j in range(8):
        for pb in (0, 32, 64):
            ind[pb + j, j, :] = 1.0
    sh["indall"] = ind.reshape(72, 1024)
    bm = np.zeros((128, 3, 8, 8), f)
    for b in range(8):
        for j in range(8):
            bm[:, 0, b, j] = 0.0 if j < b else -1e30
            bm[:, 1, b, j] = 1.0 if j < b else 0.0
            bm[:, 2, b, j] = 1.0 if j == b else 0.0
    sh["bmask"] = bm.reshape(128, 192)
    x = np.asarray(inputs["x"], f)
    c = np.asarray(inputs["c"], f)
    per = []
    for b in range(x.shape[0]):
        m = dict(sh)
        m["xT"] = np.ascontiguousarray(x[b].T)
        m["cT"] = np.ascontiguousarray(c[b].reshape(8, 128).T)
        per.append(m)
    return per


_PROG_CACHE = {}


def _get_prog(phases, debug=False, ngroups=8, stop=99):
    key = (tuple(phases), debug, ngroups, stop)
    if key not in _PROG_CACHE:
        _PROG_CACHE[key] = Prog(list(phases), debug=debug, ngroups=ngroups, stop=stop)
    return _PROG_CACHE[key]


def run_phases(inputs, phases, n_cores=8, trace=False, debug=False, ngroups=8, stop=99):
    per = _host_prep(inputs)[:n_cores]
    prog = _get_prog(phases, debug, ngroups, stop)
    res = run_bass_kernel_spmd(prog.nc, per, core_ids=list(range(n_cores)), trace=trace)
    outs = [np.ascontiguousarray(r["outT"].T) for r in res.results]
    return np.stack(outs, axis=0), res


def kernel(**inputs):
    phases = []
    for l in range(DEPTH):
        phases += [("attn", l), ("ffn", l)]
    out, _ = run_phases(inputs, phases)
    return out.astype(np.float32)
```
